# Optimizing a Trainium2 kernel written in Bass

```python
import math
import jax, jax.numpy as jnp
from jax import lax
import numpy as np

D_MODEL = 1024
BATCH = 2
SEQ = 8192
DEPTH = 2

N_MIXERS = 2
DIFF_HEADS = 8
DIFF_HEAD_DIM = D_MODEL // DIFF_HEADS // 2
MOBA_HEADS = 16
MOBA_HEAD_DIM = D_MODEL // MOBA_HEADS
MOBA_BLOCK = 256
MOBA_TOPK = 3
D_FF = 4 * D_MODEL
Q_BLOCK = 128
MOBA_Q_CHUNK = 32
RMS_EPS = 1e-6
NEG_INF = -1e30
N_DIFF_LAYERS = (DEPTH + N_MIXERS - 1) // N_MIXERS

kernel_name = "diffattn_moba_interleaved_hybrid"


def alibi_slopes(n_heads):
    return np.array([2.0 ** (-8.0 * (h + 1) / n_heads) for h in range(n_heads)], dtype=np.float32)


def lambda_init_fn(layer_idx):
    return 0.8 - 0.6 * math.exp(-0.3 * layer_idx)


def rmsnorm(x, g):
    xf = x.astype(jnp.float32)
    y = xf * lax.rsqrt(jnp.mean(xf * xf, axis=-1, keepdims=True) + RMS_EPS) * g.astype(jnp.float32)
    return y.astype(x.dtype)


def squared_relu_mlp(h, w1, w2):
    return jnp.square(jax.nn.relu(h @ w1)) @ w2


def diff_attention(h, w_in, w_out, lam_params, subln_w, lam_init):
    B, T, _ = h.shape
    H, d = DIFF_HEADS, DIFF_HEAD_DIM
    q, k, v = jnp.split(h @ w_in, 3, axis=-1)
    q = q.reshape(B, T, H, 2, d).transpose(3, 0, 2, 1, 4)
    kf = k.reshape(B, T, H, 2, d).transpose(3, 0, 2, 1, 4).astype(jnp.float32)
    v = v.reshape(B, T, H, 2 * d).transpose(0, 2, 1, 3)
    lf = lam_params.astype(jnp.float32)
    lam = jnp.exp(jnp.sum(lf[0] * lf[1])) - jnp.exp(jnp.sum(lf[2] * lf[3])) + lam_init
    slopes = jnp.asarray(alibi_slopes(H))
    kpos = jnp.arange(T)
    scale = d ** -0.5

    def block(i):
        t0 = i * Q_BLOCK
        qb = lax.dynamic_slice_in_dim(q, t0, Q_BLOCK, axis=3).astype(jnp.float32)
        s = jnp.einsum('mbhqd,mbhkd->mbhqk', qb, kf) * scale
        dist = (t0 + jnp.arange(Q_BLOCK))[:, None] - kpos[None, :]
        bias = -slopes[:, None, None] * dist.astype(jnp.float32)
        s = jnp.where(dist >= 0, s + bias, NEG_INF)
        p = jax.nn.softmax(s, axis=-1)
        a = p[0] - lam * p[1]
        return jnp.einsum('bhqk,bhke->bhqe', a.astype(v.dtype), v)

    o = lax.map(block, jnp.arange(T // Q_BLOCK))
    o = o.transpose(1, 0, 3, 2, 4).reshape(B, T, H, 2 * d)
    o = rmsnorm(o, subln_w) * (1.0 - lam_init)
    return o.reshape(B, T, H * 2 * d) @ w_out


def moba_attention(h, w_in, w_out):
    B, T, _ = h.shape
    H, dh, BLK, QC = MOBA_HEADS, MOBA_HEAD_DIM, MOBA_BLOCK, MOBA_Q_CHUNK
    q, k, v = jnp.split(h @ w_in, 3, axis=-1)
    q = q.reshape(B, T, H, dh).transpose(0, 2, 1, 3)
    k = k.reshape(B, T, H, dh).transpose(0, 2, 1, 3)
    v = v.reshape(B, T, H, dh).transpose(0, 2, 1, 3)
    NB = -(-T // BLK)
    pad = NB * BLK - T
    kp = jnp.pad(k, ((0, 0), (0, 0), (0, pad), (0, 0)))
    vp = jnp.pad(v, ((0, 0), (0, 0), (0, pad), (0, 0)))
    kb = kp.reshape(B, H, NB, BLK, dh)
    vb = vp.reshape(B, H, NB, BLK, dh)
    kmean = jnp.mean(kb.astype(jnp.float32), axis=3)
    K_SEL = min(MOBA_TOPK, NB)
    slopes = jnp.asarray(alibi_slopes(H))
    scale = dh ** -0.5
    bi = jnp.arange(B)[:, None, None, None]
    hi = jnp.arange(H)[None, :, None, None]

    def chunk(c):
        t0 = c * QC
        j = t0 // BLK
        qc = lax.dynamic_slice_in_dim(q, t0, QC, axis=2).astype(jnp.float32)
        qpos = t0 + jnp.arange(QC)
        gate = jnp.einsum('bhqd,bhnd->bhqn', qc, kmean)
        gate = jnp.where(jnp.arange(NB) < j, gate, NEG_INF)
        _, idx = lax.top_k(gate, K_SEL)
        valid = jnp.arange(K_SEL) < j
        s0 = j * BLK
        k_own = lax.dynamic_slice_in_dim(kp, s0, BLK, axis=2).astype(jnp.float32)
        v_own = lax.dynamic_slice_in_dim(vp, s0, BLK, axis=2)
        d_own = (qpos[:, None] - (s0 + jnp.arange(BLK))[None, :]).astype(jnp.float32)
        sc_own = jnp.einsum('bhqd,bhkd->bhqk', qc, k_own) * scale - slopes[:, None, None] * d_own
        sc_own = jnp.where(d_own >= 0, sc_own, NEG_INF)
        k_sel = kb[bi, hi, idx]
        v_sel = vb[bi, hi, idx]
        sel_pos = idx[..., None] * BLK + jnp.arange(BLK)
        d_sel = (qpos[None, None, :, None, None] - sel_pos).astype(jnp.float32)
        sc_sel = (jnp.einsum('bhqd,bhqkld->bhqkl', qc, k_sel.astype(jnp.float32)) * scale
                  - slopes[None, :, None, None, None] * d_sel)
        sc_sel = jnp.where(valid[:, None], sc_sel, NEG_INF)
        sc = jnp.concatenate([sc_own, sc_sel.reshape(B, H, QC, K_SEL * BLK)], axis=-1)
        p = jax.nn.softmax(sc, axis=-1)
        p_own = p[..., :BLK].astype(v.dtype)
        p_sel = p[..., BLK:].reshape(B, H, QC, K_SEL, BLK).astype(v.dtype)
        return (jnp.einsum('bhqk,bhkd->bhqd', p_own, v_own)
                + jnp.einsum('bhqkl,bhqkld->bhqd', p_sel, v_sel))

    o = lax.map(chunk, jnp.arange(T // QC))
    o = o.transpose(1, 0, 3, 2, 4).reshape(B, T, H * dh)
    return o @ w_out


def setup_inputs(seed: int = 0) -> dict:
    key = jax.random.key(seed)
    ks = jax.random.split(key, 10)
    f32 = jnp.float32
    x = jax.random.normal(ks[0], (BATCH, SEQ, D_MODEL), f32)
    attn_norm = 1.0 + 0.02 * jax.random.normal(ks[1], (DEPTH, D_MODEL), f32)
    w_in = jax.random.normal(ks[2], (DEPTH, D_MODEL, 3 * D_MODEL), f32) * D_MODEL ** -0.5
    w_out = jax.random.normal(ks[3], (DEPTH, D_MODEL, D_MODEL), f32) * D_MODEL ** -0.5
    diff_lambda = 0.1 * jax.random.normal(ks[4], (N_DIFF_LAYERS, 4, DIFF_HEAD_DIM), f32)
    diff_subln = 1.0 + 0.02 * jax.random.normal(ks[5], (N_DIFF_LAYERS, 2 * DIFF_HEAD_DIM), f32)
    mlp_norm = 1.0 + 0.02 * jax.random.normal(ks[6], (DEPTH, D_MODEL), f32)
    w_ff1 = jax.random.normal(ks[7], (DEPTH, D_MODEL, D_FF), f32) * D_MODEL ** -0.5
    w_ff2 = jax.random.normal(ks[8], (DEPTH, D_FF, D_MODEL), f32) * D_FF ** -0.5
    final_norm = 1.0 + 0.02 * jax.random.normal(ks[9], (D_MODEL,), f32)
    return {"x": x, "attn_norm": attn_norm, "w_in": w_in, "w_out": w_out,
            "diff_lambda": diff_lambda, "diff_subln": diff_subln, "mlp_norm": mlp_norm,
            "w_ff1": w_ff1, "w_ff2": w_ff2, "final_norm": final_norm}


def reference(x, attn_norm, w_in, w_out, diff_lambda, diff_subln, mlp_norm, w_ff1, w_ff2, final_norm):
    h = x
    for i in range(DEPTH):
        hn = rmsnorm(h, attn_norm[i])
        if i % N_MIXERS == 0:
            li = i // N_MIXERS
            m = diff_attention(hn, w_in[i], w_out[i], diff_lambda[li], diff_subln[li], lambda_init_fn(i))
        else:
            m = moba_attention(hn, w_in[i], w_out[i])
        h = h + m
        h = h + squared_relu_mlp(rmsnorm(h, mlp_norm[i]), w_ff1[i], w_ff2[i])
    return rmsnorm(h, final_norm)
```

```python
import math
import os
from contextlib import ExitStack

import numpy as np
import ml_dtypes
import concourse.bass as bass
import concourse.mybir as mybir
from concourse.bass_utils import run_bass_kernel_spmd

F32 = mybir.dt.float32
BF16 = mybir.dt.bfloat16
ALU = mybir.AluOpType
AF = mybir.ActivationFunctionType
AX = mybir.AxisListType

D = 1024
T = 8192
NT = 2048
DFF = 4096
EPS = 1e-6
SCALE = 0.125
NEG = -30000.0
NDC = 67
GROUPS = [[0, 1, 2, 3], [4, 5, 6, 7]]


class Op:
    __slots__ = ("eng", "fn", "deps", "dma", "cc", "signal", "count", "idx")

    def __init__(self, eng, fn, dma=None, cc=None):
        self.eng = eng
        self.fn = fn
        self.deps = set()
        self.dma = dma
        self.cc = cc
        self.signal = False
        self.count = 0
        self.idx = 0


class Prog:
    ENGS = ["sp", "act", "dve", "pool", "pe"]

    def __init__(self):
        self.ops = {e: [] for e in self.ENGS}
        self.last_w = {}
        self.readers = {}
        self.dma_keys = {}
        self.cc_keys = []
        self.n = 0

    def add(self, eng, fn, reads=(), writes=(), dma=None, cc=None):
        op = Op(eng, fn, dma=dma, cc=cc)
        op.idx = self.n
        self.n += 1
        deps = set()
        for r in reads:
            w = self.last_w.get(r)
            if w is not None:
                deps.add(w)
        for w_ in writes:
            w = self.last_w.get(w_)
            if w is not None:
                deps.add(w)
            for rd in self.readers.get(w_, ()):
                deps.add(rd)
        deps.discard(op)
        if eng == "pe":
            deps = {d for d in deps if not (d.eng == "pe" and d.dma is None and d.cc is None)}
        op.deps = deps
        for r in reads:
            self.readers.setdefault(r, []).append(op)
        for w_ in writes:
            self.last_w[w_] = op
            self.readers[w_] = []
        self.ops[eng].append(op)
        if dma is not None:
            self.dma_keys.setdefault(dma, 0)
        if cc is not None:
            self.cc_keys.append(cc)
        return op

    def barrier(self):
        lasts = []
        for e in self.ENGS:
            for op in reversed(self.ops[e]):
                if op.dma is None and op.cc is None and op.fn is not None:
                    lasts.append(op)
                    break
        seen = {}
        for e in self.ENGS:
            for op in self.ops[e]:
                if op.dma is not None:
                    seen[op.dma] = op
                if op.cc is not None:
                    seen[("cc", op.cc)] = op
        lasts += list(seen.values())
        for e in self.ENGS:
            b = Op(e, None)
            b.idx = self.n
            self.n += 1
            b.deps = set(lasts)
            self.ops[e].append(b)
        self.last_w = {}
        self.readers = {}

    def finalize(self):
        for e in self.ENGS:
            for op in self.ops[e]:
                for d in op.deps:
                    d.signal = True
        cnt = {e: 0 for e in self.ENGS}
        dcnt = {k: 0 for k in self.dma_keys}
        for e in self.ENGS:
            for op in self.ops[e]:
                if op.dma is not None:
                    dcnt[op.dma] += 16
                    op.count = dcnt[op.dma]
                elif op.cc is not None:
                    op.count = 1
                elif op.signal and op.fn is not None:
                    cnt[e] += 1
                    op.count = cnt[e]

    def emit(self, eng_name, eng, sems):
        waited = {}
        for op in self.ops[eng_name]:
            need = {}
            for d in op.deps:
                if d.dma is not None:
                    key = ("dma", d.dma)
                elif d.cc is not None:
                    key = ("cc", d.cc)
                else:
                    key = ("eng", d.eng)
                if d.count > need.get(key, 0):
                    need[key] = d.count
            for key, val in need.items():
                if waited.get(key, 0) >= val:
                    continue
                eng.wait_ge(sems[key], val)
                waited[key] = val
            if op.fn is None:
                continue
            ins = op.fn(eng)
            if op.dma is not None:
                ins.then_inc(sems[("dma", op.dma)], 16)
            elif op.cc is not None:
                ins.then_inc(sems[("cc", op.cc)])
            elif op.signal:
                ins.then_inc(sems[("eng", eng_name)], 1)


def build_program(stage="full"):
    nc = bass.Bass("TRN2", target_bir_lowering=False)
    P = Prog()

    def ext_in(name, shape, dt):
        return nc.dram_tensor(name, list(shape), dt, kind="ExternalInput").ap()

    x_in = ext_in("x", [NT, D], F32)
    win_in = ext_in("win", [4, D, 384], F32)
    wout_in = ext_in("wout", [2, D, D], F32)
    wff1_in = ext_in("wff1", [2, D, DFF], F32)
    wff2_in = ext_in("wff2", [2, DFF, D], F32)
    gains_in = ext_in("gains", [128, 64], F32)
    fnorm_in = ext_in("fnormb", [128, D], F32)
    lamb_in = ext_in("lamb", [128, 256], F32)
    ident_in = ext_in("ident", [128, 128], BF16)
    tri_in = ext_in("trimask", [128, 128], BF16)
    kaug_in = ext_in("kaug", [2, 32, T], BF16)
    qaug0_in = ext_in("qaug0", [2, 32, 512], BF16)
    btab_in = ext_in("biastab", [128, 8 * NDC], F32)
    augq1_in = ext_in("augq1", [128, 16], F32)
    bsel_in = ext_in("bsel", [128, 256], F32)
    y_out = nc.dram_tensor("y", [NT, D], F32, kind="ExternalOutput").ap()

    HTin = [[nc.dram_tensor(f"htin{l}_{i}", [D, 512], BF16) for i in range(4)] for l in range(2)]
    HT = [[nc.dram_tensor(f"ht{l}_{i}", [4 * D, 512], BF16) for i in range(4)] for l in range(2)]
    OSin = [[[nc.dram_tensor(f"osin{l}_{p}_{q}", [128, 2048], BF16) for q in range(4)]
             for p in range(2)] for l in range(2)]
    OS = [[nc.dram_tensor(f"os{l}_{p}", [2048, 2048], BF16) for p in range(2)] for l in range(2)]

    off = [16512]

    def sb(name, shape, dt, at=None):
        nbytes = int(np.prod(shape[1:])) * (4 if dt == F32 else 2)
        if at is None:
            o = off[0]
            off[0] += (nbytes + 31) // 32 * 32
        else:
            o = at
        return nc.alloc_sbuf_tensor_at(name, list(shape), dt, offset=o), o + (nbytes + 31) // 32 * 32

    H, _ = sb("H", [128, 16, D], F32)
    ident, _ = sb("identt", [128, 128], BF16)
    tri, _ = sb("trit", [128, 128], BF16)
    ones128, _ = sb("ones128", [128, 128], F32)
    bsel, _ = sb("bselt", [128, 256], F32)
    btab, _ = sb("btab", [128, 8 * NDC], F32)
    augq1, _ = sb("augq1t", [128, 16], F32)
    gains, _ = sb("gainst", [128, 64], F32)
    lamb, _ = sb("lambt", [128, 256], F32)
    small, _ = sb("small", [128, 64], F32)
    epsc, _ = sb("epsc", [128, 1], F32)
    PH = off[0]

    o = PH
    KT = []
    for x_ in range(2):
        t_, o = sb(f"KT{x_}", [128, T], BF16, at=o)
        KT.append(t_)
    Vb, o = sb("Vb", [128, 64 * 192], BF16, at=o)
    WIN, o = sb("WIN", [128, 8, 384], BF16, at=o)
    HNTc = []
    for s_ in range(2):
        t_, o = sb(f"HNTc{s_}", [128, 8, 512], BF16, at=o)
        HNTc.append(t_)
    QT = []
    for s_ in range(2):
        row = []
        for x_ in range(2):
            t_, o = sb(f"QT{s_}_{x_}", [128, 512], BF16, at=o)
            row.append(t_)
        QT.append(row)
    PT = []
    for s_ in range(4):
        t_, o = sb(f"PT{s_}", [128, 512], BF16, at=o)
        PT.append(t_)
    Zs = []
    for x_ in range(2):
        t_, o = sb(f"Zs{x_}", [128, 512], F32, at=o)
        Zs.append(t_)
    FT = []
    for i_ in range(4):
        t_, o = sb(f"FT{i_}", [128, 512], F32, at=o)
        FT.append(t_)
    FTo = []
    for i_ in range(2):
        t_, o = sb(f"FTo{i_}", [128, 512], F32, at=o)
        FTo.append(t_)
    OSB = [FT[2], FT[3]]
    Ocat = []
    for s_ in range(2):
        t_, o = sb(f"Ocat{s_}", [128, 512], BF16, at=o)
        Ocat.append(t_)
    gate_sb, o = sb("gate_sb", [128, 8, 32], F32, at=o)
    m8, o = sb("m8", [128, 8, 8], F32, at=o)
    selt, o = sb("selt", [128, 32], F32, at=o)
    NS = []
    for x_ in range(2):
        t_, o = sb(f"NS{x_}", [128, 4, 96], BF16, at=o)
        NS.append(t_)
    kmT, o = sb("kmT", [128, 32], BF16, at=o)
    km32, o = sb("km32", [128, 2], F32, at=o)
    STG, o = sb("STG", [128, 2048], F32, at=o)
    STG2, o = sb("STG2", [128, 2048], F32, at=o)
    P2END = o
    assert P2END <= 229376, P2END

    o = PH
    HNT, o = sb("HNT", [128, 8, NT], BF16, at=o)
    OT = HNT
    WO, o = sb("WO", [128, 8, D], BF16, at=o)
    W1e, W2e = [], []
    for s_ in range(2):
        t_, o = sb(f"W1e{s_}", [128, 8, 512], BF16, at=o)
        W1e.append(t_)
        t_, o = sb(f"W2e{s_}", [128, 4, D], BF16, at=o)
        W2e.append(t_)
    hnb = []
    for s_ in range(2):
        t_, o = sb(f"hnb{s_}", [128, D], BF16, at=o)
        hnb.append(t_)
    UT = []
    for s_ in range(2):
        t_, o = sb(f"UT{s_}", [128, 4, 512], BF16, at=o)
        UT.append(t_)
    RT = []
    for s_ in range(2):
        t_, o = sb(f"RT{s_}", [128, 512], BF16, at=o)
        RT.append(t_)
    assert o <= P2END - 2 * 8192, (o, P2END)
    YT = [STG, STG2]

    pb = [nc.alloc_psum_tensor(f"pb{i}", [128, 512], F32) for i in range(8)]
    psS = pb[0:4]
    psO = pb[4:6]
    psP = pb[6]
    psX = pb[7]
    psXb = psX[:, 256:512].bitcast(BF16)
    psT = pb[6][:, :].bitcast(BF16)
    psU = pb[0:2]
    psY = pb[2:4]

    ctx = {}

    def dma(eng, key, out, in_, reads, writes):
        return P.add(eng, lambda e: e.dma_start(out=out, in_=in_), reads=reads, writes=writes, dma=key)

    def allgather(name, in_ap, out_ap, reads, writes):
        return P.add("pool", lambda e: e.collective_compute(
            "AllGather", ALU.bypass, replica_groups=GROUPS, ins=[in_ap.opt()], outs=[out_ap.opt()]),
            reads=reads, writes=writes, cc=name)

    dma("sp", "c0", ident[:, :], ident_in, [], ["ident"])
    dma("sp", "c0", tri[:, :], tri_in, [], ["tri"])
    dma("sp", "c0", bsel[:, :], bsel_in, [], ["bsel"])
    dma("sp", "c0", btab[:, :], btab_in, [], ["btab"])
    dma("sp", "c0", augq1[:, :], augq1_in, [], ["augq1"])
    dma("sp", "c0", gains[:, :], gains_in, [], ["gains"])
    dma("sp", "c0", lamb[:, :], lamb_in, [], ["lamb"])
    P.add("dve", lambda e: e.memset(ones128[:, :], 1.0), writes=["ones128"])
    P.add("dve", lambda e: e.memset(epsc[:, :], EPS), writes=["epsc"])
    P.add("dve", lambda e: e.memset(small[:, :], 0.0), writes=["small"])
    P.add("dve", lambda e: e.tensor_tensor(out=FT[0][:, 0:64], in0=lamb[:, 0:64], in1=lamb[:, 64:128], op=ALU.mult),
          reads=["lamb"], writes=["ft0"])
    P.add("dve", lambda e: e.tensor_tensor(out=FT[0][:, 64:128], in0=lamb[:, 128:192], in1=lamb[:, 192:256], op=ALU.mult),
          reads=["lamb"], writes=["ft0b"])
    P.add("dve", lambda e: e.tensor_reduce(out=small[:, 34:36], in_=FT[0][:, 0:128].rearrange("p (a b) -> p a b", a=2),
                                           axis=AX.X, op=ALU.add),
          reads=["ft0", "ft0b", "small"], writes=["lam_s"])
    P.add("act", lambda e: e.activation(out=small[:, 36:38], in_=small[:, 34:36], func=AF.Exp),
          reads=["lam_s"], writes=["lam_e"])
    P.add("dve", lambda e: e.tensor_tensor(out=small[:, 38:39], in0=small[:, 37:38], in1=small[:, 36:37], op=ALU.subtract),
          reads=["lam_e"], writes=["lam_d"])
    P.add("dve", lambda e: e.tensor_scalar(out=small[:, 32:33], in0=small[:, 38:39], scalar1=-0.2, scalar2=None, op0=ALU.add),
          reads=["lam_d"], writes=["neglam"])
    P.add("dve", lambda e: e.tensor_scalar(out=small[:, 33:34], in0=gains[:, 32:33], scalar1=0.8, scalar2=None, op0=ALU.mult),
          reads=["gains", "small"], writes=["sub08"])
    for t in range(16):
        dma("sp", "xload", H[:, t, :], x_in[t * 128:(t + 1) * 128, :], [], [("H", t, 0), ("H", t, 1)])

    P.barrier()

    def rms_tile(t, slot):
        P.add("act", lambda e: e.activation(out=hnb[slot][:, :], in_=H[:, t, :], func=AF.Square,
                                            accum_out=small[:, t:t + 1]),
              reads=[("H", t, 0), ("H", t, 1), "small"], writes=[("hnb", slot), ("ssq", t)])
        P.add("act", lambda e: e.activation(out=small[:, 16 + t:17 + t], in_=small[:, t:t + 1], func=AF.Ln,
                                            bias=epsc[:, 0:1], scale=1.0 / D),
              reads=[("ssq", t), "epsc"], writes=[("rstd", t)])
        P.add("act", lambda e: e.activation(out=small[:, 16 + t:17 + t], in_=small[:, 16 + t:17 + t], func=AF.Exp, scale=-0.5),
              reads=[("rstd", t)], writes=[("rstd", t)])

    def norm_transpose(t):
        slot = t % 2
        P.add("dve", lambda e: e.memset(small[:, t:t + 1], 0.0), reads=[("ssq", t)], writes=[("ssq", t)])
        rms_tile(t, slot)
        P.add("dve", lambda e: e.tensor_scalar(out=hnb[slot][:, :], in0=H[:, t, :], scalar1=small[:, 16 + t:17 + t],
                                               scalar2=None, op0=ALU.mult),
              reads=[("H", t, 0), ("H", t, 1), ("rstd", t)], writes=[("hnb", slot)])
        for kc in range(8):
            P.add("pe", lambda e, kc=kc: e.transpose(out=psT[:, kc * 128:(kc + 1) * 128],
                                                     in_=hnb[slot][:, kc * 128:(kc + 1) * 128], identity=ident[:, :]),
                  reads=[("hnb", slot), "ident"], writes=["psT"])
        P.add("act", lambda e: e.copy(out=HNT[:, :, t * 128:(t + 1) * 128],
                                      in_=psT.rearrange("p (k q) -> p k q", k=8)),
              reads=["psT"], writes=[("HNT", t)])

    def phase1(l):
        for t in range(16):
            norm_transpose(t)
            if t % 4 == 3:
                i = t // 4
                dma("sp", f"htst{i}", HTin[l][i].ap().rearrange("(kc p) t -> p kc t", p=128),
                    HNT[:, :, i * 512:(i + 1) * 512], [("HNT", tt) for tt in range(4 * i, 4 * i + 4)],
                    [("HTin", l, i)])
                allgather(f"agh{l}_{i}", HTin[l][i].ap(), HT[l][i].ap(), [("HTin", l, i)], [("HT", l, i)])

    def load_cast_win(l, p):
        wsrc = win_in[l * 2 + p].rearrange("(kc p) n -> p kc n", p=128)
        for hf in range(2):
            stg = (STG if hf == 0 else STG2)[:, 0:4 * 384].rearrange("p (k n) -> p k n", k=4)
            dma("sp", f"stg{hf}", stg, wsrc[:, hf * 4:hf * 4 + 4, :], [], [("STG", hf)])
            for k4 in range(4):
                kc = hf * 4 + k4
                P.add("pool", lambda e, stg=stg, k4=k4, kc=kc: e.tensor_scalar(
                    out=WIN[:, kc, :], in0=stg[:, k4, :], scalar1=gains[:, l * 8 + kc:l * 8 + kc + 1],
                    scalar2=None, op0=ALU.mult),
                    reads=[("STG", hf), "gains"], writes=[("WIN", kc)])

    def phase2(l):
        V0 = Vb[:, 0:64 * 128].rearrange("p (t c) -> p t c", c=128)
        V1 = Vb[:, :].rearrange("p (t c) -> p t c", c=192)
        dma("sp", "kaug0", KT[0][64:96, :], kaug_in[l], [], [("KTaug", 0)])
        dma("sp", "kaug1", KT[1][0:32, :], kaug_in[l], [], [("KTaug", 1)])
        P.add("pool", lambda e: e.memset(KT[1][32:64, :], 0.0), writes=[("KTz", 1)])
        for s_ in range(2):
            for x_ in range(2):
                P.add("pool", lambda e, s_=s_, x_=x_: e.memset(QT[s_][x_][:, :], 0.0), writes=[("QT", s_, x_), ("QTaug", s_, x_)])
        if l == 1:
            P.add("pool", lambda e: e.memset(Vb[:, :], 0.0), writes=["Vall"])
            P.add("pool", lambda e: e.memset(V1[:, :, 64:65], 1.0), reads=["Vall"], writes=["Vall"])
            for x_ in range(2):
                P.add("pool", lambda e, x_=x_: e.memset(NS[x_][:, :, :], 0.0), writes=[("NS", x_)])
                P.add("pool", lambda e, x_=x_: e.memset(OSB[x_][:, :], 0.0), writes=[("OSB", x_), ("FT", 2 + x_)])
            P.add("pool", lambda e: e.memset(kmT[:, :], 0.0), writes=["kmT"])
        for p in range(2):
            load_cast_win(l, p)
            if l == 0:
                for s_ in range(2):
                    dma("sp", f"qaug{s_}0", QT[s_][0][64:96, :], qaug0_in[p], [("QTaug", s_, 0)], [("QTaug", s_, 0)])
                    dma("sp", f"qaug{s_}1", QT[s_][1][0:32, :], qaug0_in[p], [("QTaug", s_, 1)], [("QTaug", s_, 1)])
            for _ in chunk_proj(l, p, 0, V0, V1):
                pass
            for g in range(16):
                gen = chunk_proj(l, p, g + 1, V0, V1) if g + 1 < 16 else None
                chunk_attn(l, p, g, V0, V1, gen)
                if g % 4 == 3:
                    q = g // 4
                    allgather(f"ago{l}_{p}_{q}", OSin[l][p][q].ap(), OS[l][p][q * 512:(q + 1) * 512, :],
                              [("OSin", l, p, q)], [("OS", l, p, q)])

    bidx = [0]
    NSLOT = 4

    def chunk_proj(l, p, g, V0, V1):
        s = g % 2
        hsrc = HT[l][g % 4][(g // 4) * D:(g // 4 + 1) * D, :].rearrange("(kc p) t -> p kc t", p=128)
        dma("sp", f"hntc{s}", HNTc[s][:, :, :], hsrc, [("HT", l, g % 4)], [("HNTc", s)])
        win_r = [("WIN", kc) for kc in range(8)]
        for kc in range(8):
            P.add("pe", lambda e, kc=kc: e.matmul(psP[:, :], lhsT=WIN[:, kc, 0:128], rhs=HNTc[s][:, kc, :],
                                                  start=(kc == 0), stop=(kc == 7)),
                  reads=[("HNTc", s)] + win_r, writes=["psP"])
        P.add("dve", lambda e: e.tensor_copy(out=QT[s][0][0:64, :], in_=psP[0:64, :]), reads=["psP"], writes=[("QT", s, 0)])
        P.add("dve", lambda e: e.tensor_copy(out=QT[s][1][64:128, :], in_=psP[64:128, :]), reads=["psP"], writes=[("QT", s, 1)])
        yield
        for kc in range(8):
            P.add("pe", lambda e, kc=kc: e.matmul(psP[:, :], lhsT=WIN[:, kc, 128:256], rhs=HNTc[s][:, kc, :],
                                                  start=(kc == 0), stop=(kc == 7)),
                  reads=[("HNTc", s)] + win_r, writes=["psP"])
        P.add("dve", lambda e: e.tensor_copy(out=KT[0][0:64, g * 512:(g + 1) * 512], in_=psP[0:64, :]),
              reads=["psP"], writes=[("KT", 0, g)])
        P.add("dve", lambda e: e.tensor_copy(out=KT[1][64:128, g * 512:(g + 1) * 512], in_=psP[64:128, :]),
              reads=["psP"], writes=[("KT", 1, g)])
        if l == 1:
            P.add("dve", lambda e: e.tensor_reduce(out=km32[:, :], in_=psP[:, :].rearrange("p (a b) -> p a b", a=2),
                                                   axis=AX.X, op=ALU.add), reads=["psP"], writes=["km32"])
            P.add("dve", lambda e: e.tensor_scalar(out=kmT[:, 2 * g:2 * g + 2], in0=km32[:, :], scalar1=1.0 / 256,
                                                   scalar2=None, op0=ALU.mult), reads=["km32"], writes=["kmT"])
        yield
        for tt in range(4):
            for kc in range(8):
                P.add("pe", lambda e, kc=kc, tt=tt: e.matmul(psP[:, tt * 128:(tt + 1) * 128],
                                                             lhsT=HNTc[s][:, kc, tt * 128:(tt + 1) * 128],
                                                             rhs=WIN[:, kc, 256:384], start=(kc == 0), stop=(kc == 7)),
                      reads=[("HNTc", s)] + win_r, writes=["psP"])
            if tt == 1:
                yield
        psP4 = psP[:, :].rearrange("p (t c) -> p t c", t=4)
        if l == 0:
            P.add("dve", lambda e: e.tensor_copy(out=V0[:, 4 * g:4 * g + 4, :], in_=psP4), reads=["psP"], writes=[("V", g)])
        else:
            P.add("dve", lambda e: e.tensor_copy(out=V1[:, 4 * g:4 * g + 4, 0:64], in_=psP4[:, :, 0:64]),
                  reads=["psP", "Vall"], writes=[("V", g)])
            P.add("dve", lambda e: e.tensor_copy(out=V1[:, 4 * g:4 * g + 4, 128:192], in_=psP4[:, :, 64:128]),
                  reads=["psP", "Vall"], writes=[("Vb_", g)])
        yield
        if l == 1 and not os.environ.get("SKIP_GATE"):
            moba_gate(p, g, s)
        yield

    def chunk_attn(l, p, g, V0, V1, gen=None):
        s = g % 2
        nk = 4 * g + 4
        if l == 1 and os.environ.get("SKIP_ATTN"):
            nk = 0
        items = [(kt, x_) for kt in range(nk) for x_ in range(2)]
        LOOK = 3
        slots = {}

        def emit_s(kt, x_):
            j = kt - 4 * g
            c0 = 128 * j if j >= 0 else 0
            b = bidx[0] % NSLOT
            bidx[0] += 1
            slots[(kt, x_)] = b
            kx = 96 if x_ == 0 else 128
            kr = [("KT", x_, kt // 4), ("KTaug", x_), ("QT", s, x_), ("QTaug", s, x_)] + ([("KTz", 1)] if x_ == 1 else [])
            ks = slice(kt * 128, (kt + 1) * 128)
            if j >= 0:
                P.add("pe", lambda e: e.matmul(
                    psS[b][:, c0:c0 + 128], lhsT=KT[x_][0:kx, ks], rhs=QT[s][x_][0:kx, c0:c0 + 128],
                    start=True, stop=False), reads=kr, writes=[("psS", b)])
                P.add("pe", lambda e: e.matmul(
                    psS[b][:, c0:c0 + 128], lhsT=ident[:, :], rhs=tri[:, :], start=False, stop=True),
                    reads=["ident", "tri"], writes=[("psS", b)])
                if c0 + 128 < 512:
                    P.add("pe", lambda e: e.matmul(
                        psS[b][:, c0 + 128:512], lhsT=KT[x_][0:kx, ks], rhs=QT[s][x_][0:kx, c0 + 128:512],
                        start=True, stop=True), reads=kr, writes=[("psS", b)])
            else:
                P.add("pe", lambda e: e.matmul(
                    psS[b][:, :], lhsT=KT[x_][0:kx, ks], rhs=QT[s][x_][0:kx, :], start=True, stop=True),
                    reads=kr, writes=[("psS", b)])
            col = (((l * 2 + p) * 2 + x_) * NDC) + (4 * g - kt + 3)
            P.add("act", lambda e: e.activation(
                out=PT[b][:, c0:512], in_=psS[b][:, c0:512], func=AF.Exp, bias=btab[:, col:col + 1], scale=SCALE),
                reads=[("psS", b), "btab"], writes=[("PT", b)])
            if l == 0:
                zeng = "dve" if x_ == 0 else "pool"
                if kt == 0:
                    P.add(zeng, lambda e: e.tensor_copy(out=Zs[x_][:, :], in_=PT[b][:, :]),
                          reads=[("PT", b)], writes=[("Zs", x_)])
                else:
                    P.add(zeng, lambda e: e.tensor_tensor(
                        out=Zs[x_][:, c0:512], in0=Zs[x_][:, c0:512], in1=PT[b][:, c0:512], op=ALU.add),
                        reads=[("PT", b), ("Zs", x_)], writes=[("Zs", x_)])

        def emit_pv(kt, x_):
            j = kt - 4 * g
            c0 = 128 * j if j >= 0 else 0
            b = slots[(kt, x_)]
            if l == 0:
                vl = V0[:, kt, :]
                mo = 128
            else:
                vl = V1[:, kt, 0:65] if x_ == 0 else V1[:, kt, 64:192]
                mo = 65 if x_ == 0 else 128
            P.add("pe", lambda e: e.matmul(
                psO[x_][0:mo, c0:512], lhsT=vl, rhs=PT[b][:, c0:512], start=(kt == 0), stop=(kt == nk - 1)),
                reads=[("PT", b), ("V", kt // 4), ("Vb_", kt // 4), "Vall"], writes=[("psO", x_)])

        for n in range(len(items) + LOOK):
            if n < len(items):
                emit_s(*items[n])
            if n - LOOK >= 0:
                emit_pv(*items[n - LOOK])
            if gen is not None and n >= 4 and n % 3 == 1:
                next(gen, None)
        if gen is not None:
            for _ in gen:
                pass
        oc = Ocat[s]
        if l == 1 and os.environ.get("SKIP_FIN"):
            pass
        elif l == 0:
            for x_ in range(2):
                P.add("dve", lambda e, x_=x_: e.tensor_copy(out=FTo[x_][:, :], in_=psO[x_][:, :]),
                      reads=[("psO", x_)], writes=[("FTo", x_)])
            for x_ in range(2):
                P.add("pe", lambda e, x_=x_: e.matmul(psX[:, :], lhsT=ones128[:, :], rhs=Zs[x_][:, :], start=True, stop=True),
                      reads=[("Zs", x_), "ones128"], writes=["psX"])
                P.add("dve", lambda e, x_=x_: e.reciprocal(out=FT[x_][:, :], in_=psX[:, :]), reads=["psX"], writes=[("FT", x_)])
                P.add("dve", lambda e, x_=x_: e.tensor_tensor(out=FT[x_][:, :], in0=FTo[x_][:, :], in1=FT[x_][:, :], op=ALU.mult),
                      reads=[("FTo", x_), ("FT", x_)], writes=[("FT", x_)])
            P.add("dve", lambda e: e.scalar_tensor_tensor(out=FT[2][:, :], in0=FT[1][:, :], scalar=small[:, 32:33], in1=FT[0][:, :],
                                                          op0=ALU.mult, op1=ALU.add),
                  reads=[("FT", 0), ("FT", 1), "neglam"], writes=[("FT", 2)])
            P.add("pool", lambda e: e.tensor_tensor(out=FT[3][:, :], in0=FT[2][:, :], in1=FT[2][:, :], op=ALU.mult),
                  reads=[("FT", 2)], writes=[("FT", 3)])
            P.add("pe", lambda e: e.matmul(psX[:, :], lhsT=ones128[:, :], rhs=FT[3][:, :], start=True, stop=True),
                  reads=[("FT", 3), "ones128"], writes=["psX"])
            P.add("act", lambda e: e.activation(out=FT[3][:, :], in_=psX[:, :], func=AF.Ln, bias=epsc[:, 0:1], scale=1.0 / 128),
                  reads=["psX", "epsc"], writes=[("FT", 3)])
            P.add("act", lambda e: e.activation(out=FT[3][:, :], in_=FT[3][:, :], func=AF.Exp, scale=-0.5),
                  reads=[("FT", 3)], writes=[("FT", 3)])
            P.add("dve", lambda e: e.tensor_tensor(out=oc[:, :], in0=FT[2][:, :], in1=FT[3][:, :], op=ALU.mult),
                  reads=[("FT", 2), ("FT", 3)], writes=[("Ocat", s)])
        else:
            for x_ in range(2):
                mo = 65 if x_ == 0 else 128
                rows = slice(0, 64) if x_ == 0 else slice(64, 128)
                P.add("dve", lambda e, x_=x_, mo=mo: e.tensor_copy(out=OSB[x_][0:mo, :], in_=psO[x_][0:mo, :]),
                      reads=[("psO", x_)], writes=[("OSB", x_)])
                P.add("pe", lambda e, x_=x_: e.matmul(psX[:, :], lhsT=bsel[:, x_ * 128:(x_ + 1) * 128], rhs=OSB[x_][:, :],
                                                      start=True, stop=True), reads=[("OSB", x_), "bsel"], writes=["psX"])
                P.add("dve", lambda e, x_=x_, rows=rows: e.reciprocal(out=FT[x_][rows, :], in_=psX[rows, :]),
                      reads=["psX"], writes=[("FT", x_)])
                P.add("dve", lambda e, x_=x_, rows=rows: e.tensor_tensor(out=oc[rows, :], in0=OSB[x_][rows, :], in1=FT[x_][rows, :],
                                                                        op=ALU.mult),
                      reads=[("OSB", x_), ("FT", x_)], writes=[("Ocat", s, x_)])
        dma("sp", f"ost{s}", OSin[l][p][g // 4][:, (g % 4) * 512:(g % 4 + 1) * 512], oc[:, :],
            [("Ocat", s), ("Ocat", s, 0), ("Ocat", s, 1)], [("OSin", l, p, g // 4)])

    def moba_gate(p, g, s):
        gbank = [psX, psP]
        gkey = ["psX", "psP"]
        for x_ in range(2):
            rows = slice(0, 64) if x_ == 0 else slice(64, 128)
            for qt in range(4):
                P.add("pe", lambda e, x_=x_, qt=qt, rows=rows: e.matmul(
                    gbank[x_][:, qt * 32:(qt + 1) * 32], lhsT=QT[s][x_][rows, qt * 128:(qt + 1) * 128], rhs=kmT[rows, :],
                    start=True, stop=True), reads=[("QT", s, x_), "kmT"], writes=[gkey[x_]])
        P.add("dve", lambda e: e.memset(gate_sb[:, :, :], -1e30), writes=["gate"])
        gsv = gate_sb[:, :, :].rearrange("p (x h q) n -> p x h q n", x=2, h=2)
        for hf in range(2):
            jb = 2 * g + hf
            if jb > 0:
                for x_ in range(2):
                    psg = gbank[x_][:, 0:128].rearrange("p (h q n) -> p h q n", h=2, q=2)
                    P.add("dve", lambda e, hf=hf, jb=jb, x_=x_, psg=psg: e.tensor_copy(out=gsv[:, x_, hf, :, 0:jb], in_=psg[:, hf, :, 0:jb]),
                          reads=[gkey[x_], "gate"], writes=["gate"])
        glev = int(os.environ.get("GATE_LEVEL", "3"))
        if glev < 2:
            return
        for x_ in range(2):
            base = 64 if x_ == 0 else 0
            for qt in range(4):
                xq = x_ * 4 + qt
                jb = 2 * g + qt // 2
                ai = (p * 2 + x_) * 4 + qt
                P.add("dve", lambda e, xq=xq: e.max(out=m8[:, xq, :], in_=gate_sb[:, xq, :]), reads=["gate"], writes=[("m8", xq)])
                P.add("dve", lambda e, xq=xq: e.tensor_scalar(out=selt[:, :], in0=gate_sb[:, xq, :], scalar1=m8[:, xq, 2:3],
                                                              scalar2=None, op0=ALU.is_ge),
                      reads=["gate", ("m8", xq)], writes=["selt"])
                P.add("dve", lambda e, x_=x_, qt=qt, base=base, ai=ai: e.tensor_scalar(
                    out=NS[x_][:, qt, base:base + 32], in0=selt[:, :], scalar1=-NEG, scalar2=augq1[:, ai:ai + 1],
                    op0=ALU.mult, op1=ALU.add), reads=["selt", "augq1", ("NS", x_)], writes=[("NS", x_)])
                P.add("dve", lambda e, x_=x_, qt=qt, base=base, ai=ai, jb=jb: e.tensor_scalar(
                    out=NS[x_][:, qt, base + jb:base + jb + 1], in0=augq1[:, ai:ai + 1], scalar1=-NEG, scalar2=None,
                    op0=ALU.add), reads=["augq1", ("NS", x_)], writes=[("NS", x_)])
        if glev < 3:
            return
        for x_ in range(2):
            rows = slice(64, 96) if x_ == 0 else slice(0, 32)
            for qt in range(4):
                P.add("pe", lambda e, x_=x_, qt=qt: e.transpose(out=psXb[0:96, qt * 128:(qt + 1) * 128],
                                                                in_=NS[x_][:, qt, :], identity=ident[:, :]),
                      reads=[("NS", x_), "ident"], writes=["psX"])
            P.add("dve", lambda e, x_=x_, rows=rows: e.tensor_copy(out=QT[s][x_][rows, :], in_=psXb[rows, :]),
                  reads=["psX"], writes=[("QTaug", s, x_)])

    def phase3(l, do_ffn=True):
        def rank_of(e):
            if "rank" not in ctx:
                ctx["rank"] = e.partition_id() % 4
            return ctx["rank"]
        for kc in range(8):
            src, p = kc // 2, kc % 2
            P.add("pool", lambda e, kc=kc, src=src, p=p: e.dma_start(
                out=OT[:, kc, :], in_=OS[l][p][bass.ds(rank_of(e) * 512 + src * 128, 128), :]),
                reads=[("OS", l, p, q) for q in range(4)], writes=[("OT", kc)], dma="otld")
        wsrc = wout_in[l].rearrange("(kc p) n -> p kc n", p=128)
        for i in range(4):
            stg = (STG if i % 2 == 0 else STG2)[:, :].rearrange("p (k n) -> p k n", k=2)
            dma("sp", f"stg{i % 2}", stg, wsrc[:, 2 * i:2 * i + 2, :], [], [("STG", i % 2)])
            if l == 0:
                P.add("pool", lambda e, stg=stg, i=i: e.tensor_scalar(out=WO[:, 2 * i:2 * i + 2, :], in0=stg, scalar1=small[:, 33:34],
                                                                      scalar2=None, op0=ALU.mult),
                      reads=[("STG", i % 2), "sub08"], writes=[("WO", i)])
            else:
                P.add("pool", lambda e, stg=stg, i=i: e.tensor_copy(out=WO[:, 2 * i:2 * i + 2, :], in_=stg),
                      reads=[("STG", i % 2)], writes=[("WO", i)])
        yb = 0
        for t in range(16):
            for n2 in range(2):
                b = yb % 2
                yb += 1
                for kc in range(8):
                    P.add("pe", lambda e, kc=kc, b=b, t=t, n2=n2: e.matmul(
                        psY[b][:, :], lhsT=OT[:, kc, t * 128:(t + 1) * 128], rhs=WO[:, kc, n2 * 512:(n2 + 1) * 512],
                        start=(kc == 0), stop=(kc == 7)), reads=[("OT", k_) for k_ in range(8)] + [("WO", kc // 2)], writes=[("psY", b)])
                P.add("dve", lambda e, b=b, t=t, n2=n2: e.tensor_tensor(
                    out=H[:, t, n2 * 512:(n2 + 1) * 512], in0=H[:, t, n2 * 512:(n2 + 1) * 512], in1=psY[b][:, :], op=ALU.add),
                    reads=[("psY", b), ("H", t, n2)], writes=[("H", t, n2)])
        if not do_ffn:
            return
        P.barrier()
        for t in range(16):
            norm_transpose(t)
        w1src = wff1_in[l].rearrange("(kc p) n -> p kc n", p=128)
        w2src = wff2_in[l].rearrange("(fc p) n -> p fc n", p=128)
        ub = 0
        for ei in range(8):
            ws = ei % 2
            for hf in range(2):
                stg = (STG if hf == 0 else STG2)[:, :].rearrange("p (k n) -> p k n", k=4)
                dma("sp", f"stg{hf}", stg, w1src[:, hf * 4:hf * 4 + 4, ei * 512:(ei + 1) * 512], [], [("STG", hf)])
                for k4 in range(4):
                    kc = hf * 4 + k4
                    P.add("pool", lambda e, stg=stg, k4=k4, kc=kc, ws=ws: e.tensor_scalar(
                        out=W1e[ws][:, kc, :], in0=stg[:, k4, :], scalar1=gains[:, 16 + l * 8 + kc:17 + l * 8 + kc],
                        scalar2=None, op0=ALU.mult), reads=[("STG", hf), "gains"], writes=[("W1e", ws)])
            for hf in range(2):
                stg = (STG if hf == 0 else STG2)[:, :].rearrange("p (k n) -> p k n", k=2)
                dma("sp", f"stg{hf}", stg, w2src[:, ei * 4 + hf * 2:ei * 4 + hf * 2 + 2, :], [], [("STG", hf)])
                P.add("pool", lambda e, stg=stg, hf=hf, ws=ws: e.tensor_copy(out=W2e[ws][:, 2 * hf:2 * hf + 2, :], in_=stg),
                      reads=[("STG", hf)], writes=[("W2e", ws)])
            for c in range(4):
                us = (ei * 4 + c) % 2
                for fc in range(4):
                    b = ub % 2
                    ub += 1
                    for kc in range(8):
                        P.add("pe", lambda e, kc=kc, b=b, fc=fc, c=c, ws=ws: e.matmul(
                            psU[b][:, :], lhsT=W1e[ws][:, kc, fc * 128:(fc + 1) * 128], rhs=HNT[:, kc, c * 512:(c + 1) * 512],
                            start=(kc == 0), stop=(kc == 7)),
                            reads=[("W1e", ws)] + [("HNT", tt) for tt in range(4 * c, 4 * c + 4)], writes=[("psU", b)])
                    P.add("act", lambda e, b=b: e.activation(out=RT[b][:, :], in_=psU[b][:, :], func=AF.Relu),
                          reads=[("psU", b)], writes=[("RT", b)])
                    P.add("dve", lambda e, b=b, us=us, fc=fc: e.tensor_tensor(out=UT[us][:, fc, :], in0=RT[b][:, :], in1=RT[b][:, :],
                                                                             op=ALU.mult),
                          reads=[("RT", b)], writes=[("UT", us, fc)])
                for tt in range(4):
                    t = 4 * c + tt
                    for n2 in range(2):
                        b = yb % 2
                        yb += 1
                        for fc in range(4):
                            P.add("pe", lambda e, fc=fc, b=b, tt=tt, n2=n2, us=us, ws=ws: e.matmul(
                                psY[b][:, :], lhsT=UT[us][:, fc, tt * 128:(tt + 1) * 128], rhs=W2e[ws][:, fc, n2 * 512:(n2 + 1) * 512],
                                start=(fc == 0), stop=(fc == 3)),
                                reads=[("UT", us, fc), ("W2e", ws)], writes=[("psY", b)])
                        P.add("dve", lambda e, b=b, t=t, n2=n2: e.tensor_tensor(
                            out=H[:, t, n2 * 512:(n2 + 1) * 512], in0=H[:, t, n2 * 512:(n2 + 1) * 512], in1=psY[b][:, :], op=ALU.add),
                            reads=[("psY", b), ("H", t, n2)], writes=[("H", t, n2)])

    def final_out(with_norm=True):
        if with_norm:
            dma("sp", "stg0", STG[:, 0:1024], fnorm_in, [], ["fnb"])
        for t in range(16):
            slot = t % 2
            yt = STG2[:, slot * 1024:(slot + 1) * 1024]
            if with_norm:
                P.add("dve", lambda e, t=t: e.memset(small[:, t:t + 1], 0.0), reads=[("ssq", t)], writes=[("ssq", t)])
                rms_tile(t, slot)
                P.add("dve", lambda e, t=t, yt=yt: e.scalar_tensor_tensor(
                    out=yt, in0=H[:, t, :], scalar=small[:, 16 + t:17 + t], in1=STG[:, 0:1024], op0=ALU.mult, op1=ALU.mult),
                    reads=[("H", t, 0), ("H", t, 1), ("rstd", t), "fnb"], writes=[("yt", slot)])
            else:
                P.add("dve", lambda e, t=t, yt=yt: e.tensor_copy(out=yt, in_=H[:, t, :]),
                      reads=[("H", t, 0), ("H", t, 1)], writes=[("yt", slot)])
            dma("sp", f"yst{slot}", y_out[t * 128:(t + 1) * 128, :], yt, [("yt", slot)], [("yout", t)])

    phase1(0)
    P.barrier()
    if stage != "F0":
        phase2(0)
        P.barrier()
    if stage == "F0":
        phase3(0)
        P.barrier()
        final_out(with_norm=False)
    elif stage == "A0":
        phase3(0, do_ffn=False)
        P.barrier()
        final_out(with_norm=False)
    elif stage == "L0":
        phase3(0)
        P.barrier()
        final_out(with_norm=False)
    else:
        phase3(0)
        P.barrier()
        phase1(1)
        P.barrier()
        phase2(1)
        P.barrier()
        if stage == "A1":
            phase3(1, do_ffn=False)
            P.barrier()
            final_out(with_norm=False)
        else:
            phase3(1)
            P.barrier()
            final_out(with_norm=True)
    P.barrier()
    P.finalize()

    with ExitStack() as st:
        sems = {}
        for e in Prog.ENGS:
            sems[("eng", e)] = st.enter_context(nc.semaphore(f"s_{e}"))
        for k in P.dma_keys:
            sems[("dma", k)] = st.enter_context(nc.semaphore(f"d_{k}"))
        for k in P.cc_keys:
            sems[("cc", k)] = st.enter_context(nc.semaphore(f"c_{k}"))
        block = st.enter_context(nc.Block())

        @block.sync
        def _(e):
            P.emit("sp", e, sems)

        @block.scalar
        def _(e):
            P.emit("act", e, sems)

        @block.vector
        def _(e):
            P.emit("dve", e, sems)

        @block.gpsimd
        def _(e):
            P.emit("pool", e, sems)

        @block.tensor
        def _(e):
            P.emit("pe", e, sems)
    return nc


def _slopes(n):
    return np.array([2.0 ** (-8.0 * (h + 1) / n) for h in range(n)], dtype=np.float64)


def _consts(r):
    bf = ml_dtypes.bfloat16
    s0 = _slopes(8)
    s1 = _slopes(16)
    ident = np.eye(128, dtype=np.float32).astype(bf)
    ki = np.arange(128)[:, None]
    qi = np.arange(128)[None, :]
    tri = np.where(qi < ki, NEG, 0.0).astype(np.float32).astype(bf)
    kaug = np.zeros((2, 32, T), np.float32)
    kaug[0, 0, :] = 1.0
    kaug[1, np.arange(T) // 256, np.arange(T)] = 1.0
    kaug = kaug.astype(bf)
    qaug0 = np.zeros((2, 32, 512), np.float32)
    btab = np.zeros((128, 2, 2, 2, NDC), np.float64)
    augq1 = np.zeros((128, 2, 2, 4), np.float64)
    pp = np.arange(128, dtype=np.float64)
    dc = np.arange(NDC, dtype=np.float64) - 3.0
    for p in range(2):
        s = s0[2 * r + p]
        qaug0[p, 0, :] = -s * np.arange(512) / SCALE
        for x in range(2):
            btab[:, 0, p, x, :] = s * (pp[:, None] - 128.0 * dc[None, :])
            s_ = s1[4 * r + 2 * p + x]
            btab[:, 1, p, x, :] = s_ * (pp[:, None] - 128.0 * dc[None, :])
            for qt in range(4):
                augq1[:, p, x, qt] = -s_ * (qt * 128 + pp) / SCALE + NEG
    bsel = np.zeros((128, 2, 128), np.float32)
    bsel[64, 0, 0:64] = 1.0
    bsel[0, 1, 64:128] = 1.0
    return dict(ident=ident, trimask=tri, kaug=kaug, qaug0=qaug0.astype(bf),
                biastab=btab.reshape(128, -1).astype(np.float32), augq1=augq1.reshape(128, -1).astype(np.float32),
                bsel=bsel.reshape(128, 256))


_NC_CACHE = {}


def kernel(x, attn_norm, w_in, w_out, diff_lambda, diff_subln, mlp_norm, w_ff1, w_ff2, final_norm, _stage="full"):
    x = np.asarray(x, np.float32)
    attn_norm = np.asarray(attn_norm, np.float32)
    w_in = np.asarray(w_in, np.float32)
    w_out = np.ascontiguousarray(np.asarray(w_out, np.float32))
    diff_lambda = np.asarray(diff_lambda, np.float32)
    diff_subln = np.asarray(diff_subln, np.float32)
    mlp_norm = np.asarray(mlp_norm, np.float32)
    w_ff1 = np.ascontiguousarray(np.asarray(w_ff1, np.float32))
    w_ff2 = np.ascontiguousarray(np.asarray(w_ff2, np.float32))
    final_norm = np.asarray(final_norm, np.float32)

    if _stage not in _NC_CACHE:
        _NC_CACHE[_stage] = build_program(_stage)
    nc = _NC_CACHE[_stage]

    gains = np.zeros((128, 64), np.float32)
    for l in range(2):
        gains[:, l * 8:(l + 1) * 8] = attn_norm[l].reshape(8, 128).T
        gains[:, 16 + l * 8:16 + (l + 1) * 8] = mlp_norm[l].reshape(8, 128).T
    gains[:, 32] = diff_subln[0]
    fnb = np.ascontiguousarray(np.broadcast_to(final_norm[None, :], (128, D)))
    lamb = np.ascontiguousarray(np.broadcast_to(diff_lambda[0].reshape(1, 256), (128, 256)))

    in_maps = []
    for c in range(8):
        b, r = c // 4, c % 4
        win = np.zeros((4, D, 384), np.float32)
        for l in range(2):
            for p in range(2):
                c0 = 256 * r + 128 * p
                for j in range(3):
                    win[l * 2 + p, :, j * 128:(j + 1) * 128] = w_in[l, :, j * D + c0:j * D + c0 + 128]
        m = dict(x=np.ascontiguousarray(x[b, r * NT:(r + 1) * NT, :]), win=win, wout=w_out, wff1=w_ff1, wff2=w_ff2,
                 gains=gains, fnormb=fnb, lamb=lamb)
        m.update(_consts(r))
        in_maps.append(m)
    res = run_bass_kernel_spmd(nc, in_maps, core_ids=list(range(8)))
    out = np.zeros((2, T, D), np.float32)
    for c in range(8):
        b, r = c // 4, c % 4
        out[b, r * NT:(r + 1) * NT, :] = np.asarray(res.results[c]["y"], dtype=np.float32)
    return out
```

```python
import math
import os
from contextlib import ExitStack

import numpy as np
import ml_dtypes
import concourse.bass as bass
import concourse.mybir as mybir
from concourse.bass_utils import run_bass_kernel_spmd

F32 = mybir.dt.float32
BF16 = mybir.dt.bfloat16
ALU = mybir.AluOpType
AF = mybir.ActivationFunctionType
AX = mybir.AxisListType

D = 1024
T = 8192
NT = 2048
DFF = 4096
EPS = 1e-6
SCALE = 0.125
NEG = -30000.0
NDC = 67
GROUPS = [[0, 1, 2, 3], [4, 5, 6, 7]]


class Op:
    __slots__ = ("eng", "fn", "deps", "dma", "cc", "signal", "count", "idx")

    def __init__(self, eng, fn, dma=None, cc=None):
        self.eng = eng
        self.fn = fn
        self.deps = set()
        self.dma = dma
        self.cc = cc
        self.signal = False
        self.count = 0
        self.idx = 0


class Prog:
    ENGS = ["sp", "act", "dve", "pool", "pe"]

    def __init__(self):
        self.ops = {e: [] for e in self.ENGS}
        self.last_w = {}
        self.readers = {}
        self.dma_keys = {}
        self.cc_keys = []
        self.n = 0

    def add(self, eng, fn, reads=(), writes=(), dma=None, cc=None):
        op = Op(eng, fn, dma=dma, cc=cc)
        op.idx = self.n
        self.n += 1
        deps = set()
        for r in reads:
            w = self.last_w.get(r)
            if w is not None:
                deps.add(w)
        for w_ in writes:
            w = self.last_w.get(w_)
            if w is not None:
                deps.add(w)
            for rd in self.readers.get(w_, ()):
                deps.add(rd)
        deps.discard(op)
        if eng == "pe":
            deps = {d for d in deps if not (d.eng == "pe" and d.dma is None and d.cc is None)}
        op.deps = deps
        for r in reads:
            self.readers.setdefault(r, []).append(op)
        for w_ in writes:
            self.last_w[w_] = op
            self.readers[w_] = []
        self.ops[eng].append(op)
        if dma is not None:
            self.dma_keys.setdefault(dma, 0)
        if cc is not None:
            self.cc_keys.append(cc)
        return op

    def barrier(self):
        lasts = []
        for e in self.ENGS:
            for op in reversed(self.ops[e]):
                if op.dma is None and op.cc is None and op.fn is not None:
                    lasts.append(op)
                    break
        seen = {}
        for e in self.ENGS:
            for op in self.ops[e]:
                if op.dma is not None:
                    seen[op.dma] = op
                if op.cc is not None:
                    seen[("cc", op.cc)] = op
        lasts += list(seen.values())
        for e in self.ENGS:
            b = Op(e, None)
            b.idx = self.n
            self.n += 1
            b.deps = set(lasts)
            self.ops[e].append(b)
        self.last_w = {}
        self.readers = {}

    def finalize(self):
        for e in self.ENGS:
            for op in self.ops[e]:
                for d in op.deps:
                    d.signal = True
        cnt = {e: 0 for e in self.ENGS}
        dcnt = {k: 0 for k in self.dma_keys}
        for e in self.ENGS:
            for op in self.ops[e]:
                if op.dma is not None:
                    dcnt[op.dma] += 16
                    op.count = dcnt[op.dma]
                elif op.cc is not None:
                    op.count = 1
                elif op.signal and op.fn is not None:
                    cnt[e] += 1
                    op.count = cnt[e]

    def emit(self, eng_name, eng, sems):
        waited = {}
        for op in self.ops[eng_name]:
            need = {}
            for d in op.deps:
                if d.dma is not None:
                    key = ("dma", d.dma)
                elif d.cc is not None:
                    key = ("cc", d.cc)
                else:
                    key = ("eng", d.eng)
                if d.count > need.get(key, 0):
                    need[key] = d.count
            for key, val in need.items():
                if waited.get(key, 0) >= val:
                    continue
                eng.wait_ge(sems[key], val)
                waited[key] = val
            if op.fn is None:
                continue
            ins = op.fn(eng)
            if op.dma is not None:
                ins.then_inc(sems[("dma", op.dma)], 16)
            elif op.cc is not None:
                ins.then_inc(sems[("cc", op.cc)])
            elif op.signal:
                ins.then_inc(sems[("eng", eng_name)], 1)


def build_program(stage="full"):
    nc = bass.Bass("TRN2", target_bir_lowering=False)
    P = Prog()

    def ext_in(name, shape, dt):
        return nc.dram_tensor(name, list(shape), dt, kind="ExternalInput").ap()

    x_in = ext_in("x", [NT, D], F32)
    win_in = ext_in("win", [4, D, 384], F32)
    wout_in = ext_in("wout", [2, D, D], F32)
    wff1_in = ext_in("wff1", [2, D, DFF], F32)
    wff2_in = ext_in("wff2", [2, DFF, D], F32)
    gains_in = ext_in("gains", [128, 64], F32)
    fnorm_in = ext_in("fnormb", [128, D], F32)
    lamb_in = ext_in("lamb", [128, 256], F32)
    ident_in = ext_in("ident", [128, 128], BF16)
    tri_in = ext_in("trimask", [128, 128], BF16)
    kaug_in = ext_in("kaug", [2, 32, T], BF16)
    qaug0_in = ext_in("qaug0", [2, 32, 512], BF16)
    btab_in = ext_in("biastab", [128, 8 * NDC], F32)
    augq1_in = ext_in("augq1", [128, 16], F32)
    bsel_in = ext_in("bsel", [128, 256], F32)
    y_out = nc.dram_tensor("y", [NT, D], F32, kind="ExternalOutput").ap()

    HTin = [[nc.dram_tensor(f"htin{l}_{i}", [D, 512], BF16) for i in range(4)] for l in range(2)]
    HT = [[nc.dram_tensor(f"ht{l}_{i}", [4 * D, 512], BF16) for i in range(4)] for l in range(2)]
    OSin = [[[nc.dram_tensor(f"osin{l}_{p}_{q}", [128, 2048], BF16) for q in range(4)]
             for p in range(2)] for l in range(2)]
    OS = [[nc.dram_tensor(f"os{l}_{p}", [2048, 2048], BF16) for p in range(2)] for l in range(2)]

    off = [16512]

    def sb(name, shape, dt, at=None):
        nbytes = int(np.prod(shape[1:])) * (4 if dt == F32 else 2)
        if at is None:
            o = off[0]
            off[0] += (nbytes + 31) // 32 * 32
        else:
            o = at
        return nc.alloc_sbuf_tensor_at(name, list(shape), dt, offset=o), o + (nbytes + 31) // 32 * 32

    H, _ = sb("H", [128, 16, D], F32)
    ident, _ = sb("identt", [128, 128], BF16)
    tri, _ = sb("trit", [128, 128], BF16)
    ones128, _ = sb("ones128", [128, 128], F32)
    bsel, _ = sb("bselt", [128, 256], F32)
    btab, _ = sb("btab", [128, 8 * NDC], F32)
    augq1, _ = sb("augq1t", [128, 16], F32)
    gains, _ = sb("gainst", [128, 64], F32)
    lamb, _ = sb("lambt", [128, 256], F32)
    small, _ = sb("small", [128, 64], F32)
    epsc, _ = sb("epsc", [128, 1], F32)
    PH = off[0]

    o = PH
    KT = []
    for x_ in range(2):
        t_, o = sb(f"KT{x_}", [128, T], BF16, at=o)
        KT.append(t_)
    Vb, o = sb("Vb", [128, 64 * 192], BF16, at=o)
    WIN, o = sb("WIN", [128, 8, 384], BF16, at=o)
    HNTc = []
    for s_ in range(2):
        t_, o = sb(f"HNTc{s_}", [128, 8, 512], BF16, at=o)
        HNTc.append(t_)
    QT = []
    for s_ in range(2):
        row = []
        for x_ in range(2):
            t_, o = sb(f"QT{s_}_{x_}", [128, 512], BF16, at=o)
            row.append(t_)
        QT.append(row)
    PT = []
    for s_ in range(4):
        t_, o = sb(f"PT{s_}", [128, 512], BF16, at=o)
        PT.append(t_)
    Zs = []
    for x_ in range(2):
        row = []
        for par in range(2):
            t_, o = sb(f"Zs{x_}_{par}", [128, 512], F32, at=o)
            row.append(t_)
        Zs.append(row)
    FT = []
    for i_ in range(4):
        t_, o = sb(f"FT{i_}", [128, 512], F32, at=o)
        FT.append(t_)
    FTo = []
    for i_ in range(2):
        t_, o = sb(f"FTo{i_}", [128, 512], F32, at=o)
        FTo.append(t_)
    OSB = [FT[2], FT[3]]
    Ocat = []
    for s_ in range(2):
        t_, o = sb(f"Ocat{s_}", [128, 512], BF16, at=o)
        Ocat.append(t_)
    gate_sb, o = sb("gate_sb", [128, 8, 32], F32, at=o)
    m8, o = sb("m8", [128, 8, 8], F32, at=o)
    selt, o = sb("selt", [128, 32], F32, at=o)
    NS = []
    for x_ in range(2):
        t_, o = sb(f"NS{x_}", [128, 4, 96], BF16, at=o)
        NS.append(t_)
    kmT, o = sb("kmT", [128, 32], BF16, at=o)
    km32, o = sb("km32", [128, 2], F32, at=o)
    STG, o = sb("STG", [128, 2048], F32, at=o)
    STG2, o = sb("STG2", [128, 2048], F32, at=o)
    P2END = o
    assert P2END <= 229376, P2END

    o = PH
    HNT, o = sb("HNT", [128, 8, NT], BF16, at=o)
    OT = HNT
    WO, o = sb("WO", [128, 8, D], BF16, at=o)
    W1e, W2e = [], []
    for s_ in range(2):
        t_, o = sb(f"W1e{s_}", [128, 8, 512], BF16, at=o)
        W1e.append(t_)
        t_, o = sb(f"W2e{s_}", [128, 4, D], BF16, at=o)
        W2e.append(t_)
    hnb = []
    for s_ in range(2):
        t_, o = sb(f"hnb{s_}", [128, D], BF16, at=o)
        hnb.append(t_)
    UT = []
    for s_ in range(2):
        t_, o = sb(f"UT{s_}", [128, 4, 512], BF16, at=o)
        UT.append(t_)
    RT = []
    for s_ in range(2):
        t_, o = sb(f"RT{s_}", [128, 512], BF16, at=o)
        RT.append(t_)
    assert o <= P2END - 2 * 8192, (o, P2END)
    YT = [STG, STG2]

    pb = [nc.alloc_psum_tensor(f"pb{i}", [128, 512], F32) for i in range(8)]
    psS = pb[0:4]
    psO = pb[4:6]
    psP = pb[6]
    psX = pb[7]
    psXb = psX[:, 256:512].bitcast(BF16)
    psT = pb[6][:, :].bitcast(BF16)
    psU = pb[0:2]
    psY = pb[2:4]

    ctx = {}

    def dma(eng, key, out, in_, reads, writes):
        return P.add(eng, lambda e: e.dma_start(out=out, in_=in_), reads=reads, writes=writes, dma=key)

    def allgather(name, in_ap, out_ap, reads, writes):
        return P.add("pool", lambda e: e.collective_compute(
            "AllGather", ALU.bypass, replica_groups=GROUPS, ins=[in_ap.opt()], outs=[out_ap.opt()]),
            reads=reads, writes=writes, cc=name)

    dma("sp", "c0", ident[:, :], ident_in, [], ["ident"])
    dma("sp", "c0", tri[:, :], tri_in, [], ["tri"])
    dma("sp", "c0", bsel[:, :], bsel_in, [], ["bsel"])
    dma("sp", "c0", btab[:, :], btab_in, [], ["btab"])
    dma("sp", "c0", augq1[:, :], augq1_in, [], ["augq1"])
    dma("sp", "c0", gains[:, :], gains_in, [], ["gains"])
    dma("sp", "c0", lamb[:, :], lamb_in, [], ["lamb"])
    P.add("dve", lambda e: e.memset(ones128[:, :], 1.0), writes=["ones128"])
    P.add("dve", lambda e: e.memset(epsc[:, :], EPS), writes=["epsc"])
    P.add("dve", lambda e: e.memset(small[:, :], 0.0), writes=["small"])
    P.add("dve", lambda e: e.tensor_tensor(out=FT[0][:, 0:64], in0=lamb[:, 0:64], in1=lamb[:, 64:128], op=ALU.mult),
          reads=["lamb"], writes=["ft0"])
    P.add("dve", lambda e: e.tensor_tensor(out=FT[0][:, 64:128], in0=lamb[:, 128:192], in1=lamb[:, 192:256], op=ALU.mult),
          reads=["lamb"], writes=["ft0b"])
    P.add("dve", lambda e: e.tensor_reduce(out=small[:, 34:36], in_=FT[0][:, 0:128].rearrange("p (a b) -> p a b", a=2),
                                           axis=AX.X, op=ALU.add),
          reads=["ft0", "ft0b", "small"], writes=["lam_s"])
    P.add("act", lambda e: e.activation(out=small[:, 36:38], in_=small[:, 34:36], func=AF.Exp),
          reads=["lam_s"], writes=["lam_e"])
    P.add("dve", lambda e: e.tensor_tensor(out=small[:, 38:39], in0=small[:, 37:38], in1=small[:, 36:37], op=ALU.subtract),
          reads=["lam_e"], writes=["lam_d"])
    P.add("dve", lambda e: e.tensor_scalar(out=small[:, 32:33], in0=small[:, 38:39], scalar1=-0.2, scalar2=None, op0=ALU.add),
          reads=["lam_d"], writes=["neglam"])
    P.add("dve", lambda e: e.tensor_scalar(out=small[:, 33:34], in0=gains[:, 32:33], scalar1=0.8, scalar2=None, op0=ALU.mult),
          reads=["gains", "small"], writes=["sub08"])
    for t in range(16):
        dma("sp", "xload", H[:, t, :], x_in[t * 128:(t + 1) * 128, :], [], [("H", t, 0), ("H", t, 1)])

    P.barrier()

    def rms_tile(t, slot):
        P.add("act", lambda e: e.activation(out=hnb[slot][:, :], in_=H[:, t, :], func=AF.Square,
                                            accum_out=small[:, t:t + 1]),
              reads=[("H", t, 0), ("H", t, 1), "small"], writes=[("hnb", slot), ("ssq", t)])
        P.add("act", lambda e: e.activation(out=small[:, 16 + t:17 + t], in_=small[:, t:t + 1], func=AF.Ln,
                                            bias=epsc[:, 0:1], scale=1.0 / D),
              reads=[("ssq", t), "epsc"], writes=[("rstd", t)])
        P.add("act", lambda e: e.activation(out=small[:, 16 + t:17 + t], in_=small[:, 16 + t:17 + t], func=AF.Exp, scale=-0.5),
              reads=[("rstd", t)], writes=[("rstd", t)])

    def norm_transpose(t):
        slot = t % 2
        P.add("dve", lambda e: e.memset(small[:, t:t + 1], 0.0), reads=[("ssq", t)], writes=[("ssq", t)])
        rms_tile(t, slot)
        P.add("dve", lambda e: e.tensor_scalar(out=hnb[slot][:, :], in0=H[:, t, :], scalar1=small[:, 16 + t:17 + t],
                                               scalar2=None, op0=ALU.mult),
              reads=[("H", t, 0), ("H", t, 1), ("rstd", t)], writes=[("hnb", slot)])
        for kc in range(8):
            P.add("pe", lambda e, kc=kc: e.transpose(out=psT[:, kc * 128:(kc + 1) * 128],
                                                     in_=hnb[slot][:, kc * 128:(kc + 1) * 128], identity=ident[:, :]),
                  reads=[("hnb", slot), "ident"], writes=["psT"])
        P.add("act", lambda e: e.copy(out=HNT[:, :, t * 128:(t + 1) * 128],
                                      in_=psT.rearrange("p (k q) -> p k q", k=8)),
              reads=["psT"], writes=[("HNT", t)])

    def phase1(l):
        for t in range(16):
            norm_transpose(t)
            if t % 4 == 3:
                i = t // 4
                dma("sp", f"htst{i}", HTin[l][i].ap().rearrange("(kc p) t -> p kc t", p=128),
                    HNT[:, :, i * 512:(i + 1) * 512], [("HNT", tt) for tt in range(4 * i, 4 * i + 4)],
                    [("HTin", l, i)])
                allgather(f"agh{l}_{i}", HTin[l][i].ap(), HT[l][i].ap(), [("HTin", l, i)], [("HT", l, i)])

    def load_cast_win(l, p):
        wsrc = win_in[l * 2 + p].rearrange("(kc p) n -> p kc n", p=128)
        for hf in range(2):
            stg = (STG if hf == 0 else STG2)[:, 0:4 * 384].rearrange("p (k n) -> p k n", k=4)
            dma("sp", f"stg{hf}", stg, wsrc[:, hf * 4:hf * 4 + 4, :], [], [("STG", hf)])
            for k4 in range(4):
                kc = hf * 4 + k4
                P.add("pool", lambda e, stg=stg, k4=k4, kc=kc: e.tensor_scalar(
                    out=WIN[:, kc, :], in0=stg[:, k4, :], scalar1=gains[:, l * 8 + kc:l * 8 + kc + 1],
                    scalar2=None, op0=ALU.mult),
                    reads=[("STG", hf), "gains"], writes=[("WIN", kc)])

    def phase2(l):
        V0 = Vb[:, 0:64 * 128].rearrange("p (t c) -> p t c", c=128)
        V1 = Vb[:, :].rearrange("p (t c) -> p t c", c=192)
        dma("sp", "kaug0", KT[0][64:96, :], kaug_in[l], [], [("KTaug", 0)])
        dma("sp", "kaug1", KT[1][0:32, :], kaug_in[l], [], [("KTaug", 1)])
        P.add("pool", lambda e: e.memset(KT[1][32:64, :], 0.0), writes=[("KTz", 1)])
        for s_ in range(2):
            for x_ in range(2):
                P.add("pool", lambda e, s_=s_, x_=x_: e.memset(QT[s_][x_][:, :], 0.0), writes=[("QT", s_, x_), ("QTaug", s_, x_)])
        if l == 1:
            P.add("pool", lambda e: e.memset(Vb[:, :], 0.0), writes=["Vall"])
            P.add("pool", lambda e: e.memset(V1[:, :, 64:65], 1.0), reads=["Vall"], writes=["Vall"])
            for x_ in range(2):
                P.add("pool", lambda e, x_=x_: e.memset(NS[x_][:, :, :], 0.0), writes=[("NS", x_)])
                P.add("pool", lambda e, x_=x_: e.memset(OSB[x_][:, :], 0.0), writes=[("OSB", x_), ("FT", 2 + x_)])
            P.add("pool", lambda e: e.memset(kmT[:, :], 0.0), writes=["kmT"])
        for p in range(2):
            load_cast_win(l, p)
            if l == 0:
                for s_ in range(2):
                    dma("sp", f"qaug{s_}0", QT[s_][0][64:96, :], qaug0_in[p], [("QTaug", s_, 0)], [("QTaug", s_, 0)])
                    dma("sp", f"qaug{s_}1", QT[s_][1][0:32, :], qaug0_in[p], [("QTaug", s_, 1)], [("QTaug", s_, 1)])
            for _ in chunk_proj(l, p, 0, V0, V1):
                pass
            fin = None
            for g in range(16):
                gen = chunk_proj(l, p, g + 1, V0, V1) if g + 1 < 16 else None
                chunk_attn(l, p, g, V0, V1, gen, fin)
                fin = fin_gen(l, p, g)
                next(fin)
            for _ in fin:
                pass

    bidx = [0]
    NSLOT = 4

    def chunk_proj(l, p, g, V0, V1):
        s = g % 2
        hsrc = HT[l][g % 4][(g // 4) * D:(g // 4 + 1) * D, :].rearrange("(kc p) t -> p kc t", p=128)
        dma("sp", f"hntc{s}", HNTc[s][:, :, :], hsrc, [("HT", l, g % 4)], [("HNTc", s)])
        win_r = [("WIN", kc) for kc in range(8)]
        for kc in range(8):
            P.add("pe", lambda e, kc=kc: e.matmul(psP[:, :], lhsT=WIN[:, kc, 0:128], rhs=HNTc[s][:, kc, :],
                                                  start=(kc == 0), stop=(kc == 7)),
                  reads=[("HNTc", s)] + win_r, writes=["psP"])
        P.add("dve", lambda e: e.tensor_copy(out=QT[s][0][0:64, :], in_=psP[0:64, :]), reads=["psP"], writes=[("QT", s, 0)])
        P.add("dve", lambda e: e.tensor_copy(out=QT[s][1][64:128, :], in_=psP[64:128, :]), reads=["psP"], writes=[("QT", s, 1)])
        yield
        for kc in range(8):
            P.add("pe", lambda e, kc=kc: e.matmul(psP[:, :], lhsT=WIN[:, kc, 128:256], rhs=HNTc[s][:, kc, :],
                                                  start=(kc == 0), stop=(kc == 7)),
                  reads=[("HNTc", s)] + win_r, writes=["psP"])
        P.add("dve", lambda e: e.tensor_copy(out=KT[0][0:64, g * 512:(g + 1) * 512], in_=psP[0:64, :]),
              reads=["psP"], writes=[("KT", 0, g)])
        P.add("dve", lambda e: e.tensor_copy(out=KT[1][64:128, g * 512:(g + 1) * 512], in_=psP[64:128, :]),
              reads=["psP"], writes=[("KT", 1, g)])
        if l == 1:
            P.add("dve", lambda e: e.tensor_reduce(out=km32[:, :], in_=psP[:, :].rearrange("p (a b) -> p a b", a=2),
                                                   axis=AX.X, op=ALU.add), reads=["psP"], writes=["km32"])
            P.add("dve", lambda e: e.tensor_scalar(out=kmT[:, 2 * g:2 * g + 2], in0=km32[:, :], scalar1=1.0 / 256,
                                                   scalar2=None, op0=ALU.mult), reads=["km32"], writes=["kmT"])
        yield
        for tt in range(4):
            for kc in range(8):
                P.add("pe", lambda e, kc=kc, tt=tt: e.matmul(psP[:, tt * 128:(tt + 1) * 128],
                                                             lhsT=HNTc[s][:, kc, tt * 128:(tt + 1) * 128],
                                                             rhs=WIN[:, kc, 256:384], start=(kc == 0), stop=(kc == 7)),
                      reads=[("HNTc", s)] + win_r, writes=["psP"])
            if tt == 1:
                yield
        psP4 = psP[:, :].rearrange("p (t c) -> p t c", t=4)
        if l == 0:
            P.add("dve", lambda e: e.tensor_copy(out=V0[:, 4 * g:4 * g + 4, :], in_=psP4), reads=["psP"], writes=[("V", g)])
        else:
            P.add("dve", lambda e: e.tensor_copy(out=V1[:, 4 * g:4 * g + 4, 0:64], in_=psP4[:, :, 0:64]),
                  reads=["psP", "Vall"], writes=[("V", g)])
            P.add("dve", lambda e: e.tensor_copy(out=V1[:, 4 * g:4 * g + 4, 128:192], in_=psP4[:, :, 64:128]),
                  reads=["psP", "Vall"], writes=[("Vb_", g)])
        yield
        if l == 1 and not os.environ.get("SKIP_GATE"):
            moba_gate(p, g, s)
        yield

    def chunk_attn(l, p, g, V0, V1, gen=None, fin=None):
        s = g % 2
        nk = 4 * g + 4
        if l == 1 and os.environ.get("SKIP_ATTN"):
            nk = 0
        items = [(kt, x_) for kt in range(nk) for x_ in range(2)]
        LOOK = 3
        slots = {}

        def emit_s(kt, x_):
            j = kt - 4 * g
            c0 = 128 * j if j >= 0 else 0
            b = bidx[0] % NSLOT
            bidx[0] += 1
            slots[(kt, x_)] = b
            kx = 96 if x_ == 0 else 128
            kr = [("KT", x_, kt // 4), ("KTaug", x_), ("QT", s, x_), ("QTaug", s, x_)] + ([("KTz", 1)] if x_ == 1 else [])
            ks = slice(kt * 128, (kt + 1) * 128)
            if j >= 0:
                P.add("pe", lambda e: e.matmul(
                    psS[b][:, c0:c0 + 128], lhsT=KT[x_][0:kx, ks], rhs=QT[s][x_][0:kx, c0:c0 + 128],
                    start=True, stop=False), reads=kr, writes=[("psS", b)])
                P.add("pe", lambda e: e.matmul(
                    psS[b][:, c0:c0 + 128], lhsT=ident[:, :], rhs=tri[:, :], start=False, stop=True),
                    reads=["ident", "tri"], writes=[("psS", b)])
                if c0 + 128 < 512:
                    P.add("pe", lambda e: e.matmul(
                        psS[b][:, c0 + 128:512], lhsT=KT[x_][0:kx, ks], rhs=QT[s][x_][0:kx, c0 + 128:512],
                        start=True, stop=True), reads=kr, writes=[("psS", b)])
            else:
                P.add("pe", lambda e: e.matmul(
                    psS[b][:, :], lhsT=KT[x_][0:kx, ks], rhs=QT[s][x_][0:kx, :], start=True, stop=True),
                    reads=kr, writes=[("psS", b)])
            col = (((l * 2 + p) * 2 + x_) * NDC) + (4 * g - kt + 3)
            P.add("act", lambda e: e.activation(
                out=PT[b][:, c0:512], in_=psS[b][:, c0:512], func=AF.Exp, bias=btab[:, col:col + 1], scale=SCALE),
                reads=[("psS", b), "btab"], writes=[("PT", b)])
            if l == 0:
                zeng = "dve" if x_ == 0 else "pool"
                zt = Zs[x_][g % 2]
                zk = ("Zs", x_, g % 2)
                if kt == 0:
                    P.add(zeng, lambda e: e.tensor_copy(out=zt[:, :], in_=PT[b][:, :]),
                          reads=[("PT", b)], writes=[zk])
                else:
                    P.add(zeng, lambda e: e.tensor_tensor(
                        out=zt[:, c0:512], in0=zt[:, c0:512], in1=PT[b][:, c0:512], op=ALU.add),
                        reads=[("PT", b), zk], writes=[zk])

        def emit_pv(kt, x_):
            j = kt - 4 * g
            c0 = 128 * j if j >= 0 else 0
            b = slots[(kt, x_)]
            if l == 0:
                vl = V0[:, kt, :]
                mo = 128
            else:
                vl = V1[:, kt, 0:65] if x_ == 0 else V1[:, kt, 64:192]
                mo = 65 if x_ == 0 else 128
            P.add("pe", lambda e: e.matmul(
                psO[x_][0:mo, c0:512], lhsT=vl, rhs=PT[b][:, c0:512], start=(kt == 0), stop=(kt == nk - 1)),
                reads=[("PT", b), ("V", kt // 4), ("Vb_", kt // 4), "Vall"], writes=[("psO", x_)])

        for n in range(len(items) + LOOK):
            if n < len(items):
                emit_s(*items[n])
            if n - LOOK >= 0:
                emit_pv(*items[n - LOOK])
            if n >= 4 and n % 3 == 1 and gen is not None:
                next(gen, None)
            if n >= 2 and n % 3 == 2 and fin is not None:
                next(fin, None)
        if fin is not None:
            for _ in fin:
                pass
        if gen is not None:
            for _ in gen:
                pass

    def fin_gen(l, p, g):
        s = g % 2
        oc = Ocat[s]
        zp = g % 2
        if l == 0:
            for x_ in range(2):
                P.add("dve", lambda e, x_=x_: e.tensor_copy(out=FTo[x_][:, :], in_=psO[x_][:, :]),
                      reads=[("psO", x_)], writes=[("FTo", x_)])
            yield
            for x_ in range(2):
                P.add("pe", lambda e, x_=x_: e.matmul(psX[:, :], lhsT=ones128[:, :], rhs=Zs[x_][zp][:, :], start=True, stop=True),
                      reads=[("Zs", x_, zp), "ones128"], writes=["psX"])
                P.add("dve", lambda e, x_=x_: e.reciprocal(out=FT[x_][:, :], in_=psX[:, :]), reads=["psX"], writes=[("FT", x_)])
                yield
                P.add("dve", lambda e, x_=x_: e.tensor_tensor(out=FT[x_][:, :], in0=FTo[x_][:, :], in1=FT[x_][:, :], op=ALU.mult),
                      reads=[("FTo", x_), ("FT", x_)], writes=[("FT", x_)])
            P.add("dve", lambda e: e.scalar_tensor_tensor(out=FT[2][:, :], in0=FT[1][:, :], scalar=small[:, 32:33], in1=FT[0][:, :],
                                                          op0=ALU.mult, op1=ALU.add),
                  reads=[("FT", 0), ("FT", 1), "neglam"], writes=[("FT", 2)])
            P.add("pool", lambda e: e.tensor_tensor(out=FT[3][:, :], in0=FT[2][:, :], in1=FT[2][:, :], op=ALU.mult),
                  reads=[("FT", 2)], writes=[("FT", 3)])
            yield
            P.add("pe", lambda e: e.matmul(psX[:, :], lhsT=ones128[:, :], rhs=FT[3][:, :], start=True, stop=True),
                  reads=[("FT", 3), "ones128"], writes=["psX"])
            P.add("act", lambda e: e.activation(out=FT[3][:, :], in_=psX[:, :], func=AF.Ln, bias=epsc[:, 0:1], scale=1.0 / 128),
                  reads=["psX", "epsc"], writes=[("FT", 3)])
            P.add("act", lambda e: e.activation(out=FT[3][:, :], in_=FT[3][:, :], func=AF.Exp, scale=-0.5),
                  reads=[("FT", 3)], writes=[("FT", 3)])
            yield
            P.add("dve", lambda e: e.tensor_tensor(out=oc[:, :], in0=FT[2][:, :], in1=FT[3][:, :], op=ALU.mult),
                  reads=[("FT", 2), ("FT", 3)], writes=[("Ocat", s)])
        else:
            for x_ in range(2):
                mo = 65 if x_ == 0 else 128
                P.add("dve", lambda e, x_=x_, mo=mo: e.tensor_copy(out=OSB[x_][0:mo, :], in_=psO[x_][0:mo, :]),
                      reads=[("psO", x_)], writes=[("OSB", x_)])
            yield
            for x_ in range(2):
                rows = slice(0, 64) if x_ == 0 else slice(64, 128)
                P.add("pe", lambda e, x_=x_: e.matmul(psX[:, :], lhsT=bsel[:, x_ * 128:(x_ + 1) * 128], rhs=OSB[x_][:, :],
                                                      start=True, stop=True), reads=[("OSB", x_), "bsel"], writes=["psX"])
                P.add("dve", lambda e, x_=x_, rows=rows: e.reciprocal(out=FT[x_][rows, :], in_=psX[rows, :]),
                      reads=["psX"], writes=[("FT", x_)])
                yield
                P.add("dve", lambda e, x_=x_, rows=rows: e.tensor_tensor(out=oc[rows, :], in0=OSB[x_][rows, :], in1=FT[x_][rows, :],
                                                                        op=ALU.mult),
                      reads=[("OSB", x_), ("FT", x_)], writes=[("Ocat", s, x_)])
        dma("sp", f"ost{s}", OSin[l][p][g // 4][:, (g % 4) * 512:(g % 4 + 1) * 512], oc[:, :],
            [("Ocat", s), ("Ocat", s, 0), ("Ocat", s, 1)], [("OSin", l, p, g // 4)])
        yield
        if g % 4 == 3:
            q = g // 4
            allgather(f"ago{l}_{p}_{q}", OSin[l][p][q].ap(), OS[l][p][q * 512:(q + 1) * 512, :],
                      [("OSin", l, p, q)], [("OS", l, p, q)])
        yield

    def moba_gate(p, g, s):
        gbank = [psX, psP]
        gkey = ["psX", "psP"]
        for x_ in range(2):
            rows = slice(0, 64) if x_ == 0 else slice(64, 128)
            for qt in range(4):
                P.add("pe", lambda e, x_=x_, qt=qt, rows=rows: e.matmul(
                    gbank[x_][:, qt * 32:(qt + 1) * 32], lhsT=QT[s][x_][rows, qt * 128:(qt + 1) * 128], rhs=kmT[rows, :],
                    start=True, stop=True), reads=[("QT", s, x_), "kmT"], writes=[gkey[x_]])
        P.add("dve", lambda e: e.memset(gate_sb[:, :, :], -1e30), writes=["gate"])
        gsv = gate_sb[:, :, :].rearrange("p (x h q) n -> p x h q n", x=2, h=2)
        for hf in range(2):
            jb = 2 * g + hf
            if jb > 0:
                for x_ in range(2):
                    psg = gbank[x_][:, 0:128].rearrange("p (h q n) -> p h q n", h=2, q=2)
                    P.add("dve", lambda e, hf=hf, jb=jb, x_=x_, psg=psg: e.tensor_copy(out=gsv[:, x_, hf, :, 0:jb], in_=psg[:, hf, :, 0:jb]),
                          reads=[gkey[x_], "gate"], writes=["gate"])
        glev = int(os.environ.get("GATE_LEVEL", "3"))
        if glev < 2:
            return
        for x_ in range(2):
            base = 64 if x_ == 0 else 0
            for qt in range(4):
                xq = x_ * 4 + qt
                jb = 2 * g + qt // 2
                ai = (p * 2 + x_) * 4 + qt
                P.add("dve", lambda e, xq=xq: e.max(out=m8[:, xq, :], in_=gate_sb[:, xq, :]), reads=["gate"], writes=[("m8", xq)])
                P.add("dve", lambda e, xq=xq: e.tensor_scalar(out=selt[:, :], in0=gate_sb[:, xq, :], scalar1=m8[:, xq, 2:3],
                                                              scalar2=None, op0=ALU.is_ge),
                      reads=["gate", ("m8", xq)], writes=["selt"])
                P.add("dve", lambda e, x_=x_, qt=qt, base=base, ai=ai: e.tensor_scalar(
                    out=NS[x_][:, qt, base:base + 32], in0=selt[:, :], scalar1=-NEG, scalar2=augq1[:, ai:ai + 1],
                    op0=ALU.mult, op1=ALU.add), reads=["selt", "augq1", ("NS", x_)], writes=[("NS", x_)])
                P.add("dve", lambda e, x_=x_, qt=qt, base=base, ai=ai, jb=jb: e.tensor_scalar(
                    out=NS[x_][:, qt, base + jb:base + jb + 1], in0=augq1[:, ai:ai + 1], scalar1=-NEG, scalar2=None,
                    op0=ALU.add), reads=["augq1", ("NS", x_)], writes=[("NS", x_)])
        if glev < 3:
            return
        for x_ in range(2):
            rows = slice(64, 96) if x_ == 0 else slice(0, 32)
            for qt in range(4):
                P.add("pe", lambda e, x_=x_, qt=qt: e.transpose(out=psXb[0:96, qt * 128:(qt + 1) * 128],
                                                                in_=NS[x_][:, qt, :], identity=ident[:, :]),
                      reads=[("NS", x_), "ident"], writes=["psX"])
            P.add("dve", lambda e, x_=x_, rows=rows: e.tensor_copy(out=QT[s][x_][rows, :], in_=psXb[rows, :]),
                  reads=["psX"], writes=[("QTaug", s, x_)])

    def phase3(l, do_ffn=True):
        def rank_of(e):
            if "rank" not in ctx:
                ctx["rank"] = e.partition_id() % 4
            return ctx["rank"]
        for kc in range(8):
            src, p = kc // 2, kc % 2
            P.add("pool", lambda e, kc=kc, src=src, p=p: e.dma_start(
                out=OT[:, kc, :], in_=OS[l][p][bass.ds(rank_of(e) * 512 + src * 128, 128), :]),
                reads=[("OS", l, p, q) for q in range(4)], writes=[("OT", kc)], dma="otld")
        wsrc = wout_in[l].rearrange("(kc p) n -> p kc n", p=128)
        for i in range(4):
            stg = (STG if i % 2 == 0 else STG2)[:, :].rearrange("p (k n) -> p k n", k=2)
            dma("sp", f"stg{i % 2}", stg, wsrc[:, 2 * i:2 * i + 2, :], [], [("STG", i % 2)])
            if l == 0:
                P.add("pool", lambda e, stg=stg, i=i: e.tensor_scalar(out=WO[:, 2 * i:2 * i + 2, :], in0=stg, scalar1=small[:, 33:34],
                                                                      scalar2=None, op0=ALU.mult),
                      reads=[("STG", i % 2), "sub08"], writes=[("WO", i)])
            else:
                P.add("pool", lambda e, stg=stg, i=i: e.tensor_copy(out=WO[:, 2 * i:2 * i + 2, :], in_=stg),
                      reads=[("STG", i % 2)], writes=[("WO", i)])
        yb = 0
        for t in range(16):
            for n2 in range(2):
                b = yb % 2
                yb += 1
                for kc in range(8):
                    P.add("pe", lambda e, kc=kc, b=b, t=t, n2=n2: e.matmul(
                        psY[b][:, :], lhsT=OT[:, kc, t * 128:(t + 1) * 128], rhs=WO[:, kc, n2 * 512:(n2 + 1) * 512],
                        start=(kc == 0), stop=(kc == 7)), reads=[("OT", k_) for k_ in range(8)] + [("WO", kc // 2)], writes=[("psY", b)])
                P.add("dve", lambda e, b=b, t=t, n2=n2: e.tensor_tensor(
                    out=H[:, t, n2 * 512:(n2 + 1) * 512], in0=H[:, t, n2 * 512:(n2 + 1) * 512], in1=psY[b][:, :], op=ALU.add),
                    reads=[("psY", b), ("H", t, n2)], writes=[("H", t, n2)])
        if not do_ffn:
            return
        P.barrier()
        for t in range(16):
            norm_transpose(t)
        w1src = wff1_in[l].rearrange("(kc p) n -> p kc n", p=128)
        w2src = wff2_in[l].rearrange("(fc p) n -> p fc n", p=128)
        ub = 0
        for ei in range(8):
            ws = ei % 2
            for hf in range(2):
                stg = (STG if hf == 0 else STG2)[:, :].rearrange("p (k n) -> p k n", k=4)
                dma("sp", f"stg{hf}", stg, w1src[:, hf * 4:hf * 4 + 4, ei * 512:(ei + 1) * 512], [], [("STG", hf)])
                for k4 in range(4):
                    kc = hf * 4 + k4
                    P.add("pool", lambda e, stg=stg, k4=k4, kc=kc, ws=ws: e.tensor_scalar(
                        out=W1e[ws][:, kc, :], in0=stg[:, k4, :], scalar1=gains[:, 16 + l * 8 + kc:17 + l * 8 + kc],
                        scalar2=None, op0=ALU.mult), reads=[("STG", hf), "gains"], writes=[("W1e", ws)])
            for hf in range(2):
                stg = (STG if hf == 0 else STG2)[:, :].rearrange("p (k n) -> p k n", k=2)
                dma("sp", f"stg{hf}", stg, w2src[:, ei * 4 + hf * 2:ei * 4 + hf * 2 + 2, :], [], [("STG", hf)])
                P.add("pool", lambda e, stg=stg, hf=hf, ws=ws: e.tensor_copy(out=W2e[ws][:, 2 * hf:2 * hf + 2, :], in_=stg),
                      reads=[("STG", hf)], writes=[("W2e", ws)])
            for c in range(4):
                us = (ei * 4 + c) % 2
                for fc in range(4):
                    b = ub % 2
                    ub += 1
                    for kc in range(8):
                        P.add("pe", lambda e, kc=kc, b=b, fc=fc, c=c, ws=ws: e.matmul(
                            psU[b][:, :], lhsT=W1e[ws][:, kc, fc * 128:(fc + 1) * 128], rhs=HNT[:, kc, c * 512:(c + 1) * 512],
                            start=(kc == 0), stop=(kc == 7)),
                            reads=[("W1e", ws)] + [("HNT", tt) for tt in range(4 * c, 4 * c + 4)], writes=[("psU", b)])
                    P.add("act", lambda e, b=b: e.activation(out=RT[b][:, :], in_=psU[b][:, :], func=AF.Relu),
                          reads=[("psU", b)], writes=[("RT", b)])
                    P.add("dve", lambda e, b=b, us=us, fc=fc: e.tensor_tensor(out=UT[us][:, fc, :], in0=RT[b][:, :], in1=RT[b][:, :],
                                                                             op=ALU.mult),
                          reads=[("RT", b)], writes=[("UT", us, fc)])
                for tt in range(4):
                    t = 4 * c + tt
                    for n2 in range(2):
                        b = yb % 2
                        yb += 1
                        for fc in range(4):
                            P.add("pe", lambda e, fc=fc, b=b, tt=tt, n2=n2, us=us, ws=ws: e.matmul(
                                psY[b][:, :], lhsT=UT[us][:, fc, tt * 128:(tt + 1) * 128], rhs=W2e[ws][:, fc, n2 * 512:(n2 + 1) * 512],
                                start=(fc == 0), stop=(fc == 3)),
                                reads=[("UT", us, fc), ("W2e", ws)], writes=[("psY", b)])
                        P.add("dve", lambda e, b=b, t=t, n2=n2: e.tensor_tensor(
                            out=H[:, t, n2 * 512:(n2 + 1) * 512], in0=H[:, t, n2 * 512:(n2 + 1) * 512], in1=psY[b][:, :], op=ALU.add),
                            reads=[("psY", b), ("H", t, n2)], writes=[("H", t, n2)])

    def final_out(with_norm=True):
        if with_norm:
            dma("sp", "stg0", STG[:, 0:1024], fnorm_in, [], ["fnb"])
        for t in range(16):
            slot = t % 2
            yt = STG2[:, slot * 1024:(slot + 1) * 1024]
            if with_norm:
                P.add("dve", lambda e, t=t: e.memset(small[:, t:t + 1], 0.0), reads=[("ssq", t)], writes=[("ssq", t)])
                rms_tile(t, slot)
                P.add("dve", lambda e, t=t, yt=yt: e.scalar_tensor_tensor(
                    out=yt, in0=H[:, t, :], scalar=small[:, 16 + t:17 + t], in1=STG[:, 0:1024], op0=ALU.mult, op1=ALU.mult),
                    reads=[("H", t, 0), ("H", t, 1), ("rstd", t), "fnb"], writes=[("yt", slot)])
            else:
                P.add("dve", lambda e, t=t, yt=yt: e.tensor_copy(out=yt, in_=H[:, t, :]),
                      reads=[("H", t, 0), ("H", t, 1)], writes=[("yt", slot)])
            dma("sp", f"yst{slot}", y_out[t * 128:(t + 1) * 128, :], yt, [("yt", slot)], [("yout", t)])

    phase1(0)
    P.barrier()
    if stage != "F0":
        phase2(0)
        P.barrier()
    if stage == "F0":
        phase3(0)
        P.barrier()
        final_out(with_norm=False)
    elif stage == "A0":
        phase3(0, do_ffn=False)
        P.barrier()
        final_out(with_norm=False)
    elif stage == "L0":
        phase3(0)
        P.barrier()
        final_out(with_norm=False)
    else:
        phase3(0)
        P.barrier()
        phase1(1)
        P.barrier()
        phase2(1)
        P.barrier()
        if stage == "A1":
            phase3(1, do_ffn=False)
            P.barrier()
            final_out(with_norm=False)
        else:
            phase3(1)
            P.barrier()
            final_out(with_norm=True)
    P.barrier()
    P.finalize()

    with ExitStack() as st:
        sems = {}
        for e in Prog.ENGS:
            sems[("eng", e)] = st.enter_context(nc.semaphore(f"s_{e}"))
        for k in P.dma_keys:
            sems[("dma", k)] = st.enter_context(nc.semaphore(f"d_{k}"))
        for k in P.cc_keys:
            sems[("cc", k)] = st.enter_context(nc.semaphore(f"c_{k}"))
        block = st.enter_context(nc.Block())

        @block.sync
        def _(e):
            P.emit("sp", e, sems)

        @block.scalar
        def _(e):
            P.emit("act", e, sems)

        @block.vector
        def _(e):
            P.emit("dve", e, sems)

        @block.gpsimd
        def _(e):
            P.emit("pool", e, sems)

        @block.tensor
        def _(e):
            P.emit("pe", e, sems)
    return nc


def _slopes(n):
    return np.array([2.0 ** (-8.0 * (h + 1) / n) for h in range(n)], dtype=np.float64)


def _consts(r):
    bf = ml_dtypes.bfloat16
    s0 = _slopes(8)
    s1 = _slopes(16)
    ident = np.eye(128, dtype=np.float32).astype(bf)
    ki = np.arange(128)[:, None]
    qi = np.arange(128)[None, :]
    tri = np.where(qi < ki, NEG, 0.0).astype(np.float32).astype(bf)
    kaug = np.zeros((2, 32, T), np.float32)
    kaug[0, 0, :] = 1.0
    kaug[1, np.arange(T) // 256, np.arange(T)] = 1.0
    kaug = kaug.astype(bf)
    qaug0 = np.zeros((2, 32, 512), np.float32)
    btab = np.zeros((128, 2, 2, 2, NDC), np.float64)
    augq1 = np.zeros((128, 2, 2, 4), np.float64)
    pp = np.arange(128, dtype=np.float64)
    dc = np.arange(NDC, dtype=np.float64) - 3.0
    for p in range(2):
        s = s0[2 * r + p]
        qaug0[p, 0, :] = -s * np.arange(512) / SCALE
        for x in range(2):
            btab[:, 0, p, x, :] = s * (pp[:, None] - 128.0 * dc[None, :])
            s_ = s1[4 * r + 2 * p + x]
            btab[:, 1, p, x, :] = s_ * (pp[:, None] - 128.0 * dc[None, :])
            for qt in range(4):
                augq1[:, p, x, qt] = -s_ * (qt * 128 + pp) / SCALE + NEG
    bsel = np.zeros((128, 2, 128), np.float32)
    bsel[64, 0, 0:64] = 1.0
    bsel[0, 1, 64:128] = 1.0
    return dict(ident=ident, trimask=tri, kaug=kaug, qaug0=qaug0.astype(bf),
                biastab=btab.reshape(128, -1).astype(np.float32), augq1=augq1.reshape(128, -1).astype(np.float32),
                bsel=bsel.reshape(128, 256))


_NC_CACHE = {}


def kernel(x, attn_norm, w_in, w_out, diff_lambda, diff_subln, mlp_norm, w_ff1, w_ff2, final_norm, _stage="full"):
    x = np.asarray(x, np.float32)
    attn_norm = np.asarray(attn_norm, np.float32)
    w_in = np.asarray(w_in, np.float32)
    w_out = np.ascontiguousarray(np.asarray(w_out, np.float32))
    diff_lambda = np.asarray(diff_lambda, np.float32)
    diff_subln = np.asarray(diff_subln, np.float32)
    mlp_norm = np.asarray(mlp_norm, np.float32)
    w_ff1 = np.ascontiguousarray(np.asarray(w_ff1, np.float32))
    w_ff2 = np.ascontiguousarray(np.asarray(w_ff2, np.float32))
    final_norm = np.asarray(final_norm, np.float32)

    if _stage not in _NC_CACHE:
        _NC_CACHE[_stage] = build_program(_stage)
    nc = _NC_CACHE[_stage]

    gains = np.zeros((128, 64), np.float32)
    for l in range(2):
        gains[:, l * 8:(l + 1) * 8] = attn_norm[l].reshape(8, 128).T
        gains[:, 16 + l * 8:16 + (l + 1) * 8] = mlp_norm[l].reshape(8, 128).T
    gains[:, 32] = diff_subln[0]
    fnb = np.ascontiguousarray(np.broadcast_to(final_norm[None, :], (128, D)))
    lamb = np.ascontiguousarray(np.broadcast_to(diff_lambda[0].reshape(1, 256), (128, 256)))

    in_maps = []
    for c in range(8):
        b, r = c // 4, c % 4
        win = np.zeros((4, D, 384), np.float32)
        for l in range(2):
            for p in range(2):
                c0 = 256 * r + 128 * p
                for j in range(3):
                    win[l * 2 + p, :, j * 128:(j + 1) * 128] = w_in[l, :, j * D + c0:j * D + c0 + 128]
        m = dict(x=np.ascontiguousarray(x[b, r * NT:(r + 1) * NT, :]), win=win, wout=w_out, wff1=w_ff1, wff2=w_ff2,
                 gains=gains, fnormb=fnb, lamb=lamb)
        m.update(_consts(r))
        in_maps.append(m)
    res = run_bass_kernel_spmd(nc, in_maps, core_ids=list(range(8)))
    out = np.zeros((2, T, D), np.float32)
    for c in range(8):
        b, r = c // 4, c % 4
        out[b, r * NT:(r + 1) * NT, :] = np.asarray(res.results[c]["y"], dtype=np.float32)
    return out
```

```python
import math
import os
from contextlib import ExitStack

import numpy as np
import ml_dtypes
import concourse.bass as bass
import concourse.mybir as mybir
from concourse.bass_utils import run_bass_kernel_spmd

F32 = mybir.dt.float32
BF16 = mybir.dt.bfloat16
ALU = mybir.AluOpType
AF = mybir.ActivationFunctionType
AX = mybir.AxisListType

D = 1024
T = 8192
NT = 2048
DFF = 4096
EPS = 1e-6
SCALE = 0.125
NEG = -30000.0
NDC = 67
GROUPS = [[0, 1, 2, 3], [4, 5, 6, 7]]


class Op:
    __slots__ = ("eng", "fn", "deps", "dma", "cc", "signal", "count", "idx")

    def __init__(self, eng, fn, dma=None, cc=None):
        self.eng = eng
        self.fn = fn
        self.deps = set()
        self.dma = dma
        self.cc = cc
        self.signal = False
        self.count = 0
        self.idx = 0


class Prog:
    ENGS = ["sp", "act", "dve", "pool", "pe"]

    def __init__(self):
        self.ops = {e: [] for e in self.ENGS}
        self.last_w = {}
        self.readers = {}
        self.dma_keys = {}
        self.cc_keys = []
        self.n = 0

    def add(self, eng, fn, reads=(), writes=(), dma=None, cc=None):
        op = Op(eng, fn, dma=dma, cc=cc)
        op.idx = self.n
        self.n += 1
        deps = set()
        for r in reads:
            w = self.last_w.get(r)
            if w is not None:
                deps.add(w)
        for w_ in writes:
            w = self.last_w.get(w_)
            if w is not None:
                deps.add(w)
            for rd in self.readers.get(w_, ()):
                deps.add(rd)
        deps.discard(op)
        if eng == "pe":
            deps = {d for d in deps if not (d.eng == "pe" and d.dma is None and d.cc is None)}
        op.deps = deps
        for r in reads:
            self.readers.setdefault(r, []).append(op)
        for w_ in writes:
            self.last_w[w_] = op
            self.readers[w_] = []
        self.ops[eng].append(op)
        if dma is not None:
            self.dma_keys.setdefault(dma, 0)
        if cc is not None:
            self.cc_keys.append(cc)
        return op

    def barrier(self):
        lasts = []
        for e in self.ENGS:
            for op in reversed(self.ops[e]):
                if op.dma is None and op.cc is None and op.fn is not None:
                    lasts.append(op)
                    break
        seen = {}
        for e in self.ENGS:
            for op in self.ops[e]:
                if op.dma is not None:
                    seen[op.dma] = op
                if op.cc is not None:
                    seen[("cc", op.cc)] = op
        lasts += list(seen.values())
        for e in self.ENGS:
            b = Op(e, None)
            b.idx = self.n
            self.n += 1
            b.deps = set(lasts)
            self.ops[e].append(b)
        self.last_w = {}
        self.readers = {}

    def finalize(self):
        for e in self.ENGS:
            for op in self.ops[e]:
                for d in op.deps:
                    d.signal = True
        cnt = {e: 0 for e in self.ENGS}
        dcnt = {k: 0 for k in self.dma_keys}
        for e in self.ENGS:
            for op in self.ops[e]:
                if op.dma is not None:
                    dcnt[op.dma] += 16
                    op.count = dcnt[op.dma]
                elif op.cc is not None:
                    op.count = 1
                elif op.signal and op.fn is not None:
                    cnt[e] += 1
                    op.count = cnt[e]

    def emit(self, eng_name, eng, sems):
        waited = {}
        for op in self.ops[eng_name]:
            need = {}
            for d in op.deps:
                if d.dma is not None:
                    key = ("dma", d.dma)
                elif d.cc is not None:
                    key = ("cc", d.cc)
                else:
                    key = ("eng", d.eng)
                if d.count > need.get(key, 0):
                    need[key] = d.count
            for key, val in need.items():
                if waited.get(key, 0) >= val:
                    continue
                eng.wait_ge(sems[key], val)
                waited[key] = val
            if op.fn is None:
                continue
            ins = op.fn(eng)
            if op.dma is not None:
                ins.then_inc(sems[("dma", op.dma)], 16)
            elif op.cc is not None:
                ins.then_inc(sems[("cc", op.cc)])
            elif op.signal:
                ins.then_inc(sems[("eng", eng_name)], 1)


def build_program(stage="full"):
    nc = bass.Bass("TRN2", target_bir_lowering=False)
    P = Prog()

    def ext_in(name, shape, dt):
        return nc.dram_tensor(name, list(shape), dt, kind="ExternalInput").ap()

    x_in = ext_in("x", [NT, D], F32)
    win_in = ext_in("win", [4, D, 384], F32)
    wout_in = ext_in("wout", [2, D, D], F32)
    wff1_in = ext_in("wff1", [2, D, DFF], F32)
    wff2_in = ext_in("wff2", [2, DFF, D], F32)
    gains_in = ext_in("gains", [128, 64], F32)
    fnorm_in = ext_in("fnormb", [128, D], F32)
    lamb_in = ext_in("lamb", [128, 256], F32)
    ident_in = ext_in("ident", [128, 128], BF16)
    tri_in = ext_in("trimask", [128, 128], BF16)
    kaug_in = ext_in("kaug", [2, 32, T], BF16)
    qaug0_in = ext_in("qaug0", [2, 32, 512], BF16)
    btab_in = ext_in("biastab", [128, 8 * NDC], F32)
    augq1_in = ext_in("augq1", [128, 16], F32)
    bsel_in = ext_in("bsel", [128, 256], F32)
    y_out = nc.dram_tensor("y", [NT, D], F32, kind="ExternalOutput").ap()

    HTin = [[nc.dram_tensor(f"htin{l}_{i}", [D, 512], BF16) for i in range(4)] for l in range(2)]
    HT = [[nc.dram_tensor(f"ht{l}_{i}", [4 * D, 512], BF16) for i in range(4)] for l in range(2)]
    OSin = [[[nc.dram_tensor(f"osin{l}_{p}_{q}", [128, 2048], BF16) for q in range(4)]
             for p in range(2)] for l in range(2)]
    OS = [[nc.dram_tensor(f"os{l}_{p}", [2048, 2048], BF16) for p in range(2)] for l in range(2)]

    off = [16512]

    def sb(name, shape, dt, at=None):
        nbytes = int(np.prod(shape[1:])) * (4 if dt == F32 else 2)
        if at is None:
            o = off[0]
            off[0] += (nbytes + 31) // 32 * 32
        else:
            o = at
        return nc.alloc_sbuf_tensor_at(name, list(shape), dt, offset=o), o + (nbytes + 31) // 32 * 32

    H, _ = sb("H", [128, 16, D], F32)
    ident, _ = sb("identt", [128, 128], BF16)
    tri, _ = sb("trit", [128, 128], BF16)
    ones128, _ = sb("ones128", [128, 128], F32)
    bsel, _ = sb("bselt", [128, 256], F32)
    btab, _ = sb("btab", [128, 8 * NDC], F32)
    augq1, _ = sb("augq1t", [128, 16], F32)
    gains, _ = sb("gainst", [128, 64], F32)
    lamb, _ = sb("lambt", [128, 256], F32)
    small, _ = sb("small", [128, 64], F32)
    epsc, _ = sb("epsc", [128, 1], F32)
    PH = off[0]

    o = PH
    KT = []
    for x_ in range(2):
        t_, o = sb(f"KT{x_}", [128, T], BF16, at=o)
        KT.append(t_)
    Vb, o = sb("Vb", [128, 64 * 192], BF16, at=o)
    WIN, o = sb("WIN", [128, 8, 384], BF16, at=o)
    HNTc = []
    for s_ in range(2):
        t_, o = sb(f"HNTc{s_}", [128, 8, 512], BF16, at=o)
        HNTc.append(t_)
    QT = []
    for s_ in range(2):
        row = []
        for x_ in range(2):
            t_, o = sb(f"QT{s_}_{x_}", [128, 512], BF16, at=o)
            row.append(t_)
        QT.append(row)
    PT = []
    for s_ in range(4):
        t_, o = sb(f"PT{s_}", [128, 512], BF16, at=o)
        PT.append(t_)
    Zs = []
    for x_ in range(2):
        row = []
        for par in range(2):
            t_, o = sb(f"Zs{x_}_{par}", [128, 512], F32, at=o)
            row.append(t_)
        Zs.append(row)
    FT = []
    for i_ in range(4):
        t_, o = sb(f"FT{i_}", [128, 512], F32, at=o)
        FT.append(t_)
    FTo = []
    for i_ in range(2):
        t_, o = sb(f"FTo{i_}", [128, 512], F32, at=o)
        FTo.append(t_)
    OSB = [FT[2], FT[3]]
    Ocat = []
    for s_ in range(2):
        t_, o = sb(f"Ocat{s_}", [128, 512], BF16, at=o)
        Ocat.append(t_)
    gate_sb, o = sb("gate_sb", [128, 8, 32], F32, at=o)
    m8, o = sb("m8", [128, 8, 8], F32, at=o)
    selt, o = sb("selt", [128, 32], F32, at=o)
    NS = []
    for x_ in range(2):
        t_, o = sb(f"NS{x_}", [128, 4, 96], BF16, at=o)
        NS.append(t_)
    kmT, o = sb("kmT", [128, 32], BF16, at=o)
    km32, o = sb("km32", [128, 2], F32, at=o)
    STG, o = sb("STG", [128, 2048], F32, at=o)
    STG2, o = sb("STG2", [128, 2048], F32, at=o)
    P2END = o
    assert P2END <= 229376, P2END

    o = PH
    HNT, o = sb("HNT", [128, 8, NT], BF16, at=o)
    OT = HNT
    WO, o = sb("WO", [128, 8, D], BF16, at=o)
    W1e, W2e = [], []
    for s_ in range(2):
        t_, o = sb(f"W1e{s_}", [128, 8, 512], BF16, at=o)
        W1e.append(t_)
        t_, o = sb(f"W2e{s_}", [128, 4, D], BF16, at=o)
        W2e.append(t_)
    hnb = []
    for s_ in range(2):
        t_, o = sb(f"hnb{s_}", [128, D], BF16, at=o)
        hnb.append(t_)
    UT = []
    for s_ in range(2):
        t_, o = sb(f"UT{s_}", [128, 4, 512], BF16, at=o)
        UT.append(t_)
    RT = []
    for s_ in range(2):
        t_, o = sb(f"RT{s_}", [128, 512], BF16, at=o)
        RT.append(t_)
    assert o <= P2END - 2 * 8192, (o, P2END)
    YT = [STG, STG2]

    pb = [nc.alloc_psum_tensor(f"pb{i}", [128, 512], F32) for i in range(8)]
    psS = pb[0:4]
    psO = pb[4:6]
    psP = pb[6]
    psX = pb[7]
    psXb = psX[:, 256:512].bitcast(BF16)
    psT = pb[6][:, :].bitcast(BF16)
    psU = pb[0:2]
    psY = pb[2:4]

    ctx = {}

    def dma(eng, key, out, in_, reads, writes):
        return P.add(eng, lambda e: e.dma_start(out=out, in_=in_), reads=reads, writes=writes, dma=key)

    def allgather(name, in_ap, out_ap, reads, writes):
        return P.add("pool", lambda e: e.collective_compute(
            "AllGather", ALU.bypass, replica_groups=GROUPS, ins=[in_ap.opt()], outs=[out_ap.opt()]),
            reads=reads, writes=writes, cc=name)

    dma("sp", "c0", ident[:, :], ident_in, [], ["ident"])
    dma("sp", "c0", tri[:, :], tri_in, [], ["tri"])
    dma("sp", "c0", bsel[:, :], bsel_in, [], ["bsel"])
    dma("sp", "c0", btab[:, :], btab_in, [], ["btab"])
    dma("sp", "c0", augq1[:, :], augq1_in, [], ["augq1"])
    dma("sp", "c0", gains[:, :], gains_in, [], ["gains"])
    dma("sp", "c0", lamb[:, :], lamb_in, [], ["lamb"])
    P.add("dve", lambda e: e.memset(ones128[:, :], 1.0), writes=["ones128"])
    P.add("dve", lambda e: e.memset(epsc[:, :], EPS), writes=["epsc"])
    P.add("dve", lambda e: e.memset(small[:, :], 0.0), writes=["small"])
    P.add("dve", lambda e: e.tensor_tensor(out=FT[0][:, 0:64], in0=lamb[:, 0:64], in1=lamb[:, 64:128], op=ALU.mult),
          reads=["lamb"], writes=["ft0"])
    P.add("dve", lambda e: e.tensor_tensor(out=FT[0][:, 64:128], in0=lamb[:, 128:192], in1=lamb[:, 192:256], op=ALU.mult),
          reads=["lamb"], writes=["ft0b"])
    P.add("dve", lambda e: e.tensor_reduce(out=small[:, 34:36], in_=FT[0][:, 0:128].rearrange("p (a b) -> p a b", a=2),
                                           axis=AX.X, op=ALU.add),
          reads=["ft0", "ft0b", "small"], writes=["lam_s"])
    P.add("act", lambda e: e.activation(out=small[:, 36:38], in_=small[:, 34:36], func=AF.Exp),
          reads=["lam_s"], writes=["lam_e"])
    P.add("dve", lambda e: e.tensor_tensor(out=small[:, 38:39], in0=small[:, 37:38], in1=small[:, 36:37], op=ALU.subtract),
          reads=["lam_e"], writes=["lam_d"])
    P.add("dve", lambda e: e.tensor_scalar(out=small[:, 32:33], in0=small[:, 38:39], scalar1=-0.2, scalar2=None, op0=ALU.add),
          reads=["lam_d"], writes=["neglam"])
    P.add("dve", lambda e: e.tensor_scalar(out=small[:, 33:34], in0=gains[:, 32:33], scalar1=0.8, scalar2=None, op0=ALU.mult),
          reads=["gains", "small"], writes=["sub08"])
    for t in range(16):
        dma("sp", "xload", H[:, t, :], x_in[t * 128:(t + 1) * 128, :], [], [("H", t, 0), ("H", t, 1)])

    P.barrier()

    def rms_tile(t, slot):
        P.add("act", lambda e: e.activation(out=hnb[slot][:, :], in_=H[:, t, :], func=AF.Square,
                                            accum_out=small[:, t:t + 1]),
              reads=[("H", t, 0), ("H", t, 1), "small"], writes=[("hnb", slot), ("ssq", t)])
        P.add("act", lambda e: e.activation(out=small[:, 16 + t:17 + t], in_=small[:, t:t + 1], func=AF.Ln,
                                            bias=epsc[:, 0:1], scale=1.0 / D),
              reads=[("ssq", t), "epsc"], writes=[("rstd", t)])
        P.add("act", lambda e: e.activation(out=small[:, 16 + t:17 + t], in_=small[:, 16 + t:17 + t], func=AF.Exp, scale=-0.5),
              reads=[("rstd", t)], writes=[("rstd", t)])

    def norm_transpose(t):
        slot = t % 2
        P.add("dve", lambda e: e.memset(small[:, t:t + 1], 0.0), reads=[("ssq", t)], writes=[("ssq", t)])
        rms_tile(t, slot)
        P.add("dve", lambda e: e.tensor_scalar(out=hnb[slot][:, :], in0=H[:, t, :], scalar1=small[:, 16 + t:17 + t],
                                               scalar2=None, op0=ALU.mult),
              reads=[("H", t, 0), ("H", t, 1), ("rstd", t)], writes=[("hnb", slot)])
        for kc in range(8):
            P.add("pe", lambda e, kc=kc: e.transpose(out=psT[:, kc * 128:(kc + 1) * 128],
                                                     in_=hnb[slot][:, kc * 128:(kc + 1) * 128], identity=ident[:, :]),
                  reads=[("hnb", slot), "ident"], writes=["psT"])
        P.add("act", lambda e: e.copy(out=HNT[:, :, t * 128:(t + 1) * 128],
                                      in_=psT.rearrange("p (k q) -> p k q", k=8)),
              reads=["psT"], writes=[("HNT", t)])

    def phase1(l):
        for t in range(16):
            norm_transpose(t)
            if t % 4 == 3:
                i = t // 4
                dma("sp", f"htst{i}", HTin[l][i].ap().rearrange("(kc p) t -> p kc t", p=128),
                    HNT[:, :, i * 512:(i + 1) * 512], [("HNT", tt) for tt in range(4 * i, 4 * i + 4)],
                    [("HTin", l, i)])
                allgather(f"agh{l}_{i}", HTin[l][i].ap(), HT[l][i].ap(), [("HTin", l, i)], [("HT", l, i)])

    def load_cast_win(l, p):
        wsrc = win_in[l * 2 + p].rearrange("(kc p) n -> p kc n", p=128)
        for hf in range(2):
            stg = (STG if hf == 0 else STG2)[:, 0:4 * 384].rearrange("p (k n) -> p k n", k=4)
            dma("sp", f"stg{hf}", stg, wsrc[:, hf * 4:hf * 4 + 4, :], [], [("STG", hf)])
            for k4 in range(4):
                kc = hf * 4 + k4
                P.add("pool", lambda e, stg=stg, k4=k4, kc=kc: e.tensor_scalar(
                    out=WIN[:, kc, :], in0=stg[:, k4, :], scalar1=gains[:, l * 8 + kc:l * 8 + kc + 1],
                    scalar2=None, op0=ALU.mult),
                    reads=[("STG", hf), "gains"], writes=[("WIN", kc)])

    def phase2(l):
        V0 = Vb[:, 0:64 * 128].rearrange("p (t c) -> p t c", c=128)
        V1 = Vb[:, :].rearrange("p (t c) -> p t c", c=192)
        dma("sp", "kaug0", KT[0][64:96, :], kaug_in[l], [], [("KTaug", 0)])
        dma("sp", "kaug1", KT[1][0:32, :], kaug_in[l], [], [("KTaug", 1)])
        P.add("pool", lambda e: e.memset(KT[1][32:64, :], 0.0), writes=[("KTz", 1)])
        for s_ in range(2):
            for x_ in range(2):
                P.add("pool", lambda e, s_=s_, x_=x_: e.memset(QT[s_][x_][:, :], 0.0), writes=[("QT", s_, x_), ("QTaug", s_, x_)])
        if l == 1:
            P.add("pool", lambda e: e.memset(Vb[:, :], 0.0), writes=["Vall"])
            P.add("pool", lambda e: e.memset(V1[:, :, 64:65], 1.0), reads=["Vall"], writes=["Vall"])
            for x_ in range(2):
                P.add("pool", lambda e, x_=x_: e.memset(NS[x_][:, :, :], 0.0), writes=[("NS", x_)])
                P.add("pool", lambda e, x_=x_: e.memset(OSB[x_][:, :], 0.0), writes=[("OSB", x_), ("FT", 2 + x_)])
            P.add("pool", lambda e: e.memset(kmT[:, :], 0.0), writes=["kmT"])
        for p in range(2):
            load_cast_win(l, p)
            if l == 0:
                for s_ in range(2):
                    dma("sp", f"qaug{s_}0", QT[s_][0][64:96, :], qaug0_in[p], [("QTaug", s_, 0)], [("QTaug", s_, 0)])
                    dma("sp", f"qaug{s_}1", QT[s_][1][0:32, :], qaug0_in[p], [("QTaug", s_, 1)], [("QTaug", s_, 1)])
            for _ in chunk_proj(l, p, 0, V0, V1):
                pass
            fin = None
            for g in range(16):
                gen = chunk_proj(l, p, g + 1, V0, V1) if g + 1 < 16 else None
                chunk_attn(l, p, g, V0, V1, gen, fin)
                fin = fin_gen(l, p, g)
                next(fin)
            for _ in fin:
                pass

    bidx = [0]
    NSLOT = 4

    def chunk_proj(l, p, g, V0, V1):
        s = g % 2
        hsrc = HT[l][g % 4][(g // 4) * D:(g // 4 + 1) * D, :].rearrange("(kc p) t -> p kc t", p=128)
        dma("sp", f"hntc{s}", HNTc[s][:, :, :], hsrc, [("HT", l, g % 4)], [("HNTc", s)])
        win_r = [("WIN", kc) for kc in range(8)]
        for kc in range(8):
            P.add("pe", lambda e, kc=kc: e.matmul(psP[:, :], lhsT=WIN[:, kc, 0:128], rhs=HNTc[s][:, kc, :],
                                                  start=(kc == 0), stop=(kc == 7)),
                  reads=[("HNTc", s)] + win_r, writes=["psP"])
        P.add("dve", lambda e: e.tensor_copy(out=QT[s][0][0:64, :], in_=psP[0:64, :]), reads=["psP"], writes=[("QT", s, 0)])
        P.add("dve", lambda e: e.tensor_copy(out=QT[s][1][64:128, :], in_=psP[64:128, :]), reads=["psP"], writes=[("QT", s, 1)])
        yield
        for kc in range(8):
            P.add("pe", lambda e, kc=kc: e.matmul(psP[:, :], lhsT=WIN[:, kc, 128:256], rhs=HNTc[s][:, kc, :],
                                                  start=(kc == 0), stop=(kc == 7)),
                  reads=[("HNTc", s)] + win_r, writes=["psP"])
        P.add("dve", lambda e: e.tensor_copy(out=KT[0][0:64, g * 512:(g + 1) * 512], in_=psP[0:64, :]),
              reads=["psP"], writes=[("KT", 0, g)])
        P.add("dve", lambda e: e.tensor_copy(out=KT[1][64:128, g * 512:(g + 1) * 512], in_=psP[64:128, :]),
              reads=["psP"], writes=[("KT", 1, g)])
        if l == 1:
            P.add("dve", lambda e: e.tensor_reduce(out=km32[:, :], in_=psP[:, :].rearrange("p (a b) -> p a b", a=2),
                                                   axis=AX.X, op=ALU.add), reads=["psP"], writes=["km32"])
            P.add("dve", lambda e: e.tensor_scalar(out=kmT[:, 2 * g:2 * g + 2], in0=km32[:, :], scalar1=1.0 / 256,
                                                   scalar2=None, op0=ALU.mult), reads=["km32"], writes=["kmT"])
        yield
        for tt in range(4):
            for kc in range(8):
                P.add("pe", lambda e, kc=kc, tt=tt: e.matmul(psP[:, tt * 128:(tt + 1) * 128],
                                                             lhsT=HNTc[s][:, kc, tt * 128:(tt + 1) * 128],
                                                             rhs=WIN[:, kc, 256:384], start=(kc == 0), stop=(kc == 7)),
                      reads=[("HNTc", s)] + win_r, writes=["psP"])
            if tt == 1:
                yield
        psP4 = psP[:, :].rearrange("p (t c) -> p t c", t=4)
        if l == 0:
            P.add("dve", lambda e: e.tensor_copy(out=V0[:, 4 * g:4 * g + 4, :], in_=psP4), reads=["psP"], writes=[("V", g)])
        else:
            P.add("dve", lambda e: e.tensor_copy(out=V1[:, 4 * g:4 * g + 4, 0:64], in_=psP4[:, :, 0:64]),
                  reads=["psP", "Vall"], writes=[("V", g)])
            P.add("dve", lambda e: e.tensor_copy(out=V1[:, 4 * g:4 * g + 4, 128:192], in_=psP4[:, :, 64:128]),
                  reads=["psP", "Vall"], writes=[("Vb_", g)])
        yield
        if l == 1 and not os.environ.get("SKIP_GATE"):
            moba_gate(p, g, s)
        yield

    def chunk_attn(l, p, g, V0, V1, gen=None, fin=None):
        s = g % 2
        nk = 4 * g + 4
        if l == 1 and os.environ.get("SKIP_ATTN"):
            nk = 0
        kt0 = max(0, 4 * g - 16) if p == 0 else 0
        items = [(kt, x_) for kt in range(kt0, nk) for x_ in range(2)]
        LOOK = 3
        slots = {}

        def emit_s(kt, x_):
            j = kt - 4 * g
            c0 = 128 * j if j >= 0 else 0
            b = bidx[0] % NSLOT
            bidx[0] += 1
            slots[(kt, x_)] = b
            kx = 96 if x_ == 0 else 128
            kr = [("KT", x_, kt // 4), ("KTaug", x_), ("QT", s, x_), ("QTaug", s, x_)] + ([("KTz", 1)] if x_ == 1 else [])
            ks = slice(kt * 128, (kt + 1) * 128)
            if j >= 0:
                P.add("pe", lambda e: e.matmul(
                    psS[b][:, c0:c0 + 128], lhsT=KT[x_][0:kx, ks], rhs=QT[s][x_][0:kx, c0:c0 + 128],
                    start=True, stop=False), reads=kr, writes=[("psS", b)])
                P.add("pe", lambda e: e.matmul(
                    psS[b][:, c0:c0 + 128], lhsT=ident[:, :], rhs=tri[:, :], start=False, stop=True),
                    reads=["ident", "tri"], writes=[("psS", b)])
                if c0 + 128 < 512:
                    P.add("pe", lambda e: e.matmul(
                        psS[b][:, c0 + 128:512], lhsT=KT[x_][0:kx, ks], rhs=QT[s][x_][0:kx, c0 + 128:512],
                        start=True, stop=True), reads=kr, writes=[("psS", b)])
            else:
                P.add("pe", lambda e: e.matmul(
                    psS[b][:, :], lhsT=KT[x_][0:kx, ks], rhs=QT[s][x_][0:kx, :], start=True, stop=True),
                    reads=kr, writes=[("psS", b)])
            col = (((l * 2 + p) * 2 + x_) * NDC) + (4 * g - kt + 3)
            P.add("act", lambda e: e.activation(
                out=PT[b][:, c0:512], in_=psS[b][:, c0:512], func=AF.Exp, bias=btab[:, col:col + 1], scale=SCALE),
                reads=[("psS", b), "btab"], writes=[("PT", b)])
            if l == 0:
                zeng = "dve" if x_ == 0 else "pool"
                zt = Zs[x_][g % 2]
                zk = ("Zs", x_, g % 2)
                if kt == kt0:
                    P.add(zeng, lambda e: e.tensor_copy(out=zt[:, :], in_=PT[b][:, :]),
                          reads=[("PT", b)], writes=[zk])
                else:
                    P.add(zeng, lambda e: e.tensor_tensor(
                        out=zt[:, c0:512], in0=zt[:, c0:512], in1=PT[b][:, c0:512], op=ALU.add),
                        reads=[("PT", b), zk], writes=[zk])

        def emit_pv(kt, x_):
            j = kt - 4 * g
            c0 = 128 * j if j >= 0 else 0
            b = slots[(kt, x_)]
            if l == 0:
                vl = V0[:, kt, :]
                mo = 128
            else:
                vl = V1[:, kt, 0:65] if x_ == 0 else V1[:, kt, 64:192]
                mo = 65 if x_ == 0 else 128
            P.add("pe", lambda e: e.matmul(
                psO[x_][0:mo, c0:512], lhsT=vl, rhs=PT[b][:, c0:512], start=(kt == kt0), stop=(kt == nk - 1)),
                reads=[("PT", b), ("V", kt // 4), ("Vb_", kt // 4), "Vall"], writes=[("psO", x_)])

        for n in range(len(items) + LOOK):
            if n < len(items):
                emit_s(*items[n])
            if n - LOOK >= 0:
                emit_pv(*items[n - LOOK])
            if n >= 4 and n % 3 == 1 and gen is not None:
                next(gen, None)
            if n >= 2 and n % 3 == 2 and fin is not None:
                next(fin, None)
        if fin is not None:
            for _ in fin:
                pass
        if gen is not None:
            for _ in gen:
                pass

    def fin_gen(l, p, g):
        s = g % 2
        oc = Ocat[s]
        zp = g % 2
        if l == 0:
            for x_ in range(2):
                P.add("dve", lambda e, x_=x_: e.tensor_copy(out=FTo[x_][:, :], in_=psO[x_][:, :]),
                      reads=[("psO", x_)], writes=[("FTo", x_)])
            yield
            for x_ in range(2):
                P.add("pe", lambda e, x_=x_: e.matmul(psX[:, :], lhsT=ones128[:, :], rhs=Zs[x_][zp][:, :], start=True, stop=True),
                      reads=[("Zs", x_, zp), "ones128"], writes=["psX"])
                P.add("dve", lambda e, x_=x_: e.reciprocal(out=FT[x_][:, :], in_=psX[:, :]), reads=["psX"], writes=[("FT", x_)])
                yield
                P.add("dve", lambda e, x_=x_: e.tensor_tensor(out=FT[x_][:, :], in0=FTo[x_][:, :], in1=FT[x_][:, :], op=ALU.mult),
                      reads=[("FTo", x_), ("FT", x_)], writes=[("FT", x_)])
            P.add("dve", lambda e: e.scalar_tensor_tensor(out=FT[2][:, :], in0=FT[1][:, :], scalar=small[:, 32:33], in1=FT[0][:, :],
                                                          op0=ALU.mult, op1=ALU.add),
                  reads=[("FT", 0), ("FT", 1), "neglam"], writes=[("FT", 2)])
            P.add("pool", lambda e: e.tensor_tensor(out=FT[3][:, :], in0=FT[2][:, :], in1=FT[2][:, :], op=ALU.mult),
                  reads=[("FT", 2)], writes=[("FT", 3)])
            yield
            P.add("pe", lambda e: e.matmul(psX[:, :], lhsT=ones128[:, :], rhs=FT[3][:, :], start=True, stop=True),
                  reads=[("FT", 3), "ones128"], writes=["psX"])
            P.add("act", lambda e: e.activation(out=FT[3][:, :], in_=psX[:, :], func=AF.Ln, bias=epsc[:, 0:1], scale=1.0 / 128),
                  reads=["psX", "epsc"], writes=[("FT", 3)])
            P.add("act", lambda e: e.activation(out=FT[3][:, :], in_=FT[3][:, :], func=AF.Exp, scale=-0.5),
                  reads=[("FT", 3)], writes=[("FT", 3)])
            yield
            P.add("dve", lambda e: e.tensor_tensor(out=oc[:, :], in0=FT[2][:, :], in1=FT[3][:, :], op=ALU.mult),
                  reads=[("FT", 2), ("FT", 3)], writes=[("Ocat", s)])
        else:
            for x_ in range(2):
                mo = 65 if x_ == 0 else 128
                P.add("dve", lambda e, x_=x_, mo=mo: e.tensor_copy(out=OSB[x_][0:mo, :], in_=psO[x_][0:mo, :]),
                      reads=[("psO", x_)], writes=[("OSB", x_)])
            yield
            for x_ in range(2):
                rows = slice(0, 64) if x_ == 0 else slice(64, 128)
                P.add("pe", lambda e, x_=x_: e.matmul(psX[:, :], lhsT=bsel[:, x_ * 128:(x_ + 1) * 128], rhs=OSB[x_][:, :],
                                                      start=True, stop=True), reads=[("OSB", x_), "bsel"], writes=["psX"])
                P.add("dve", lambda e, x_=x_, rows=rows: e.reciprocal(out=FT[x_][rows, :], in_=psX[rows, :]),
                      reads=["psX"], writes=[("FT", x_)])
                yield
                P.add("dve", lambda e, x_=x_, rows=rows: e.tensor_tensor(out=oc[rows, :], in0=OSB[x_][rows, :], in1=FT[x_][rows, :],
                                                                        op=ALU.mult),
                      reads=[("OSB", x_), ("FT", x_)], writes=[("Ocat", s, x_)])
        dma("sp", f"ost{s}", OSin[l][p][g // 4][:, (g % 4) * 512:(g % 4 + 1) * 512], oc[:, :],
            [("Ocat", s), ("Ocat", s, 0), ("Ocat", s, 1)], [("OSin", l, p, g // 4)])
        yield
        if g % 4 == 3:
            q = g // 4
            allgather(f"ago{l}_{p}_{q}", OSin[l][p][q].ap(), OS[l][p][q * 512:(q + 1) * 512, :],
                      [("OSin", l, p, q)], [("OS", l, p, q)])
        yield

    def moba_gate(p, g, s):
        gbank = [psX, psP]
        gkey = ["psX", "psP"]
        for x_ in range(2):
            rows = slice(0, 64) if x_ == 0 else slice(64, 128)
            for qt in range(4):
                P.add("pe", lambda e, x_=x_, qt=qt, rows=rows: e.matmul(
                    gbank[x_][:, qt * 32:(qt + 1) * 32], lhsT=QT[s][x_][rows, qt * 128:(qt + 1) * 128], rhs=kmT[rows, :],
                    start=True, stop=True), reads=[("QT", s, x_), "kmT"], writes=[gkey[x_]])
        P.add("dve", lambda e: e.memset(gate_sb[:, :, :], -1e30), writes=["gate"])
        gsv = gate_sb[:, :, :].rearrange("p (x h q) n -> p x h q n", x=2, h=2)
        for hf in range(2):
            jb = 2 * g + hf
            if jb > 0:
                for x_ in range(2):
                    psg = gbank[x_][:, 0:128].rearrange("p (h q n) -> p h q n", h=2, q=2)
                    P.add("dve", lambda e, hf=hf, jb=jb, x_=x_, psg=psg: e.tensor_copy(out=gsv[:, x_, hf, :, 0:jb], in_=psg[:, hf, :, 0:jb]),
                          reads=[gkey[x_], "gate"], writes=["gate"])
        glev = int(os.environ.get("GATE_LEVEL", "3"))
        if glev < 2:
            return
        for x_ in range(2):
            base = 64 if x_ == 0 else 0
            for qt in range(4):
                xq = x_ * 4 + qt
                jb = 2 * g + qt // 2
                ai = (p * 2 + x_) * 4 + qt
                P.add("dve", lambda e, xq=xq: e.max(out=m8[:, xq, :], in_=gate_sb[:, xq, :]), reads=["gate"], writes=[("m8", xq)])
                P.add("dve", lambda e, xq=xq: e.tensor_scalar(out=selt[:, :], in0=gate_sb[:, xq, :], scalar1=m8[:, xq, 2:3],
                                                              scalar2=None, op0=ALU.is_ge),
                      reads=["gate", ("m8", xq)], writes=["selt"])
                P.add("dve", lambda e, x_=x_, qt=qt, base=base, ai=ai: e.tensor_scalar(
                    out=NS[x_][:, qt, base:base + 32], in0=selt[:, :], scalar1=-NEG, scalar2=augq1[:, ai:ai + 1],
                    op0=ALU.mult, op1=ALU.add), reads=["selt", "augq1", ("NS", x_)], writes=[("NS", x_)])
                P.add("dve", lambda e, x_=x_, qt=qt, base=base, ai=ai, jb=jb: e.tensor_scalar(
                    out=NS[x_][:, qt, base + jb:base + jb + 1], in0=augq1[:, ai:ai + 1], scalar1=-NEG, scalar2=None,
                    op0=ALU.add), reads=["augq1", ("NS", x_)], writes=[("NS", x_)])
        if glev < 3:
            return
        for x_ in range(2):
            rows = slice(64, 96) if x_ == 0 else slice(0, 32)
            for qt in range(4):
                P.add("pe", lambda e, x_=x_, qt=qt: e.transpose(out=psXb[0:96, qt * 128:(qt + 1) * 128],
                                                                in_=NS[x_][:, qt, :], identity=ident[:, :]),
                      reads=[("NS", x_), "ident"], writes=["psX"])
            P.add("dve", lambda e, x_=x_, rows=rows: e.tensor_copy(out=QT[s][x_][rows, :], in_=psXb[rows, :]),
                  reads=["psX"], writes=[("QTaug", s, x_)])

    def phase3(l, do_ffn=True):
        def rank_of(e):
            if "rank" not in ctx:
                ctx["rank"] = e.partition_id() % 4
            return ctx["rank"]
        for kc in range(8):
            src, p = kc // 2, kc % 2
            P.add("pool", lambda e, kc=kc, src=src, p=p: e.dma_start(
                out=OT[:, kc, :], in_=OS[l][p][bass.ds(rank_of(e) * 512 + src * 128, 128), :]),
                reads=[("OS", l, p, q) for q in range(4)], writes=[("OT", kc)], dma="otld")
        wsrc = wout_in[l].rearrange("(kc p) n -> p kc n", p=128)
        for i in range(4):
            stg = (STG if i % 2 == 0 else STG2)[:, :].rearrange("p (k n) -> p k n", k=2)
            dma("sp", f"stg{i % 2}", stg, wsrc[:, 2 * i:2 * i + 2, :], [], [("STG", i % 2)])
            if l == 0:
                P.add("pool", lambda e, stg=stg, i=i: e.tensor_scalar(out=WO[:, 2 * i:2 * i + 2, :], in0=stg, scalar1=small[:, 33:34],
                                                                      scalar2=None, op0=ALU.mult),
                      reads=[("STG", i % 2), "sub08"], writes=[("WO", i)])
            else:
                P.add("pool", lambda e, stg=stg, i=i: e.tensor_copy(out=WO[:, 2 * i:2 * i + 2, :], in_=stg),
                      reads=[("STG", i % 2)], writes=[("WO", i)])
        yb = 0
        for t in range(16):
            for n2 in range(2):
                b = yb % 2
                yb += 1
                for kc in range(8):
                    wk = kc // 2 + 4 * (kc % 2)
                    P.add("pe", lambda e, kc=kc, wk=wk, b=b, t=t, n2=n2: e.matmul(
                        psY[b][:, :], lhsT=OT[:, kc, t * 128:(t + 1) * 128], rhs=WO[:, wk, n2 * 512:(n2 + 1) * 512],
                        start=(kc == 0), stop=(kc == 7)), reads=[("OT", k_) for k_ in range(8)] + [("WO", wk // 2)], writes=[("psY", b)])
                P.add("dve", lambda e, b=b, t=t, n2=n2: e.tensor_tensor(
                    out=H[:, t, n2 * 512:(n2 + 1) * 512], in0=H[:, t, n2 * 512:(n2 + 1) * 512], in1=psY[b][:, :], op=ALU.add),
                    reads=[("psY", b), ("H", t, n2)], writes=[("H", t, n2)])
        if not do_ffn:
            return
        P.barrier()
        for t in range(16):
            norm_transpose(t)
        w1src = wff1_in[l].rearrange("(kc p) n -> p kc n", p=128)
        w2src = wff2_in[l].rearrange("(fc p) n -> p fc n", p=128)
        ub = 0
        for ei in range(8):
            ws = ei % 2
            for hf in range(2):
                stg = (STG if hf == 0 else STG2)[:, :].rearrange("p (k n) -> p k n", k=4)
                dma("sp", f"stg{hf}", stg, w1src[:, hf * 4:hf * 4 + 4, ei * 512:(ei + 1) * 512], [], [("STG", hf)])
                for k4 in range(4):
                    kc = hf * 4 + k4
                    P.add("pool", lambda e, stg=stg, k4=k4, kc=kc, ws=ws: e.tensor_scalar(
                        out=W1e[ws][:, kc, :], in0=stg[:, k4, :], scalar1=gains[:, 16 + l * 8 + kc:17 + l * 8 + kc],
                        scalar2=None, op0=ALU.mult), reads=[("STG", hf), "gains"], writes=[("W1e", ws)])
            for hf in range(2):
                stg = (STG if hf == 0 else STG2)[:, :].rearrange("p (k n) -> p k n", k=2)
                dma("sp", f"stg{hf}", stg, w2src[:, ei * 4 + hf * 2:ei * 4 + hf * 2 + 2, :], [], [("STG", hf)])
                P.add("pool", lambda e, stg=stg, hf=hf, ws=ws: e.tensor_copy(out=W2e[ws][:, 2 * hf:2 * hf + 2, :], in_=stg),
                      reads=[("STG", hf)], writes=[("W2e", ws)])
            for c in range(4):
                us = (ei * 4 + c) % 2
                for fc in range(4):
                    b = ub % 2
                    ub += 1
                    for kc in range(8):
                        P.add("pe", lambda e, kc=kc, b=b, fc=fc, c=c, ws=ws: e.matmul(
                            psU[b][:, :], lhsT=W1e[ws][:, kc, fc * 128:(fc + 1) * 128], rhs=HNT[:, kc, c * 512:(c + 1) * 512],
                            start=(kc == 0), stop=(kc == 7)),
                            reads=[("W1e", ws)] + [("HNT", tt) for tt in range(4 * c, 4 * c + 4)], writes=[("psU", b)])
                    P.add("act", lambda e, b=b: e.activation(out=RT[b][:, :], in_=psU[b][:, :], func=AF.Relu),
                          reads=[("psU", b)], writes=[("RT", b)])
                    P.add("dve", lambda e, b=b, us=us, fc=fc: e.tensor_tensor(out=UT[us][:, fc, :], in0=RT[b][:, :], in1=RT[b][:, :],
                                                                             op=ALU.mult),
                          reads=[("RT", b)], writes=[("UT", us, fc)])
                for tt in range(4):
                    t = 4 * c + tt
                    for n2 in range(2):
                        b = yb % 2
                        yb += 1
                        for fc in range(4):
                            P.add("pe", lambda e, fc=fc, b=b, tt=tt, n2=n2, us=us, ws=ws: e.matmul(
                                psY[b][:, :], lhsT=UT[us][:, fc, tt * 128:(tt + 1) * 128], rhs=W2e[ws][:, fc, n2 * 512:(n2 + 1) * 512],
                                start=(fc == 0), stop=(fc == 3)),
                                reads=[("UT", us, fc), ("W2e", ws)], writes=[("psY", b)])
                        P.add("dve", lambda e, b=b, t=t, n2=n2: e.tensor_tensor(
                            out=H[:, t, n2 * 512:(n2 + 1) * 512], in0=H[:, t, n2 * 512:(n2 + 1) * 512], in1=psY[b][:, :], op=ALU.add),
                            reads=[("psY", b), ("H", t, n2)], writes=[("H", t, n2)])

    def final_out(with_norm=True):
        if with_norm:
            dma("sp", "stg0", STG[:, 0:1024], fnorm_in, [], ["fnb"])
        for t in range(16):
            slot = t % 2
            yt = STG2[:, slot * 1024:(slot + 1) * 1024]
            if with_norm:
                P.add("dve", lambda e, t=t: e.memset(small[:, t:t + 1], 0.0), reads=[("ssq", t)], writes=[("ssq", t)])
                rms_tile(t, slot)
                P.add("dve", lambda e, t=t, yt=yt: e.scalar_tensor_tensor(
                    out=yt, in0=H[:, t, :], scalar=small[:, 16 + t:17 + t], in1=STG[:, 0:1024], op0=ALU.mult, op1=ALU.mult),
                    reads=[("H", t, 0), ("H", t, 1), ("rstd", t), "fnb"], writes=[("yt", slot)])
            else:
                P.add("dve", lambda e, t=t, yt=yt: e.tensor_copy(out=yt, in_=H[:, t, :]),
                      reads=[("H", t, 0), ("H", t, 1)], writes=[("yt", slot)])
            dma("sp", f"yst{slot}", y_out[t * 128:(t + 1) * 128, :], yt, [("yt", slot)], [("yout", t)])

    phase1(0)
    P.barrier()
    if stage != "F0":
        phase2(0)
        P.barrier()
    if stage == "F0":
        phase3(0)
        P.barrier()
        final_out(with_norm=False)
    elif stage == "A0":
        phase3(0, do_ffn=False)
        P.barrier()
        final_out(with_norm=False)
    elif stage == "L0":
        phase3(0)
        P.barrier()
        final_out(with_norm=False)
    else:
        phase3(0)
        P.barrier()
        phase1(1)
        P.barrier()
        phase2(1)
        P.barrier()
        if stage == "A1":
            phase3(1, do_ffn=False)
            P.barrier()
            final_out(with_norm=False)
        else:
            phase3(1)
            P.barrier()
            final_out(with_norm=True)
    P.barrier()
    P.finalize()

    with ExitStack() as st:
        sems = {}
        for e in Prog.ENGS:
            sems[("eng", e)] = st.enter_context(nc.semaphore(f"s_{e}"))
        for k in P.dma_keys:
            sems[("dma", k)] = st.enter_context(nc.semaphore(f"d_{k}"))
        for k in P.cc_keys:
            sems[("cc", k)] = st.enter_context(nc.semaphore(f"c_{k}"))
        block = st.enter_context(nc.Block())

        @block.sync
        def _(e):
            P.emit("sp", e, sems)

        @block.scalar
        def _(e):
            P.emit("act", e, sems)

        @block.vector
        def _(e):
            P.emit("dve", e, sems)

        @block.gpsimd
        def _(e):
            P.emit("pool", e, sems)

        @block.tensor
        def _(e):
            P.emit("pe", e, sems)
    return nc


def _slopes(n):
    return np.array([2.0 ** (-8.0 * (h + 1) / n) for h in range(n)], dtype=np.float64)


def _consts(r):
    bf = ml_dtypes.bfloat16
    s0 = _slopes(8)
    s1 = _slopes(16)
    ident = np.eye(128, dtype=np.float32).astype(bf)
    ki = np.arange(128)[:, None]
    qi = np.arange(128)[None, :]
    tri = np.where(qi < ki, NEG, 0.0).astype(np.float32).astype(bf)
    kaug = np.zeros((2, 32, T), np.float32)
    kaug[0, 0, :] = 1.0
    kaug[1, np.arange(T) // 256, np.arange(T)] = 1.0
    kaug = kaug.astype(bf)
    qaug0 = np.zeros((2, 32, 512), np.float32)
    btab = np.zeros((128, 2, 2, 2, NDC), np.float64)
    augq1 = np.zeros((128, 2, 2, 4), np.float64)
    pp = np.arange(128, dtype=np.float64)
    dc = np.arange(NDC, dtype=np.float64) - 3.0
    for p in range(2):
        s = s0[r + 4 * p]
        qaug0[p, 0, :] = -s * np.arange(512) / SCALE
        for x in range(2):
            btab[:, 0, p, x, :] = s * (pp[:, None] - 128.0 * dc[None, :])
            s_ = s1[2 * (r + 4 * p) + x]
            btab[:, 1, p, x, :] = s_ * (pp[:, None] - 128.0 * dc[None, :])
            for qt in range(4):
                augq1[:, p, x, qt] = -s_ * (qt * 128 + pp) / SCALE + NEG
    bsel = np.zeros((128, 2, 128), np.float32)
    bsel[64, 0, 0:64] = 1.0
    bsel[0, 1, 64:128] = 1.0
    return dict(ident=ident, trimask=tri, kaug=kaug, qaug0=qaug0.astype(bf),
                biastab=btab.reshape(128, -1).astype(np.float32), augq1=augq1.reshape(128, -1).astype(np.float32),
                bsel=bsel.reshape(128, 256))


_NC_CACHE = {}


def kernel(x, attn_norm, w_in, w_out, diff_lambda, diff_subln, mlp_norm, w_ff1, w_ff2, final_norm, _stage="full"):
    x = np.asarray(x, np.float32)
    attn_norm = np.asarray(attn_norm, np.float32)
    w_in = np.asarray(w_in, np.float32)
    w_out = np.ascontiguousarray(np.asarray(w_out, np.float32))
    diff_lambda = np.asarray(diff_lambda, np.float32)
    diff_subln = np.asarray(diff_subln, np.float32)
    mlp_norm = np.asarray(mlp_norm, np.float32)
    w_ff1 = np.ascontiguousarray(np.asarray(w_ff1, np.float32))
    w_ff2 = np.ascontiguousarray(np.asarray(w_ff2, np.float32))
    final_norm = np.asarray(final_norm, np.float32)

    if _stage not in _NC_CACHE:
        _NC_CACHE[_stage] = build_program(_stage)
    nc = _NC_CACHE[_stage]

    gains = np.zeros((128, 64), np.float32)
    for l in range(2):
        gains[:, l * 8:(l + 1) * 8] = attn_norm[l].reshape(8, 128).T
        gains[:, 16 + l * 8:16 + (l + 1) * 8] = mlp_norm[l].reshape(8, 128).T
    gains[:, 32] = diff_subln[0]
    fnb = np.ascontiguousarray(np.broadcast_to(final_norm[None, :], (128, D)))
    lamb = np.ascontiguousarray(np.broadcast_to(diff_lambda[0].reshape(1, 256), (128, 256)))

    in_maps = []
    for c in range(8):
        b, r = c // 4, c % 4
        win = np.zeros((4, D, 384), np.float32)
        for l in range(2):
            for p in range(2):
                c0 = 128 * (r + 4 * p)
                for j in range(3):
                    win[l * 2 + p, :, j * 128:(j + 1) * 128] = w_in[l, :, j * D + c0:j * D + c0 + 128]
        m = dict(x=np.ascontiguousarray(x[b, r * NT:(r + 1) * NT, :]), win=win, wout=w_out, wff1=w_ff1, wff2=w_ff2,
                 gains=gains, fnormb=fnb, lamb=lamb)
        m.update(_consts(r))
        in_maps.append(m)
    res = run_bass_kernel_spmd(nc, in_maps, core_ids=list(range(8)))
    out = np.zeros((2, T, D), np.float32)
    for c in range(8):
        b, r = c // 4, c % 4
        out[b, r * NT:(r + 1) * NT, :] = np.asarray(res.results[c]["y"], dtype=np.float32)
    return out
```

```python
import math
import os
from contextlib import ExitStack

import numpy as np
import ml_dtypes
import concourse.bass as bass
import concourse.mybir as mybir
from concourse.bass_utils import run_bass_kernel_spmd

F32 = mybir.dt.float32
BF16 = mybir.dt.bfloat16
ALU = mybir.AluOpType
AF = mybir.ActivationFunctionType
AX = mybir.AxisListType

D = 1024
T = 8192
NT = 2048
DFF = 4096
EPS = 1e-6
SCALE = 0.125
NEG = -30000.0
NDC = 67
GROUPS = [[0, 1, 2, 3], [4, 5, 6, 7]]


class Op:
    __slots__ = ("eng", "fn", "deps", "dma", "cc", "signal", "count", "idx")

    def __init__(self, eng, fn, dma=None, cc=None):
        self.eng = eng
        self.fn = fn
        self.deps = set()
        self.dma = dma
        self.cc = cc
        self.signal = False
        self.count = 0
        self.idx = 0


class Prog:
    ENGS = ["sp", "act", "dve", "pool", "pe"]

    def __init__(self):
        self.ops = {e: [] for e in self.ENGS}
        self.last_w = {}
        self.readers = {}
        self.dma_keys = {}
        self.cc_keys = []
        self.n = 0

    def add(self, eng, fn, reads=(), writes=(), dma=None, cc=None):
        op = Op(eng, fn, dma=dma, cc=cc)
        op.idx = self.n
        self.n += 1
        deps = set()
        for r in reads:
            w = self.last_w.get(r)
            if w is not None:
                deps.add(w)
        for w_ in writes:
            w = self.last_w.get(w_)
            if w is not None:
                deps.add(w)
            for rd in self.readers.get(w_, ()):
                deps.add(rd)
        deps.discard(op)
        if eng == "pe":
            deps = {d for d in deps if not (d.eng == "pe" and d.dma is None and d.cc is None)}
        op.deps = deps
        for r in reads:
            self.readers.setdefault(r, []).append(op)
        for w_ in writes:
            self.last_w[w_] = op
            self.readers[w_] = []
        self.ops[eng].append(op)
        if dma is not None:
            self.dma_keys.setdefault(dma, 0)
        if cc is not None:
            self.cc_keys.append(cc)
        return op

    def barrier(self, soft_cc=False):
        lasts = []
        for e in self.ENGS:
            for op in reversed(self.ops[e]):
                if op.dma is None and op.cc is None and op.fn is not None:
                    lasts.append(op)
                    break
        seen = {}
        for e in self.ENGS:
            for op in self.ops[e]:
                if op.dma is not None:
                    seen[op.dma] = op
                if op.cc is not None and not soft_cc:
                    seen[("cc", op.cc)] = op
        lasts += list(seen.values())
        for e in self.ENGS:
            b = Op(e, None)
            b.idx = self.n
            self.n += 1
            b.deps = set(lasts)
            self.ops[e].append(b)
        self.last_w = {k: v for k, v in self.last_w.items() if soft_cc and v.cc is not None}
        self.readers = {}

    def finalize(self):
        for e in self.ENGS:
            for op in self.ops[e]:
                for d in op.deps:
                    d.signal = True
        cnt = {e: 0 for e in self.ENGS}
        dcnt = {k: 0 for k in self.dma_keys}
        for e in self.ENGS:
            for op in self.ops[e]:
                if op.dma is not None:
                    dcnt[op.dma] += 16
                    op.count = dcnt[op.dma]
                elif op.cc is not None:
                    op.count = 1
                elif op.signal and op.fn is not None:
                    cnt[e] += 1
                    op.count = cnt[e]

    def emit(self, eng_name, eng, sems):
        waited = {}
        for op in self.ops[eng_name]:
            need = {}
            for d in op.deps:
                if d.dma is not None:
                    key = ("dma", d.dma)
                elif d.cc is not None:
                    key = ("cc", d.cc)
                else:
                    key = ("eng", d.eng)
                if d.count > need.get(key, 0):
                    need[key] = d.count
            for key, val in need.items():
                if waited.get(key, 0) >= val:
                    continue
                eng.wait_ge(sems[key], val)
                waited[key] = val
            if op.fn is None:
                continue
            ins = op.fn(eng)
            if op.dma is not None:
                ins.then_inc(sems[("dma", op.dma)], 16)
            elif op.cc is not None:
                ins.then_inc(sems[("cc", op.cc)])
            elif op.signal:
                ins.then_inc(sems[("eng", eng_name)], 1)


def build_program(stage="full"):
    nc = bass.Bass("TRN2", target_bir_lowering=False)
    P = Prog()

    def ext_in(name, shape, dt):
        return nc.dram_tensor(name, list(shape), dt, kind="ExternalInput").ap()

    x_in = ext_in("x", [NT, D], F32)
    win_in = ext_in("win", [4, D, 384], F32)
    wout_in = ext_in("wout", [2, D, D], F32)
    wff1_in = ext_in("wff1", [2, D, DFF], F32)
    wff2_in = ext_in("wff2", [2, DFF, D], F32)
    gains_in = ext_in("gains", [128, 64], F32)
    fnorm_in = ext_in("fnormb", [128, D], F32)
    lamb_in = ext_in("lamb", [128, 256], F32)
    ident_in = ext_in("ident", [128, 128], BF16)
    tri_in = ext_in("trimask", [128, 128], BF16)
    kaug_in = ext_in("kaug", [2, 32, T], BF16)
    qaug0_in = ext_in("qaug0", [2, 32, 512], BF16)
    btab_in = ext_in("biastab", [128, 8 * NDC], F32)
    augq1_in = ext_in("augq1", [128, 16], F32)
    bsel_in = ext_in("bsel", [128, 256], F32)
    y_out = nc.dram_tensor("y", [NT, D], F32, kind="ExternalOutput").ap()

    HTin = [[nc.dram_tensor(f"htin{l}_{i}", [D, 512], BF16) for i in range(4)] for l in range(2)]
    HT = [[nc.dram_tensor(f"ht{l}_{i}", [4 * D, 512], BF16) for i in range(4)] for l in range(2)]
    OSin = [[[nc.dram_tensor(f"osin{l}_{p}_{q}", [128, 2048], BF16) for q in range(4)]
             for p in range(2)] for l in range(2)]
    OS = [[nc.dram_tensor(f"os{l}_{p}", [2048, 2048], BF16) for p in range(2)] for l in range(2)]

    off = [16512]

    def sb(name, shape, dt, at=None):
        nbytes = int(np.prod(shape[1:])) * (4 if dt == F32 else 2)
        if at is None:
            o = off[0]
            off[0] += (nbytes + 31) // 32 * 32
        else:
            o = at
        return nc.alloc_sbuf_tensor_at(name, list(shape), dt, offset=o), o + (nbytes + 31) // 32 * 32

    H, _ = sb("H", [128, 16, D], F32)
    ident, _ = sb("identt", [128, 128], BF16)
    tri, _ = sb("trit", [128, 128], BF16)
    ones128, _ = sb("ones128", [128, 128], F32)
    bsel, _ = sb("bselt", [128, 256], F32)
    btab, _ = sb("btab", [128, 8 * NDC], F32)
    augq1, _ = sb("augq1t", [128, 16], F32)
    gains, _ = sb("gainst", [128, 64], F32)
    lamb, _ = sb("lambt", [128, 256], F32)
    small, _ = sb("small", [128, 64], F32)
    epsc, _ = sb("epsc", [128, 1], F32)
    PH = off[0]

    o = PH
    KT = []
    for x_ in range(2):
        t_, o = sb(f"KT{x_}", [128, T], BF16, at=o)
        KT.append(t_)
    Vb, o = sb("Vb", [128, 64 * 192], BF16, at=o)
    WIN, o = sb("WIN", [128, 8, 384], BF16, at=o)
    HNTc = []
    for s_ in range(2):
        t_, o = sb(f"HNTc{s_}", [128, 8, 512], BF16, at=o)
        HNTc.append(t_)
    QT = []
    for s_ in range(2):
        row = []
        for x_ in range(2):
            t_, o = sb(f"QT{s_}_{x_}", [128, 512], BF16, at=o)
            row.append(t_)
        QT.append(row)
    PT = []
    for s_ in range(4):
        t_, o = sb(f"PT{s_}", [128, 512], BF16, at=o)
        PT.append(t_)
    Zs = []
    for x_ in range(2):
        row = []
        for par in range(2):
            t_, o = sb(f"Zs{x_}_{par}", [128, 512], F32, at=o)
            row.append(t_)
        Zs.append(row)
    FT = []
    for i_ in range(4):
        t_, o = sb(f"FT{i_}", [128, 512], F32, at=o)
        FT.append(t_)
    FTo = []
    for i_ in range(2):
        t_, o = sb(f"FTo{i_}", [128, 512], F32, at=o)
        FTo.append(t_)
    OSB = [FT[2], FT[3]]
    Ocat = []
    for s_ in range(2):
        t_, o = sb(f"Ocat{s_}", [128, 512], BF16, at=o)
        Ocat.append(t_)
    gate_sb, o = sb("gate_sb", [128, 8, 32], F32, at=o)
    m8, o = sb("m8", [128, 8, 8], F32, at=o)
    selt, o = sb("selt", [128, 32], F32, at=o)
    NS = []
    for x_ in range(2):
        t_, o = sb(f"NS{x_}", [128, 4, 96], BF16, at=o)
        NS.append(t_)
    kmT, o = sb("kmT", [128, 32], BF16, at=o)
    km32, o = sb("km32", [128, 2], F32, at=o)
    STG, o = sb("STG", [128, 2048], F32, at=o)
    STG2, o = sb("STG2", [128, 2048], F32, at=o)
    P2END = o
    assert P2END <= 229376, P2END

    o = PH
    HNT, o = sb("HNT", [128, 8, NT], BF16, at=o)
    OT = HNT
    WO, o = sb("WO", [128, 8, D], BF16, at=o)
    W1e, W2e = [], []
    for s_ in range(2):
        t_, o = sb(f"W1e{s_}", [128, 8, 512], BF16, at=o)
        W1e.append(t_)
        t_, o = sb(f"W2e{s_}", [128, 4, D], BF16, at=o)
        W2e.append(t_)
    hnb = []
    for s_ in range(2):
        t_, o = sb(f"hnb{s_}", [128, D], BF16, at=o)
        hnb.append(t_)
    UT = []
    for s_ in range(2):
        t_, o = sb(f"UT{s_}", [128, 4, 512], BF16, at=o)
        UT.append(t_)
    RT = []
    for s_ in range(2):
        t_, o = sb(f"RT{s_}", [128, 512], BF16, at=o)
        RT.append(t_)
    assert o <= P2END - 2 * 8192, (o, P2END)
    YT = [STG, STG2]

    pb = [nc.alloc_psum_tensor(f"pb{i}", [128, 512], F32) for i in range(8)]
    psS = pb[0:4]
    psO = pb[4:6]
    psP = pb[6]
    psX = pb[7]
    psXb = psX[:, 256:512].bitcast(BF16)
    psT = pb[6][:, :].bitcast(BF16)
    psU = pb[0:2]
    psY = pb[2:4]

    ctx = {}
    OTQ = os.environ.get("OTQ", "sp")

    def dma(eng, key, out, in_, reads, writes):
        return P.add(eng, lambda e: e.dma_start(out=out, in_=in_), reads=reads, writes=writes, dma=key)

    def allgather(name, in_ap, out_ap, reads, writes):
        return P.add("pool", lambda e: e.collective_compute(
            "AllGather", ALU.bypass, replica_groups=GROUPS, ins=[in_ap.opt()], outs=[out_ap.opt()]),
            reads=reads, writes=writes, cc=name)

    dma("sp", "c0", ident[:, :], ident_in, [], ["ident"])
    dma("sp", "c0", tri[:, :], tri_in, [], ["tri"])
    dma("sp", "c0", bsel[:, :], bsel_in, [], ["bsel"])
    dma("sp", "c0", btab[:, :], btab_in, [], ["btab"])
    dma("sp", "c0", augq1[:, :], augq1_in, [], ["augq1"])
    dma("sp", "c0", gains[:, :], gains_in, [], ["gains"])
    dma("sp", "c0", lamb[:, :], lamb_in, [], ["lamb"])
    P.add("dve", lambda e: e.memset(ones128[:, :], 1.0), writes=["ones128"])
    P.add("dve", lambda e: e.memset(epsc[:, :], EPS), writes=["epsc"])
    P.add("dve", lambda e: e.memset(small[:, :], 0.0), writes=["small"])
    P.add("dve", lambda e: e.tensor_tensor(out=FT[0][:, 0:64], in0=lamb[:, 0:64], in1=lamb[:, 64:128], op=ALU.mult),
          reads=["lamb"], writes=["ft0"])
    P.add("dve", lambda e: e.tensor_tensor(out=FT[0][:, 64:128], in0=lamb[:, 128:192], in1=lamb[:, 192:256], op=ALU.mult),
          reads=["lamb"], writes=["ft0b"])
    P.add("dve", lambda e: e.tensor_reduce(out=small[:, 34:36], in_=FT[0][:, 0:128].rearrange("p (a b) -> p a b", a=2),
                                           axis=AX.X, op=ALU.add),
          reads=["ft0", "ft0b", "small"], writes=["lam_s"])
    P.add("act", lambda e: e.activation(out=small[:, 36:38], in_=small[:, 34:36], func=AF.Exp),
          reads=["lam_s"], writes=["lam_e"])
    P.add("dve", lambda e: e.tensor_tensor(out=small[:, 38:39], in0=small[:, 37:38], in1=small[:, 36:37], op=ALU.subtract),
          reads=["lam_e"], writes=["lam_d"])
    P.add("dve", lambda e: e.tensor_scalar(out=small[:, 32:33], in0=small[:, 38:39], scalar1=-0.2, scalar2=None, op0=ALU.add),
          reads=["lam_d"], writes=["neglam"])
    P.add("dve", lambda e: e.tensor_scalar(out=small[:, 33:34], in0=gains[:, 32:33], scalar1=0.8, scalar2=None, op0=ALU.mult),
          reads=["gains", "small"], writes=["sub08"])
    for t in range(16):
        dma("sp", "xload", H[:, t, :], x_in[t * 128:(t + 1) * 128, :], [], [("H", t, 0), ("H", t, 1)])

    P.barrier()

    def rms_tile(t, slot):
        P.add("act", lambda e: e.activation(out=hnb[slot][:, :], in_=H[:, t, :], func=AF.Square,
                                            accum_out=small[:, t:t + 1]),
              reads=[("H", t, 0), ("H", t, 1), "small"], writes=[("hnb", slot), ("ssq", t)])
        P.add("act", lambda e: e.activation(out=small[:, 16 + t:17 + t], in_=small[:, t:t + 1], func=AF.Ln,
                                            bias=epsc[:, 0:1], scale=1.0 / D),
              reads=[("ssq", t), "epsc"], writes=[("rstd", t)])
        P.add("act", lambda e: e.activation(out=small[:, 16 + t:17 + t], in_=small[:, 16 + t:17 + t], func=AF.Exp, scale=-0.5),
              reads=[("rstd", t)], writes=[("rstd", t)])

    def norm_transpose(t):
        slot = t % 2
        P.add("dve", lambda e: e.memset(small[:, t:t + 1], 0.0), reads=[("ssq", t)], writes=[("ssq", t)])
        rms_tile(t, slot)
        P.add("dve", lambda e: e.tensor_scalar(out=hnb[slot][:, :], in0=H[:, t, :], scalar1=small[:, 16 + t:17 + t],
                                               scalar2=None, op0=ALU.mult),
              reads=[("H", t, 0), ("H", t, 1), ("rstd", t)], writes=[("hnb", slot)])
        for kc in range(8):
            P.add("pe", lambda e, kc=kc: e.transpose(out=psT[:, kc * 128:(kc + 1) * 128],
                                                     in_=hnb[slot][:, kc * 128:(kc + 1) * 128], identity=ident[:, :]),
                  reads=[("hnb", slot), "ident"], writes=["psT"])
        P.add("act", lambda e: e.copy(out=HNT[:, :, t * 128:(t + 1) * 128],
                                      in_=psT.rearrange("p (k q) -> p k q", k=8)),
              reads=["psT"], writes=[("HNT", t)])

    def phase1(l):
        for t in range(16):
            norm_transpose(t)
            if t % 4 == 3:
                i = t // 4
                dma("sp", f"htst{i}", HTin[l][i].ap().rearrange("(kc p) t -> p kc t", p=128),
                    HNT[:, :, i * 512:(i + 1) * 512], [("HNT", tt) for tt in range(4 * i, 4 * i + 4)],
                    [("HTin", l, i)])
                allgather(f"agh{l}_{i}", HTin[l][i].ap(), HT[l][i].ap(), [("HTin", l, i)], [("HT", l, i)])

    def load_cast_win(l, p):
        wsrc = win_in[l * 2 + p].rearrange("(kc p) n -> p kc n", p=128)
        for hf in range(2):
            stg = (STG if hf == 0 else STG2)[:, 0:4 * 384].rearrange("p (k n) -> p k n", k=4)
            dma("sp", f"stg{hf}", stg, wsrc[:, hf * 4:hf * 4 + 4, :], [], [("STG", hf)])
            for k4 in range(4):
                kc = hf * 4 + k4
                P.add("pool", lambda e, stg=stg, k4=k4, kc=kc: e.tensor_scalar(
                    out=WIN[:, kc, :], in0=stg[:, k4, :], scalar1=gains[:, l * 8 + kc:l * 8 + kc + 1],
                    scalar2=None, op0=ALU.mult),
                    reads=[("STG", hf), "gains"], writes=[("WIN", kc)])

    def phase2(l):
        V0 = Vb[:, 0:64 * 128].rearrange("p (t c) -> p t c", c=128)
        V1 = Vb[:, :].rearrange("p (t c) -> p t c", c=192)
        dma("sp", "kaug0", KT[0][64:96, :], kaug_in[l], [], [("KTaug", 0)])
        dma("sp", "kaug1", KT[1][0:32, :], kaug_in[l], [], [("KTaug", 1)])
        P.add("pool", lambda e: e.memset(KT[1][32:64, :], 0.0), writes=[("KTz", 1)])
        for s_ in range(2):
            for x_ in range(2):
                P.add("pool", lambda e, s_=s_, x_=x_: e.memset(QT[s_][x_][:, :], 0.0), writes=[("QT", s_, x_), ("QTaug", s_, x_)])
        if l == 1:
            P.add("pool", lambda e: e.memset(Vb[:, :], 0.0), writes=["Vall"])
            P.add("pool", lambda e: e.memset(V1[:, :, 64:65], 1.0), reads=["Vall"], writes=["Vall"])
            for x_ in range(2):
                P.add("pool", lambda e, x_=x_: e.memset(NS[x_][:, :, :], 0.0), writes=[("NS", x_)])
                P.add("pool", lambda e, x_=x_: e.memset(OSB[x_][:, :], 0.0), writes=[("OSB", x_), ("FT", 2 + x_)])
            P.add("pool", lambda e: e.memset(kmT[:, :], 0.0), writes=["kmT"])
        for p in range(2):
            load_cast_win(l, p)
            if l == 0:
                for s_ in range(2):
                    dma("sp", f"qaug{s_}0", QT[s_][0][64:96, :], qaug0_in[p], [("QTaug", s_, 0)], [("QTaug", s_, 0)])
                    dma("sp", f"qaug{s_}1", QT[s_][1][0:32, :], qaug0_in[p], [("QTaug", s_, 1)], [("QTaug", s_, 1)])
            for _ in chunk_proj(l, p, 0, V0, V1):
                pass
            fin = None
            for g in range(16):
                gen = chunk_proj(l, p, g + 1, V0, V1) if g + 1 < 16 else None
                chunk_attn(l, p, g, V0, V1, gen, fin)
                fin = fin_gen(l, p, g)
                next(fin)
            for _ in fin:
                pass

    bidx = [0]
    NSLOT = 4

    def chunk_proj(l, p, g, V0, V1):
        s = g % 2
        hsrc = HT[l][g // 4][(g % 4) * D:(g % 4 + 1) * D, :].rearrange("(kc p) t -> p kc t", p=128)
        dma("sp", f"hntc{s}", HNTc[s][:, :, :], hsrc, [("HT", l, g // 4)], [("HNTc", s)])
        win_r = [("WIN", kc) for kc in range(8)]
        for kc in range(8):
            P.add("pe", lambda e, kc=kc: e.matmul(psP[:, :], lhsT=WIN[:, kc, 0:128], rhs=HNTc[s][:, kc, :],
                                                  start=(kc == 0), stop=(kc == 7)),
                  reads=[("HNTc", s)] + win_r, writes=["psP"])
        P.add("dve", lambda e: e.tensor_copy(out=QT[s][0][0:64, :], in_=psP[0:64, :]), reads=["psP"], writes=[("QT", s, 0)])
        P.add("dve", lambda e: e.tensor_copy(out=QT[s][1][64:128, :], in_=psP[64:128, :]), reads=["psP"], writes=[("QT", s, 1)])
        yield
        for kc in range(8):
            P.add("pe", lambda e, kc=kc: e.matmul(psP[:, :], lhsT=WIN[:, kc, 128:256], rhs=HNTc[s][:, kc, :],
                                                  start=(kc == 0), stop=(kc == 7)),
                  reads=[("HNTc", s)] + win_r, writes=["psP"])
        P.add("dve", lambda e: e.tensor_copy(out=KT[0][0:64, g * 512:(g + 1) * 512], in_=psP[0:64, :]),
              reads=["psP"], writes=[("KT", 0, g)])
        P.add("dve", lambda e: e.tensor_copy(out=KT[1][64:128, g * 512:(g + 1) * 512], in_=psP[64:128, :]),
              reads=["psP"], writes=[("KT", 1, g)])
        if l == 1:
            P.add("dve", lambda e: e.tensor_reduce(out=km32[:, :], in_=psP[:, :].rearrange("p (a b) -> p a b", a=2),
                                                   axis=AX.X, op=ALU.add), reads=["psP"], writes=["km32"])
            P.add("dve", lambda e: e.tensor_scalar(out=kmT[:, 2 * g:2 * g + 2], in0=km32[:, :], scalar1=1.0 / 256,
                                                   scalar2=None, op0=ALU.mult), reads=["km32"], writes=["kmT"])
        yield
        for tt in range(4):
            for kc in range(8):
                P.add("pe", lambda e, kc=kc, tt=tt: e.matmul(psP[:, tt * 128:(tt + 1) * 128],
                                                             lhsT=HNTc[s][:, kc, tt * 128:(tt + 1) * 128],
                                                             rhs=WIN[:, kc, 256:384], start=(kc == 0), stop=(kc == 7)),
                      reads=[("HNTc", s)] + win_r, writes=["psP"])
            if tt == 1:
                yield
        psP4 = psP[:, :].rearrange("p (t c) -> p t c", t=4)
        if l == 0:
            P.add("dve", lambda e: e.tensor_copy(out=V0[:, 4 * g:4 * g + 4, :], in_=psP4), reads=["psP"], writes=[("V", g)])
        else:
            P.add("dve", lambda e: e.tensor_copy(out=V1[:, 4 * g:4 * g + 4, 0:64], in_=psP4[:, :, 0:64]),
                  reads=["psP", "Vall"], writes=[("V", g)])
            P.add("dve", lambda e: e.tensor_copy(out=V1[:, 4 * g:4 * g + 4, 128:192], in_=psP4[:, :, 64:128]),
                  reads=["psP", "Vall"], writes=[("Vb_", g)])
        yield
        if l == 1 and not os.environ.get("SKIP_GATE"):
            moba_gate(p, g, s)
        yield

    def chunk_attn(l, p, g, V0, V1, gen=None, fin=None):
        s = g % 2
        nk = 4 * g + 4
        if l == 1 and os.environ.get("SKIP_ATTN"):
            nk = 0
        kt0 = max(0, 4 * g - 16) if p == 0 else 0
        items = [(kt, x_) for kt in range(kt0, nk) for x_ in range(2)]
        LOOK = 3
        slots = {}

        def emit_s(kt, x_):
            j = kt - 4 * g
            c0 = 128 * j if j >= 0 else 0
            b = bidx[0] % NSLOT
            bidx[0] += 1
            slots[(kt, x_)] = b
            kx = 96 if x_ == 0 else 128
            kr = [("KT", x_, kt // 4), ("KTaug", x_), ("QT", s, x_), ("QTaug", s, x_)] + ([("KTz", 1)] if x_ == 1 else [])
            ks = slice(kt * 128, (kt + 1) * 128)
            if j >= 0:
                P.add("pe", lambda e: e.matmul(
                    psS[b][:, c0:c0 + 128], lhsT=KT[x_][0:kx, ks], rhs=QT[s][x_][0:kx, c0:c0 + 128],
                    start=True, stop=False), reads=kr, writes=[("psS", b)])
                P.add("pe", lambda e: e.matmul(
                    psS[b][:, c0:c0 + 128], lhsT=ident[:, :], rhs=tri[:, :], start=False, stop=True),
                    reads=["ident", "tri"], writes=[("psS", b)])
                if c0 + 128 < 512:
                    P.add("pe", lambda e: e.matmul(
                        psS[b][:, c0 + 128:512], lhsT=KT[x_][0:kx, ks], rhs=QT[s][x_][0:kx, c0 + 128:512],
                        start=True, stop=True), reads=kr, writes=[("psS", b)])
            else:
                P.add("pe", lambda e: e.matmul(
                    psS[b][:, :], lhsT=KT[x_][0:kx, ks], rhs=QT[s][x_][0:kx, :], start=True, stop=True),
                    reads=kr, writes=[("psS", b)])
            col = (((l * 2 + p) * 2 + x_) * NDC) + (4 * g - kt + 3)
            P.add("act", lambda e: e.activation(
                out=PT[b][:, c0:512], in_=psS[b][:, c0:512], func=AF.Exp, bias=btab[:, col:col + 1], scale=SCALE),
                reads=[("psS", b), "btab"], writes=[("PT", b)])
            if l == 0:
                zeng = "dve" if x_ == 0 else "pool"
                zt = Zs[x_][g % 2]
                zk = ("Zs", x_, g % 2)
                if kt == kt0:
                    P.add(zeng, lambda e: e.tensor_copy(out=zt[:, :], in_=PT[b][:, :]),
                          reads=[("PT", b)], writes=[zk])
                else:
                    P.add(zeng, lambda e: e.tensor_tensor(
                        out=zt[:, c0:512], in0=zt[:, c0:512], in1=PT[b][:, c0:512], op=ALU.add),
                        reads=[("PT", b), zk], writes=[zk])

        def emit_pv(kt, x_):
            j = kt - 4 * g
            c0 = 128 * j if j >= 0 else 0
            b = slots[(kt, x_)]
            if l == 0:
                vl = V0[:, kt, :]
                mo = 128
            else:
                vl = V1[:, kt, 0:65] if x_ == 0 else V1[:, kt, 64:192]
                mo = 65 if x_ == 0 else 128
            P.add("pe", lambda e: e.matmul(
                psO[x_][0:mo, c0:512], lhsT=vl, rhs=PT[b][:, c0:512], start=(kt == kt0), stop=(kt == nk - 1)),
                reads=[("PT", b), ("V", kt // 4), ("Vb_", kt // 4), "Vall"], writes=[("psO", x_)])

        for n in range(len(items) + LOOK):
            if n < len(items):
                emit_s(*items[n])
            if n - LOOK >= 0:
                emit_pv(*items[n - LOOK])
            if n >= 4 and n % 3 == 1 and gen is not None:
                next(gen, None)
            if n >= 2 and n % 3 == 2 and fin is not None:
                next(fin, None)
        if fin is not None:
            for _ in fin:
                pass
        if gen is not None:
            for _ in gen:
                pass

    def fin_gen(l, p, g):
        s = g % 2
        oc = Ocat[s]
        zp = g % 2
        if l == 0:
            for x_ in range(2):
                P.add("dve", lambda e, x_=x_: e.tensor_copy(out=FTo[x_][:, :], in_=psO[x_][:, :]),
                      reads=[("psO", x_)], writes=[("FTo", x_)])
            yield
            for x_ in range(2):
                P.add("pe", lambda e, x_=x_: e.matmul(psX[:, :], lhsT=ones128[:, :], rhs=Zs[x_][zp][:, :], start=True, stop=True),
                      reads=[("Zs", x_, zp), "ones128"], writes=["psX"])
                P.add("dve", lambda e, x_=x_: e.reciprocal(out=FT[x_][:, :], in_=psX[:, :]), reads=["psX"], writes=[("FT", x_)])
                yield
                P.add("dve", lambda e, x_=x_: e.tensor_tensor(out=FT[x_][:, :], in0=FTo[x_][:, :], in1=FT[x_][:, :], op=ALU.mult),
                      reads=[("FTo", x_), ("FT", x_)], writes=[("FT", x_)])
            P.add("dve", lambda e: e.scalar_tensor_tensor(out=FT[2][:, :], in0=FT[1][:, :], scalar=small[:, 32:33], in1=FT[0][:, :],
                                                          op0=ALU.mult, op1=ALU.add),
                  reads=[("FT", 0), ("FT", 1), "neglam"], writes=[("FT", 2)])
            P.add("pool", lambda e: e.tensor_tensor(out=FT[3][:, :], in0=FT[2][:, :], in1=FT[2][:, :], op=ALU.mult),
                  reads=[("FT", 2)], writes=[("FT", 3)])
            yield
            P.add("pe", lambda e: e.matmul(psX[:, :], lhsT=ones128[:, :], rhs=FT[3][:, :], start=True, stop=True),
                  reads=[("FT", 3), "ones128"], writes=["psX"])
            P.add("act", lambda e: e.activation(out=FT[3][:, :], in_=psX[:, :], func=AF.Ln, bias=epsc[:, 0:1], scale=1.0 / 128),
                  reads=["psX", "epsc"], writes=[("FT", 3)])
            P.add("act", lambda e: e.activation(out=FT[3][:, :], in_=FT[3][:, :], func=AF.Exp, scale=-0.5),
                  reads=[("FT", 3)], writes=[("FT", 3)])
            yield
            P.add("dve", lambda e: e.tensor_tensor(out=oc[:, :], in0=FT[2][:, :], in1=FT[3][:, :], op=ALU.mult),
                  reads=[("FT", 2), ("FT", 3)], writes=[("Ocat", s)])
        else:
            for x_ in range(2):
                mo = 65 if x_ == 0 else 128
                P.add("dve", lambda e, x_=x_, mo=mo: e.tensor_copy(out=OSB[x_][0:mo, :], in_=psO[x_][0:mo, :]),
                      reads=[("psO", x_)], writes=[("OSB", x_)])
            yield
            for x_ in range(2):
                rows = slice(0, 64) if x_ == 0 else slice(64, 128)
                P.add("pe", lambda e, x_=x_: e.matmul(psX[:, :], lhsT=bsel[:, x_ * 128:(x_ + 1) * 128], rhs=OSB[x_][:, :],
                                                      start=True, stop=True), reads=[("OSB", x_), "bsel"], writes=["psX"])
                P.add("dve", lambda e, x_=x_, rows=rows: e.reciprocal(out=FT[x_][rows, :], in_=psX[rows, :]),
                      reads=["psX"], writes=[("FT", x_)])
                yield
                P.add("dve", lambda e, x_=x_, rows=rows: e.tensor_tensor(out=oc[rows, :], in0=OSB[x_][rows, :], in1=FT[x_][rows, :],
                                                                        op=ALU.mult),
                      reads=[("OSB", x_), ("FT", x_)], writes=[("Ocat", s, x_)])
        dma("sp", f"ost{s}", OSin[l][p][g // 4][:, (g % 4) * 512:(g % 4 + 1) * 512], oc[:, :],
            [("Ocat", s), ("Ocat", s, 0), ("Ocat", s, 1)], [("OSin", l, p, g // 4)])
        yield
        if g % 4 == 3:
            q = g // 4
            allgather(f"ago{l}_{p}_{q}", OSin[l][p][q].ap(), OS[l][p][q * 512:(q + 1) * 512, :],
                      [("OSin", l, p, q)], [("OS", l, p, q)])
        yield

    def moba_gate(p, g, s):
        gbank = [psX, psP]
        gkey = ["psX", "psP"]
        for x_ in range(2):
            rows = slice(0, 64) if x_ == 0 else slice(64, 128)
            for qt in range(4):
                P.add("pe", lambda e, x_=x_, qt=qt, rows=rows: e.matmul(
                    gbank[x_][:, qt * 32:(qt + 1) * 32], lhsT=QT[s][x_][rows, qt * 128:(qt + 1) * 128], rhs=kmT[rows, :],
                    start=True, stop=True), reads=[("QT", s, x_), "kmT"], writes=[gkey[x_]])
        P.add("dve", lambda e: e.memset(gate_sb[:, :, :], -1e30), writes=["gate"])
        gsv = gate_sb[:, :, :].rearrange("p (x h q) n -> p x h q n", x=2, h=2)
        for hf in range(2):
            jb = 2 * g + hf
            if jb > 0:
                for x_ in range(2):
                    psg = gbank[x_][:, 0:128].rearrange("p (h q n) -> p h q n", h=2, q=2)
                    P.add("dve", lambda e, hf=hf, jb=jb, x_=x_, psg=psg: e.tensor_copy(out=gsv[:, x_, hf, :, 0:jb], in_=psg[:, hf, :, 0:jb]),
                          reads=[gkey[x_], "gate"], writes=["gate"])
        glev = int(os.environ.get("GATE_LEVEL", "3"))
        if glev < 2:
            return
        for x_ in range(2):
            base = 64 if x_ == 0 else 0
            for qt in range(4):
                xq = x_ * 4 + qt
                jb = 2 * g + qt // 2
                ai = (p * 2 + x_) * 4 + qt
                P.add("dve", lambda e, xq=xq: e.max(out=m8[:, xq, :], in_=gate_sb[:, xq, :]), reads=["gate"], writes=[("m8", xq)])
                P.add("dve", lambda e, xq=xq: e.tensor_scalar(out=selt[:, :], in0=gate_sb[:, xq, :], scalar1=m8[:, xq, 2:3],
                                                              scalar2=None, op0=ALU.is_ge),
                      reads=["gate", ("m8", xq)], writes=["selt"])
                P.add("dve", lambda e, x_=x_, qt=qt, base=base, ai=ai: e.tensor_scalar(
                    out=NS[x_][:, qt, base:base + 32], in0=selt[:, :], scalar1=-NEG, scalar2=augq1[:, ai:ai + 1],
                    op0=ALU.mult, op1=ALU.add), reads=["selt", "augq1", ("NS", x_)], writes=[("NS", x_)])
                P.add("dve", lambda e, x_=x_, qt=qt, base=base, ai=ai, jb=jb: e.tensor_scalar(
                    out=NS[x_][:, qt, base + jb:base + jb + 1], in0=augq1[:, ai:ai + 1], scalar1=-NEG, scalar2=None,
                    op0=ALU.add), reads=["augq1", ("NS", x_)], writes=[("NS", x_)])
        if glev < 3:
            return
        for x_ in range(2):
            rows = slice(64, 96) if x_ == 0 else slice(0, 32)
            for qt in range(4):
                P.add("pe", lambda e, x_=x_, qt=qt: e.transpose(out=psXb[0:96, qt * 128:(qt + 1) * 128],
                                                                in_=NS[x_][:, qt, :], identity=ident[:, :]),
                      reads=[("NS", x_), "ident"], writes=["psX"])
            P.add("dve", lambda e, x_=x_, rows=rows: e.tensor_copy(out=QT[s][x_][rows, :], in_=psXb[rows, :]),
                  reads=["psX"], writes=[("QTaug", s, x_)])

    def phase3(l, do_ffn=True):
        def rank_of(e):
            if "rank" not in ctx:
                ctx["rank"] = e.partition_id() % 4
            return ctx["rank"]
        for q in range(4):
            for kc in range(8):
                src, p = kc // 2, kc % 2
                P.add(OTQ, lambda e, kc=kc, src=src, p=p, q=q: e.dma_start(
                    out=OT[:, kc, q * 512:(q + 1) * 512],
                    in_=OS[l][p][q * 512 + src * 128:q * 512 + src * 128 + 128, bass.ds(rank_of(e) * 512, 512)]),
                    reads=[("OS", l, p, q)], writes=[("OT", kc, q)], dma="otld")
        wsrc = wout_in[l].rearrange("(kc p) n -> p kc n", p=128)
        for i in range(4):
            stg = (STG if i % 2 == 0 else STG2)[:, :].rearrange("p (k n) -> p k n", k=2)
            dma("sp", f"stg{i % 2}", stg, wsrc[:, 2 * i:2 * i + 2, :], [], [("STG", i % 2)])
            if l == 0:
                P.add("pool", lambda e, stg=stg, i=i: e.tensor_scalar(out=WO[:, 2 * i:2 * i + 2, :], in0=stg, scalar1=small[:, 33:34],
                                                                      scalar2=None, op0=ALU.mult),
                      reads=[("STG", i % 2), "sub08"], writes=[("WO", i)])
            else:
                P.add("pool", lambda e, stg=stg, i=i: e.tensor_copy(out=WO[:, 2 * i:2 * i + 2, :], in_=stg),
                      reads=[("STG", i % 2)], writes=[("WO", i)])
        yb = 0
        for t in range(16):
            for n2 in range(2):
                b = yb % 2
                yb += 1
                for kc in range(8):
                    wk = kc // 2 + 4 * (kc % 2)
                    P.add("pe", lambda e, kc=kc, wk=wk, b=b, t=t, n2=n2: e.matmul(
                        psY[b][:, :], lhsT=OT[:, kc, t * 128:(t + 1) * 128], rhs=WO[:, wk, n2 * 512:(n2 + 1) * 512],
                        start=(kc == 0), stop=(kc == 7)), reads=[("OT", k_, q_) for k_ in range(8) for q_ in range(4)] + [("WO", wk // 2)], writes=[("psY", b)])
                P.add("dve", lambda e, b=b, t=t, n2=n2: e.tensor_tensor(
                    out=H[:, t, n2 * 512:(n2 + 1) * 512], in0=H[:, t, n2 * 512:(n2 + 1) * 512], in1=psY[b][:, :], op=ALU.add),
                    reads=[("psY", b), ("H", t, n2)], writes=[("H", t, n2)])
        if not do_ffn:
            return
        P.barrier()
        for t in range(16):
            norm_transpose(t)
        w1src = wff1_in[l].rearrange("(kc p) n -> p kc n", p=128)
        w2src = wff2_in[l].rearrange("(fc p) n -> p fc n", p=128)
        ub = 0
        for ei in range(8):
            ws = ei % 2
            for hf in range(2):
                stg = (STG if hf == 0 else STG2)[:, :].rearrange("p (k n) -> p k n", k=4)
                dma("sp", f"stg{hf}", stg, w1src[:, hf * 4:hf * 4 + 4, ei * 512:(ei + 1) * 512], [], [("STG", hf)])
                for k4 in range(4):
                    kc = hf * 4 + k4
                    P.add("pool", lambda e, stg=stg, k4=k4, kc=kc, ws=ws: e.tensor_scalar(
                        out=W1e[ws][:, kc, :], in0=stg[:, k4, :], scalar1=gains[:, 16 + l * 8 + kc:17 + l * 8 + kc],
                        scalar2=None, op0=ALU.mult), reads=[("STG", hf), "gains"], writes=[("W1e", ws)])
            for hf in range(2):
                stg = (STG if hf == 0 else STG2)[:, :].rearrange("p (k n) -> p k n", k=2)
                dma("sp", f"stg{hf}", stg, w2src[:, ei * 4 + hf * 2:ei * 4 + hf * 2 + 2, :], [], [("STG", hf)])
                P.add("act", lambda e, stg=stg, hf=hf, ws=ws: e.copy(out=W2e[ws][:, 2 * hf:2 * hf + 2, :], in_=stg),
                      reads=[("STG", hf)], writes=[("W2e", ws)])
            for c in range(4):
                us = (ei * 4 + c) % 2
                for fc in range(4):
                    b = ub % 2
                    ub += 1
                    for kc in range(8):
                        P.add("pe", lambda e, kc=kc, b=b, fc=fc, c=c, ws=ws: e.matmul(
                            psU[b][:, :], lhsT=W1e[ws][:, kc, fc * 128:(fc + 1) * 128], rhs=HNT[:, kc, c * 512:(c + 1) * 512],
                            start=(kc == 0), stop=(kc == 7)),
                            reads=[("W1e", ws)] + [("HNT", tt) for tt in range(4 * c, 4 * c + 4)], writes=[("psU", b)])
                    P.add("act", lambda e, b=b: e.activation(out=RT[b][:, :], in_=psU[b][:, :], func=AF.Relu),
                          reads=[("psU", b)], writes=[("RT", b)])
                    P.add("dve", lambda e, b=b, us=us, fc=fc: e.tensor_tensor(out=UT[us][:, fc, :], in0=RT[b][:, :], in1=RT[b][:, :],
                                                                             op=ALU.mult),
                          reads=[("RT", b)], writes=[("UT", us, fc)])
                for tt in range(4):
                    t = 4 * c + tt
                    for n2 in range(2):
                        b = yb % 2
                        yb += 1
                        for fc in range(4):
                            P.add("pe", lambda e, fc=fc, b=b, tt=tt, n2=n2, us=us, ws=ws: e.matmul(
                                psY[b][:, :], lhsT=UT[us][:, fc, tt * 128:(tt + 1) * 128], rhs=W2e[ws][:, fc, n2 * 512:(n2 + 1) * 512],
                                start=(fc == 0), stop=(fc == 3)),
                                reads=[("UT", us, fc), ("W2e", ws)], writes=[("psY", b)])
                        P.add("dve", lambda e, b=b, t=t, n2=n2: e.tensor_tensor(
                            out=H[:, t, n2 * 512:(n2 + 1) * 512], in0=H[:, t, n2 * 512:(n2 + 1) * 512], in1=psY[b][:, :], op=ALU.add),
                            reads=[("psY", b), ("H", t, n2)], writes=[("H", t, n2)])

    def final_out(with_norm=True):
        if with_norm:
            dma("sp", "stg0", STG[:, 0:1024], fnorm_in, [], ["fnb"])
        for t in range(16):
            slot = t % 2
            yt = STG2[:, slot * 1024:(slot + 1) * 1024]
            if with_norm:
                P.add("dve", lambda e, t=t: e.memset(small[:, t:t + 1], 0.0), reads=[("ssq", t)], writes=[("ssq", t)])
                rms_tile(t, slot)
                P.add("dve", lambda e, t=t, yt=yt: e.scalar_tensor_tensor(
                    out=yt, in0=H[:, t, :], scalar=small[:, 16 + t:17 + t], in1=STG[:, 0:1024], op0=ALU.mult, op1=ALU.mult),
                    reads=[("H", t, 0), ("H", t, 1), ("rstd", t), "fnb"], writes=[("yt", slot)])
            else:
                P.add("dve", lambda e, t=t, yt=yt: e.tensor_copy(out=yt, in_=H[:, t, :]),
                      reads=[("H", t, 0), ("H", t, 1)], writes=[("yt", slot)])
            dma("sp", f"yst{slot}", y_out[t * 128:(t + 1) * 128, :], yt, [("yt", slot)], [("yout", t)])

    phase1(0)
    P.barrier(soft_cc=True)
    if stage != "F0":
        phase2(0)
        P.barrier()
    if stage == "F0":
        phase3(0)
        P.barrier()
        final_out(with_norm=False)
    elif stage == "A0":
        phase3(0, do_ffn=False)
        P.barrier()
        final_out(with_norm=False)
    elif stage == "L0":
        phase3(0)
        P.barrier()
        final_out(with_norm=False)
    else:
        phase3(0)
        P.barrier()
        phase1(1)
        P.barrier(soft_cc=True)
        phase2(1)
        P.barrier()
        if stage == "A1":
            phase3(1, do_ffn=False)
            P.barrier()
            final_out(with_norm=False)
        else:
            phase3(1)
            P.barrier()
            final_out(with_norm=True)
    P.barrier()
    P.finalize()

    with ExitStack() as st:
        sems = {}
        for e in Prog.ENGS:
            sems[("eng", e)] = st.enter_context(nc.semaphore(f"s_{e}"))
        for k in P.dma_keys:
            sems[("dma", k)] = st.enter_context(nc.semaphore(f"d_{k}"))
        for k in P.cc_keys:
            sems[("cc", k)] = st.enter_context(nc.semaphore(f"c_{k}"))
        block = st.enter_context(nc.Block())

        @block.sync
        def _(e):
            P.emit("sp", e, sems)

        @block.scalar
        def _(e):
            P.emit("act", e, sems)

        @block.vector
        def _(e):
            P.emit("dve", e, sems)

        @block.gpsimd
        def _(e):
            P.emit("pool", e, sems)

        @block.tensor
        def _(e):
            P.emit("pe", e, sems)
    return nc


def _slopes(n):
    return np.array([2.0 ** (-8.0 * (h + 1) / n) for h in range(n)], dtype=np.float64)


def _consts(r):
    bf = ml_dtypes.bfloat16
    s0 = _slopes(8)
    s1 = _slopes(16)
    ident = np.eye(128, dtype=np.float32).astype(bf)
    ki = np.arange(128)[:, None]
    qi = np.arange(128)[None, :]
    tri = np.where(qi < ki, NEG, 0.0).astype(np.float32).astype(bf)
    kaug = np.zeros((2, 32, T), np.float32)
    kaug[0, 0, :] = 1.0
    kaug[1, np.arange(T) // 256, np.arange(T)] = 1.0
    kaug = kaug.astype(bf)
    qaug0 = np.zeros((2, 32, 512), np.float32)
    btab = np.zeros((128, 2, 2, 2, NDC), np.float64)
    augq1 = np.zeros((128, 2, 2, 4), np.float64)
    pp = np.arange(128, dtype=np.float64)
    dc = np.arange(NDC, dtype=np.float64) - 3.0
    for p in range(2):
        s = s0[r + 4 * p]
        qaug0[p, 0, :] = -s * np.arange(512) / SCALE
        for x in range(2):
            btab[:, 0, p, x, :] = s * (pp[:, None] - 128.0 * dc[None, :])
            s_ = s1[2 * (r + 4 * p) + x]
            btab[:, 1, p, x, :] = s_ * (pp[:, None] - 128.0 * dc[None, :])
            for qt in range(4):
                augq1[:, p, x, qt] = -s_ * (qt * 128 + pp) / SCALE + NEG
    bsel = np.zeros((128, 2, 128), np.float32)
    bsel[64, 0, 0:64] = 1.0
    bsel[0, 1, 64:128] = 1.0
    return dict(ident=ident, trimask=tri, kaug=kaug, qaug0=qaug0.astype(bf),
                biastab=btab.reshape(128, -1).astype(np.float32), augq1=augq1.reshape(128, -1).astype(np.float32),
                bsel=bsel.reshape(128, 256))


_NC_CACHE = {}


def kernel(x, attn_norm, w_in, w_out, diff_lambda, diff_subln, mlp_norm, w_ff1, w_ff2, final_norm, _stage="full"):
    x = np.asarray(x, np.float32)
    attn_norm = np.asarray(attn_norm, np.float32)
    w_in = np.asarray(w_in, np.float32)
    w_out = np.ascontiguousarray(np.asarray(w_out, np.float32))
    diff_lambda = np.asarray(diff_lambda, np.float32)
    diff_subln = np.asarray(diff_subln, np.float32)
    mlp_norm = np.asarray(mlp_norm, np.float32)
    w_ff1 = np.ascontiguousarray(np.asarray(w_ff1, np.float32))
    w_ff2 = np.ascontiguousarray(np.asarray(w_ff2, np.float32))
    final_norm = np.asarray(final_norm, np.float32)

    if _stage not in _NC_CACHE:
        _NC_CACHE[_stage] = build_program(_stage)
    nc = _NC_CACHE[_stage]

    gains = np.zeros((128, 64), np.float32)
    for l in range(2):
        gains[:, l * 8:(l + 1) * 8] = attn_norm[l].reshape(8, 128).T
        gains[:, 16 + l * 8:16 + (l + 1) * 8] = mlp_norm[l].reshape(8, 128).T
    gains[:, 32] = diff_subln[0]
    fnb = np.ascontiguousarray(np.broadcast_to(final_norm[None, :], (128, D)))
    lamb = np.ascontiguousarray(np.broadcast_to(diff_lambda[0].reshape(1, 256), (128, 256)))

    in_maps = []
    for c in range(8):
        b, r = c // 4, c % 4
        win = np.zeros((4, D, 384), np.float32)
        for l in range(2):
            for p in range(2):
                c0 = 128 * (r + 4 * p)
                for j in range(3):
                    win[l * 2 + p, :, j * 128:(j + 1) * 128] = w_in[l, :, j * D + c0:j * D + c0 + 128]
        xr = x[b].reshape(4, 4, 512, D)[:, r].reshape(NT, D)
        m = dict(x=np.ascontiguousarray(xr), win=win, wout=w_out, wff1=w_ff1, wff2=w_ff2,
                 gains=gains, fnormb=fnb, lamb=lamb)
        m.update(_consts(r))
        in_maps.append(m)
    res = run_bass_kernel_spmd(nc, in_maps, core_ids=list(range(8)))
    out = np.zeros((2, T, D), np.float32)
    for c in range(8):
        b, r = c // 4, c % 4
        out[b].reshape(4, 4, 512, D)[:, r] = np.asarray(res.results[c]["y"], dtype=np.float32).reshape(4, 512, D)
    return out
```

```python
import math
import os
from contextlib import ExitStack

import numpy as np
import ml_dtypes
import concourse.bass as bass
import concourse.mybir as mybir
from concourse.bass_utils import run_bass_kernel_spmd

F32 = mybir.dt.float32
BF16 = mybir.dt.bfloat16
ALU = mybir.AluOpType
AF = mybir.ActivationFunctionType
AX = mybir.AxisListType

D = 1024
T = 8192
NT = 2048
DFF = 4096
EPS = 1e-6
SCALE = 0.125
NEG = -30000.0
NDC = 67
GROUPS = [[0, 1, 2, 3], [4, 5, 6, 7]]


class Op:
    __slots__ = ("eng", "fn", "deps", "dma", "cc", "signal", "count", "idx")

    def __init__(self, eng, fn, dma=None, cc=None):
        self.eng = eng
        self.fn = fn
        self.deps = set()
        self.dma = dma
        self.cc = cc
        self.signal = False
        self.count = 0
        self.idx = 0


class Prog:
    ENGS = ["sp", "act", "dve", "pool", "pe"]

    def __init__(self):
        self.ops = {e: [] for e in self.ENGS}
        self.last_w = {}
        self.readers = {}
        self.dma_keys = {}
        self.cc_keys = []
        self.n = 0

    def add(self, eng, fn, reads=(), writes=(), dma=None, cc=None):
        op = Op(eng, fn, dma=dma, cc=cc)
        op.idx = self.n
        self.n += 1
        deps = set()
        for r in reads:
            w = self.last_w.get(r)
            if w is not None:
                deps.add(w)
        for w_ in writes:
            w = self.last_w.get(w_)
            if w is not None:
                deps.add(w)
            for rd in self.readers.get(w_, ()):
                deps.add(rd)
        deps.discard(op)
        if eng == "pe":
            deps = {d for d in deps if not (d.eng == "pe" and d.dma is None and d.cc is None)}
        op.deps = deps
        for r in reads:
            self.readers.setdefault(r, []).append(op)
        for w_ in writes:
            self.last_w[w_] = op
            self.readers[w_] = []
        self.ops[eng].append(op)
        if dma is not None:
            self.dma_keys.setdefault(dma, 0)
        if cc is not None:
            self.cc_keys.append(cc)
        return op

    def barrier(self, soft_cc=False):
        lasts = []
        for e in self.ENGS:
            for op in reversed(self.ops[e]):
                if op.dma is None and op.cc is None and op.fn is not None:
                    lasts.append(op)
                    break
        seen = {}
        for e in self.ENGS:
            for op in self.ops[e]:
                if op.dma is not None:
                    seen[op.dma] = op
                if op.cc is not None and not soft_cc:
                    seen[("cc", op.cc)] = op
        lasts += list(seen.values())
        for e in self.ENGS:
            b = Op(e, None)
            b.idx = self.n
            self.n += 1
            b.deps = set(lasts)
            self.ops[e].append(b)
        self.last_w = {k: v for k, v in self.last_w.items() if soft_cc and v.cc is not None}
        self.readers = {}

    def finalize(self):
        for e in self.ENGS:
            for op in self.ops[e]:
                for d in op.deps:
                    d.signal = True
        cnt = {e: 0 for e in self.ENGS}
        dcnt = {k: 0 for k in self.dma_keys}
        for e in self.ENGS:
            for op in self.ops[e]:
                if op.dma is not None:
                    dcnt[op.dma] += 16
                    op.count = dcnt[op.dma]
                elif op.cc is not None:
                    op.count = 1
                elif op.signal and op.fn is not None:
                    cnt[e] += 1
                    op.count = cnt[e]

    def emit(self, eng_name, eng, sems):
        waited = {}
        for op in self.ops[eng_name]:
            need = {}
            for d in op.deps:
                if d.dma is not None:
                    key = ("dma", d.dma)
                elif d.cc is not None:
                    key = ("cc", d.cc)
                else:
                    key = ("eng", d.eng)
                if d.count > need.get(key, 0):
                    need[key] = d.count
            for key, val in need.items():
                if waited.get(key, 0) >= val:
                    continue
                eng.wait_ge(sems[key], val)
                waited[key] = val
            if op.fn is None:
                continue
            ins = op.fn(eng)
            if op.dma is not None:
                ins.then_inc(sems[("dma", op.dma)], 16)
            elif op.cc is not None:
                ins.then_inc(sems[("cc", op.cc)])
            elif op.signal:
                ins.then_inc(sems[("eng", eng_name)], 1)


def build_program(stage="full"):
    nc = bass.Bass("TRN2", target_bir_lowering=False)
    P = Prog()

    def ext_in(name, shape, dt):
        return nc.dram_tensor(name, list(shape), dt, kind="ExternalInput").ap()

    x_in = ext_in("x", [NT, D], F32)
    win_in = ext_in("win", [4, D, 384], F32)
    wout_in = ext_in("wout", [2, D, D], F32)
    wff1_in = ext_in("wff1", [2, D, DFF], F32)
    wff2_in = ext_in("wff2", [2, DFF, D], F32)
    gains_in = ext_in("gains", [128, 64], F32)
    fnorm_in = ext_in("fnormb", [128, D], F32)
    lamb_in = ext_in("lamb", [128, 256], F32)
    ident_in = ext_in("ident", [128, 128], BF16)
    tri_in = ext_in("trimask", [128, 128], BF16)
    kaug_in = ext_in("kaug", [2, 32, T], BF16)
    qaug0_in = ext_in("qaug0", [2, 32, 512], BF16)
    btab_in = ext_in("biastab", [128, 8 * NDC], F32)
    augq1_in = ext_in("augq1", [128, 16], F32)
    bsel_in = ext_in("bsel", [128, 256], F32)
    y_out = nc.dram_tensor("y", [NT, D], F32, kind="ExternalOutput").ap()

    HTin = [[nc.dram_tensor(f"htin{l}_{i}", [D, 512], BF16) for i in range(4)] for l in range(2)]
    HT = [[nc.dram_tensor(f"ht{l}_{i}", [4 * D, 512], BF16) for i in range(4)] for l in range(2)]
    OSin = [[[nc.dram_tensor(f"osin{l}_{p}_{q}", [128, 2048], BF16) for q in range(4)]
             for p in range(2)] for l in range(2)]
    OS = [[nc.dram_tensor(f"os{l}_{p}", [2048, 2048], BF16) for p in range(2)] for l in range(2)]

    off = [16512]

    def sb(name, shape, dt, at=None):
        nbytes = int(np.prod(shape[1:])) * (4 if dt == F32 else 2)
        if at is None:
            o = off[0]
            off[0] += (nbytes + 31) // 32 * 32
        else:
            o = at
        return nc.alloc_sbuf_tensor_at(name, list(shape), dt, offset=o), o + (nbytes + 31) // 32 * 32

    H, _ = sb("H", [128, 16, D], F32)
    ident, _ = sb("identt", [128, 128], BF16)
    tri, _ = sb("trit", [128, 128], BF16)
    ones128, _ = sb("ones128", [128, 128], F32)
    bsel, _ = sb("bselt", [128, 256], F32)
    btab, _ = sb("btab", [128, 8 * NDC], F32)
    augq1, _ = sb("augq1t", [128, 16], F32)
    gains, _ = sb("gainst", [128, 64], F32)
    lamb, _ = sb("lambt", [128, 256], F32)
    small, _ = sb("small", [128, 64], F32)
    epsc, _ = sb("epsc", [128, 1], F32)
    PH = off[0]

    o = PH
    KT = []
    for x_ in range(2):
        t_, o = sb(f"KT{x_}", [128, T], BF16, at=o)
        KT.append(t_)
    Vb, o = sb("Vb", [128, 64 * 192], BF16, at=o)
    WIN, o = sb("WIN", [128, 8, 384], BF16, at=o)
    HNTc = []
    for s_ in range(2):
        t_, o = sb(f"HNTc{s_}", [128, 8, 512], BF16, at=o)
        HNTc.append(t_)
    QT = []
    for s_ in range(2):
        row = []
        for x_ in range(2):
            t_, o = sb(f"QT{s_}_{x_}", [128, 512], BF16, at=o)
            row.append(t_)
        QT.append(row)
    PT = []
    for s_ in range(4):
        t_, o = sb(f"PT{s_}", [128, 512], BF16, at=o)
        PT.append(t_)
    Zs = []
    for x_ in range(2):
        row = []
        for par in range(2):
            t_, o = sb(f"Zs{x_}_{par}", [128, 512], F32, at=o)
            row.append(t_)
        Zs.append(row)
    FT = []
    for i_ in range(4):
        t_, o = sb(f"FT{i_}", [128, 512], F32, at=o)
        FT.append(t_)
    FTo = []
    for i_ in range(2):
        t_, o = sb(f"FTo{i_}", [128, 512], F32, at=o)
        FTo.append(t_)
    OSB = [FT[2], FT[3]]
    Ocat = []
    for s_ in range(2):
        t_, o = sb(f"Ocat{s_}", [128, 512], BF16, at=o)
        Ocat.append(t_)
    gate_sb, o = sb("gate_sb", [128, 8, 32], F32, at=o)
    m8, o = sb("m8", [128, 8, 8], F32, at=o)
    selt, o = sb("selt", [128, 32], F32, at=o)
    NS = []
    for x_ in range(2):
        t_, o = sb(f"NS{x_}", [128, 4, 96], BF16, at=o)
        NS.append(t_)
    kmT, o = sb("kmT", [128, 32], BF16, at=o)
    km32, o = sb("km32", [128, 2], F32, at=o)
    STG, o = sb("STG", [128, 2048], F32, at=o)
    STG2, o = sb("STG2", [128, 2048], F32, at=o)
    P2END = o
    assert P2END <= 229376, P2END

    o = PH
    HNT, o = sb("HNT", [128, 8, NT], BF16, at=o)
    OT = HNT
    WO, o = sb("WO", [128, 8, D], BF16, at=o)
    W1e, W2e = [], []
    for s_ in range(2):
        t_, o = sb(f"W1e{s_}", [128, 8, 512], BF16, at=o)
        W1e.append(t_)
        t_, o = sb(f"W2e{s_}", [128, 4, D], BF16, at=o)
        W2e.append(t_)
    hnb = []
    for s_ in range(2):
        t_, o = sb(f"hnb{s_}", [128, D], BF16, at=o)
        hnb.append(t_)
    UT = []
    for s_ in range(2):
        t_, o = sb(f"UT{s_}", [128, 4, 512], BF16, at=o)
        UT.append(t_)
    RT = []
    for s_ in range(2):
        t_, o = sb(f"RT{s_}", [128, 512], BF16, at=o)
        RT.append(t_)
    YS = []
    for s_ in range(2):
        t_, o = sb(f"YS{s_}", [128, 512], F32, at=o)
        YS.append(t_)
    assert o <= P2END - 2 * 8192, (o, P2END)
    YT = [STG, STG2]

    pb = [nc.alloc_psum_tensor(f"pb{i}", [128, 512], F32) for i in range(8)]
    psS = pb[0:4]
    psO = pb[4:6]
    psP = pb[6]
    psX = pb[7]
    psXb = psX[:, 256:512].bitcast(BF16)
    psT = pb[6][:, :].bitcast(BF16)
    psU = pb[0:2]
    psY = pb[2:4]

    ctx = {}
    OTQ = os.environ.get("OTQ", "sp")

    def dma(eng, key, out, in_, reads, writes):
        return P.add(eng, lambda e: e.dma_start(out=out, in_=in_), reads=reads, writes=writes, dma=key)

    def allgather(name, in_ap, out_ap, reads, writes):
        return P.add("pool", lambda e: e.collective_compute(
            "AllGather", ALU.bypass, replica_groups=GROUPS, ins=[in_ap.opt()], outs=[out_ap.opt()]),
            reads=reads, writes=writes, cc=name)

    dma("sp", "c0", ident[:, :], ident_in, [], ["ident"])
    dma("sp", "c0", tri[:, :], tri_in, [], ["tri"])
    dma("sp", "c0", bsel[:, :], bsel_in, [], ["bsel"])
    dma("sp", "c0", btab[:, :], btab_in, [], ["btab"])
    dma("sp", "c0", augq1[:, :], augq1_in, [], ["augq1"])
    dma("sp", "c0", gains[:, :], gains_in, [], ["gains"])
    dma("sp", "c0", lamb[:, :], lamb_in, [], ["lamb"])
    P.add("dve", lambda e: e.memset(ones128[:, :], 1.0), writes=["ones128"])
    P.add("dve", lambda e: e.memset(epsc[:, :], EPS), writes=["epsc"])
    P.add("dve", lambda e: e.memset(small[:, :], 0.0), writes=["small"])
    P.add("dve", lambda e: e.tensor_tensor(out=FT[0][:, 0:64], in0=lamb[:, 0:64], in1=lamb[:, 64:128], op=ALU.mult),
          reads=["lamb"], writes=["ft0"])
    P.add("dve", lambda e: e.tensor_tensor(out=FT[0][:, 64:128], in0=lamb[:, 128:192], in1=lamb[:, 192:256], op=ALU.mult),
          reads=["lamb"], writes=["ft0b"])
    P.add("dve", lambda e: e.tensor_reduce(out=small[:, 34:36], in_=FT[0][:, 0:128].rearrange("p (a b) -> p a b", a=2),
                                           axis=AX.X, op=ALU.add),
          reads=["ft0", "ft0b", "small"], writes=["lam_s"])
    P.add("act", lambda e: e.activation(out=small[:, 36:38], in_=small[:, 34:36], func=AF.Exp),
          reads=["lam_s"], writes=["lam_e"])
    P.add("dve", lambda e: e.tensor_tensor(out=small[:, 38:39], in0=small[:, 37:38], in1=small[:, 36:37], op=ALU.subtract),
          reads=["lam_e"], writes=["lam_d"])
    P.add("dve", lambda e: e.tensor_scalar(out=small[:, 32:33], in0=small[:, 38:39], scalar1=-0.2, scalar2=None, op0=ALU.add),
          reads=["lam_d"], writes=["neglam"])
    P.add("dve", lambda e: e.tensor_scalar(out=small[:, 33:34], in0=gains[:, 32:33], scalar1=0.8, scalar2=None, op0=ALU.mult),
          reads=["gains", "small"], writes=["sub08"])
    for t in range(16):
        dma("sp", "xload", H[:, t, :], x_in[t * 128:(t + 1) * 128, :], [], [("H", t, 0), ("H", t, 1)])

    P.barrier()

    def rms_tile(t, slot):
        P.add("act", lambda e: e.activation(out=hnb[slot][:, :], in_=H[:, t, :], func=AF.Square,
                                            accum_out=small[:, t:t + 1]),
              reads=[("H", t, 0), ("H", t, 1), "small"], writes=[("hnb", slot), ("ssq", t)])
        P.add("act", lambda e: e.activation(out=small[:, 16 + t:17 + t], in_=small[:, t:t + 1], func=AF.Ln,
                                            bias=epsc[:, 0:1], scale=1.0 / D),
              reads=[("ssq", t), "epsc"], writes=[("rstd", t)])
        P.add("act", lambda e: e.activation(out=small[:, 16 + t:17 + t], in_=small[:, 16 + t:17 + t], func=AF.Exp, scale=-0.5),
              reads=[("rstd", t)], writes=[("rstd", t)])

    def norm_a(t):
        slot = t % 2
        P.add("dve", lambda e: e.memset(small[:, t:t + 1], 0.0), reads=[("ssq", t)], writes=[("ssq", t)])
        rms_tile(t, slot)
        P.add("dve", lambda e: e.tensor_scalar(out=hnb[slot][:, :], in0=H[:, t, :], scalar1=small[:, 16 + t:17 + t],
                                               scalar2=None, op0=ALU.mult),
              reads=[("H", t, 0), ("H", t, 1), ("rstd", t)], writes=[("hnb", slot)])

    def norm_b(t):
        slot = t % 2
        for kc in range(8):
            P.add("pe", lambda e, kc=kc: e.transpose(out=psT[:, kc * 128:(kc + 1) * 128],
                                                     in_=hnb[slot][:, kc * 128:(kc + 1) * 128], identity=ident[:, :]),
                  reads=[("hnb", slot), "ident"], writes=["psT"])
        P.add("dve", lambda e: e.tensor_copy(out=HNT[:, :, t * 128:(t + 1) * 128],
                                             in_=psT.rearrange("p (k q) -> p k q", k=8)),
              reads=["psT"], writes=[("HNT", t)])

    def norm_all(after_b=None):
        norm_a(0)
        for t in range(16):
            if t + 1 < 16:
                norm_a(t + 1)
            norm_b(t)
            if after_b is not None:
                after_b(t)

    def phase1(l):
        def after_b(t):
            if t % 4 == 3:
                i = t // 4
                dma("sp", f"htst{i}", HTin[l][i].ap().rearrange("(kc p) t -> p kc t", p=128),
                    HNT[:, :, i * 512:(i + 1) * 512], [("HNT", tt) for tt in range(4 * i, 4 * i + 4)],
                    [("HTin", l, i)])
                allgather(f"agh{l}_{i}", HTin[l][i].ap(), HT[l][i].ap(), [("HTin", l, i)], [("HT", l, i)])
        norm_all(after_b)

    def load_cast_win(l, p):
        wsrc = win_in[l * 2 + p].rearrange("(kc p) n -> p kc n", p=128)
        for hf in range(2):
            stg = (STG if hf == 0 else STG2)[:, 0:4 * 384].rearrange("p (k n) -> p k n", k=4)
            dma("sp", f"stg{hf}", stg, wsrc[:, hf * 4:hf * 4 + 4, :], [], [("STG", hf)])
            for k4 in range(4):
                kc = hf * 4 + k4
                P.add("pool", lambda e, stg=stg, k4=k4, kc=kc: e.tensor_scalar(
                    out=WIN[:, kc, :], in0=stg[:, k4, :], scalar1=gains[:, l * 8 + kc:l * 8 + kc + 1],
                    scalar2=None, op0=ALU.mult),
                    reads=[("STG", hf), "gains"], writes=[("WIN", kc)])

    def phase2(l):
        V0 = Vb[:, 0:64 * 128].rearrange("p (t c) -> p t c", c=128)
        V1 = Vb[:, :].rearrange("p (t c) -> p t c", c=192)
        dma("sp", "kaug0", KT[0][64:96, :], kaug_in[l], [], [("KTaug", 0)])
        dma("sp", "kaug1", KT[1][0:32, :], kaug_in[l], [], [("KTaug", 1)])
        P.add("pool", lambda e: e.memset(KT[1][32:64, :], 0.0), writes=[("KTz", 1)])
        for s_ in range(2):
            for x_ in range(2):
                P.add("pool", lambda e, s_=s_, x_=x_: e.memset(QT[s_][x_][:, :], 0.0), writes=[("QT", s_, x_), ("QTaug", s_, x_)])
        if l == 1:
            P.add("pool", lambda e: e.memset(Vb[:, :], 0.0), writes=["Vall"])
            P.add("pool", lambda e: e.memset(V1[:, :, 64:65], 1.0), reads=["Vall"], writes=["Vall"])
            for x_ in range(2):
                P.add("pool", lambda e, x_=x_: e.memset(NS[x_][:, :, :], 0.0), writes=[("NS", x_)])
                P.add("pool", lambda e, x_=x_: e.memset(OSB[x_][:, :], 0.0), writes=[("OSB", x_), ("FT", 2 + x_)])
            P.add("pool", lambda e: e.memset(kmT[:, :], 0.0), writes=["kmT"])
        for p in range(2):
            load_cast_win(l, p)
            if l == 0:
                for s_ in range(2):
                    dma("sp", f"qaug{s_}0", QT[s_][0][64:96, :], qaug0_in[p], [("QTaug", s_, 0)], [("QTaug", s_, 0)])
                    dma("sp", f"qaug{s_}1", QT[s_][1][0:32, :], qaug0_in[p], [("QTaug", s_, 1)], [("QTaug", s_, 1)])
            for _ in chunk_proj(l, p, 0, V0, V1):
                pass
            fin = None
            for g in range(16):
                gen = chunk_proj(l, p, g + 1, V0, V1) if g + 1 < 16 else None
                chunk_attn(l, p, g, V0, V1, gen, fin)
                fin = fin_gen(l, p, g)
                next(fin)
            for _ in fin:
                pass

    bidx = [0]
    NSLOT = 4

    def chunk_proj(l, p, g, V0, V1):
        s = g % 2
        hsrc = HT[l][g // 4][(g % 4) * D:(g % 4 + 1) * D, :].rearrange("(kc p) t -> p kc t", p=128)
        dma("sp", f"hntc{s}", HNTc[s][:, :, :], hsrc, [("HT", l, g // 4)], [("HNTc", s)])
        win_r = [("WIN", kc) for kc in range(8)]
        for kc in range(8):
            P.add("pe", lambda e, kc=kc: e.matmul(psP[:, :], lhsT=WIN[:, kc, 0:128], rhs=HNTc[s][:, kc, :],
                                                  start=(kc == 0), stop=(kc == 7)),
                  reads=[("HNTc", s)] + win_r, writes=["psP"])
        P.add("dve", lambda e: e.tensor_copy(out=QT[s][0][0:64, :], in_=psP[0:64, :]), reads=["psP"], writes=[("QT", s, 0)])
        P.add("dve", lambda e: e.tensor_copy(out=QT[s][1][64:128, :], in_=psP[64:128, :]), reads=["psP"], writes=[("QT", s, 1)])
        yield
        for kc in range(8):
            P.add("pe", lambda e, kc=kc: e.matmul(psP[:, :], lhsT=WIN[:, kc, 128:256], rhs=HNTc[s][:, kc, :],
                                                  start=(kc == 0), stop=(kc == 7)),
                  reads=[("HNTc", s)] + win_r, writes=["psP"])
        P.add("dve", lambda e: e.tensor_copy(out=KT[0][0:64, g * 512:(g + 1) * 512], in_=psP[0:64, :]),
              reads=["psP"], writes=[("KT", 0, g)])
        P.add("dve", lambda e: e.tensor_copy(out=KT[1][64:128, g * 512:(g + 1) * 512], in_=psP[64:128, :]),
              reads=["psP"], writes=[("KT", 1, g)])
        if l == 1:
            P.add("dve", lambda e: e.tensor_reduce(out=km32[:, :], in_=psP[:, :].rearrange("p (a b) -> p a b", a=2),
                                                   axis=AX.X, op=ALU.add), reads=["psP"], writes=["km32"])
            P.add("dve", lambda e: e.tensor_scalar(out=kmT[:, 2 * g:2 * g + 2], in0=km32[:, :], scalar1=1.0 / 256,
                                                   scalar2=None, op0=ALU.mult), reads=["km32"], writes=["kmT"])
        yield
        for tt in range(4):
            for kc in range(8):
                P.add("pe", lambda e, kc=kc, tt=tt: e.matmul(psP[:, tt * 128:(tt + 1) * 128],
                                                             lhsT=HNTc[s][:, kc, tt * 128:(tt + 1) * 128],
                                                             rhs=WIN[:, kc, 256:384], start=(kc == 0), stop=(kc == 7)),
                      reads=[("HNTc", s)] + win_r, writes=["psP"])
            if tt == 1:
                yield
        psP4 = psP[:, :].rearrange("p (t c) -> p t c", t=4)
        if l == 0:
            P.add("dve", lambda e: e.tensor_copy(out=V0[:, 4 * g:4 * g + 4, :], in_=psP4), reads=["psP"], writes=[("V", g)])
        else:
            P.add("dve", lambda e: e.tensor_copy(out=V1[:, 4 * g:4 * g + 4, 0:64], in_=psP4[:, :, 0:64]),
                  reads=["psP", "Vall"], writes=[("V", g)])
            P.add("dve", lambda e: e.tensor_copy(out=V1[:, 4 * g:4 * g + 4, 128:192], in_=psP4[:, :, 64:128]),
                  reads=["psP", "Vall"], writes=[("Vb_", g)])
        yield
        if l == 1:
            yield from moba_gate(p, g, s)
        yield

    def chunk_attn(l, p, g, V0, V1, gen=None, fin=None):
        s = g % 2
        nk = 4 * g + 4
        if l == 1 and os.environ.get("SKIP_ATTN"):
            nk = 0
        kt0 = max(0, 4 * g - 16) if p == 0 else 0
        items = [(kt, x_) for kt in range(kt0, nk) for x_ in range(2)]
        LOOK = 3
        slots = {}

        def emit_s(kt, x_):
            j = kt - 4 * g
            c0 = 128 * j if j >= 0 else 0
            b = bidx[0] % NSLOT
            bidx[0] += 1
            slots[(kt, x_)] = b
            kx = 96 if x_ == 0 else 128
            kr = [("KT", x_, kt // 4), ("KTaug", x_), ("QT", s, x_), ("QTaug", s, x_)] + ([("KTz", 1)] if x_ == 1 else [])
            ks = slice(kt * 128, (kt + 1) * 128)
            if j >= 0:
                P.add("pe", lambda e: e.matmul(
                    psS[b][:, c0:c0 + 128], lhsT=KT[x_][0:kx, ks], rhs=QT[s][x_][0:kx, c0:c0 + 128],
                    start=True, stop=False), reads=kr, writes=[("psS", b)])
                P.add("pe", lambda e: e.matmul(
                    psS[b][:, c0:c0 + 128], lhsT=ident[:, :], rhs=tri[:, :], start=False, stop=True),
                    reads=["ident", "tri"], writes=[("psS", b)])
                if c0 + 128 < 512:
                    P.add("pe", lambda e: e.matmul(
                        psS[b][:, c0 + 128:512], lhsT=KT[x_][0:kx, ks], rhs=QT[s][x_][0:kx, c0 + 128:512],
                        start=True, stop=True), reads=kr, writes=[("psS", b)])
            else:
                P.add("pe", lambda e: e.matmul(
                    psS[b][:, :], lhsT=KT[x_][0:kx, ks], rhs=QT[s][x_][0:kx, :], start=True, stop=True),
                    reads=kr, writes=[("psS", b)])
            col = (((l * 2 + p) * 2 + x_) * NDC) + (4 * g - kt + 3)
            P.add("act", lambda e: e.activation(
                out=PT[b][:, c0:512], in_=psS[b][:, c0:512], func=AF.Exp, bias=btab[:, col:col + 1], scale=SCALE),
                reads=[("psS", b), "btab"], writes=[("PT", b)])
            if l == 0:
                zeng = "dve" if x_ == 0 else "pool"
                zt = Zs[x_][g % 2]
                zk = ("Zs", x_, g % 2)
                if kt == kt0:
                    P.add(zeng, lambda e: e.tensor_copy(out=zt[:, :], in_=PT[b][:, :]),
                          reads=[("PT", b)], writes=[zk])
                else:
                    P.add(zeng, lambda e: e.tensor_tensor(
                        out=zt[:, c0:512], in0=zt[:, c0:512], in1=PT[b][:, c0:512], op=ALU.add),
                        reads=[("PT", b), zk], writes=[zk])

        def emit_pv(kt, x_):
            j = kt - 4 * g
            c0 = 128 * j if j >= 0 else 0
            b = slots[(kt, x_)]
            if l == 0:
                vl = V0[:, kt, :]
                mo = 128
            else:
                vl = V1[:, kt, 0:65] if x_ == 0 else V1[:, kt, 64:192]
                mo = 65 if x_ == 0 else 128
            P.add("pe", lambda e: e.matmul(
                psO[x_][0:mo, c0:512], lhsT=vl, rhs=PT[b][:, c0:512], start=(kt == kt0), stop=(kt == nk - 1)),
                reads=[("PT", b), ("V", kt // 4), ("Vb_", kt // 4), "Vall"], writes=[("psO", x_)])

        for n in range(len(items) + LOOK):
            if n < len(items):
                emit_s(*items[n])
            if n - LOOK >= 0:
                emit_pv(*items[n - LOOK])
            if n >= 4 and n % 3 == 1 and gen is not None:
                next(gen, None)
            if n >= 2 and n % 3 == 2 and fin is not None:
                next(fin, None)
        if fin is not None:
            for _ in fin:
                pass
        if gen is not None:
            for _ in gen:
                pass

    def fin_gen(l, p, g):
        s = g % 2
        oc = Ocat[s]
        zp = g % 2
        if l == 0:
            for x_ in range(2):
                P.add("dve", lambda e, x_=x_: e.tensor_copy(out=FTo[x_][:, :], in_=psO[x_][:, :]),
                      reads=[("psO", x_)], writes=[("FTo", x_)])
            yield
            for x_ in range(2):
                P.add("pe", lambda e, x_=x_: e.matmul(psX[:, :], lhsT=ones128[:, :], rhs=Zs[x_][zp][:, :], start=True, stop=True),
                      reads=[("Zs", x_, zp), "ones128"], writes=["psX"])
                P.add("dve", lambda e, x_=x_: e.reciprocal(out=FT[x_][:, :], in_=psX[:, :]), reads=["psX"], writes=[("FT", x_)])
                yield
                P.add("dve", lambda e, x_=x_: e.tensor_tensor(out=FT[x_][:, :], in0=FTo[x_][:, :], in1=FT[x_][:, :], op=ALU.mult),
                      reads=[("FTo", x_), ("FT", x_)], writes=[("FT", x_)])
            P.add("dve", lambda e: e.scalar_tensor_tensor(out=FT[2][:, :], in0=FT[1][:, :], scalar=small[:, 32:33], in1=FT[0][:, :],
                                                          op0=ALU.mult, op1=ALU.add),
                  reads=[("FT", 0), ("FT", 1), "neglam"], writes=[("FT", 2)])
            P.add("pool", lambda e: e.tensor_tensor(out=FT[3][:, :], in0=FT[2][:, :], in1=FT[2][:, :], op=ALU.mult),
                  reads=[("FT", 2)], writes=[("FT", 3)])
            yield
            P.add("pe", lambda e: e.matmul(psX[:, :], lhsT=ones128[:, :], rhs=FT[3][:, :], start=True, stop=True),
                  reads=[("FT", 3), "ones128"], writes=["psX"])
            P.add("act", lambda e: e.activation(out=FT[3][:, :], in_=psX[:, :], func=AF.Ln, bias=epsc[:, 0:1], scale=1.0 / 128),
                  reads=["psX", "epsc"], writes=[("FT", 3)])
            P.add("act", lambda e: e.activation(out=FT[3][:, :], in_=FT[3][:, :], func=AF.Exp, scale=-0.5),
                  reads=[("FT", 3)], writes=[("FT", 3)])
            yield
            P.add("dve", lambda e: e.tensor_tensor(out=oc[:, :], in0=FT[2][:, :], in1=FT[3][:, :], op=ALU.mult),
                  reads=[("FT", 2), ("FT", 3)], writes=[("Ocat", s)])
        else:
            for x_ in range(2):
                mo = 65 if x_ == 0 else 128
                P.add("dve", lambda e, x_=x_, mo=mo: e.tensor_copy(out=OSB[x_][0:mo, :], in_=psO[x_][0:mo, :]),
                      reads=[("psO", x_)], writes=[("OSB", x_)])
            yield
            for x_ in range(2):
                rows = slice(0, 64) if x_ == 0 else slice(64, 128)
                P.add("pe", lambda e, x_=x_: e.matmul(psX[:, :], lhsT=bsel[:, x_ * 128:(x_ + 1) * 128], rhs=OSB[x_][:, :],
                                                      start=True, stop=True), reads=[("OSB", x_), "bsel"], writes=["psX"])
                P.add("dve", lambda e, x_=x_, rows=rows: e.reciprocal(out=FT[x_][rows, :], in_=psX[rows, :]),
                      reads=["psX"], writes=[("FT", x_)])
                yield
                P.add("dve", lambda e, x_=x_, rows=rows: e.tensor_tensor(out=oc[rows, :], in0=OSB[x_][rows, :], in1=FT[x_][rows, :],
                                                                        op=ALU.mult),
                      reads=[("OSB", x_), ("FT", x_)], writes=[("Ocat", s, x_)])
        dma("sp", f"ost{s}", OSin[l][p][g // 4][:, (g % 4) * 512:(g % 4 + 1) * 512], oc[:, :],
            [("Ocat", s), ("Ocat", s, 0), ("Ocat", s, 1)], [("OSin", l, p, g // 4)])
        yield
        if g % 4 == 3:
            q = g // 4
            allgather(f"ago{l}_{p}_{q}", OSin[l][p][q].ap(), OS[l][p][q * 512:(q + 1) * 512, :],
                      [("OSin", l, p, q)], [("OS", l, p, q)])
        yield

    def moba_gate(p, g, s):
        gbank = [psX, psP]
        gkey = ["psX", "psP"]
        for x_ in range(2):
            rows = slice(0, 64) if x_ == 0 else slice(64, 128)
            for qt in range(4):
                P.add("pe", lambda e, x_=x_, qt=qt, rows=rows: e.matmul(
                    gbank[x_][:, qt * 32:(qt + 1) * 32], lhsT=QT[s][x_][rows, qt * 128:(qt + 1) * 128], rhs=kmT[rows, :],
                    start=True, stop=True), reads=[("QT", s, x_), "kmT"], writes=[gkey[x_]])
        P.add("dve", lambda e: e.memset(gate_sb[:, :, :], -1e30), writes=["gate"])
        gsv = gate_sb[:, :, :].rearrange("p (x h q) n -> p x h q n", x=2, h=2)
        for hf in range(2):
            jb = 2 * g + hf
            if jb > 0:
                for x_ in range(2):
                    psg = gbank[x_][:, 0:128].rearrange("p (h q n) -> p h q n", h=2, q=2)
                    P.add("dve", lambda e, hf=hf, jb=jb, x_=x_, psg=psg: e.tensor_copy(out=gsv[:, x_, hf, :, 0:jb], in_=psg[:, hf, :, 0:jb]),
                          reads=[gkey[x_], "gate"], writes=["gate"])
        for x_ in range(2):
            base = 64 if x_ == 0 else 0
            for qt in range(4):
                xq = x_ * 4 + qt
                jb = 2 * g + qt // 2
                ai = (p * 2 + x_) * 4 + qt
                P.add("dve", lambda e, xq=xq: e.max(out=m8[:, xq, :], in_=gate_sb[:, xq, :]), reads=["gate"], writes=[("m8", xq)])
                P.add("dve", lambda e, xq=xq: e.tensor_scalar(out=selt[:, :], in0=gate_sb[:, xq, :], scalar1=m8[:, xq, 2:3],
                                                              scalar2=None, op0=ALU.is_ge),
                      reads=["gate", ("m8", xq)], writes=["selt"])
                P.add("dve", lambda e, x_=x_, qt=qt, base=base, ai=ai: e.tensor_scalar(
                    out=NS[x_][:, qt, base:base + 32], in0=selt[:, :], scalar1=-NEG, scalar2=augq1[:, ai:ai + 1],
                    op0=ALU.mult, op1=ALU.add), reads=["selt", "augq1", ("NS", x_)], writes=[("NS", x_)])
                P.add("dve", lambda e, x_=x_, qt=qt, base=base, ai=ai, jb=jb: e.tensor_scalar(
                    out=NS[x_][:, qt, base + jb:base + jb + 1], in0=augq1[:, ai:ai + 1], scalar1=-NEG, scalar2=None,
                    op0=ALU.add), reads=["augq1", ("NS", x_)], writes=[("NS", x_)])
        for _ in range(4):
            yield
        for x_ in range(2):
            rows = slice(64, 96) if x_ == 0 else slice(0, 32)
            yield
            for qt in range(4):
                P.add("pe", lambda e, x_=x_, qt=qt: e.transpose(out=psXb[0:96, qt * 128:(qt + 1) * 128],
                                                                in_=NS[x_][:, qt, :], identity=ident[:, :]),
                      reads=[("NS", x_), "ident"], writes=["psX"])
            P.add("dve", lambda e, x_=x_, rows=rows: e.tensor_copy(out=QT[s][x_][rows, :], in_=psXb[rows, :]),
                  reads=["psX"], writes=[("QTaug", s, x_)])

    def phase3(l, do_ffn=True):
        def rank_of(e):
            if "rank" not in ctx:
                ctx["rank"] = e.partition_id() % 4
            return ctx["rank"]
        for q in range(4):
            for kc in range(8):
                src, p = kc // 2, kc % 2
                P.add(OTQ, lambda e, kc=kc, src=src, p=p, q=q: e.dma_start(
                    out=OT[:, kc, q * 512:(q + 1) * 512],
                    in_=OS[l][p][q * 512 + src * 128:q * 512 + src * 128 + 128, bass.ds(rank_of(e) * 512, 512)]),
                    reads=[("OS", l, p, q)], writes=[("OT", kc, q)], dma="otld")
        wsrc = wout_in[l].rearrange("(kc p) n -> p kc n", p=128)
        for i in range(4):
            stg = (STG if i % 2 == 0 else STG2)[:, :].rearrange("p (k n) -> p k n", k=2)
            dma("sp", f"stg{i % 2}", stg, wsrc[:, 2 * i:2 * i + 2, :], [], [("STG", i % 2)])
            if l == 0:
                P.add("act", lambda e, stg=stg, i=i: e.activation(out=WO[:, 2 * i:2 * i + 2, :], in_=stg, func=AF.Copy,
                                                                  scale=small[:, 33:34]),
                      reads=[("STG", i % 2), "sub08"], writes=[("WO", i)])
            else:
                P.add("act", lambda e, stg=stg, i=i: e.copy(out=WO[:, 2 * i:2 * i + 2, :], in_=stg),
                      reads=[("STG", i % 2)], writes=[("WO", i)])
        yb = 0
        for t in range(16):
            for n2 in range(2):
                b = yb % 2
                yb += 1
                for kc in range(8):
                    wk = kc // 2 + 4 * (kc % 2)
                    P.add("pe", lambda e, kc=kc, wk=wk, b=b, t=t, n2=n2: e.matmul(
                        psY[b][:, :], lhsT=OT[:, kc, t * 128:(t + 1) * 128], rhs=WO[:, wk, n2 * 512:(n2 + 1) * 512],
                        start=(kc == 0), stop=(kc == 7)), reads=[("OT", k_, q_) for k_ in range(8) for q_ in range(4)] + [("WO", wk // 2)], writes=[("psY", b)])
                P.add("dve", lambda e, b=b, t=t, n2=n2: e.tensor_tensor(
                    out=H[:, t, n2 * 512:(n2 + 1) * 512], in0=H[:, t, n2 * 512:(n2 + 1) * 512], in1=psY[b][:, :], op=ALU.add),
                    reads=[("psY", b), ("H", t, n2)], writes=[("H", t, n2)])
        if not do_ffn:
            return
        P.barrier()
        norm_all()
        w1src = wff1_in[l].rearrange("(kc p) n -> p kc n", p=128)
        w2src = wff2_in[l].rearrange("(fc p) n -> p fc n", p=128)
        ub = 0
        for ei in range(8):
            ws = ei % 2
            for hf in range(2):
                stg = (STG if hf == 0 else STG2)[:, :].rearrange("p (k n) -> p k n", k=4)
                dma("sp", f"stg{hf}", stg, w1src[:, hf * 4:hf * 4 + 4, ei * 512:(ei + 1) * 512], [], [("STG", hf)])
                for k4 in range(4):
                    kc = hf * 4 + k4
                    P.add("act", lambda e, stg=stg, k4=k4, kc=kc, ws=ws: e.activation(
                        out=W1e[ws][:, kc, :], in_=stg[:, k4, :], func=AF.Copy, scale=gains[:, 16 + l * 8 + kc:17 + l * 8 + kc]),
                        reads=[("STG", hf), "gains"], writes=[("W1e", ws)])
            for hf in range(2):
                stg = (STG if hf == 0 else STG2)[:, :].rearrange("p (k n) -> p k n", k=2)
                dma("sp", f"stg{hf}", stg, w2src[:, ei * 4 + hf * 2:ei * 4 + hf * 2 + 2, :], [], [("STG", hf)])
                P.add("act", lambda e, stg=stg, hf=hf, ws=ws: e.copy(out=W2e[ws][:, 2 * hf:2 * hf + 2, :], in_=stg),
                      reads=[("STG", hf)], writes=[("W2e", ws)])
            for c in range(4):
                us = (ei * 4 + c) % 2
                for fc in range(4):
                    b = ub % 2
                    ub += 1
                    for kc in range(8):
                        P.add("pe", lambda e, kc=kc, b=b, fc=fc, c=c, ws=ws: e.matmul(
                            psU[b][:, :], lhsT=W1e[ws][:, kc, fc * 128:(fc + 1) * 128], rhs=HNT[:, kc, c * 512:(c + 1) * 512],
                            start=(kc == 0), stop=(kc == 7)),
                            reads=[("W1e", ws)] + [("HNT", tt) for tt in range(4 * c, 4 * c + 4)], writes=[("psU", b)])
                    P.add("act", lambda e, b=b: e.activation(out=RT[b][:, :], in_=psU[b][:, :], func=AF.Relu),
                          reads=[("psU", b)], writes=[("RT", b)])
                    P.add("dve", lambda e, b=b, us=us, fc=fc: e.tensor_tensor(out=UT[us][:, fc, :], in0=RT[b][:, :], in1=RT[b][:, :],
                                                                             op=ALU.mult),
                          reads=[("RT", b)], writes=[("UT", us, fc)])
                for tt in range(4):
                    t = 4 * c + tt
                    for n2 in range(2):
                        b = yb % 2
                        yb += 1
                        for fc in range(4):
                            P.add("pe", lambda e, fc=fc, b=b, tt=tt, n2=n2, us=us, ws=ws: e.matmul(
                                psY[b][:, :], lhsT=UT[us][:, fc, tt * 128:(tt + 1) * 128], rhs=W2e[ws][:, fc, n2 * 512:(n2 + 1) * 512],
                                start=(fc == 0), stop=(fc == 3)),
                                reads=[("UT", us, fc), ("W2e", ws)], writes=[("psY", b)])
                        if n2 == 0:
                            P.add("dve", lambda e, b=b, t=t, n2=n2: e.tensor_tensor(
                                out=H[:, t, n2 * 512:(n2 + 1) * 512], in0=H[:, t, n2 * 512:(n2 + 1) * 512], in1=psY[b][:, :], op=ALU.add),
                                reads=[("psY", b), ("H", t, n2)], writes=[("H", t, n2)])
                        else:
                            ys = tt % 2
                            P.add("act", lambda e, b=b, ys=ys: e.copy(out=YS[ys][:, :], in_=psY[b][:, :]),
                                  reads=[("psY", b)], writes=[("YS", ys)])
                            P.add("pool", lambda e, t=t, n2=n2, ys=ys: e.tensor_tensor(
                                out=H[:, t, n2 * 512:(n2 + 1) * 512], in0=H[:, t, n2 * 512:(n2 + 1) * 512], in1=YS[ys][:, :], op=ALU.add),
                                reads=[("YS", ys), ("H", t, n2)], writes=[("H", t, n2)])

    def final_out(with_norm=True):
        if with_norm:
            dma("sp", "stg0", STG[:, 0:1024], fnorm_in, [], ["fnb"])
        for t in range(16):
            slot = t % 2
            yt = STG2[:, slot * 1024:(slot + 1) * 1024]
            if with_norm:
                P.add("dve", lambda e, t=t: e.memset(small[:, t:t + 1], 0.0), reads=[("ssq", t)], writes=[("ssq", t)])
                rms_tile(t, slot)
                P.add("dve", lambda e, t=t, yt=yt: e.scalar_tensor_tensor(
                    out=yt, in0=H[:, t, :], scalar=small[:, 16 + t:17 + t], in1=STG[:, 0:1024], op0=ALU.mult, op1=ALU.mult),
                    reads=[("H", t, 0), ("H", t, 1), ("rstd", t), "fnb"], writes=[("yt", slot)])
            else:
                P.add("dve", lambda e, t=t, yt=yt: e.tensor_copy(out=yt, in_=H[:, t, :]),
                      reads=[("H", t, 0), ("H", t, 1)], writes=[("yt", slot)])
            dma("sp", f"yst{slot}", y_out[t * 128:(t + 1) * 128, :], yt, [("yt", slot)], [("yout", t)])

    phase1(0)
    P.barrier(soft_cc=True)
    if stage != "F0":
        phase2(0)
        P.barrier(soft_cc=True)
    if stage == "F0":
        phase3(0)
        P.barrier()
        final_out(with_norm=False)
    elif stage == "A0":
        phase3(0, do_ffn=False)
        P.barrier()
        final_out(with_norm=False)
    elif stage == "L0":
        phase3(0)
        P.barrier()
        final_out(with_norm=False)
    else:
        phase3(0)
        P.barrier()
        phase1(1)
        P.barrier(soft_cc=True)
        phase2(1)
        P.barrier(soft_cc=True)
        if stage == "A1":
            phase3(1, do_ffn=False)
            P.barrier()
            final_out(with_norm=False)
        else:
            phase3(1)
            P.barrier()
            final_out(with_norm=True)
    P.barrier()
    P.finalize()

    with ExitStack() as st:
        sems = {}
        for e in Prog.ENGS:
            sems[("eng", e)] = st.enter_context(nc.semaphore(f"s_{e}"))
        for k in P.dma_keys:
            sems[("dma", k)] = st.enter_context(nc.semaphore(f"d_{k}"))
        for k in P.cc_keys:
            sems[("cc", k)] = st.enter_context(nc.semaphore(f"c_{k}"))
        block = st.enter_context(nc.Block())

        @block.sync
        def _(e):
            P.emit("sp", e, sems)

        @block.scalar
        def _(e):
            P.emit("act", e, sems)

        @block.vector
        def _(e):
            P.emit("dve", e, sems)

        @block.gpsimd
        def _(e):
            P.emit("pool", e, sems)

        @block.tensor
        def _(e):
            P.emit("pe", e, sems)
    return nc


def _slopes(n):
    return np.array([2.0 ** (-8.0 * (h + 1) / n) for h in range(n)], dtype=np.float64)


def _consts(r):
    bf = ml_dtypes.bfloat16
    s0 = _slopes(8)
    s1 = _slopes(16)
    ident = np.eye(128, dtype=np.float32).astype(bf)
    ki = np.arange(128)[:, None]
    qi = np.arange(128)[None, :]
    tri = np.where(qi < ki, NEG, 0.0).astype(np.float32).astype(bf)
    kaug = np.zeros((2, 32, T), np.float32)
    kaug[0, 0, :] = 1.0
    kaug[1, np.arange(T) // 256, np.arange(T)] = 1.0
    kaug = kaug.astype(bf)
    qaug0 = np.zeros((2, 32, 512), np.float32)
    btab = np.zeros((128, 2, 2, 2, NDC), np.float64)
    augq1 = np.zeros((128, 2, 2, 4), np.float64)
    pp = np.arange(128, dtype=np.float64)
    dc = np.arange(NDC, dtype=np.float64) - 3.0
    for p in range(2):
        s = s0[r + 4 * p]
        qaug0[p, 0, :] = -s * np.arange(512) / SCALE
        for x in range(2):
            btab[:, 0, p, x, :] = s * (pp[:, None] - 128.0 * dc[None, :])
            s_ = s1[2 * (r + 4 * p) + x]
            btab[:, 1, p, x, :] = s_ * (pp[:, None] - 128.0 * dc[None, :])
            for qt in range(4):
                augq1[:, p, x, qt] = -s_ * (qt * 128 + pp) / SCALE + NEG
    bsel = np.zeros((128, 2, 128), np.float32)
    bsel[64, 0, 0:64] = 1.0
    bsel[0, 1, 64:128] = 1.0
    return dict(ident=ident, trimask=tri, kaug=kaug, qaug0=qaug0.astype(bf),
                biastab=btab.reshape(128, -1).astype(np.float32), augq1=augq1.reshape(128, -1).astype(np.float32),
                bsel=bsel.reshape(128, 256))


_NC_CACHE = {}


def kernel(x, attn_norm, w_in, w_out, diff_lambda, diff_subln, mlp_norm, w_ff1, w_ff2, final_norm, _stage="full"):
    x = np.asarray(x, np.float32)
    attn_norm = np.asarray(attn_norm, np.float32)
    w_in = np.asarray(w_in, np.float32)
    w_out = np.ascontiguousarray(np.asarray(w_out, np.float32))
    diff_lambda = np.asarray(diff_lambda, np.float32)
    diff_subln = np.asarray(diff_subln, np.float32)
    mlp_norm = np.asarray(mlp_norm, np.float32)
    w_ff1 = np.ascontiguousarray(np.asarray(w_ff1, np.float32))
    w_ff2 = np.ascontiguousarray(np.asarray(w_ff2, np.float32))
    final_norm = np.asarray(final_norm, np.float32)

    if _stage not in _NC_CACHE:
        _NC_CACHE[_stage] = build_program(_stage)
    nc = _NC_CACHE[_stage]

    gains = np.zeros((128, 64), np.float32)
    for l in range(2):
        gains[:, l * 8:(l + 1) * 8] = attn_norm[l].reshape(8, 128).T
        gains[:, 16 + l * 8:16 + (l + 1) * 8] = mlp_norm[l].reshape(8, 128).T
    gains[:, 32] = diff_subln[0]
    fnb = np.ascontiguousarray(np.broadcast_to(final_norm[None, :], (128, D)))
    lamb = np.ascontiguousarray(np.broadcast_to(diff_lambda[0].reshape(1, 256), (128, 256)))

    in_maps = []
    for c in range(8):
        b, r = c // 4, c % 4
        win = np.zeros((4, D, 384), np.float32)
        for l in range(2):
            for p in range(2):
                c0 = 128 * (r + 4 * p)
                for j in range(3):
                    win[l * 2 + p, :, j * 128:(j + 1) * 128] = w_in[l, :, j * D + c0:j * D + c0 + 128]
        xr = x[b].reshape(4, 4, 512, D)[:, r].reshape(NT, D)
        m = dict(x=np.ascontiguousarray(xr), win=win, wout=w_out, wff1=w_ff1, wff2=w_ff2,
                 gains=gains, fnormb=fnb, lamb=lamb)
        m.update(_consts(r))
        in_maps.append(m)
    res = run_bass_kernel_spmd(nc, in_maps, core_ids=list(range(8)))
    out = np.zeros((2, T, D), np.float32)
    for c in range(8):
        b, r = c // 4, c % 4
        out[b].reshape(4, 4, 512, D)[:, r] = np.asarray(res.results[c]["y"], dtype=np.float32).reshape(4, 512, D)
    return out
```

```python
import math
import os
from contextlib import ExitStack

import numpy as np
import ml_dtypes
import concourse.bass as bass
import concourse.mybir as mybir
from concourse.bass_utils import run_bass_kernel_spmd

F32 = mybir.dt.float32
BF16 = mybir.dt.bfloat16
ALU = mybir.AluOpType
AF = mybir.ActivationFunctionType
AX = mybir.AxisListType

D = 1024
T = 8192
NT = 2048
DFF = 4096
EPS = 1e-6
SCALE = 0.125
NEG = -30000.0
NDC = 67
GROUPS = [[0, 1, 2, 3], [4, 5, 6, 7]]


class Op:
    __slots__ = ("eng", "fn", "deps", "dma", "cc", "signal", "count", "idx")

    def __init__(self, eng, fn, dma=None, cc=None):
        self.eng = eng
        self.fn = fn
        self.deps = set()
        self.dma = dma
        self.cc = cc
        self.signal = False
        self.count = 0
        self.idx = 0


class Prog:
    ENGS = ["sp", "act", "dve", "pool", "pe"]

    def __init__(self):
        self.ops = {e: [] for e in self.ENGS}
        self.last_w = {}
        self.readers = {}
        self.dma_keys = {}
        self.cc_keys = []
        self.n = 0

    def add(self, eng, fn, reads=(), writes=(), dma=None, cc=None):
        op = Op(eng, fn, dma=dma, cc=cc)
        op.idx = self.n
        self.n += 1
        deps = set()
        for r in reads:
            w = self.last_w.get(r)
            if w is not None:
                deps.add(w)
        for w_ in writes:
            w = self.last_w.get(w_)
            if w is not None:
                deps.add(w)
            for rd in self.readers.get(w_, ()):
                deps.add(rd)
        deps.discard(op)
        if eng == "pe":
            deps = {d for d in deps if not (d.eng == "pe" and d.dma is None and d.cc is None)}
        op.deps = deps
        for r in reads:
            self.readers.setdefault(r, []).append(op)
        for w_ in writes:
            self.last_w[w_] = op
            self.readers[w_] = []
        self.ops[eng].append(op)
        if dma is not None:
            self.dma_keys.setdefault(dma, 0)
        if cc is not None:
            self.cc_keys.append(cc)
        return op

    def barrier(self, soft_cc=False):
        lasts = []
        for e in self.ENGS:
            for op in reversed(self.ops[e]):
                if op.dma is None and op.cc is None and op.fn is not None:
                    lasts.append(op)
                    break
        seen = {}
        for e in self.ENGS:
            for op in self.ops[e]:
                if op.dma is not None:
                    seen[op.dma] = op
                if op.cc is not None and not soft_cc:
                    seen[("cc", op.cc)] = op
        lasts += list(seen.values())
        for e in self.ENGS:
            b = Op(e, None)
            b.idx = self.n
            self.n += 1
            b.deps = set(lasts)
            self.ops[e].append(b)
        self.last_w = {k: v for k, v in self.last_w.items() if soft_cc and v.cc is not None}
        self.readers = {}

    def finalize(self):
        for e in self.ENGS:
            for op in self.ops[e]:
                for d in op.deps:
                    d.signal = True
        cnt = {e: 0 for e in self.ENGS}
        dcnt = {k: 0 for k in self.dma_keys}
        for e in self.ENGS:
            for op in self.ops[e]:
                if op.dma is not None:
                    dcnt[op.dma] += 16
                    op.count = dcnt[op.dma]
                elif op.cc is not None:
                    op.count = 1
                elif op.signal and op.fn is not None:
                    cnt[e] += 1
                    op.count = cnt[e]

    def emit(self, eng_name, eng, sems):
        waited = {}
        for op in self.ops[eng_name]:
            need = {}
            for d in op.deps:
                if d.dma is not None:
                    key = ("dma", d.dma)
                elif d.cc is not None:
                    key = ("cc", d.cc)
                else:
                    key = ("eng", d.eng)
                if d.count > need.get(key, 0):
                    need[key] = d.count
            for key, val in need.items():
                if waited.get(key, 0) >= val:
                    continue
                eng.wait_ge(sems[key], val)
                waited[key] = val
            if op.fn is None:
                continue
            ins = op.fn(eng)
            if op.dma is not None:
                ins.then_inc(sems[("dma", op.dma)], 16)
            elif op.cc is not None:
                ins.then_inc(sems[("cc", op.cc)])
            elif op.signal:
                ins.then_inc(sems[("eng", eng_name)], 1)


def build_program(stage="full"):
    nc = bass.Bass("TRN2", target_bir_lowering=False)
    P = Prog()

    def ext_in(name, shape, dt):
        return nc.dram_tensor(name, list(shape), dt, kind="ExternalInput").ap()

    x_in = ext_in("x", [NT, D], F32)
    win_in = ext_in("win", [4, D, 384], F32)
    wout_in = ext_in("wout", [2, D, D], F32)
    wff1_in = ext_in("wff1", [2, D, DFF], F32)
    wff2_in = ext_in("wff2", [2, DFF, D], F32)
    gains_in = ext_in("gains", [128, 64], F32)
    fnorm_in = ext_in("fnormb", [128, D], F32)
    lamb_in = ext_in("lamb", [128, 256], F32)
    ident_in = ext_in("ident", [128, 128], BF16)
    tri_in = ext_in("trimask", [128, 128], BF16)
    kaug_in = ext_in("kaug", [2, 32, T], BF16)
    qaug0_in = ext_in("qaug0", [2, 32, 512], BF16)
    btab_in = ext_in("biastab", [128, 8 * NDC], F32)
    augq1_in = ext_in("augq1", [128, 16], F32)
    bsel_in = ext_in("bsel", [128, 256], F32)
    y_out = nc.dram_tensor("y", [NT, D], F32, kind="ExternalOutput").ap()

    HTin = [[nc.dram_tensor(f"htin{l}_{i}", [D, 512], BF16) for i in range(4)] for l in range(2)]
    HT = [[nc.dram_tensor(f"ht{l}_{i}", [4 * D, 512], BF16) for i in range(4)] for l in range(2)]
    OSin = [[[nc.dram_tensor(f"osin{l}_{p}_{q}", [128, 2048], BF16) for q in range(4)]
             for p in range(2)] for l in range(2)]
    OS = [[nc.dram_tensor(f"os{l}_{p}", [2048, 2048], BF16) for p in range(2)] for l in range(2)]

    off = [16512]

    def sb(name, shape, dt, at=None):
        nbytes = int(np.prod(shape[1:])) * (4 if dt == F32 else 2)
        if at is None:
            o = off[0]
            off[0] += (nbytes + 31) // 32 * 32
        else:
            o = at
        return nc.alloc_sbuf_tensor_at(name, list(shape), dt, offset=o), o + (nbytes + 31) // 32 * 32

    H, _ = sb("H", [128, 16, D], F32)
    ident, _ = sb("identt", [128, 128], BF16)
    tri, _ = sb("trit", [128, 128], BF16)
    ones128, _ = sb("ones128", [128, 128], F32)
    onesbf, _ = sb("onesbf", [128, 128], BF16)
    bsel, _ = sb("bselt", [128, 256], F32)
    btab, _ = sb("btab", [128, 8 * NDC], F32)
    augq1, _ = sb("augq1t", [128, 16], F32)
    gains, _ = sb("gainst", [128, 64], F32)
    lamb, _ = sb("lambt", [128, 256], F32)
    small, _ = sb("small", [128, 64], F32)
    epsc, _ = sb("epsc", [128, 1], F32)
    PH = off[0]

    o = PH
    KT = []
    for x_ in range(2):
        t_, o = sb(f"KT{x_}", [128, T], BF16, at=o)
        KT.append(t_)
    Vb, o = sb("Vb", [128, 64 * 192], BF16, at=o)
    WINs = []
    for s_ in range(2):
        t_, o = sb(f"WIN{s_}", [128, 8, 384], BF16, at=o)
        WINs.append(t_)
    HNTc = []
    for s_ in range(2):
        t_, o = sb(f"HNTc{s_}", [128, 8, 512], BF16, at=o)
        HNTc.append(t_)
    QT = []
    for s_ in range(2):
        row = []
        for x_ in range(2):
            t_, o = sb(f"QT{s_}_{x_}", [128, 512], BF16, at=o)
            row.append(t_)
        QT.append(row)
    PT = []
    for s_ in range(4):
        t_, o = sb(f"PT{s_}", [128, 512], BF16, at=o)
        PT.append(t_)
    Zs = []
    for x_ in range(2):
        row = []
        for par in range(2):
            t_, o = sb(f"Zs{x_}_{par}", [128, 512], F32, at=o)
            row.append(t_)
        Zs.append(row)
    FT = []
    for i_ in range(4):
        t_, o = sb(f"FT{i_}", [128, 512], F32, at=o)
        FT.append(t_)
    FTo = []
    for i_ in range(2):
        t_, o = sb(f"FTo{i_}", [128, 512], F32, at=o)
        FTo.append(t_)
    FTz, o = sb("FTz", [128, 512], F32, at=o)
    OSB = [FT[2], FT[3]]
    Ocat = []
    for s_ in range(2):
        t_, o = sb(f"Ocat{s_}", [128, 512], BF16, at=o)
        Ocat.append(t_)
    gate_sb, o = sb("gate_sb", [128, 8, 32], F32, at=o)
    m8, o = sb("m8", [128, 8, 8], F32, at=o)
    selt, o = sb("selt", [128, 32], F32, at=o)
    NS = []
    for x_ in range(2):
        t_, o = sb(f"NS{x_}", [128, 4, 96], BF16, at=o)
        NS.append(t_)
    kmT, o = sb("kmT", [128, 32], BF16, at=o)
    km32, o = sb("km32", [128, 2], F32, at=o)
    STG, o = sb("STG", [128, 2048], F32, at=o)
    STG2, o = sb("STG2", [128, 2048], F32, at=o)
    P2END = o
    assert P2END <= 229376, P2END

    o = PH
    HNT, o = sb("HNT", [128, 8, NT], BF16, at=o)
    OT = HNT
    WO, o = sb("WO", [128, 8, D], BF16, at=o)
    W1e, W2e = [], []
    for s_ in range(2):
        t_, o = sb(f"W1e{s_}", [128, 8, 512], BF16, at=o)
        W1e.append(t_)
        t_, o = sb(f"W2e{s_}", [128, 4, D], BF16, at=o)
        W2e.append(t_)
    hnb = []
    for s_ in range(2):
        t_, o = sb(f"hnb{s_}", [128, D], BF16, at=o)
        hnb.append(t_)
    UT = []
    for s_ in range(2):
        t_, o = sb(f"UT{s_}", [128, 4, 512], BF16, at=o)
        UT.append(t_)
    RT = []
    for s_ in range(2):
        t_, o = sb(f"RT{s_}", [128, 512], BF16, at=o)
        RT.append(t_)
    YS = []
    for s_ in range(2):
        t_, o = sb(f"YS{s_}", [128, 512], F32, at=o)
        YS.append(t_)
    assert o <= P2END - 2 * 8192, (o, P2END)
    YT = [STG, STG2]

    pb = [nc.alloc_psum_tensor(f"pb{i}", [128, 512], F32) for i in range(8)]
    psS = pb[0:4]
    psO = pb[4:6]
    psP = pb[6]
    psX = pb[7]
    psXb = psX[:, 256:512].bitcast(BF16)
    psZ = pb[7]
    psT = pb[6][:, :].bitcast(BF16)
    psU = pb[0:2]
    psY = pb[2:4]

    ctx = {}
    OTQ = os.environ.get("OTQ", "sp")

    def dma(eng, key, out, in_, reads, writes):
        return P.add(eng, lambda e: e.dma_start(out=out, in_=in_), reads=reads, writes=writes, dma=key)

    def allgather(name, in_ap, out_ap, reads, writes):
        return P.add("pool", lambda e: e.collective_compute(
            "AllGather", ALU.bypass, replica_groups=GROUPS, ins=[in_ap.opt()], outs=[out_ap.opt()]),
            reads=reads, writes=writes, cc=name)

    dma("sp", "c0", ident[:, :], ident_in, [], ["ident"])
    dma("sp", "c0", tri[:, :], tri_in, [], ["tri"])
    dma("sp", "c0", bsel[:, :], bsel_in, [], ["bsel"])
    dma("sp", "c0", btab[:, :], btab_in, [], ["btab"])
    dma("sp", "c0", augq1[:, :], augq1_in, [], ["augq1"])
    dma("sp", "c0", gains[:, :], gains_in, [], ["gains"])
    dma("sp", "c0", lamb[:, :], lamb_in, [], ["lamb"])
    P.add("dve", lambda e: e.memset(ones128[:, :], 1.0), writes=["ones128"])
    P.add("dve", lambda e: e.memset(onesbf[:, :], 1.0), writes=["onesbf"])
    P.add("dve", lambda e: e.memset(epsc[:, :], EPS), writes=["epsc"])
    P.add("dve", lambda e: e.memset(small[:, :], 0.0), writes=["small"])
    P.add("dve", lambda e: e.tensor_tensor(out=FT[0][:, 0:64], in0=lamb[:, 0:64], in1=lamb[:, 64:128], op=ALU.mult),
          reads=["lamb"], writes=["ft0"])
    P.add("dve", lambda e: e.tensor_tensor(out=FT[0][:, 64:128], in0=lamb[:, 128:192], in1=lamb[:, 192:256], op=ALU.mult),
          reads=["lamb"], writes=["ft0b"])
    P.add("dve", lambda e: e.tensor_reduce(out=small[:, 34:36], in_=FT[0][:, 0:128].rearrange("p (a b) -> p a b", a=2),
                                           axis=AX.X, op=ALU.add),
          reads=["ft0", "ft0b", "small"], writes=["lam_s"])
    P.add("act", lambda e: e.activation(out=small[:, 36:38], in_=small[:, 34:36], func=AF.Exp),
          reads=["lam_s"], writes=["lam_e"])
    P.add("dve", lambda e: e.tensor_tensor(out=small[:, 38:39], in0=small[:, 37:38], in1=small[:, 36:37], op=ALU.subtract),
          reads=["lam_e"], writes=["lam_d"])
    P.add("dve", lambda e: e.tensor_scalar(out=small[:, 32:33], in0=small[:, 38:39], scalar1=-0.2, scalar2=None, op0=ALU.add),
          reads=["lam_d"], writes=["neglam"])
    P.add("dve", lambda e: e.tensor_scalar(out=small[:, 33:34], in0=gains[:, 32:33], scalar1=0.8, scalar2=None, op0=ALU.mult),
          reads=["gains", "small"], writes=["sub08"])
    for t in range(16):
        dma("sp", "xload", H[:, t, :], x_in[t * 128:(t + 1) * 128, :], [], [("H", t, 0), ("H", t, 1)])


    def rms_tile(t, slot):
        P.add("act", lambda e: e.activation(out=hnb[slot][:, :], in_=H[:, t, :], func=AF.Square,
                                            accum_out=small[:, t:t + 1]),
              reads=[("H", t, 0), ("H", t, 1), "small"], writes=[("hnb", slot), ("ssq", t)])
        P.add("act", lambda e: e.activation(out=small[:, 16 + t:17 + t], in_=small[:, t:t + 1], func=AF.Ln,
                                            bias=epsc[:, 0:1], scale=1.0 / D),
              reads=[("ssq", t), "epsc"], writes=[("rstd", t)])
        P.add("act", lambda e: e.activation(out=small[:, 16 + t:17 + t], in_=small[:, 16 + t:17 + t], func=AF.Exp, scale=-0.5),
              reads=[("rstd", t)], writes=[("rstd", t)])

    def norm_a(t):
        slot = t % 2
        P.add("dve", lambda e: e.memset(small[:, t:t + 1], 0.0), reads=[("ssq", t)], writes=[("ssq", t)])
        rms_tile(t, slot)
        P.add("dve", lambda e: e.tensor_scalar(out=hnb[slot][:, :], in0=H[:, t, :], scalar1=small[:, 16 + t:17 + t],
                                               scalar2=None, op0=ALU.mult),
              reads=[("H", t, 0), ("H", t, 1), ("rstd", t)], writes=[("hnb", slot)])

    def norm_b(t):
        slot = t % 2
        for kc in range(8):
            P.add("pe", lambda e, kc=kc: e.transpose(out=psT[:, kc * 128:(kc + 1) * 128],
                                                     in_=hnb[slot][:, kc * 128:(kc + 1) * 128], identity=ident[:, :]),
                  reads=[("hnb", slot), "ident"], writes=["psT"])
        P.add("dve", lambda e: e.tensor_copy(out=HNT[:, :, t * 128:(t + 1) * 128],
                                             in_=psT.rearrange("p (k q) -> p k q", k=8)),
              reads=["psT"], writes=[("HNT", t)])

    def norm_all(after_b=None):
        norm_a(0)
        for t in range(16):
            if t + 1 < 16:
                norm_a(t + 1)
            norm_b(t)
            if after_b is not None:
                after_b(t)

    def phase1(l):
        def after_b(t):
            if t % 4 == 3:
                i = t // 4
                dma("sp", f"htst{i}", HTin[l][i].ap().rearrange("(kc p) t -> p kc t", p=128),
                    HNT[:, :, i * 512:(i + 1) * 512], [("HNT", tt) for tt in range(4 * i, 4 * i + 4)],
                    [("HTin", l, i)])
                allgather(f"agh{l}_{i}", HTin[l][i].ap(), HT[l][i].ap(), [("HTin", l, i)], [("HT", l, i)])
        norm_all(after_b)

    def load_cast_win(l, p):
        wsrc = win_in[l * 2 + p].rearrange("(kc p) n -> p kc n", p=128)
        for hf in range(2):
            stg = (STG if hf == 0 else STG2)[:, 0:4 * 384].rearrange("p (k n) -> p k n", k=4)
            dma("sp", f"stg{hf}", stg, wsrc[:, hf * 4:hf * 4 + 4, :], [], [("STG", hf)])
            for k4 in range(4):
                kc = hf * 4 + k4
                P.add("act", lambda e, stg=stg, k4=k4, kc=kc: e.activation(
                    out=WINs[p][:, kc, :], in_=stg[:, k4, :], func=AF.Copy, scale=gains[:, l * 8 + kc:l * 8 + kc + 1]),
                    reads=[("STG", hf), "gains"], writes=[("WIN", p, kc)])

    def phase2(l):
        V0 = Vb[:, 0:64 * 128].rearrange("p (t c) -> p t c", c=128)
        V1 = Vb[:, :].rearrange("p (t c) -> p t c", c=192)
        dma("sp", "kaug0", KT[0][64:96, :], kaug_in[l], [], [("KTaug", 0)])
        dma("sp", "kaug1", KT[1][0:32, :], kaug_in[l], [], [("KTaug", 1)])
        P.add("dve", lambda e: e.memset(KT[1][32:64, :], 0.0), writes=[("KTz", 1)])
        for s_ in range(2):
            for x_ in range(2):
                P.add("dve", lambda e, s_=s_, x_=x_: e.memset(QT[s_][x_][:, :], 0.0), writes=[("QT", s_, x_), ("QTaug", s_, x_)])
        if l == 1:
            P.add("dve", lambda e: e.memset(V1[:, :, 64:128], 0.0), writes=["Vall"])
            P.add("dve", lambda e: e.memset(V1[:, :, 64:65], 1.0), reads=["Vall"], writes=["Vall"])
            for x_ in range(2):
                P.add("dve", lambda e, x_=x_: e.memset(NS[x_][:, :, :], 0.0), writes=[("NS", x_)])
                P.add("dve", lambda e, x_=x_: e.memset(OSB[x_][:, :], 0.0), writes=[("OSB", x_), ("FT", 2 + x_)])
            P.add("dve", lambda e: e.memset(kmT[:, :], 0.0), writes=["kmT"])
        load_cast_win(l, 0)
        for p in range(2):
            if l == 0:
                for s_ in range(2):
                    dma("sp", f"qaug{s_}0", QT[s_][0][64:96, :], qaug0_in[p], [("QTaug", s_, 0)], [("QTaug", s_, 0)])
                    dma("sp", f"qaug{s_}1", QT[s_][1][0:32, :], qaug0_in[p], [("QTaug", s_, 1)], [("QTaug", s_, 1)])
            for _ in chunk_proj(l, p, 0, V0, V1):
                pass
            fin = None
            for g in range(16):
                gen = chunk_proj(l, p, g + 1, V0, V1) if g + 1 < 16 else None
                if g == 8 and p == 0:
                    load_cast_win(l, 1)
                chunk_attn(l, p, g, V0, V1, gen, fin)
                fin = fin_gen(l, p, g)
                next(fin)
            for _ in fin:
                pass

    bidx = [0]
    NSLOT = 4

    def chunk_proj(l, p, g, V0, V1):
        s = g % 2
        hsrc = HT[l][g // 4][(g % 4) * D:(g % 4 + 1) * D, :].rearrange("(kc p) t -> p kc t", p=128)
        dma("sp", f"hntc{s}", HNTc[s][:, :, :], hsrc, [("HT", l, g // 4)], [("HNTc", s)])
        WIN = WINs[p]
        win_r = [("WIN", p, kc) for kc in range(8)]
        for kc in range(8):
            P.add("pe", lambda e, kc=kc: e.matmul(psP[:, :], lhsT=WIN[:, kc, 0:128], rhs=HNTc[s][:, kc, :],
                                                  start=(kc == 0), stop=(kc == 7)),
                  reads=[("HNTc", s)] + win_r, writes=["psP"])
        P.add("dve", lambda e: e.tensor_copy(out=QT[s][0][0:64, :], in_=psP[0:64, :]), reads=["psP"], writes=[("QT", s, 0)])
        P.add("dve", lambda e: e.tensor_copy(out=QT[s][1][64:128, :], in_=psP[64:128, :]), reads=["psP"], writes=[("QT", s, 1)])
        yield
        for kc in range(8):
            P.add("pe", lambda e, kc=kc: e.matmul(psP[:, :], lhsT=WIN[:, kc, 128:256], rhs=HNTc[s][:, kc, :],
                                                  start=(kc == 0), stop=(kc == 7)),
                  reads=[("HNTc", s)] + win_r, writes=["psP"])
        P.add("dve", lambda e: e.tensor_copy(out=KT[0][0:64, g * 512:(g + 1) * 512], in_=psP[0:64, :]),
              reads=["psP"], writes=[("KT", 0, g)])
        P.add("dve", lambda e: e.tensor_copy(out=KT[1][64:128, g * 512:(g + 1) * 512], in_=psP[64:128, :]),
              reads=["psP"], writes=[("KT", 1, g)])
        if l == 1:
            P.add("dve", lambda e: e.tensor_reduce(out=km32[:, :], in_=psP[:, :].rearrange("p (a b) -> p a b", a=2),
                                                   axis=AX.X, op=ALU.add), reads=["psP"], writes=["km32"])
            P.add("dve", lambda e: e.tensor_scalar(out=kmT[:, 2 * g:2 * g + 2], in0=km32[:, :], scalar1=1.0 / 256,
                                                   scalar2=None, op0=ALU.mult), reads=["km32"], writes=["kmT"])
        yield
        for tt in range(4):
            for kc in range(8):
                P.add("pe", lambda e, kc=kc, tt=tt: e.matmul(psP[:, tt * 128:(tt + 1) * 128],
                                                             lhsT=HNTc[s][:, kc, tt * 128:(tt + 1) * 128],
                                                             rhs=WIN[:, kc, 256:384], start=(kc == 0), stop=(kc == 7)),
                      reads=[("HNTc", s)] + win_r, writes=["psP"])
        psP4 = psP[:, :].rearrange("p (t c) -> p t c", t=4)
        if l == 0:
            P.add("dve", lambda e: e.tensor_copy(out=V0[:, 4 * g:4 * g + 4, :], in_=psP4), reads=["psP"], writes=[("V", g)])
        else:
            P.add("dve", lambda e: e.tensor_copy(out=V1[:, 4 * g:4 * g + 4, 0:64], in_=psP4[:, :, 0:64]),
                  reads=["psP", "Vall"], writes=[("V", g)])
            P.add("dve", lambda e: e.tensor_copy(out=V1[:, 4 * g:4 * g + 4, 128:192], in_=psP4[:, :, 64:128]),
                  reads=["psP", "Vall"], writes=[("Vb_", g)])
        yield
        if l == 1:
            yield from moba_gate(p, g, s)
        yield

    def chunk_attn(l, p, g, V0, V1, gen=None, fin=None):
        s = g % 2
        nk = 4 * g + 4
        if l == 1 and os.environ.get("SKIP_ATTN"):
            nk = 0
        kt0 = max(0, 4 * g - 16) if p == 0 else 0
        items = [(kt, x_) for kt in range(kt0, nk) for x_ in range(2)]
        if l == 0:
            for za, zeng in ((0, "dve"), (1, "pool")):
                P.add(zeng, lambda e, za=za: e.memset(Zs[za][g % 2][:, :], 0.0), writes=[("Zs", za, g % 2)])
        LOOK = 3
        slots = {}

        def emit_s(kt, x_):
            j = kt - 4 * g
            c0 = 128 * j if j >= 0 else 0
            b = bidx[0] % NSLOT
            bidx[0] += 1
            slots[(kt, x_)] = b
            kx = 96 if x_ == 0 else 128
            kr = [("KT", x_, kt // 4), ("KTaug", x_), ("QT", s, x_), ("QTaug", s, x_)] + ([("KTz", 1)] if x_ == 1 else [])
            ks = slice(kt * 128, (kt + 1) * 128)
            if j >= 0:
                P.add("pe", lambda e: e.matmul(
                    psS[b][:, c0:c0 + 128], lhsT=KT[x_][0:kx, ks], rhs=QT[s][x_][0:kx, c0:c0 + 128],
                    start=True, stop=False), reads=kr, writes=[("psS", b)])
                P.add("pe", lambda e: e.matmul(
                    psS[b][:, c0:c0 + 128], lhsT=ident[:, :], rhs=tri[:, :], start=False, stop=True),
                    reads=["ident", "tri"], writes=[("psS", b)])
                if c0 + 128 < 512:
                    P.add("pe", lambda e: e.matmul(
                        psS[b][:, c0 + 128:512], lhsT=KT[x_][0:kx, ks], rhs=QT[s][x_][0:kx, c0 + 128:512],
                        start=True, stop=True), reads=kr, writes=[("psS", b)])
            else:
                P.add("pe", lambda e: e.matmul(
                    psS[b][:, :], lhsT=KT[x_][0:kx, ks], rhs=QT[s][x_][0:kx, :], start=True, stop=True),
                    reads=kr, writes=[("psS", b)])
            col = (((l * 2 + p) * 2 + x_) * NDC) + (4 * g - kt + 3)
            P.add("act", lambda e: e.activation(
                out=PT[b][:, c0:512], in_=psS[b][:, c0:512], func=AF.Exp, bias=btab[:, col:col + 1], scale=SCALE),
                reads=[("psS", b), "btab"], writes=[("PT", b)])
            if l == 0 and x_ == 0:
                za = kt % 2
                zeng = "dve" if za == 0 else "pool"
                zt = Zs[za][g % 2]
                zk = ("Zs", za, g % 2)
                P.add(zeng, lambda e: e.tensor_tensor(
                    out=zt[:, c0:512], in0=zt[:, c0:512], in1=PT[b][:, c0:512], op=ALU.add),
                    reads=[("PT", b), zk], writes=[zk])

        def emit_pv(kt, x_):
            j = kt - 4 * g
            c0 = 128 * j if j >= 0 else 0
            b = slots[(kt, x_)]
            if l == 0:
                vl = V0[:, kt, :]
                mo = 128
            else:
                vl = V1[:, kt, 0:65] if x_ == 0 else V1[:, kt, 64:192]
                mo = 65 if x_ == 0 else 128
            P.add("pe", lambda e: e.matmul(
                psO[x_][0:mo, c0:512], lhsT=vl, rhs=PT[b][:, c0:512], start=(kt == kt0), stop=(kt == nk - 1)),
                reads=[("PT", b), ("V", kt // 4), ("Vb_", kt // 4), "Vall"], writes=[("psO", x_)])
            if l == 0 and x_ == 1:
                P.add("pe", lambda e: e.matmul(
                    psZ[:, c0:512], lhsT=onesbf[:, :], rhs=PT[b][:, c0:512], start=(kt == kt0), stop=(kt == nk - 1)),
                    reads=[("PT", b), "onesbf"], writes=["psZ"])

        for n in range(len(items) + LOOK):
            if n < len(items):
                emit_s(*items[n])
            if n - LOOK >= 0:
                emit_pv(*items[n - LOOK])
            if n >= 4 and n % 3 == 1 and gen is not None:
                next(gen, None)
            if n >= 2 and n % 3 == 2 and fin is not None:
                next(fin, None)
        if fin is not None:
            for _ in fin:
                pass
        if gen is not None:
            for _ in gen:
                pass

    def fin_gen(l, p, g):
        s = g % 2
        oc = Ocat[s]
        zp = g % 2
        if l == 0:
            for x_ in range(2):
                P.add("dve", lambda e, x_=x_: e.tensor_copy(out=FTo[x_][:, :], in_=psO[x_][:, :]),
                      reads=[("psO", x_)], writes=[("FTo", x_)])
            P.add("dve", lambda e: e.tensor_copy(out=FTz[:, :], in_=psZ[:, :]), reads=["psZ"], writes=["FTz"])
            yield
            P.add("dve", lambda e: e.tensor_tensor(out=Zs[0][zp][:, :], in0=Zs[0][zp][:, :], in1=Zs[1][zp][:, :], op=ALU.add),
                  reads=[("Zs", 0, zp), ("Zs", 1, zp)], writes=[("Zs", 0, zp)])
            P.add("pe", lambda e: e.matmul(psP[:, :], lhsT=ones128[:, :], rhs=Zs[0][zp][:, :], start=True, stop=True),
                  reads=[("Zs", 0, zp), "ones128"], writes=["psP"])
            P.add("dve", lambda e: e.reciprocal(out=FT[0][:, :], in_=psP[:, :]), reads=["psP"], writes=[("FT", 0)])
            yield
            P.add("dve", lambda e: e.reciprocal(out=FT[1][:, :], in_=FTz[:, :]), reads=["FTz"], writes=[("FT", 1)])
            yield
            for x_ in range(2):
                P.add("dve", lambda e, x_=x_: e.tensor_tensor(out=FT[x_][:, :], in0=FTo[x_][:, :], in1=FT[x_][:, :], op=ALU.mult),
                      reads=[("FTo", x_), ("FT", x_)], writes=[("FT", x_)])
            P.add("dve", lambda e: e.scalar_tensor_tensor(out=FT[2][:, :], in0=FT[1][:, :], scalar=small[:, 32:33], in1=FT[0][:, :],
                                                          op0=ALU.mult, op1=ALU.add),
                  reads=[("FT", 0), ("FT", 1), "neglam"], writes=[("FT", 2)])
            P.add("pool", lambda e: e.tensor_tensor(out=FT[3][:, :], in0=FT[2][:, :], in1=FT[2][:, :], op=ALU.mult),
                  reads=[("FT", 2)], writes=[("FT", 3)])
            yield
            P.add("pe", lambda e: e.matmul(psP[:, :], lhsT=ones128[:, :], rhs=FT[3][:, :], start=True, stop=True),
                  reads=[("FT", 3), "ones128"], writes=["psP"])
            P.add("act", lambda e: e.activation(out=FT[3][:, :], in_=psP[:, :], func=AF.Ln, bias=epsc[:, 0:1], scale=1.0 / 128),
                  reads=["psP", "epsc"], writes=[("FT", 3)])
            P.add("act", lambda e: e.activation(out=FT[3][:, :], in_=FT[3][:, :], func=AF.Exp, scale=-0.5),
                  reads=[("FT", 3)], writes=[("FT", 3)])
            yield
            P.add("dve", lambda e: e.tensor_tensor(out=oc[:, :], in0=FT[2][:, :], in1=FT[3][:, :], op=ALU.mult),
                  reads=[("FT", 2), ("FT", 3)], writes=[("Ocat", s)])
        else:
            for x_ in range(2):
                mo = 65 if x_ == 0 else 128
                P.add("dve", lambda e, x_=x_, mo=mo: e.tensor_copy(out=OSB[x_][0:mo, :], in_=psO[x_][0:mo, :]),
                      reads=[("psO", x_)], writes=[("OSB", x_)])
            yield
            for x_ in range(2):
                rows = slice(0, 64) if x_ == 0 else slice(64, 128)
                P.add("pe", lambda e, x_=x_: e.matmul(psX[:, :], lhsT=bsel[:, x_ * 128:(x_ + 1) * 128], rhs=OSB[x_][:, :],
                                                      start=True, stop=True), reads=[("OSB", x_), "bsel"], writes=["psX"])
                P.add("dve", lambda e, x_=x_, rows=rows: e.reciprocal(out=FT[x_][rows, :], in_=psX[rows, :]),
                      reads=["psX"], writes=[("FT", x_)])
                yield
                P.add("dve", lambda e, x_=x_, rows=rows: e.tensor_tensor(out=oc[rows, :], in0=OSB[x_][rows, :], in1=FT[x_][rows, :],
                                                                        op=ALU.mult),
                      reads=[("OSB", x_), ("FT", x_)], writes=[("Ocat", s, x_)])
        dma("sp", f"ost{s}", OSin[l][p][g // 4][:, (g % 4) * 512:(g % 4 + 1) * 512], oc[:, :],
            [("Ocat", s), ("Ocat", s, 0), ("Ocat", s, 1)], [("OSin", l, p, g // 4)])
        yield
        if g % 4 == 3:
            q = g // 4
            allgather(f"ago{l}_{p}_{q}", OSin[l][p][q].ap(), OS[l][p][q * 512:(q + 1) * 512, :],
                      [("OSin", l, p, q)], [("OS", l, p, q)])
        yield

    def moba_gate(p, g, s):
        gbank = [psX, psP]
        gkey = ["psX", "psP"]
        for x_ in range(2):
            rows = slice(0, 64) if x_ == 0 else slice(64, 128)
            for qt in range(4):
                P.add("pe", lambda e, x_=x_, qt=qt, rows=rows: e.matmul(
                    gbank[x_][:, qt * 32:(qt + 1) * 32], lhsT=QT[s][x_][rows, qt * 128:(qt + 1) * 128], rhs=kmT[rows, :],
                    start=True, stop=True), reads=[("QT", s, x_), "kmT"], writes=[gkey[x_]])
        P.add("dve", lambda e: e.memset(gate_sb[:, :, :], -1e30), writes=["gate"])
        gsv = gate_sb[:, :, :].rearrange("p (x h q) n -> p x h q n", x=2, h=2)
        for hf in range(2):
            jb = 2 * g + hf
            if jb > 0:
                for x_ in range(2):
                    psg = gbank[x_][:, 0:128].rearrange("p (h q n) -> p h q n", h=2, q=2)
                    P.add("dve", lambda e, hf=hf, jb=jb, x_=x_, psg=psg: e.tensor_copy(out=gsv[:, x_, hf, :, 0:jb], in_=psg[:, hf, :, 0:jb]),
                          reads=[gkey[x_], "gate"], writes=["gate"])
        for x_ in range(2):
            base = 64 if x_ == 0 else 0
            for qt in range(4):
                xq = x_ * 4 + qt
                jb = 2 * g + qt // 2
                ai = (p * 2 + x_) * 4 + qt
                P.add("dve", lambda e, xq=xq: e.max(out=m8[:, xq, :], in_=gate_sb[:, xq, :]), reads=["gate"], writes=[("m8", xq)])
                P.add("dve", lambda e, xq=xq: e.tensor_scalar(out=selt[:, :], in0=gate_sb[:, xq, :], scalar1=m8[:, xq, 2:3],
                                                              scalar2=None, op0=ALU.is_ge),
                      reads=["gate", ("m8", xq)], writes=["selt"])
                P.add("dve", lambda e, x_=x_, qt=qt, base=base, ai=ai: e.tensor_scalar(
                    out=NS[x_][:, qt, base:base + 32], in0=selt[:, :], scalar1=-NEG, scalar2=augq1[:, ai:ai + 1],
                    op0=ALU.mult, op1=ALU.add), reads=["selt", "augq1", ("NS", x_)], writes=[("NS", x_)])
                P.add("dve", lambda e, x_=x_, qt=qt, base=base, ai=ai, jb=jb: e.tensor_scalar(
                    out=NS[x_][:, qt, base + jb:base + jb + 1], in0=augq1[:, ai:ai + 1], scalar1=-NEG, scalar2=None,
                    op0=ALU.add), reads=["augq1", ("NS", x_)], writes=[("NS", x_)])
        for _ in range(4):
            yield
        for x_ in range(2):
            rows = slice(64, 96) if x_ == 0 else slice(0, 32)
            yield
            for qt in range(4):
                P.add("pe", lambda e, x_=x_, qt=qt: e.transpose(out=psXb[0:96, qt * 128:(qt + 1) * 128],
                                                                in_=NS[x_][:, qt, :], identity=ident[:, :]),
                      reads=[("NS", x_), "ident"], writes=["psX"])
            P.add("dve", lambda e, x_=x_, rows=rows: e.tensor_copy(out=QT[s][x_][rows, :], in_=psXb[rows, :]),
                  reads=["psX"], writes=[("QTaug", s, x_)])

    def phase3(l, do_ffn=True):
        def rank_of(e):
            if "rank" not in ctx:
                ctx["rank"] = e.partition_id() % 4
            return ctx["rank"]
        for q in range(4):
            for kc in range(8):
                src, p = kc // 2, kc % 2
                P.add(OTQ, lambda e, kc=kc, src=src, p=p, q=q: e.dma_start(
                    out=OT[:, kc, q * 512:(q + 1) * 512],
                    in_=OS[l][p][q * 512 + src * 128:q * 512 + src * 128 + 128, bass.ds(rank_of(e) * 512, 512)]),
                    reads=[("OS", l, p, q)], writes=[("OT", kc, q)], dma="otld")
        wsrc = wout_in[l].rearrange("(kc p) n -> p kc n", p=128)
        for i in range(4):
            stg = (STG if i % 2 == 0 else STG2)[:, :].rearrange("p (k n) -> p k n", k=2)
            dma("sp", f"stg{i % 2}", stg, wsrc[:, 2 * i:2 * i + 2, :], [], [("STG", i % 2)])
            if l == 0:
                P.add("act", lambda e, stg=stg, i=i: e.activation(out=WO[:, 2 * i:2 * i + 2, :], in_=stg, func=AF.Copy,
                                                                  scale=small[:, 33:34]),
                      reads=[("STG", i % 2), "sub08"], writes=[("WO", i)])
            else:
                P.add("act", lambda e, stg=stg, i=i: e.copy(out=WO[:, 2 * i:2 * i + 2, :], in_=stg),
                      reads=[("STG", i % 2)], writes=[("WO", i)])
        yb = 0
        for t in range(16):
            for n2 in range(2):
                b = yb % 2
                yb += 1
                for kc in range(8):
                    wk = kc // 2 + 4 * (kc % 2)
                    P.add("pe", lambda e, kc=kc, wk=wk, b=b, t=t, n2=n2: e.matmul(
                        psY[b][:, :], lhsT=OT[:, kc, t * 128:(t + 1) * 128], rhs=WO[:, wk, n2 * 512:(n2 + 1) * 512],
                        start=(kc == 0), stop=(kc == 7)), reads=[("OT", k_, q_) for k_ in range(8) for q_ in range(4)] + [("WO", wk // 2)], writes=[("psY", b)])
                P.add("dve", lambda e, b=b, t=t, n2=n2: e.tensor_tensor(
                    out=H[:, t, n2 * 512:(n2 + 1) * 512], in0=H[:, t, n2 * 512:(n2 + 1) * 512], in1=psY[b][:, :], op=ALU.add),
                    reads=[("psY", b), ("H", t, n2)], writes=[("H", t, n2)])
        if not do_ffn:
            return
        P.barrier()
        norm_all()
        w1src = wff1_in[l].rearrange("(kc p) n -> p kc n", p=128)
        w2src = wff2_in[l].rearrange("(fc p) n -> p fc n", p=128)
        ub = 0
        for ei in range(8):
            ws = ei % 2
            for hf in range(2):
                stg = (STG if hf == 0 else STG2)[:, :].rearrange("p (k n) -> p k n", k=4)
                dma("sp", f"stg{hf}", stg, w1src[:, hf * 4:hf * 4 + 4, ei * 512:(ei + 1) * 512], [], [("STG", hf)])
                for k4 in range(4):
                    kc = hf * 4 + k4
                    P.add("act", lambda e, stg=stg, k4=k4, kc=kc, ws=ws: e.activation(
                        out=W1e[ws][:, kc, :], in_=stg[:, k4, :], func=AF.Copy, scale=gains[:, 16 + l * 8 + kc:17 + l * 8 + kc]),
                        reads=[("STG", hf), "gains"], writes=[("W1e", ws)])
            for hf in range(2):
                stg = (STG if hf == 0 else STG2)[:, :].rearrange("p (k n) -> p k n", k=2)
                dma("sp", f"stg{hf}", stg, w2src[:, ei * 4 + hf * 2:ei * 4 + hf * 2 + 2, :], [], [("STG", hf)])
                P.add("act", lambda e, stg=stg, hf=hf, ws=ws: e.copy(out=W2e[ws][:, 2 * hf:2 * hf + 2, :], in_=stg),
                      reads=[("STG", hf)], writes=[("W2e", ws)])
            for c in range(4):
                us = (ei * 4 + c) % 2
                for fc in range(4):
                    b = ub % 2
                    ub += 1
                    for kc in range(8):
                        P.add("pe", lambda e, kc=kc, b=b, fc=fc, c=c, ws=ws: e.matmul(
                            psU[b][:, :], lhsT=W1e[ws][:, kc, fc * 128:(fc + 1) * 128], rhs=HNT[:, kc, c * 512:(c + 1) * 512],
                            start=(kc == 0), stop=(kc == 7)),
                            reads=[("W1e", ws)] + [("HNT", tt) for tt in range(4 * c, 4 * c + 4)], writes=[("psU", b)])
                    P.add("act", lambda e, b=b: e.activation(out=RT[b][:, :], in_=psU[b][:, :], func=AF.Relu),
                          reads=[("psU", b)], writes=[("RT", b)])
                    P.add("dve", lambda e, b=b, us=us, fc=fc: e.tensor_tensor(out=UT[us][:, fc, :], in0=RT[b][:, :], in1=RT[b][:, :],
                                                                             op=ALU.mult),
                          reads=[("RT", b)], writes=[("UT", us, fc)])
                for tt in range(4):
                    t = 4 * c + tt
                    for n2 in range(2):
                        b = yb % 2
                        yb += 1
                        for fc in range(4):
                            P.add("pe", lambda e, fc=fc, b=b, tt=tt, n2=n2, us=us, ws=ws: e.matmul(
                                psY[b][:, :], lhsT=UT[us][:, fc, tt * 128:(tt + 1) * 128], rhs=W2e[ws][:, fc, n2 * 512:(n2 + 1) * 512],
                                start=(fc == 0), stop=(fc == 3)),
                                reads=[("UT", us, fc), ("W2e", ws)], writes=[("psY", b)])
                        if n2 == 0:
                            P.add("dve", lambda e, b=b, t=t, n2=n2: e.tensor_tensor(
                                out=H[:, t, n2 * 512:(n2 + 1) * 512], in0=H[:, t, n2 * 512:(n2 + 1) * 512], in1=psY[b][:, :], op=ALU.add),
                                reads=[("psY", b), ("H", t, n2)], writes=[("H", t, n2)])
                        else:
                            ys = tt % 2
                            P.add("act", lambda e, b=b, ys=ys: e.copy(out=YS[ys][:, :], in_=psY[b][:, :]),
                                  reads=[("psY", b)], writes=[("YS", ys)])
                            P.add("pool", lambda e, t=t, n2=n2, ys=ys: e.tensor_tensor(
                                out=H[:, t, n2 * 512:(n2 + 1) * 512], in0=H[:, t, n2 * 512:(n2 + 1) * 512], in1=YS[ys][:, :], op=ALU.add),
                                reads=[("YS", ys), ("H", t, n2)], writes=[("H", t, n2)])

    def final_out(with_norm=True):
        if with_norm:
            dma("sp", "stg0", STG[:, 0:1024], fnorm_in, [], ["fnb"])
        for t in range(16):
            slot = t % 2
            yt = STG2[:, slot * 1024:(slot + 1) * 1024]
            if with_norm:
                P.add("dve", lambda e, t=t: e.memset(small[:, t:t + 1], 0.0), reads=[("ssq", t)], writes=[("ssq", t)])
                rms_tile(t, slot)
                P.add("dve", lambda e, t=t, yt=yt: e.scalar_tensor_tensor(
                    out=yt, in0=H[:, t, :], scalar=small[:, 16 + t:17 + t], in1=STG[:, 0:1024], op0=ALU.mult, op1=ALU.mult),
                    reads=[("H", t, 0), ("H", t, 1), ("rstd", t), "fnb"], writes=[("yt", slot)])
            else:
                P.add("dve", lambda e, t=t, yt=yt: e.tensor_copy(out=yt, in_=H[:, t, :]),
                      reads=[("H", t, 0), ("H", t, 1)], writes=[("yt", slot)])
            dma("sp", f"yst{slot}", y_out[t * 128:(t + 1) * 128, :], yt, [("yt", slot)], [("yout", t)])

    phase1(0)
    P.barrier(soft_cc=True)
    if stage != "F0":
        phase2(0)
        P.barrier(soft_cc=True)
    if stage == "F0":
        phase3(0)
        P.barrier()
        final_out(with_norm=False)
    elif stage == "A0":
        phase3(0, do_ffn=False)
        P.barrier()
        final_out(with_norm=False)
    elif stage == "L0":
        phase3(0)
        P.barrier()
        final_out(with_norm=False)
    else:
        phase3(0)
        P.barrier()
        phase1(1)
        P.barrier(soft_cc=True)
        phase2(1)
        P.barrier(soft_cc=True)
        if stage == "A1":
            phase3(1, do_ffn=False)
            P.barrier()
            final_out(with_norm=False)
        else:
            phase3(1)
            P.barrier()
            final_out(with_norm=True)
    P.barrier()
    P.finalize()

    with ExitStack() as st:
        sems = {}
        for e in Prog.ENGS:
            sems[("eng", e)] = st.enter_context(nc.semaphore(f"s_{e}"))
        for k in P.dma_keys:
            sems[("dma", k)] = st.enter_context(nc.semaphore(f"d_{k}"))
        for k in P.cc_keys:
            sems[("cc", k)] = st.enter_context(nc.semaphore(f"c_{k}"))
        block = st.enter_context(nc.Block())

        @block.sync
        def _(e):
            P.emit("sp", e, sems)

        @block.scalar
        def _(e):
            P.emit("act", e, sems)

        @block.vector
        def _(e):
            P.emit("dve", e, sems)

        @block.gpsimd
        def _(e):
            P.emit("pool", e, sems)

        @block.tensor
        def _(e):
            P.emit("pe", e, sems)
    return nc


def _slopes(n):
    return np.array([2.0 ** (-8.0 * (h + 1) / n) for h in range(n)], dtype=np.float64)


def _consts(r):
    bf = ml_dtypes.bfloat16
    s0 = _slopes(8)
    s1 = _slopes(16)
    ident = np.eye(128, dtype=np.float32).astype(bf)
    ki = np.arange(128)[:, None]
    qi = np.arange(128)[None, :]
    tri = np.where(qi < ki, NEG, 0.0).astype(np.float32).astype(bf)
    kaug = np.zeros((2, 32, T), np.float32)
    kaug[0, 0, :] = 1.0
    kaug[1, np.arange(T) // 256, np.arange(T)] = 1.0
    kaug = kaug.astype(bf)
    qaug0 = np.zeros((2, 32, 512), np.float32)
    btab = np.zeros((128, 2, 2, 2, NDC), np.float64)
    augq1 = np.zeros((128, 2, 2, 4), np.float64)
    pp = np.arange(128, dtype=np.float64)
    dc = np.arange(NDC, dtype=np.float64) - 3.0
    for p in range(2):
        s = s0[r + 4 * p]
        qaug0[p, 0, :] = -s * np.arange(512) / SCALE
        for x in range(2):
            btab[:, 0, p, x, :] = s * (pp[:, None] - 128.0 * dc[None, :])
            s_ = s1[2 * (r + 4 * p) + x]
            btab[:, 1, p, x, :] = s_ * (pp[:, None] - 128.0 * dc[None, :])
            for qt in range(4):
                augq1[:, p, x, qt] = -s_ * (qt * 128 + pp) / SCALE + NEG
    bsel = np.zeros((128, 2, 128), np.float32)
    bsel[64, 0, 0:64] = 1.0
    bsel[0, 1, 64:128] = 1.0
    return dict(ident=ident, trimask=tri, kaug=kaug, qaug0=qaug0.astype(bf),
                biastab=btab.reshape(128, -1).astype(np.float32), augq1=augq1.reshape(128, -1).astype(np.float32),
                bsel=bsel.reshape(128, 256))


_NC_CACHE = {}


def kernel(x, attn_norm, w_in, w_out, diff_lambda, diff_subln, mlp_norm, w_ff1, w_ff2, final_norm, _stage="full"):
    x = np.asarray(x, np.float32)
    attn_norm = np.asarray(attn_norm, np.float32)
    w_in = np.asarray(w_in, np.float32)
    w_out = np.ascontiguousarray(np.asarray(w_out, np.float32))
    diff_lambda = np.asarray(diff_lambda, np.float32)
    diff_subln = np.asarray(diff_subln, np.float32)
    mlp_norm = np.asarray(mlp_norm, np.float32)
    w_ff1 = np.ascontiguousarray(np.asarray(w_ff1, np.float32))
    w_ff2 = np.ascontiguousarray(np.asarray(w_ff2, np.float32))
    final_norm = np.asarray(final_norm, np.float32)

    if _stage not in _NC_CACHE:
        _NC_CACHE[_stage] = build_program(_stage)
    nc = _NC_CACHE[_stage]

    gains = np.zeros((128, 64), np.float32)
    for l in range(2):
        gains[:, l * 8:(l + 1) * 8] = attn_norm[l].reshape(8, 128).T
        gains[:, 16 + l * 8:16 + (l + 1) * 8] = mlp_norm[l].reshape(8, 128).T
    gains[:, 32] = diff_subln[0]
    fnb = np.ascontiguousarray(np.broadcast_to(final_norm[None, :], (128, D)))
    lamb = np.ascontiguousarray(np.broadcast_to(diff_lambda[0].reshape(1, 256), (128, 256)))

    in_maps = []
    for c in range(8):
        b, r = c // 4, c % 4
        win = np.zeros((4, D, 384), np.float32)
        for l in range(2):
            for p in range(2):
                c0 = 128 * (r + 4 * p)
                for j in range(3):
                    win[l * 2 + p, :, j * 128:(j + 1) * 128] = w_in[l, :, j * D + c0:j * D + c0 + 128]
        xr = x[b].reshape(4, 4, 512, D)[:, r].reshape(NT, D)
        m = dict(x=np.ascontiguousarray(xr), win=win, wout=w_out, wff1=w_ff1, wff2=w_ff2,
                 gains=gains, fnormb=fnb, lamb=lamb)
        m.update(_consts(r))
        in_maps.append(m)
    res = run_bass_kernel_spmd(nc, in_maps, core_ids=list(range(8)))
    out = np.zeros((2, T, D), np.float32)
    for c in range(8):
        b, r = c // 4, c % 4
        out[b].reshape(4, 4, 512, D)[:, r] = np.asarray(res.results[c]["y"], dtype=np.float32).reshape(4, 512, D)
    return out
```

```python
import math
import os
from contextlib import ExitStack

import numpy as np
import ml_dtypes
import concourse.bass as bass
import concourse.mybir as mybir
from concourse.bass_utils import run_bass_kernel_spmd

F32 = mybir.dt.float32
BF16 = mybir.dt.bfloat16
ALU = mybir.AluOpType
AF = mybir.ActivationFunctionType
AX = mybir.AxisListType

D = 1024
T = 8192
NT = 2048
DFF = 4096
EPS = 1e-6
SCALE = 0.125
NEG = -30000.0
NDC = 67
GROUPS = [[0, 1, 2, 3], [4, 5, 6, 7]]


class Op:
    __slots__ = ("eng", "fn", "deps", "dma", "cc", "signal", "count", "idx")

    def __init__(self, eng, fn, dma=None, cc=None):
        self.eng = eng
        self.fn = fn
        self.deps = set()
        self.dma = dma
        self.cc = cc
        self.signal = False
        self.count = 0
        self.idx = 0


class Prog:
    ENGS = ["sp", "act", "dve", "pool", "pe"]

    def __init__(self):
        self.ops = {e: [] for e in self.ENGS}
        self.last_w = {}
        self.readers = {}
        self.dma_keys = {}
        self.cc_keys = []
        self.n = 0

    def add(self, eng, fn, reads=(), writes=(), dma=None, cc=None):
        op = Op(eng, fn, dma=dma, cc=cc)
        op.idx = self.n
        self.n += 1
        deps = set()
        for r in reads:
            w = self.last_w.get(r)
            if w is not None:
                deps.add(w)
        for w_ in writes:
            w = self.last_w.get(w_)
            if w is not None:
                deps.add(w)
            for rd in self.readers.get(w_, ()):
                deps.add(rd)
        deps.discard(op)
        if eng == "pe":
            deps = {d for d in deps if not (d.eng == "pe" and d.dma is None and d.cc is None)}
        op.deps = deps
        for r in reads:
            self.readers.setdefault(r, []).append(op)
        for w_ in writes:
            self.last_w[w_] = op
            self.readers[w_] = []
        self.ops[eng].append(op)
        if dma is not None:
            self.dma_keys.setdefault(dma, 0)
        if cc is not None:
            self.cc_keys.append(cc)
        return op

    def barrier(self, soft_cc=False):
        lasts = []
        for e in self.ENGS:
            for op in reversed(self.ops[e]):
                if op.dma is None and op.cc is None and op.fn is not None:
                    lasts.append(op)
                    break
        seen = {}
        for e in self.ENGS:
            for op in self.ops[e]:
                if op.dma is not None:
                    seen[op.dma] = op
                if op.cc is not None and not soft_cc:
                    seen[("cc", op.cc)] = op
        lasts += list(seen.values())
        for e in self.ENGS:
            b = Op(e, None)
            b.idx = self.n
            self.n += 1
            b.deps = set(lasts)
            self.ops[e].append(b)
        self.last_w = {k: v for k, v in self.last_w.items() if soft_cc and v.cc is not None}
        self.readers = {}

    def finalize(self):
        for e in self.ENGS:
            for op in self.ops[e]:
                for d in op.deps:
                    d.signal = True
        cnt = {e: 0 for e in self.ENGS}
        dcnt = {k: 0 for k in self.dma_keys}
        for e in self.ENGS:
            for op in self.ops[e]:
                if op.dma is not None:
                    dcnt[op.dma] += 16
                    op.count = dcnt[op.dma]
                elif op.cc is not None:
                    op.count = 1
                elif op.signal and op.fn is not None:
                    cnt[e] += 1
                    op.count = cnt[e]

    def emit(self, eng_name, eng, sems):
        waited = {}
        for op in self.ops[eng_name]:
            need = {}
            for d in op.deps:
                if d.dma is not None:
                    key = ("dma", d.dma)
                elif d.cc is not None:
                    key = ("cc", d.cc)
                else:
                    key = ("eng", d.eng)
                if d.count > need.get(key, 0):
                    need[key] = d.count
            for key, val in need.items():
                if waited.get(key, 0) >= val:
                    continue
                eng.wait_ge(sems[key], val)
                waited[key] = val
            if op.fn is None:
                continue
            ins = op.fn(eng)
            if op.dma is not None:
                ins.then_inc(sems[("dma", op.dma)], 16)
            elif op.cc is not None:
                ins.then_inc(sems[("cc", op.cc)])
            elif op.signal:
                ins.then_inc(sems[("eng", eng_name)], 1)


def build_program(stage="full"):
    nc = bass.Bass("TRN2", target_bir_lowering=False)
    P = Prog()

    def ext_in(name, shape, dt):
        return nc.dram_tensor(name, list(shape), dt, kind="ExternalInput").ap()

    x_in = ext_in("x", [NT, D], F32)
    win_in = ext_in("win", [4, D, 384], F32)
    wout_in = ext_in("wout", [2, D, D], F32)
    wff1_in = ext_in("wff1", [2, D, DFF], F32)
    wff2_in = ext_in("wff2", [2, DFF, D], F32)
    gains_in = ext_in("gains", [128, 64], F32)
    fnorm_in = ext_in("fnormb", [128, D], F32)
    lamb_in = ext_in("lamb", [128, 256], F32)
    ident_in = ext_in("ident", [128, 128], BF16)
    tri_in = ext_in("trimask", [128, 128], BF16)
    kaug_in = ext_in("kaug", [2, 32, T], BF16)
    qaug0_in = ext_in("qaug0", [2, 32, 512], BF16)
    btab_in = ext_in("biastab", [128, 8 * NDC], F32)
    augq1_in = ext_in("augq1", [128, 16], F32)
    bsel_in = ext_in("bsel", [128, 256], F32)
    y_out = nc.dram_tensor("y", [NT, D], F32, kind="ExternalOutput").ap()

    HTin = [[nc.dram_tensor(f"htin{l}_{i}", [D, 512], BF16) for i in range(4)] for l in range(2)]
    HT = [[nc.dram_tensor(f"ht{l}_{i}", [4 * D, 512], BF16) for i in range(4)] for l in range(2)]
    OSin = [[[nc.dram_tensor(f"osin{l}_{p}_{q}", [128, 2048], BF16) for q in range(4)]
             for p in range(2)] for l in range(2)]
    OS = [[nc.dram_tensor(f"os{l}_{p}", [2048, 2048], BF16) for p in range(2)] for l in range(2)]

    off = [16512]

    def sb(name, shape, dt, at=None):
        nbytes = int(np.prod(shape[1:])) * (4 if dt == F32 else 2)
        if at is None:
            o = off[0]
            off[0] += (nbytes + 31) // 32 * 32
        else:
            o = at
        return nc.alloc_sbuf_tensor_at(name, list(shape), dt, offset=o), o + (nbytes + 31) // 32 * 32

    H, _ = sb("H", [128, 16, D], F32)
    ident, _ = sb("identt", [128, 128], BF16)
    tri, _ = sb("trit", [128, 128], BF16)
    ones128, _ = sb("ones128", [128, 128], F32)
    onesbf, _ = sb("onesbf", [128, 128], BF16)
    bsel, _ = sb("bselt", [128, 256], F32)
    btab, _ = sb("btab", [128, 8 * NDC], F32)
    augq1, _ = sb("augq1t", [128, 16], F32)
    gains, _ = sb("gainst", [128, 64], F32)
    lamb, _ = sb("lambt", [128, 256], F32)
    small, _ = sb("small", [128, 64], F32)
    epsc, _ = sb("epsc", [128, 1], F32)
    PH = off[0]

    o = PH
    KT = []
    for x_ in range(2):
        t_, o = sb(f"KT{x_}", [128, T], BF16, at=o)
        KT.append(t_)
    Vb, o = sb("Vb", [128, 64 * 192], BF16, at=o)
    WINs = []
    for s_ in range(2):
        t_, o = sb(f"WIN{s_}", [128, 8, 384], BF16, at=o)
        WINs.append(t_)
    HNTc = []
    for s_ in range(2):
        t_, o = sb(f"HNTc{s_}", [128, 8, 512], BF16, at=o)
        HNTc.append(t_)
    QT = []
    for s_ in range(2):
        row = []
        for x_ in range(2):
            t_, o = sb(f"QT{s_}_{x_}", [128, 512], BF16, at=o)
            row.append(t_)
        QT.append(row)
    PT = []
    for s_ in range(4):
        t_, o = sb(f"PT{s_}", [128, 512], BF16, at=o)
        PT.append(t_)
    Zs = []
    for x_ in range(2):
        row = []
        for par in range(2):
            t_, o = sb(f"Zs{x_}_{par}", [128, 512], F32, at=o)
            row.append(t_)
        Zs.append(row)
    FT = []
    for i_ in range(4):
        t_, o = sb(f"FT{i_}", [128, 512], F32, at=o)
        FT.append(t_)
    FTo = []
    for i_ in range(2):
        t_, o = sb(f"FTo{i_}", [128, 512], F32, at=o)
        FTo.append(t_)
    FTz, o = sb("FTz", [128, 512], F32, at=o)
    OSB = [FT[2], FT[3]]
    Ocat = []
    for s_ in range(2):
        t_, o = sb(f"Ocat{s_}", [128, 512], BF16, at=o)
        Ocat.append(t_)
    gate_sb, o = sb("gate_sb", [128, 8, 32], F32, at=o)
    m8, o = sb("m8", [128, 8, 8], F32, at=o)
    selt, o = sb("selt", [128, 32], F32, at=o)
    NS = []
    for x_ in range(2):
        t_, o = sb(f"NS{x_}", [128, 4, 96], BF16, at=o)
        NS.append(t_)
    kmT, o = sb("kmT", [128, 32], BF16, at=o)
    km32, o = sb("km32", [128, 2], F32, at=o)
    STG, o = sb("STG", [128, 2048], F32, at=o)
    STG2, o = sb("STG2", [128, 2048], F32, at=o)
    P2END = o
    assert P2END <= 229376, P2END

    o = PH
    HNT, o = sb("HNT", [128, 8, NT], BF16, at=o)
    OT = HNT
    WO, o = sb("WO", [128, 8, D], BF16, at=o)
    W1e, W2e = [], []
    for s_ in range(2):
        t_, o = sb(f"W1e{s_}", [128, 8, 512], BF16, at=o)
        W1e.append(t_)
        t_, o = sb(f"W2e{s_}", [128, 4, D], BF16, at=o)
        W2e.append(t_)
    hnb = []
    for s_ in range(2):
        t_, o = sb(f"hnb{s_}", [128, D], BF16, at=o)
        hnb.append(t_)
    UT = []
    for s_ in range(2):
        t_, o = sb(f"UT{s_}", [128, 4, 512], BF16, at=o)
        UT.append(t_)
    RT = []
    for s_ in range(2):
        t_, o = sb(f"RT{s_}", [128, 512], BF16, at=o)
        RT.append(t_)
    YS = []
    for s_ in range(2):
        t_, o = sb(f"YS{s_}", [128, 512], F32, at=o)
        YS.append(t_)
    FNB, o = sb("FNB", [128, D], F32, at=o)
    YO = []
    for s_ in range(2):
        t_, o = sb(f"YO{s_}", [128, D], F32, at=o)
        YO.append(t_)
    print("SBUF plan: PH", PH, "P2END", P2END, "P3 end", o)
    assert o <= P2END - 2 * 8192, (o, P2END)
    YT = [STG, STG2]

    pb = [nc.alloc_psum_tensor(f"pb{i}", [128, 512], F32) for i in range(8)]
    psS = pb[0:4]
    psO = pb[4:6]
    psP = pb[6]
    psX = pb[7]
    psXb = psX[:, 256:512].bitcast(BF16)
    psZ = pb[7]
    psT = pb[6][:, :].bitcast(BF16)
    psU = pb[0:2]
    psY = pb[2:4]

    ctx = {}
    OTQ = os.environ.get("OTQ", "sp")

    def dma(eng, key, out, in_, reads, writes):
        return P.add(eng, lambda e: e.dma_start(out=out, in_=in_), reads=reads, writes=writes, dma=key)

    def allgather(name, in_ap, out_ap, reads, writes):
        return P.add("pool", lambda e: e.collective_compute(
            "AllGather", ALU.bypass, replica_groups=GROUPS, ins=[in_ap.opt()], outs=[out_ap.opt()]),
            reads=reads, writes=writes, cc=name)

    dma("sp", "c0", ident[:, :], ident_in, [], ["ident"])
    dma("sp", "c0", tri[:, :], tri_in, [], ["tri"])
    dma("sp", "c0", bsel[:, :], bsel_in, [], ["bsel"])
    dma("sp", "c0", btab[:, :], btab_in, [], ["btab"])
    dma("sp", "c0", augq1[:, :], augq1_in, [], ["augq1"])
    dma("sp", "c0", gains[:, :], gains_in, [], ["gains"])
    dma("sp", "c0", lamb[:, :], lamb_in, [], ["lamb"])
    P.add("dve", lambda e: e.memset(ones128[:, :], 1.0), writes=["ones128"])
    P.add("dve", lambda e: e.memset(onesbf[:, :], 1.0), writes=["onesbf"])
    P.add("dve", lambda e: e.memset(epsc[:, :], EPS), writes=["epsc"])
    P.add("dve", lambda e: e.memset(small[:, :], 0.0), writes=["small"])
    P.add("dve", lambda e: e.tensor_tensor(out=FT[0][:, 0:64], in0=lamb[:, 0:64], in1=lamb[:, 64:128], op=ALU.mult),
          reads=["lamb"], writes=["ft0"])
    P.add("dve", lambda e: e.tensor_tensor(out=FT[0][:, 64:128], in0=lamb[:, 128:192], in1=lamb[:, 192:256], op=ALU.mult),
          reads=["lamb"], writes=["ft0b"])
    P.add("dve", lambda e: e.tensor_reduce(out=small[:, 34:36], in_=FT[0][:, 0:128].rearrange("p (a b) -> p a b", a=2),
                                           axis=AX.X, op=ALU.add),
          reads=["ft0", "ft0b", "small"], writes=["lam_s"])
    P.add("act", lambda e: e.activation(out=small[:, 36:38], in_=small[:, 34:36], func=AF.Exp),
          reads=["lam_s"], writes=["lam_e"])
    P.add("dve", lambda e: e.tensor_tensor(out=small[:, 38:39], in0=small[:, 37:38], in1=small[:, 36:37], op=ALU.subtract),
          reads=["lam_e"], writes=["lam_d"])
    P.add("dve", lambda e: e.tensor_scalar(out=small[:, 32:33], in0=small[:, 38:39], scalar1=-0.2, scalar2=None, op0=ALU.add),
          reads=["lam_d"], writes=["neglam"])
    P.add("dve", lambda e: e.tensor_scalar(out=small[:, 33:34], in0=gains[:, 32:33], scalar1=0.8, scalar2=None, op0=ALU.mult),
          reads=["gains", "small"], writes=["sub08"])
    for t in range(16):
        dma("sp", "xload", H[:, t, :], x_in[t * 128:(t + 1) * 128, :], [], [("H", t, 0), ("H", t, 1)])


    def rms_tile(t, slot):
        P.add("act", lambda e: e.activation(out=hnb[slot][:, :], in_=H[:, t, :], func=AF.Square,
                                            accum_out=small[:, t:t + 1]),
              reads=[("H", t, 0), ("H", t, 1), "small"], writes=[("hnb", slot), ("ssq", t)])
        P.add("act", lambda e: e.activation(out=small[:, 16 + t:17 + t], in_=small[:, t:t + 1], func=AF.Ln,
                                            bias=epsc[:, 0:1], scale=1.0 / D),
              reads=[("ssq", t), "epsc"], writes=[("rstd", t)])
        P.add("act", lambda e: e.activation(out=small[:, 16 + t:17 + t], in_=small[:, 16 + t:17 + t], func=AF.Exp, scale=-0.5),
              reads=[("rstd", t)], writes=[("rstd", t)])

    def norm_a(t):
        slot = t % 2
        P.add("dve", lambda e: e.memset(small[:, t:t + 1], 0.0), reads=[("ssq", t)], writes=[("ssq", t)])
        rms_tile(t, slot)
        P.add("dve", lambda e: e.tensor_scalar(out=hnb[slot][:, :], in0=H[:, t, :], scalar1=small[:, 16 + t:17 + t],
                                               scalar2=None, op0=ALU.mult),
              reads=[("H", t, 0), ("H", t, 1), ("rstd", t)], writes=[("hnb", slot)])

    def norm_b(t):
        slot = t % 2
        for kc in range(8):
            P.add("pe", lambda e, kc=kc: e.transpose(out=psT[:, kc * 128:(kc + 1) * 128],
                                                     in_=hnb[slot][:, kc * 128:(kc + 1) * 128], identity=ident[:, :]),
                  reads=[("hnb", slot), "ident"], writes=["psT"])
        P.add("dve", lambda e: e.tensor_copy(out=HNT[:, :, t * 128:(t + 1) * 128],
                                             in_=psT.rearrange("p (k q) -> p k q", k=8)),
              reads=["psT"], writes=[("HNT", t)])

    def norm_all(after_b=None):
        norm_a(0)
        for t in range(16):
            if t + 1 < 16:
                norm_a(t + 1)
            norm_b(t)
            if after_b is not None:
                after_b(t)

    def phase1(l):
        def after_b(t):
            if t % 4 == 3:
                i = t // 4
                dma("sp", f"htst{i}", HTin[l][i].ap().rearrange("(kc p) t -> p kc t", p=128),
                    HNT[:, :, i * 512:(i + 1) * 512], [("HNT", tt) for tt in range(4 * i, 4 * i + 4)],
                    [("HTin", l, i)])
                allgather(f"agh{l}_{i}", HTin[l][i].ap(), HT[l][i].ap(), [("HTin", l, i)], [("HT", l, i)])
        norm_all(after_b)

    def load_cast_win(l, p):
        wsrc = win_in[l * 2 + p].rearrange("(kc p) n -> p kc n", p=128)
        for hf in range(2):
            stg = (STG if hf == 0 else STG2)[:, 0:4 * 384].rearrange("p (k n) -> p k n", k=4)
            dma("sp", f"stg{hf}", stg, wsrc[:, hf * 4:hf * 4 + 4, :], [], [("STG", hf)])
            for k4 in range(4):
                kc = hf * 4 + k4
                P.add("act", lambda e, stg=stg, k4=k4, kc=kc: e.activation(
                    out=WINs[p][:, kc, :], in_=stg[:, k4, :], func=AF.Copy, scale=gains[:, l * 8 + kc:l * 8 + kc + 1]),
                    reads=[("STG", hf), "gains"], writes=[("WIN", p, kc)])

    def phase2(l):
        V0 = Vb[:, 0:64 * 128].rearrange("p (t c) -> p t c", c=128)
        V1 = Vb[:, :].rearrange("p (t c) -> p t c", c=192)
        dma("sp", "kaug0", KT[0][64:96, :], kaug_in[l], [], [("KTaug", 0)])
        dma("sp", "kaug1", KT[1][0:32, :], kaug_in[l], [], [("KTaug", 1)])
        P.add("dve", lambda e: e.memset(KT[1][32:64, :], 0.0), writes=[("KTz", 1)])
        for s_ in range(2):
            for x_ in range(2):
                P.add("dve", lambda e, s_=s_, x_=x_: e.memset(QT[s_][x_][:, :], 0.0), writes=[("QT", s_, x_), ("QTaug", s_, x_)])
        if l == 1:
            P.add("dve", lambda e: e.memset(V1[:, :, 64:128], 0.0), writes=["Vall"])
            P.add("dve", lambda e: e.memset(V1[:, :, 64:65], 1.0), reads=["Vall"], writes=["Vall"])
            for x_ in range(2):
                P.add("dve", lambda e, x_=x_: e.memset(NS[x_][:, :, :], 0.0), writes=[("NS", x_)])
                P.add("dve", lambda e, x_=x_: e.memset(OSB[x_][:, :], 0.0), writes=[("OSB", x_), ("FT", 2 + x_)])
            P.add("dve", lambda e: e.memset(kmT[:, :], 0.0), writes=["kmT"])
        load_cast_win(l, 0)
        for p in range(2):
            if l == 0:
                for s_ in range(2):
                    dma("sp", f"qaug{s_}0", QT[s_][0][64:96, :], qaug0_in[p], [("QTaug", s_, 0)], [("QTaug", s_, 0)])
                    dma("sp", f"qaug{s_}1", QT[s_][1][0:32, :], qaug0_in[p], [("QTaug", s_, 1)], [("QTaug", s_, 1)])
            for _ in chunk_proj(l, p, 0, V0, V1):
                pass
            fin = None
            for g in range(16):
                gen = chunk_proj(l, p, g + 1, V0, V1) if g + 1 < 16 else None
                if g == 8 and p == 0:
                    load_cast_win(l, 1)
                chunk_attn(l, p, g, V0, V1, gen, fin)
                fin = fin_gen(l, p, g)
                next(fin)
            for _ in fin:
                pass

    bidx = [0]
    NSLOT = 4

    def chunk_proj(l, p, g, V0, V1):
        s = g % 2
        hsrc = HT[l][g // 4][(g % 4) * D:(g % 4 + 1) * D, :].rearrange("(kc p) t -> p kc t", p=128)
        dma("sp", f"hntc{s}", HNTc[s][:, :, :], hsrc, [("HT", l, g // 4)], [("HNTc", s)])
        WIN = WINs[p]
        win_r = [("WIN", p, kc) for kc in range(8)]
        for kc in range(8):
            P.add("pe", lambda e, kc=kc: e.matmul(psP[:, :], lhsT=WIN[:, kc, 0:128], rhs=HNTc[s][:, kc, :],
                                                  start=(kc == 0), stop=(kc == 7)),
                  reads=[("HNTc", s)] + win_r, writes=["psP"])
        P.add("dve", lambda e: e.tensor_copy(out=QT[s][0][0:64, :], in_=psP[0:64, :]), reads=["psP"], writes=[("QT", s, 0)])
        P.add("dve", lambda e: e.tensor_copy(out=QT[s][1][64:128, :], in_=psP[64:128, :]), reads=["psP"], writes=[("QT", s, 1)])
        yield
        for kc in range(8):
            P.add("pe", lambda e, kc=kc: e.matmul(psP[:, :], lhsT=WIN[:, kc, 128:256], rhs=HNTc[s][:, kc, :],
                                                  start=(kc == 0), stop=(kc == 7)),
                  reads=[("HNTc", s)] + win_r, writes=["psP"])
        P.add("dve", lambda e: e.tensor_copy(out=KT[0][0:64, g * 512:(g + 1) * 512], in_=psP[0:64, :]),
              reads=["psP"], writes=[("KT", 0, g)])
        P.add("dve", lambda e: e.tensor_copy(out=KT[1][64:128, g * 512:(g + 1) * 512], in_=psP[64:128, :]),
              reads=["psP"], writes=[("KT", 1, g)])
        if l == 1:
            P.add("dve", lambda e: e.tensor_reduce(out=km32[:, :], in_=psP[:, :].rearrange("p (a b) -> p a b", a=2),
                                                   axis=AX.X, op=ALU.add), reads=["psP"], writes=["km32"])
            P.add("dve", lambda e: e.tensor_scalar(out=kmT[:, 2 * g:2 * g + 2], in0=km32[:, :], scalar1=1.0 / 256,
                                                   scalar2=None, op0=ALU.mult), reads=["km32"], writes=["kmT"])
        yield
        for tt in range(4):
            for kc in range(8):
                P.add("pe", lambda e, kc=kc, tt=tt: e.matmul(psP[:, tt * 128:(tt + 1) * 128],
                                                             lhsT=HNTc[s][:, kc, tt * 128:(tt + 1) * 128],
                                                             rhs=WIN[:, kc, 256:384], start=(kc == 0), stop=(kc == 7)),
                      reads=[("HNTc", s)] + win_r, writes=["psP"])
        psP4 = psP[:, :].rearrange("p (t c) -> p t c", t=4)
        if l == 0:
            P.add("dve", lambda e: e.tensor_copy(out=V0[:, 4 * g:4 * g + 4, :], in_=psP4), reads=["psP"], writes=[("V", g)])
        else:
            P.add("dve", lambda e: e.tensor_copy(out=V1[:, 4 * g:4 * g + 4, 0:64], in_=psP4[:, :, 0:64]),
                  reads=["psP", "Vall"], writes=[("V", g)])
            P.add("dve", lambda e: e.tensor_copy(out=V1[:, 4 * g:4 * g + 4, 128:192], in_=psP4[:, :, 64:128]),
                  reads=["psP", "Vall"], writes=[("Vb_", g)])
        yield
        if l == 1:
            yield from moba_gate(p, g, s)
        yield

    def chunk_attn(l, p, g, V0, V1, gen=None, fin=None):
        s = g % 2
        nk = 4 * g + 4
        if l == 1 and os.environ.get("SKIP_ATTN"):
            nk = 0
        kt0 = max(0, 4 * g - 16) if p == 0 else 0
        items = [(kt, x_) for kt in range(kt0, nk) for x_ in range(2)]
        if l == 0:
            for za, zeng in ((0, "dve"), (1, "pool")):
                P.add(zeng, lambda e, za=za: e.memset(Zs[za][g % 2][:, :], 0.0), writes=[("Zs", za, g % 2)])
        LOOK = 3
        slots = {}

        def emit_s(kt, x_):
            j = kt - 4 * g
            c0 = 128 * j if j >= 0 else 0
            b = bidx[0] % NSLOT
            bidx[0] += 1
            slots[(kt, x_)] = b
            kx = 96 if x_ == 0 else 128
            kr = [("KT", x_, kt // 4), ("KTaug", x_), ("QT", s, x_), ("QTaug", s, x_)] + ([("KTz", 1)] if x_ == 1 else [])
            ks = slice(kt * 128, (kt + 1) * 128)
            if j >= 0:
                P.add("pe", lambda e: e.matmul(
                    psS[b][:, c0:c0 + 128], lhsT=KT[x_][0:kx, ks], rhs=QT[s][x_][0:kx, c0:c0 + 128],
                    start=True, stop=False), reads=kr, writes=[("psS", b)])
                P.add("pe", lambda e: e.matmul(
                    psS[b][:, c0:c0 + 128], lhsT=ident[:, :], rhs=tri[:, :], start=False, stop=True),
                    reads=["ident", "tri"], writes=[("psS", b)])
                if c0 + 128 < 512:
                    P.add("pe", lambda e: e.matmul(
                        psS[b][:, c0 + 128:512], lhsT=KT[x_][0:kx, ks], rhs=QT[s][x_][0:kx, c0 + 128:512],
                        start=True, stop=True), reads=kr, writes=[("psS", b)])
            else:
                P.add("pe", lambda e: e.matmul(
                    psS[b][:, :], lhsT=KT[x_][0:kx, ks], rhs=QT[s][x_][0:kx, :], start=True, stop=True),
                    reads=kr, writes=[("psS", b)])
            col = (((l * 2 + p) * 2 + x_) * NDC) + (4 * g - kt + 3)
            P.add("act", lambda e: e.activation(
                out=PT[b][:, c0:512], in_=psS[b][:, c0:512], func=AF.Exp, bias=btab[:, col:col + 1], scale=SCALE),
                reads=[("psS", b), "btab"], writes=[("PT", b)])
            if l == 0 and x_ == 0:
                za = kt % 2
                zeng = "dve" if za == 0 else "pool"
                zt = Zs[za][g % 2]
                zk = ("Zs", za, g % 2)
                P.add(zeng, lambda e: e.tensor_tensor(
                    out=zt[:, c0:512], in0=zt[:, c0:512], in1=PT[b][:, c0:512], op=ALU.add),
                    reads=[("PT", b), zk], writes=[zk])

        def emit_pv(kt, x_):
            j = kt - 4 * g
            c0 = 128 * j if j >= 0 else 0
            b = slots[(kt, x_)]
            if l == 0:
                vl = V0[:, kt, :]
                mo = 128
            else:
                vl = V1[:, kt, 0:65] if x_ == 0 else V1[:, kt, 64:192]
                mo = 65 if x_ == 0 else 128
            P.add("pe", lambda e: e.matmul(
                psO[x_][0:mo, c0:512], lhsT=vl, rhs=PT[b][:, c0:512], start=(kt == kt0), stop=(kt == nk - 1)),
                reads=[("PT", b), ("V", kt // 4), ("Vb_", kt // 4), "Vall"], writes=[("psO", x_)])
            if l == 0 and x_ == 1:
                P.add("pe", lambda e: e.matmul(
                    psZ[:, c0:512], lhsT=onesbf[:, :], rhs=PT[b][:, c0:512], start=(kt == kt0), stop=(kt == nk - 1)),
                    reads=[("PT", b), "onesbf"], writes=["psZ"])

        for n in range(len(items) + LOOK):
            if n < len(items):
                emit_s(*items[n])
            if n - LOOK >= 0:
                emit_pv(*items[n - LOOK])
            if n >= 4 and n % 3 == 1 and gen is not None:
                next(gen, None)
            if n >= 2 and n % 3 == 2 and fin is not None:
                next(fin, None)
        if fin is not None:
            for _ in fin:
                pass
        if gen is not None:
            for _ in gen:
                pass

    def fin_gen(l, p, g):
        s = g % 2
        oc = Ocat[s]
        zp = g % 2
        if l == 0:
            for x_ in range(2):
                P.add("dve", lambda e, x_=x_: e.tensor_copy(out=FTo[x_][:, :], in_=psO[x_][:, :]),
                      reads=[("psO", x_)], writes=[("FTo", x_)])
            P.add("dve", lambda e: e.tensor_copy(out=FTz[:, :], in_=psZ[:, :]), reads=["psZ"], writes=["FTz"])
            yield
            P.add("dve", lambda e: e.tensor_tensor(out=Zs[0][zp][:, :], in0=Zs[0][zp][:, :], in1=Zs[1][zp][:, :], op=ALU.add),
                  reads=[("Zs", 0, zp), ("Zs", 1, zp)], writes=[("Zs", 0, zp)])
            P.add("pe", lambda e: e.matmul(psP[:, :], lhsT=ones128[:, :], rhs=Zs[0][zp][:, :], start=True, stop=True),
                  reads=[("Zs", 0, zp), "ones128"], writes=["psP"])
            P.add("dve", lambda e: e.reciprocal(out=FT[0][:, :], in_=psP[:, :]), reads=["psP"], writes=[("FT", 0)])
            yield
            P.add("dve", lambda e: e.reciprocal(out=FT[1][:, :], in_=FTz[:, :]), reads=["FTz"], writes=[("FT", 1)])
            yield
            for x_ in range(2):
                P.add("dve", lambda e, x_=x_: e.tensor_tensor(out=FT[x_][:, :], in0=FTo[x_][:, :], in1=FT[x_][:, :], op=ALU.mult),
                      reads=[("FTo", x_), ("FT", x_)], writes=[("FT", x_)])
            P.add("dve", lambda e: e.scalar_tensor_tensor(out=FT[2][:, :], in0=FT[1][:, :], scalar=small[:, 32:33], in1=FT[0][:, :],
                                                          op0=ALU.mult, op1=ALU.add),
                  reads=[("FT", 0), ("FT", 1), "neglam"], writes=[("FT", 2)])
            P.add("pool", lambda e: e.tensor_tensor(out=FT[3][:, :], in0=FT[2][:, :], in1=FT[2][:, :], op=ALU.mult),
                  reads=[("FT", 2)], writes=[("FT", 3)])
            yield
            P.add("pe", lambda e: e.matmul(psP[:, :], lhsT=ones128[:, :], rhs=FT[3][:, :], start=True, stop=True),
                  reads=[("FT", 3), "ones128"], writes=["psP"])
            P.add("act", lambda e: e.activation(out=FT[3][:, :], in_=psP[:, :], func=AF.Ln, bias=epsc[:, 0:1], scale=1.0 / 128),
                  reads=["psP", "epsc"], writes=[("FT", 3)])
            P.add("act", lambda e: e.activation(out=FT[3][:, :], in_=FT[3][:, :], func=AF.Exp, scale=-0.5),
                  reads=[("FT", 3)], writes=[("FT", 3)])
            yield
            P.add("dve", lambda e: e.tensor_tensor(out=oc[:, :], in0=FT[2][:, :], in1=FT[3][:, :], op=ALU.mult),
                  reads=[("FT", 2), ("FT", 3)], writes=[("Ocat", s)])
        else:
            for x_ in range(2):
                mo = 65 if x_ == 0 else 128
                P.add("dve", lambda e, x_=x_, mo=mo: e.tensor_copy(out=OSB[x_][0:mo, :], in_=psO[x_][0:mo, :]),
                      reads=[("psO", x_)], writes=[("OSB", x_)])
            yield
            for x_ in range(2):
                rows = slice(0, 64) if x_ == 0 else slice(64, 128)
                P.add("pe", lambda e, x_=x_: e.matmul(psX[:, :], lhsT=bsel[:, x_ * 128:(x_ + 1) * 128], rhs=OSB[x_][:, :],
                                                      start=True, stop=True), reads=[("OSB", x_), "bsel"], writes=["psX"])
                P.add("dve", lambda e, x_=x_, rows=rows: e.reciprocal(out=FT[x_][rows, :], in_=psX[rows, :]),
                      reads=["psX"], writes=[("FT", x_)])
                yield
                P.add("dve", lambda e, x_=x_, rows=rows: e.tensor_tensor(out=oc[rows, :], in0=OSB[x_][rows, :], in1=FT[x_][rows, :],
                                                                        op=ALU.mult),
                      reads=[("OSB", x_), ("FT", x_)], writes=[("Ocat", s, x_)])
        dma("sp", f"ost{s}", OSin[l][p][g // 4][:, (g % 4) * 512:(g % 4 + 1) * 512], oc[:, :],
            [("Ocat", s), ("Ocat", s, 0), ("Ocat", s, 1)], [("OSin", l, p, g // 4)])
        yield
        if g % 4 == 3:
            q = g // 4
            allgather(f"ago{l}_{p}_{q}", OSin[l][p][q].ap(), OS[l][p][q * 512:(q + 1) * 512, :],
                      [("OSin", l, p, q)], [("OS", l, p, q)])
        yield

    def moba_gate(p, g, s):
        gbank = [psX, psP]
        gkey = ["psX", "psP"]
        for x_ in range(2):
            rows = slice(0, 64) if x_ == 0 else slice(64, 128)
            for qt in range(4):
                P.add("pe", lambda e, x_=x_, qt=qt, rows=rows: e.matmul(
                    gbank[x_][:, qt * 32:(qt + 1) * 32], lhsT=QT[s][x_][rows, qt * 128:(qt + 1) * 128], rhs=kmT[rows, :],
                    start=True, stop=True), reads=[("QT", s, x_), "kmT"], writes=[gkey[x_]])
        P.add("dve", lambda e: e.memset(gate_sb[:, :, :], -1e30), writes=["gate"])
        gsv = gate_sb[:, :, :].rearrange("p (x h q) n -> p x h q n", x=2, h=2)
        for hf in range(2):
            jb = 2 * g + hf
            if jb > 0:
                for x_ in range(2):
                    psg = gbank[x_][:, 0:128].rearrange("p (h q n) -> p h q n", h=2, q=2)
                    P.add("dve", lambda e, hf=hf, jb=jb, x_=x_, psg=psg: e.tensor_copy(out=gsv[:, x_, hf, :, 0:jb], in_=psg[:, hf, :, 0:jb]),
                          reads=[gkey[x_], "gate"], writes=["gate"])
        for x_ in range(2):
            base = 64 if x_ == 0 else 0
            for qt in range(4):
                xq = x_ * 4 + qt
                jb = 2 * g + qt // 2
                ai = (p * 2 + x_) * 4 + qt
                P.add("dve", lambda e, xq=xq: e.max(out=m8[:, xq, :], in_=gate_sb[:, xq, :]), reads=["gate"], writes=[("m8", xq)])
                P.add("dve", lambda e, xq=xq: e.tensor_scalar(out=selt[:, :], in0=gate_sb[:, xq, :], scalar1=m8[:, xq, 2:3],
                                                              scalar2=None, op0=ALU.is_ge),
                      reads=["gate", ("m8", xq)], writes=["selt"])
                P.add("dve", lambda e, x_=x_, qt=qt, base=base, ai=ai: e.tensor_scalar(
                    out=NS[x_][:, qt, base:base + 32], in0=selt[:, :], scalar1=-NEG, scalar2=augq1[:, ai:ai + 1],
                    op0=ALU.mult, op1=ALU.add), reads=["selt", "augq1", ("NS", x_)], writes=[("NS", x_)])
                P.add("dve", lambda e, x_=x_, qt=qt, base=base, ai=ai, jb=jb: e.tensor_scalar(
                    out=NS[x_][:, qt, base + jb:base + jb + 1], in0=augq1[:, ai:ai + 1], scalar1=-NEG, scalar2=None,
                    op0=ALU.add), reads=["augq1", ("NS", x_)], writes=[("NS", x_)])
        for _ in range(4):
            yield
        for x_ in range(2):
            rows = slice(64, 96) if x_ == 0 else slice(0, 32)
            yield
            for qt in range(4):
                P.add("pe", lambda e, x_=x_, qt=qt: e.transpose(out=psXb[0:96, qt * 128:(qt + 1) * 128],
                                                                in_=NS[x_][:, qt, :], identity=ident[:, :]),
                      reads=[("NS", x_), "ident"], writes=["psX"])
            P.add("dve", lambda e, x_=x_, rows=rows: e.tensor_copy(out=QT[s][x_][rows, :], in_=psXb[rows, :]),
                  reads=["psX"], writes=[("QTaug", s, x_)])

    def phase3(l, do_ffn=True, tail=None):
        def rank_of(e):
            if "rank" not in ctx:
                ctx["rank"] = e.partition_id() % 4
            return ctx["rank"]
        for q in range(4):
            for kc in range(8):
                src, p = kc // 2, kc % 2
                P.add(OTQ, lambda e, kc=kc, src=src, p=p, q=q: e.dma_start(
                    out=OT[:, kc, q * 512:(q + 1) * 512],
                    in_=OS[l][p][q * 512 + src * 128:q * 512 + src * 128 + 128, bass.ds(rank_of(e) * 512, 512)]),
                    reads=[("OS", l, p, q)], writes=[("OT", kc, q)], dma="otld")
        wsrc = wout_in[l].rearrange("(kc p) n -> p kc n", p=128)
        for i in range(4):
            stg = (STG if i % 2 == 0 else STG2)[:, :].rearrange("p (k n) -> p k n", k=2)
            dma("sp", f"stg{i % 2}", stg, wsrc[:, 2 * i:2 * i + 2, :], [], [("STG", i % 2)])
            if l == 0:
                P.add("act", lambda e, stg=stg, i=i: e.activation(out=WO[:, 2 * i:2 * i + 2, :], in_=stg, func=AF.Copy,
                                                                  scale=small[:, 33:34]),
                      reads=[("STG", i % 2), "sub08"], writes=[("WO", i)])
            else:
                P.add("act", lambda e, stg=stg, i=i: e.copy(out=WO[:, 2 * i:2 * i + 2, :], in_=stg),
                      reads=[("STG", i % 2)], writes=[("WO", i)])
        yb = 0
        for t in range(16):
            for n2 in range(2):
                b = yb % 2
                yb += 1
                for kc in range(8):
                    wk = kc // 2 + 4 * (kc % 2)
                    P.add("pe", lambda e, kc=kc, wk=wk, b=b, t=t, n2=n2: e.matmul(
                        psY[b][:, :], lhsT=OT[:, kc, t * 128:(t + 1) * 128], rhs=WO[:, wk, n2 * 512:(n2 + 1) * 512],
                        start=(kc == 0), stop=(kc == 7)), reads=[("OT", k_, t // 4) for k_ in range(8)] + [("WO", wk // 2)], writes=[("psY", b)])
                P.add("dve", lambda e, b=b, t=t, n2=n2: e.tensor_tensor(
                    out=H[:, t, n2 * 512:(n2 + 1) * 512], in0=H[:, t, n2 * 512:(n2 + 1) * 512], in1=psY[b][:, :], op=ALU.add),
                    reads=[("psY", b), ("H", t, n2)], writes=[("H", t, n2)])
        if not do_ffn:
            return
        P.barrier()
        norm_all()
        w1src = wff1_in[l].rearrange("(kc p) n -> p kc n", p=128)
        w2src = wff2_in[l].rearrange("(fc p) n -> p fc n", p=128)
        ub = 0
        for ei in range(8):
            ws = ei % 2
            for hf in range(2):
                stg = (STG if hf == 0 else STG2)[:, :].rearrange("p (k n) -> p k n", k=4)
                dma("sp", f"stg{hf}", stg, w1src[:, hf * 4:hf * 4 + 4, ei * 512:(ei + 1) * 512], [], [("STG", hf)])
                for k4 in range(4):
                    kc = hf * 4 + k4
                    P.add("act", lambda e, stg=stg, k4=k4, kc=kc, ws=ws: e.activation(
                        out=W1e[ws][:, kc, :], in_=stg[:, k4, :], func=AF.Copy, scale=gains[:, 16 + l * 8 + kc:17 + l * 8 + kc]),
                        reads=[("STG", hf), "gains"], writes=[("W1e", ws)])
            for hf in range(2):
                stg = (STG if hf == 0 else STG2)[:, :].rearrange("p (k n) -> p k n", k=2)
                dma("sp", f"stg{hf}", stg, w2src[:, ei * 4 + hf * 2:ei * 4 + hf * 2 + 2, :], [], [("STG", hf)])
                P.add("act", lambda e, stg=stg, hf=hf, ws=ws: e.copy(out=W2e[ws][:, 2 * hf:2 * hf + 2, :], in_=stg),
                      reads=[("STG", hf)], writes=[("W2e", ws)])
            for c in range(4):
                us = (ei * 4 + c) % 2
                for fc in range(4):
                    b = ub % 2
                    ub += 1
                    for kc in range(8):
                        P.add("pe", lambda e, kc=kc, b=b, fc=fc, c=c, ws=ws: e.matmul(
                            psU[b][:, :], lhsT=W1e[ws][:, kc, fc * 128:(fc + 1) * 128], rhs=HNT[:, kc, c * 512:(c + 1) * 512],
                            start=(kc == 0), stop=(kc == 7)),
                            reads=[("W1e", ws)] + [("HNT", tt) for tt in range(4 * c, 4 * c + 4)], writes=[("psU", b)])
                    P.add("act", lambda e, b=b: e.activation(out=RT[b][:, :], in_=psU[b][:, :], func=AF.Relu),
                          reads=[("psU", b)], writes=[("RT", b)])
                    P.add("dve", lambda e, b=b, us=us, fc=fc: e.tensor_tensor(out=UT[us][:, fc, :], in0=RT[b][:, :], in1=RT[b][:, :],
                                                                             op=ALU.mult),
                          reads=[("RT", b)], writes=[("UT", us, fc)])
                for tt in range(4):
                    t = 4 * c + tt
                    for n2 in range(2):
                        b = yb % 2
                        yb += 1
                        for fc in range(4):
                            P.add("pe", lambda e, fc=fc, b=b, tt=tt, n2=n2, us=us, ws=ws: e.matmul(
                                psY[b][:, :], lhsT=UT[us][:, fc, tt * 128:(tt + 1) * 128], rhs=W2e[ws][:, fc, n2 * 512:(n2 + 1) * 512],
                                start=(fc == 0), stop=(fc == 3)),
                                reads=[("UT", us, fc), ("W2e", ws)], writes=[("psY", b)])
                        if n2 == 0:
                            P.add("dve", lambda e, b=b, t=t, n2=n2: e.tensor_tensor(
                                out=H[:, t, n2 * 512:(n2 + 1) * 512], in0=H[:, t, n2 * 512:(n2 + 1) * 512], in1=psY[b][:, :], op=ALU.add),
                                reads=[("psY", b), ("H", t, n2)], writes=[("H", t, n2)])
                        else:
                            ys = tt % 2
                            P.add("act", lambda e, b=b, ys=ys: e.copy(out=YS[ys][:, :], in_=psY[b][:, :]),
                                  reads=[("psY", b)], writes=[("YS", ys)])
                            P.add("pool", lambda e, t=t, n2=n2, ys=ys: e.tensor_tensor(
                                out=H[:, t, n2 * 512:(n2 + 1) * 512], in0=H[:, t, n2 * 512:(n2 + 1) * 512], in1=YS[ys][:, :], op=ALU.add),
                                reads=[("YS", ys), ("H", t, n2)], writes=[("H", t, n2)])
                if ei == 7 and tail is not None:
                    tail(c)

    def final_out(with_norm=True, tiles=range(16), load_fnb=True):
        if with_norm and load_fnb:
            dma("sp", "fnb", FNB[:, :], fnorm_in, [], ["fnb"])
        for t in tiles:
            slot = t % 2
            yt = YO[slot][:, :]
            if with_norm:
                P.add("dve", lambda e, t=t: e.memset(small[:, t:t + 1], 0.0), reads=[("ssq", t)], writes=[("ssq", t)])
                rms_tile(t, slot)
                P.add("dve", lambda e, t=t, yt=yt: e.scalar_tensor_tensor(
                    out=yt, in0=H[:, t, :], scalar=small[:, 16 + t:17 + t], in1=FNB[:, :], op0=ALU.mult, op1=ALU.mult),
                    reads=[("H", t, 0), ("H", t, 1), ("rstd", t), "fnb"], writes=[("yt", slot)])
            else:
                P.add("dve", lambda e, t=t, yt=yt: e.tensor_copy(out=yt, in_=H[:, t, :]),
                      reads=[("H", t, 0), ("H", t, 1)], writes=[("yt", slot)])
            dma("sp", f"yst{slot}", y_out[t * 128:(t + 1) * 128, :], yt, [("yt", slot)], [("yout", t)])

    def p1_tail(l):
        def tail(c):
            norm_a(4 * c)
            for t in range(4 * c, 4 * c + 4):
                if t + 1 < 4 * c + 4:
                    norm_a(t + 1)
                norm_b(t)
            dma("sp", f"htst{c}", HTin[l][c].ap().rearrange("(kc p) t -> p kc t", p=128),
                HNT[:, :, c * 512:(c + 1) * 512], [("HNT", tt) for tt in range(4 * c, 4 * c + 4)],
                [("HTin", l, c)])
            allgather(f"agh{l}_{c}", HTin[l][c].ap(), HT[l][c].ap(), [("HTin", l, c)], [("HT", l, c)])
        return tail

    def out_tail():
        def tail(c):
            final_out(True, tiles=range(4 * c, 4 * c + 4), load_fnb=(c == 0))
        return tail

    phase1(0)
    P.barrier(soft_cc=True)
    if stage != "F0":
        phase2(0)
        P.barrier(soft_cc=True)
    if stage == "F0":
        phase3(0)
        P.barrier()
        final_out(with_norm=False)
    elif stage == "A0":
        phase3(0, do_ffn=False)
        P.barrier()
        final_out(with_norm=False)
    elif stage == "L0":
        phase3(0)
        P.barrier()
        final_out(with_norm=False)
    else:
        phase3(0, tail=p1_tail(1))
        P.barrier(soft_cc=True)
        phase2(1)
        P.barrier(soft_cc=True)
        if stage == "A1":
            phase3(1, do_ffn=False)
            P.barrier()
            final_out(with_norm=False)
        else:
            phase3(1, tail=out_tail())
    P.barrier()
    P.finalize()

    with ExitStack() as st:
        sems = {}
        for e in Prog.ENGS:
            sems[("eng", e)] = st.enter_context(nc.semaphore(f"s_{e}"))
        for k in P.dma_keys:
            sems[("dma", k)] = st.enter_context(nc.semaphore(f"d_{k}"))
        for k in P.cc_keys:
            sems[("cc", k)] = st.enter_context(nc.semaphore(f"c_{k}"))
        block = st.enter_context(nc.Block())

        @block.sync
        def _(e):
            P.emit("sp", e, sems)

        @block.scalar
        def _(e):
            P.emit("act", e, sems)

        @block.vector
        def _(e):
            P.emit("dve", e, sems)

        @block.gpsimd
        def _(e):
            P.emit("pool", e, sems)

        @block.tensor
        def _(e):
            P.emit("pe", e, sems)
    return nc


def _slopes(n):
    return np.array([2.0 ** (-8.0 * (h + 1) / n) for h in range(n)], dtype=np.float64)


def _consts(r):
    bf = ml_dtypes.bfloat16
    s0 = _slopes(8)
    s1 = _slopes(16)
    ident = np.eye(128, dtype=np.float32).astype(bf)
    ki = np.arange(128)[:, None]
    qi = np.arange(128)[None, :]
    tri = np.where(qi < ki, NEG, 0.0).astype(np.float32).astype(bf)
    kaug = np.zeros((2, 32, T), np.float32)
    kaug[0, 0, :] = 1.0
    kaug[1, np.arange(T) // 256, np.arange(T)] = 1.0
    kaug = kaug.astype(bf)
    qaug0 = np.zeros((2, 32, 512), np.float32)
    btab = np.zeros((128, 2, 2, 2, NDC), np.float64)
    augq1 = np.zeros((128, 2, 2, 4), np.float64)
    pp = np.arange(128, dtype=np.float64)
    dc = np.arange(NDC, dtype=np.float64) - 3.0
    for p in range(2):
        s = s0[r + 4 * p]
        qaug0[p, 0, :] = -s * np.arange(512) / SCALE
        for x in range(2):
            btab[:, 0, p, x, :] = s * (pp[:, None] - 128.0 * dc[None, :])
            s_ = s1[2 * (r + 4 * p) + x]
            btab[:, 1, p, x, :] = s_ * (pp[:, None] - 128.0 * dc[None, :])
            for qt in range(4):
                augq1[:, p, x, qt] = -s_ * (qt * 128 + pp) / SCALE + NEG
    bsel = np.zeros((128, 2, 128), np.float32)
    bsel[64, 0, 0:64] = 1.0
    bsel[0, 1, 64:128] = 1.0
    return dict(ident=ident, trimask=tri, kaug=kaug, qaug0=qaug0.astype(bf),
                biastab=btab.reshape(128, -1).astype(np.float32), augq1=augq1.reshape(128, -1).astype(np.float32),
                bsel=bsel.reshape(128, 256))


_NC_CACHE = {}


def kernel(x, attn_norm, w_in, w_out, diff_lambda, diff_subln, mlp_norm, w_ff1, w_ff2, final_norm, _stage="full"):
    x = np.asarray(x, np.float32)
    attn_norm = np.asarray(attn_norm, np.float32)
    w_in = np.asarray(w_in, np.float32)
    w_out = np.ascontiguousarray(np.asarray(w_out, np.float32))
    diff_lambda = np.asarray(diff_lambda, np.float32)
    diff_subln = np.asarray(diff_subln, np.float32)
    mlp_norm = np.asarray(mlp_norm, np.float32)
    w_ff1 = np.ascontiguousarray(np.asarray(w_ff1, np.float32))
    w_ff2 = np.ascontiguousarray(np.asarray(w_ff2, np.float32))
    final_norm = np.asarray(final_norm, np.float32)

    if _stage not in _NC_CACHE:
        _NC_CACHE[_stage] = build_program(_stage)
    nc = _NC_CACHE[_stage]

    gains = np.zeros((128, 64), np.float32)
    for l in range(2):
        gains[:, l * 8:(l + 1) * 8] = attn_norm[l].reshape(8, 128).T
        gains[:, 16 + l * 8:16 + (l + 1) * 8] = mlp_norm[l].reshape(8, 128).T
    gains[:, 32] = diff_subln[0]
    fnb = np.ascontiguousarray(np.broadcast_to(final_norm[None, :], (128, D)))
    lamb = np.ascontiguousarray(np.broadcast_to(diff_lambda[0].reshape(1, 256), (128, 256)))

    in_maps = []
    for c in range(8):
        b, r = c // 4, c % 4
        win = np.zeros((4, D, 384), np.float32)
        for l in range(2):
            for p in range(2):
                c0 = 128 * (r + 4 * p)
                for j in range(3):
                    win[l * 2 + p, :, j * 128:(j + 1) * 128] = w_in[l, :, j * D + c0:j * D + c0 + 128]
        xr = x[b].reshape(4, 4, 512, D)[:, r].reshape(NT, D)
        m = dict(x=np.ascontiguousarray(xr), win=win, wout=w_out, wff1=w_ff1, wff2=w_ff2,
                 gains=gains, fnormb=fnb, lamb=lamb)
        m.update(_consts(r))
        in_maps.append(m)
    res = run_bass_kernel_spmd(nc, in_maps, core_ids=list(range(8)))
    out = np.zeros((2, T, D), np.float32)
    for c in range(8):
        b, r = c // 4, c % 4
        out[b].reshape(4, 4, 512, D)[:, r] = np.asarray(res.results[c]["y"], dtype=np.float32).reshape(4, 512, D)
    return out
```

```python
import math
import os
from contextlib import ExitStack

import numpy as np
import ml_dtypes
import concourse.bass as bass
import concourse.mybir as mybir
from concourse.bass_utils import run_bass_kernel_spmd

F32 = mybir.dt.float32
BF16 = mybir.dt.bfloat16
ALU = mybir.AluOpType
AF = mybir.ActivationFunctionType
AX = mybir.AxisListType

D = 1024
T = 8192
NT = 2048
DFF = 4096
EPS = 1e-6
SCALE = 0.125
NEG = -30000.0
NDC = 67
GROUPS = [[0, 1, 2, 3], [4, 5, 6, 7]]


class Op:
    __slots__ = ("eng", "fn", "deps", "dma", "cc", "signal", "count", "idx")

    def __init__(self, eng, fn, dma=None, cc=None):
        self.eng = eng
        self.fn = fn
        self.deps = set()
        self.dma = dma
        self.cc = cc
        self.signal = False
        self.count = 0
        self.idx = 0


class Prog:
    ENGS = ["sp", "act", "dve", "pool", "pe"]

    def __init__(self):
        self.ops = {e: [] for e in self.ENGS}
        self.last_w = {}
        self.readers = {}
        self.dma_keys = {}
        self.cc_keys = []
        self.n = 0

    def add(self, eng, fn, reads=(), writes=(), dma=None, cc=None):
        op = Op(eng, fn, dma=dma, cc=cc)
        op.idx = self.n
        self.n += 1
        deps = set()
        for r in reads:
            w = self.last_w.get(r)
            if w is not None:
                deps.add(w)
        for w_ in writes:
            w = self.last_w.get(w_)
            if w is not None:
                deps.add(w)
            for rd in self.readers.get(w_, ()):
                deps.add(rd)
        deps.discard(op)
        if eng == "pe":
            deps = {d for d in deps if not (d.eng == "pe" and d.dma is None and d.cc is None)}
        op.deps = deps
        for r in reads:
            self.readers.setdefault(r, []).append(op)
        for w_ in writes:
            self.last_w[w_] = op
            self.readers[w_] = []
        self.ops[eng].append(op)
        if dma is not None:
            self.dma_keys.setdefault(dma, 0)
        if cc is not None:
            self.cc_keys.append(cc)
        return op

    def barrier(self, soft_cc=False):
        lasts = []
        for e in self.ENGS:
            for op in reversed(self.ops[e]):
                if op.dma is None and op.cc is None and op.fn is not None:
                    lasts.append(op)
                    break
        seen = {}
        for e in self.ENGS:
            for op in self.ops[e]:
                if op.dma is not None:
                    seen[op.dma] = op
                if op.cc is not None and not soft_cc:
                    seen[("cc", op.cc)] = op
        lasts += list(seen.values())
        for e in self.ENGS:
            b = Op(e, None)
            b.idx = self.n
            self.n += 1
            b.deps = set(lasts)
            self.ops[e].append(b)
        self.last_w = {k: v for k, v in self.last_w.items() if soft_cc and v.cc is not None}
        self.readers = {}

    def finalize(self):
        for e in self.ENGS:
            for op in self.ops[e]:
                for d in op.deps:
                    d.signal = True
        cnt = {e: 0 for e in self.ENGS}
        dcnt = {k: 0 for k in self.dma_keys}
        for e in self.ENGS:
            for op in self.ops[e]:
                if op.dma is not None:
                    dcnt[op.dma] += 16
                    op.count = dcnt[op.dma]
                elif op.cc is not None:
                    op.count = 1
                elif op.signal and op.fn is not None:
                    cnt[e] += 1
                    op.count = cnt[e]

    def emit(self, eng_name, eng, sems):
        waited = {}
        for op in self.ops[eng_name]:
            need = {}
            for d in op.deps:
                if d.dma is not None:
                    key = ("dma", d.dma)
                elif d.cc is not None:
                    key = ("cc", d.cc)
                else:
                    key = ("eng", d.eng)
                if d.count > need.get(key, 0):
                    need[key] = d.count
            for key, val in need.items():
                if waited.get(key, 0) >= val:
                    continue
                eng.wait_ge(sems[key], val)
                waited[key] = val
            if op.fn is None:
                continue
            ins = op.fn(eng)
            if op.dma is not None:
                ins.then_inc(sems[("dma", op.dma)], 16)
            elif op.cc is not None:
                ins.then_inc(sems[("cc", op.cc)])
            elif op.signal:
                ins.then_inc(sems[("eng", eng_name)], 1)


def build_program(stage="full"):
    nc = bass.Bass("TRN2", target_bir_lowering=False)
    P = Prog()

    def ext_in(name, shape, dt):
        return nc.dram_tensor(name, list(shape), dt, kind="ExternalInput").ap()

    x_in = ext_in("x", [NT, D], F32)
    win_in = ext_in("win", [4, D, 384], F32)
    wout_in = ext_in("wout", [2, D, D], F32)
    wff1_in = ext_in("wff1", [2, D, DFF], F32)
    wff2_in = ext_in("wff2", [2, DFF, D], F32)
    gains_in = ext_in("gains", [128, 64], F32)
    fnorm_in = ext_in("fnormb", [128, D], F32)
    lamb_in = ext_in("lamb", [128, 256], F32)
    ident_in = ext_in("ident", [128, 128], BF16)
    tri_in = ext_in("trimask", [128, 128], BF16)
    kaug_in = ext_in("kaug", [2, 32, T], BF16)
    qaug0_in = ext_in("qaug0", [2, 32, 512], BF16)
    btab_in = ext_in("biastab", [128, 8 * NDC], F32)
    augq1_in = ext_in("augq1", [128, 16], F32)
    bsel_in = ext_in("bsel", [128, 256], F32)
    y_out = nc.dram_tensor("y", [NT, D], F32, kind="ExternalOutput").ap()

    HTin = [[nc.dram_tensor(f"htin{l}_{i}", [D, 512], BF16) for i in range(4)] for l in range(2)]
    HT = [[nc.dram_tensor(f"ht{l}_{i}", [4 * D, 512], BF16) for i in range(4)] for l in range(2)]
    OSin = [[[nc.dram_tensor(f"osin{l}_{p}_{q}", [128, 2048], BF16) for q in range(4)]
             for p in range(2)] for l in range(2)]
    OS = [[nc.dram_tensor(f"os{l}_{p}", [2048, 2048], BF16) for p in range(2)] for l in range(2)]

    off = [16512]

    def sb(name, shape, dt, at=None):
        nbytes = int(np.prod(shape[1:])) * (4 if dt == F32 else 2)
        if at is None:
            o = off[0]
            off[0] += (nbytes + 31) // 32 * 32
        else:
            o = at
        return nc.alloc_sbuf_tensor_at(name, list(shape), dt, offset=o), o + (nbytes + 31) // 32 * 32

    H, _ = sb("H", [128, 16, D], F32)
    ident, _ = sb("identt", [128, 128], BF16)
    tri, _ = sb("trit", [128, 128], BF16)
    ones128, _ = sb("ones128", [128, 128], F32)
    onesbf, _ = sb("onesbf", [128, 128], BF16)
    bsel, _ = sb("bselt", [128, 256], F32)
    btab, _ = sb("btab", [128, 8 * NDC], F32)
    augq1, _ = sb("augq1t", [128, 16], F32)
    gains, _ = sb("gainst", [128, 64], F32)
    lamb, _ = sb("lambt", [128, 256], F32)
    small, _ = sb("small", [128, 64], F32)
    epsc, _ = sb("epsc", [128, 1], F32)
    PH = off[0]

    o = PH
    KT = []
    for x_ in range(2):
        t_, o = sb(f"KT{x_}", [128, T], BF16, at=o)
        KT.append(t_)
    Vb, o = sb("Vb", [128, 64 * 192], BF16, at=o)
    WINs = []
    for s_ in range(2):
        t_, o = sb(f"WIN{s_}", [128, 8, 384], BF16, at=o)
        WINs.append(t_)
    HNTc = []
    for s_ in range(2):
        t_, o = sb(f"HNTc{s_}", [128, 8, 512], BF16, at=o)
        HNTc.append(t_)
    QT = []
    for s_ in range(2):
        row = []
        for x_ in range(2):
            t_, o = sb(f"QT{s_}_{x_}", [128, 512], BF16, at=o)
            row.append(t_)
        QT.append(row)
    PT = []
    for s_ in range(4):
        t_, o = sb(f"PT{s_}", [128, 512], BF16, at=o)
        PT.append(t_)
    Zs = []
    for x_ in range(2):
        row = []
        for par in range(2):
            t_, o = sb(f"Zs{x_}_{par}", [128, 512], F32, at=o)
            row.append(t_)
        Zs.append(row)
    FT = []
    for i_ in range(4):
        t_, o = sb(f"FT{i_}", [128, 512], F32, at=o)
        FT.append(t_)
    FTo = []
    for i_ in range(2):
        t_, o = sb(f"FTo{i_}", [128, 512], F32, at=o)
        FTo.append(t_)
    FTz, o = sb("FTz", [128, 512], F32, at=o)
    OSB = [FT[2], FT[3]]
    Ocat = []
    for s_ in range(2):
        t_, o = sb(f"Ocat{s_}", [128, 512], BF16, at=o)
        Ocat.append(t_)
    gate_sb, o = sb("gate_sb", [128, 8, 32], F32, at=o)
    m8, o = sb("m8", [128, 8, 8], F32, at=o)
    selt, o = sb("selt", [128, 32], F32, at=o)
    NS = []
    for x_ in range(2):
        t_, o = sb(f"NS{x_}", [128, 4, 96], BF16, at=o)
        NS.append(t_)
    kmT, o = sb("kmT", [128, 32], BF16, at=o)
    km32, o = sb("km32", [128, 2], F32, at=o)
    STG, o = sb("STG", [128, 2048], F32, at=o)
    STG2, o = sb("STG2", [128, 2048], F32, at=o)
    P2END = o
    assert P2END <= 229376, P2END

    o = PH
    HNT, o = sb("HNT", [128, 8, NT], BF16, at=o)
    OT = HNT
    WO, o = sb("WO", [128, 8, D], BF16, at=o)
    W1e, W2e = [], []
    for s_ in range(2):
        t_, o = sb(f"W1e{s_}", [128, 8, 512], BF16, at=o)
        W1e.append(t_)
        t_, o = sb(f"W2e{s_}", [128, 4, D], BF16, at=o)
        W2e.append(t_)
    hnb = []
    for s_ in range(2):
        t_, o = sb(f"hnb{s_}", [128, D], BF16, at=o)
        hnb.append(t_)
    UT = []
    for s_ in range(2):
        t_, o = sb(f"UT{s_}", [128, 4, 512], BF16, at=o)
        UT.append(t_)
    RT = []
    for s_ in range(2):
        t_, o = sb(f"RT{s_}", [128, 512], BF16, at=o)
        RT.append(t_)
    YS = []
    for s_ in range(2):
        t_, o = sb(f"YS{s_}", [128, 512], F32, at=o)
        YS.append(t_)
    FNB, o = sb("FNB", [128, D], F32, at=o)
    YO = []
    for s_ in range(2):
        t_, o = sb(f"YO{s_}", [128, D], F32, at=o)
        YO.append(t_)
    print("SBUF plan: PH", PH, "P2END", P2END, "P3 end", o)
    assert o <= P2END - 2 * 8192, (o, P2END)
    YT = [STG, STG2]

    pb = [nc.alloc_psum_tensor(f"pb{i}", [128, 512], F32) for i in range(8)]
    psS = pb[0:4]
    psO = pb[4:6]
    psP = pb[6]
    psX = pb[7]
    psXb = psX[:, 256:512].bitcast(BF16)
    psZ = pb[7]
    psT = pb[6][:, :].bitcast(BF16)
    psU = pb[0:2]
    psY = pb[2:6]

    ctx = {}
    OTQ = os.environ.get("OTQ", "sp")

    def dma(eng, key, out, in_, reads, writes):
        return P.add(eng, lambda e: e.dma_start(out=out, in_=in_), reads=reads, writes=writes, dma=key)

    def allgather(name, in_ap, out_ap, reads, writes):
        return P.add("pool", lambda e: e.collective_compute(
            "AllGather", ALU.bypass, replica_groups=GROUPS, ins=[in_ap.opt()], outs=[out_ap.opt()]),
            reads=reads, writes=writes, cc=name)

    dma("sp", "c0", ident[:, :], ident_in, [], ["ident"])
    dma("sp", "c0", tri[:, :], tri_in, [], ["tri"])
    dma("sp", "c0", bsel[:, :], bsel_in, [], ["bsel"])
    dma("sp", "c0", btab[:, :], btab_in, [], ["btab"])
    dma("sp", "c0", augq1[:, :], augq1_in, [], ["augq1"])
    dma("sp", "c0", gains[:, :], gains_in, [], ["gains"])
    dma("sp", "c0", lamb[:, :], lamb_in, [], ["lamb"])
    P.add("dve", lambda e: e.memset(ones128[:, :], 1.0), writes=["ones128"])
    P.add("dve", lambda e: e.memset(onesbf[:, :], 1.0), writes=["onesbf"])
    P.add("dve", lambda e: e.memset(epsc[:, :], EPS), writes=["epsc"])
    P.add("dve", lambda e: e.memset(small[:, :], 0.0), writes=["small"])
    P.add("dve", lambda e: e.tensor_tensor(out=FT[0][:, 0:64], in0=lamb[:, 0:64], in1=lamb[:, 64:128], op=ALU.mult),
          reads=["lamb"], writes=["ft0"])
    P.add("dve", lambda e: e.tensor_tensor(out=FT[0][:, 64:128], in0=lamb[:, 128:192], in1=lamb[:, 192:256], op=ALU.mult),
          reads=["lamb"], writes=["ft0b"])
    P.add("dve", lambda e: e.tensor_reduce(out=small[:, 34:36], in_=FT[0][:, 0:128].rearrange("p (a b) -> p a b", a=2),
                                           axis=AX.X, op=ALU.add),
          reads=["ft0", "ft0b", "small"], writes=["lam_s"])
    P.add("act", lambda e: e.activation(out=small[:, 36:38], in_=small[:, 34:36], func=AF.Exp),
          reads=["lam_s"], writes=["lam_e"])
    P.add("dve", lambda e: e.tensor_tensor(out=small[:, 38:39], in0=small[:, 37:38], in1=small[:, 36:37], op=ALU.subtract),
          reads=["lam_e"], writes=["lam_d"])
    P.add("dve", lambda e: e.tensor_scalar(out=small[:, 32:33], in0=small[:, 38:39], scalar1=-0.2, scalar2=None, op0=ALU.add),
          reads=["lam_d"], writes=["neglam"])
    P.add("dve", lambda e: e.tensor_scalar(out=small[:, 33:34], in0=gains[:, 32:33], scalar1=0.8, scalar2=None, op0=ALU.mult),
          reads=["gains", "small"], writes=["sub08"])
    for t in range(16):
        dma("sp", "xload", H[:, t, :], x_in[t * 128:(t + 1) * 128, :], [], [("H", t, 0), ("H", t, 1)])


    def rms_tile(t, slot):
        P.add("act", lambda e: e.activation(out=hnb[slot][:, :], in_=H[:, t, :], func=AF.Square,
                                            accum_out=small[:, t:t + 1]),
              reads=[("H", t, 0), ("H", t, 1), "small"], writes=[("hnb", slot), ("ssq", t)])
        P.add("act", lambda e: e.activation(out=small[:, 16 + t:17 + t], in_=small[:, t:t + 1], func=AF.Ln,
                                            bias=epsc[:, 0:1], scale=1.0 / D),
              reads=[("ssq", t), "epsc"], writes=[("rstd", t)])
        P.add("act", lambda e: e.activation(out=small[:, 16 + t:17 + t], in_=small[:, 16 + t:17 + t], func=AF.Exp, scale=-0.5),
              reads=[("rstd", t)], writes=[("rstd", t)])

    def norm_a(t):
        slot = t % 2
        P.add("dve", lambda e: e.memset(small[:, t:t + 1], 0.0), reads=[("ssq", t)], writes=[("ssq", t)])
        rms_tile(t, slot)
        P.add("dve", lambda e: e.tensor_scalar(out=hnb[slot][:, :], in0=H[:, t, :], scalar1=small[:, 16 + t:17 + t],
                                               scalar2=None, op0=ALU.mult),
              reads=[("H", t, 0), ("H", t, 1), ("rstd", t)], writes=[("hnb", slot)])

    def norm_b(t):
        slot = t % 2
        for kc in range(8):
            P.add("pe", lambda e, kc=kc: e.transpose(out=psT[:, kc * 128:(kc + 1) * 128],
                                                     in_=hnb[slot][:, kc * 128:(kc + 1) * 128], identity=ident[:, :]),
                  reads=[("hnb", slot), "ident"], writes=["psT"])
        P.add("dve", lambda e: e.tensor_copy(out=HNT[:, :, t * 128:(t + 1) * 128],
                                             in_=psT.rearrange("p (k q) -> p k q", k=8)),
              reads=["psT"], writes=[("HNT", t)])

    def norm_all(after_b=None):
        norm_a(0)
        for t in range(16):
            if t + 1 < 16:
                norm_a(t + 1)
            norm_b(t)
            if after_b is not None:
                after_b(t)

    def phase1(l):
        def after_b(t):
            if t % 4 == 3:
                i = t // 4
                dma("sp", f"htst{i}", HTin[l][i].ap().rearrange("(kc p) t -> p kc t", p=128),
                    HNT[:, :, i * 512:(i + 1) * 512], [("HNT", tt) for tt in range(4 * i, 4 * i + 4)],
                    [("HTin", l, i)])
                allgather(f"agh{l}_{i}", HTin[l][i].ap(), HT[l][i].ap(), [("HTin", l, i)], [("HT", l, i)])
        norm_all(after_b)

    def load_cast_win(l, p):
        wsrc = win_in[l * 2 + p].rearrange("(kc p) n -> p kc n", p=128)
        for hf in range(2):
            stg = (STG if hf == 0 else STG2)[:, 0:4 * 384].rearrange("p (k n) -> p k n", k=4)
            dma("sp", f"stg{hf}", stg, wsrc[:, hf * 4:hf * 4 + 4, :], [], [("STG", hf)])
            for k4 in range(4):
                kc = hf * 4 + k4
                P.add("act", lambda e, stg=stg, k4=k4, kc=kc: e.activation(
                    out=WINs[p][:, kc, :], in_=stg[:, k4, :], func=AF.Copy, scale=gains[:, l * 8 + kc:l * 8 + kc + 1]),
                    reads=[("STG", hf), "gains"], writes=[("WIN", p, kc)])

    def phase2(l):
        V0 = Vb[:, 0:64 * 128].rearrange("p (t c) -> p t c", c=128)
        V1 = Vb[:, :].rearrange("p (t c) -> p t c", c=192)
        dma("sp", "kaug0", KT[0][64:96, :], kaug_in[l], [], [("KTaug", 0)])
        dma("sp", "kaug1", KT[1][0:32, :], kaug_in[l], [], [("KTaug", 1)])
        P.add("dve", lambda e: e.memset(KT[1][32:64, :], 0.0), writes=[("KTz", 1)])
        for s_ in range(2):
            for x_ in range(2):
                P.add("dve", lambda e, s_=s_, x_=x_: e.memset(QT[s_][x_][:, :], 0.0), writes=[("QT", s_, x_), ("QTaug", s_, x_)])
        if l == 1:
            P.add("dve", lambda e: e.memset(V1[:, :, 64:128], 0.0), writes=["Vall"])
            P.add("dve", lambda e: e.memset(V1[:, :, 64:65], 1.0), reads=["Vall"], writes=["Vall"])
            for x_ in range(2):
                P.add("dve", lambda e, x_=x_: e.memset(NS[x_][:, :, :], 0.0), writes=[("NS", x_)])
                P.add("dve", lambda e, x_=x_: e.memset(OSB[x_][:, :], 0.0), writes=[("OSB", x_), ("FT", 2 + x_)])
            P.add("dve", lambda e: e.memset(kmT[:, :], 0.0), writes=["kmT"])
        load_cast_win(l, 0)
        for p in range(2):
            if l == 0:
                for s_ in range(2):
                    dma("sp", f"qaug{s_}0", QT[s_][0][64:96, :], qaug0_in[p], [("QTaug", s_, 0)], [("QTaug", s_, 0)])
                    dma("sp", f"qaug{s_}1", QT[s_][1][0:32, :], qaug0_in[p], [("QTaug", s_, 1)], [("QTaug", s_, 1)])
            for _ in chunk_proj(l, p, 0, V0, V1):
                pass
            fin = None
            for g in range(16):
                gen = chunk_proj(l, p, g + 1, V0, V1) if g + 1 < 16 else None
                if g == 8 and p == 0:
                    load_cast_win(l, 1)
                chunk_attn(l, p, g, V0, V1, gen, fin)
                fin = fin_gen(l, p, g)
                next(fin)
            for _ in fin:
                pass

    bidx = [0]
    NSLOT = 4

    def chunk_proj(l, p, g, V0, V1):
        s = g % 2
        hsrc = HT[l][g // 4][(g % 4) * D:(g % 4 + 1) * D, :].rearrange("(kc p) t -> p kc t", p=128)
        dma("sp", f"hntc{s}", HNTc[s][:, :, :], hsrc, [("HT", l, g // 4)], [("HNTc", s)])
        WIN = WINs[p]
        win_r = [("WIN", p, kc) for kc in range(8)]
        for kc in range(8):
            P.add("pe", lambda e, kc=kc: e.matmul(psP[:, :], lhsT=WIN[:, kc, 0:128], rhs=HNTc[s][:, kc, :],
                                                  start=(kc == 0), stop=(kc == 7)),
                  reads=[("HNTc", s)] + win_r, writes=["psP"])
        P.add("dve", lambda e: e.tensor_copy(out=QT[s][0][0:64, :], in_=psP[0:64, :]), reads=["psP"], writes=[("QT", s, 0)])
        P.add("dve", lambda e: e.tensor_copy(out=QT[s][1][64:128, :], in_=psP[64:128, :]), reads=["psP"], writes=[("QT", s, 1)])
        yield
        for kc in range(8):
            P.add("pe", lambda e, kc=kc: e.matmul(psP[:, :], lhsT=WIN[:, kc, 128:256], rhs=HNTc[s][:, kc, :],
                                                  start=(kc == 0), stop=(kc == 7)),
                  reads=[("HNTc", s)] + win_r, writes=["psP"])
        P.add("dve", lambda e: e.tensor_copy(out=KT[0][0:64, g * 512:(g + 1) * 512], in_=psP[0:64, :]),
              reads=["psP"], writes=[("KT", 0, g)])
        P.add("dve", lambda e: e.tensor_copy(out=KT[1][64:128, g * 512:(g + 1) * 512], in_=psP[64:128, :]),
              reads=["psP"], writes=[("KT", 1, g)])
        if l == 1:
            P.add("dve", lambda e: e.tensor_reduce(out=km32[:, :], in_=psP[:, :].rearrange("p (a b) -> p a b", a=2),
                                                   axis=AX.X, op=ALU.add), reads=["psP"], writes=["km32"])
            P.add("dve", lambda e: e.tensor_scalar(out=kmT[:, 2 * g:2 * g + 2], in0=km32[:, :], scalar1=1.0 / 256,
                                                   scalar2=None, op0=ALU.mult), reads=["km32"], writes=["kmT"])
        yield
        for tt in range(4):
            for kc in range(8):
                P.add("pe", lambda e, kc=kc, tt=tt: e.matmul(psP[:, tt * 128:(tt + 1) * 128],
                                                             lhsT=HNTc[s][:, kc, tt * 128:(tt + 1) * 128],
                                                             rhs=WIN[:, kc, 256:384], start=(kc == 0), stop=(kc == 7)),
                      reads=[("HNTc", s)] + win_r, writes=["psP"])
        psP4 = psP[:, :].rearrange("p (t c) -> p t c", t=4)
        if l == 0:
            P.add("dve", lambda e: e.tensor_copy(out=V0[:, 4 * g:4 * g + 4, :], in_=psP4), reads=["psP"], writes=[("V", g)])
        else:
            P.add("dve", lambda e: e.tensor_copy(out=V1[:, 4 * g:4 * g + 4, 0:64], in_=psP4[:, :, 0:64]),
                  reads=["psP", "Vall"], writes=[("V", g)])
            P.add("dve", lambda e: e.tensor_copy(out=V1[:, 4 * g:4 * g + 4, 128:192], in_=psP4[:, :, 64:128]),
                  reads=["psP", "Vall"], writes=[("Vb_", g)])
        yield
        if l == 1:
            yield from moba_gate(p, g, s)
        yield

    def chunk_attn(l, p, g, V0, V1, gen=None, fin=None):
        s = g % 2
        nk = 4 * g + 4
        if l == 1 and os.environ.get("SKIP_ATTN"):
            nk = 0
        kt0 = max(0, 4 * g - 16) if p == 0 else 0
        items = [(kt, x_) for kt in range(kt0, nk) for x_ in range(2)]
        if l == 0:
            for za, zeng in ((0, "dve"), (1, "pool")):
                P.add(zeng, lambda e, za=za: e.memset(Zs[za][g % 2][:, :], 0.0), writes=[("Zs", za, g % 2)])
        LOOK = 3
        slots = {}

        def emit_s(kt, x_):
            j = kt - 4 * g
            c0 = 128 * j if j >= 0 else 0
            b = bidx[0] % NSLOT
            bidx[0] += 1
            slots[(kt, x_)] = b
            kx = 96 if x_ == 0 else 128
            kr = [("KT", x_, kt // 4), ("KTaug", x_), ("QT", s, x_), ("QTaug", s, x_)] + ([("KTz", 1)] if x_ == 1 else [])
            ks = slice(kt * 128, (kt + 1) * 128)
            if j >= 0:
                P.add("pe", lambda e: e.matmul(
                    psS[b][:, c0:c0 + 128], lhsT=KT[x_][0:kx, ks], rhs=QT[s][x_][0:kx, c0:c0 + 128],
                    start=True, stop=False), reads=kr, writes=[("psS", b)])
                P.add("pe", lambda e: e.matmul(
                    psS[b][:, c0:c0 + 128], lhsT=ident[:, :], rhs=tri[:, :], start=False, stop=True),
                    reads=["ident", "tri"], writes=[("psS", b)])
                if c0 + 128 < 512:
                    P.add("pe", lambda e: e.matmul(
                        psS[b][:, c0 + 128:512], lhsT=KT[x_][0:kx, ks], rhs=QT[s][x_][0:kx, c0 + 128:512],
                        start=True, stop=True), reads=kr, writes=[("psS", b)])
            else:
                P.add("pe", lambda e: e.matmul(
                    psS[b][:, :], lhsT=KT[x_][0:kx, ks], rhs=QT[s][x_][0:kx, :], start=True, stop=True),
                    reads=kr, writes=[("psS", b)])
            col = (((l * 2 + p) * 2 + x_) * NDC) + (4 * g - kt + 3)
            P.add("act", lambda e: e.activation(
                out=PT[b][:, c0:512], in_=psS[b][:, c0:512], func=AF.Exp, bias=btab[:, col:col + 1], scale=SCALE),
                reads=[("psS", b), "btab"], writes=[("PT", b)])
            if l == 0 and x_ == 0:
                za = kt % 2
                zeng = "dve" if za == 0 else "pool"
                zt = Zs[za][g % 2]
                zk = ("Zs", za, g % 2)
                P.add(zeng, lambda e: e.tensor_tensor(
                    out=zt[:, c0:512], in0=zt[:, c0:512], in1=PT[b][:, c0:512], op=ALU.add),
                    reads=[("PT", b), zk], writes=[zk])

        def emit_pv(kt, x_):
            j = kt - 4 * g
            c0 = 128 * j if j >= 0 else 0
            b = slots[(kt, x_)]
            if l == 0:
                vl = V0[:, kt, :]
                mo = 128
            else:
                vl = V1[:, kt, 0:65] if x_ == 0 else V1[:, kt, 64:192]
                mo = 65 if x_ == 0 else 128
            P.add("pe", lambda e: e.matmul(
                psO[x_][0:mo, c0:512], lhsT=vl, rhs=PT[b][:, c0:512], start=(kt == kt0), stop=(kt == nk - 1)),
                reads=[("PT", b), ("V", kt // 4), ("Vb_", kt // 4), "Vall"], writes=[("psO", x_)])
            if l == 0 and x_ == 1:
                P.add("pe", lambda e: e.matmul(
                    psZ[:, c0:512], lhsT=onesbf[:, :], rhs=PT[b][:, c0:512], start=(kt == kt0), stop=(kt == nk - 1)),
                    reads=[("PT", b), "onesbf"], writes=["psZ"])

        for n in range(len(items) + LOOK):
            if n < len(items):
                emit_s(*items[n])
            if n - LOOK >= 0:
                emit_pv(*items[n - LOOK])
            if n >= 4 and n % 3 == 1 and gen is not None:
                next(gen, None)
            if n >= 2 and n % 3 == 2 and fin is not None:
                next(fin, None)
        if fin is not None:
            for _ in fin:
                pass
        if gen is not None:
            for _ in gen:
                pass

    def fin_gen(l, p, g):
        s = g % 2
        oc = Ocat[s]
        zp = g % 2
        if l == 0:
            for x_ in range(2):
                P.add("dve", lambda e, x_=x_: e.tensor_copy(out=FTo[x_][:, :], in_=psO[x_][:, :]),
                      reads=[("psO", x_)], writes=[("FTo", x_)])
            P.add("dve", lambda e: e.tensor_copy(out=FTz[:, :], in_=psZ[:, :]), reads=["psZ"], writes=["FTz"])
            yield
            P.add("dve", lambda e: e.tensor_tensor(out=Zs[0][zp][:, :], in0=Zs[0][zp][:, :], in1=Zs[1][zp][:, :], op=ALU.add),
                  reads=[("Zs", 0, zp), ("Zs", 1, zp)], writes=[("Zs", 0, zp)])
            P.add("pe", lambda e: e.matmul(psP[:, :], lhsT=ones128[:, :], rhs=Zs[0][zp][:, :], start=True, stop=True),
                  reads=[("Zs", 0, zp), "ones128"], writes=["psP"])
            P.add("dve", lambda e: e.reciprocal(out=FT[0][:, :], in_=psP[:, :]), reads=["psP"], writes=[("FT", 0)])
            yield
            P.add("dve", lambda e: e.reciprocal(out=FT[1][:, :], in_=FTz[:, :]), reads=["FTz"], writes=[("FT", 1)])
            yield
            for x_ in range(2):
                P.add("dve", lambda e, x_=x_: e.tensor_tensor(out=FT[x_][:, :], in0=FTo[x_][:, :], in1=FT[x_][:, :], op=ALU.mult),
                      reads=[("FTo", x_), ("FT", x_)], writes=[("FT", x_)])
            P.add("dve", lambda e: e.scalar_tensor_tensor(out=FT[2][:, :], in0=FT[1][:, :], scalar=small[:, 32:33], in1=FT[0][:, :],
                                                          op0=ALU.mult, op1=ALU.add),
                  reads=[("FT", 0), ("FT", 1), "neglam"], writes=[("FT", 2)])
            P.add("pool", lambda e: e.tensor_tensor(out=FT[3][:, :], in0=FT[2][:, :], in1=FT[2][:, :], op=ALU.mult),
                  reads=[("FT", 2)], writes=[("FT", 3)])
            yield
            P.add("pe", lambda e: e.matmul(psP[:, :], lhsT=ones128[:, :], rhs=FT[3][:, :], start=True, stop=True),
                  reads=[("FT", 3), "ones128"], writes=["psP"])
            P.add("act", lambda e: e.activation(out=FT[3][:, :], in_=psP[:, :], func=AF.Ln, bias=epsc[:, 0:1], scale=1.0 / 128),
                  reads=["psP", "epsc"], writes=[("FT", 3)])
            P.add("act", lambda e: e.activation(out=FT[3][:, :], in_=FT[3][:, :], func=AF.Exp, scale=-0.5),
                  reads=[("FT", 3)], writes=[("FT", 3)])
            yield
            P.add("dve", lambda e: e.tensor_tensor(out=oc[:, :], in0=FT[2][:, :], in1=FT[3][:, :], op=ALU.mult),
                  reads=[("FT", 2), ("FT", 3)], writes=[("Ocat", s)])
        else:
            for x_ in range(2):
                mo = 65 if x_ == 0 else 128
                P.add("dve", lambda e, x_=x_, mo=mo: e.tensor_copy(out=OSB[x_][0:mo, :], in_=psO[x_][0:mo, :]),
                      reads=[("psO", x_)], writes=[("OSB", x_)])
            yield
            for x_ in range(2):
                rows = slice(0, 64) if x_ == 0 else slice(64, 128)
                P.add("pe", lambda e, x_=x_: e.matmul(psX[:, :], lhsT=bsel[:, x_ * 128:(x_ + 1) * 128], rhs=OSB[x_][:, :],
                                                      start=True, stop=True), reads=[("OSB", x_), "bsel"], writes=["psX"])
                P.add("dve", lambda e, x_=x_, rows=rows: e.reciprocal(out=FT[x_][rows, :], in_=psX[rows, :]),
                      reads=["psX"], writes=[("FT", x_)])
                yield
                P.add("dve", lambda e, x_=x_, rows=rows: e.tensor_tensor(out=oc[rows, :], in0=OSB[x_][rows, :], in1=FT[x_][rows, :],
                                                                        op=ALU.mult),
                      reads=[("OSB", x_), ("FT", x_)], writes=[("Ocat", s, x_)])
        dma("sp", f"ost{s}", OSin[l][p][g // 4][:, (g % 4) * 512:(g % 4 + 1) * 512], oc[:, :],
            [("Ocat", s), ("Ocat", s, 0), ("Ocat", s, 1)], [("OSin", l, p, g // 4)])
        yield
        if g % 4 == 3:
            q = g // 4
            allgather(f"ago{l}_{p}_{q}", OSin[l][p][q].ap(), OS[l][p][q * 512:(q + 1) * 512, :],
                      [("OSin", l, p, q)], [("OS", l, p, q)])
        yield

    def moba_gate(p, g, s):
        gbank = [psX, psP]
        gkey = ["psX", "psP"]
        for x_ in range(2):
            rows = slice(0, 64) if x_ == 0 else slice(64, 128)
            for qt in range(4):
                P.add("pe", lambda e, x_=x_, qt=qt, rows=rows: e.matmul(
                    gbank[x_][:, qt * 32:(qt + 1) * 32], lhsT=QT[s][x_][rows, qt * 128:(qt + 1) * 128], rhs=kmT[rows, :],
                    start=True, stop=True), reads=[("QT", s, x_), "kmT"], writes=[gkey[x_]])
        P.add("dve", lambda e: e.memset(gate_sb[:, :, :], -1e30), writes=["gate"])
        gsv = gate_sb[:, :, :].rearrange("p (x h q) n -> p x h q n", x=2, h=2)
        for hf in range(2):
            jb = 2 * g + hf
            if jb > 0:
                for x_ in range(2):
                    psg = gbank[x_][:, 0:128].rearrange("p (h q n) -> p h q n", h=2, q=2)
                    P.add("dve", lambda e, hf=hf, jb=jb, x_=x_, psg=psg: e.tensor_copy(out=gsv[:, x_, hf, :, 0:jb], in_=psg[:, hf, :, 0:jb]),
                          reads=[gkey[x_], "gate"], writes=["gate"])
        for x_ in range(2):
            base = 64 if x_ == 0 else 0
            for qt in range(4):
                xq = x_ * 4 + qt
                jb = 2 * g + qt // 2
                ai = (p * 2 + x_) * 4 + qt
                P.add("dve", lambda e, xq=xq: e.max(out=m8[:, xq, :], in_=gate_sb[:, xq, :]), reads=["gate"], writes=[("m8", xq)])
                P.add("dve", lambda e, xq=xq: e.tensor_scalar(out=selt[:, :], in0=gate_sb[:, xq, :], scalar1=m8[:, xq, 2:3],
                                                              scalar2=None, op0=ALU.is_ge),
                      reads=["gate", ("m8", xq)], writes=["selt"])
                P.add("dve", lambda e, x_=x_, qt=qt, base=base, ai=ai: e.tensor_scalar(
                    out=NS[x_][:, qt, base:base + 32], in0=selt[:, :], scalar1=-NEG, scalar2=augq1[:, ai:ai + 1],
                    op0=ALU.mult, op1=ALU.add), reads=["selt", "augq1", ("NS", x_)], writes=[("NS", x_)])
                P.add("dve", lambda e, x_=x_, qt=qt, base=base, ai=ai, jb=jb: e.tensor_scalar(
                    out=NS[x_][:, qt, base + jb:base + jb + 1], in0=augq1[:, ai:ai + 1], scalar1=-NEG, scalar2=None,
                    op0=ALU.add), reads=["augq1", ("NS", x_)], writes=[("NS", x_)])
        for _ in range(4):
            yield
        for x_ in range(2):
            rows = slice(64, 96) if x_ == 0 else slice(0, 32)
            yield
            for qt in range(4):
                P.add("pe", lambda e, x_=x_, qt=qt: e.transpose(out=psXb[0:96, qt * 128:(qt + 1) * 128],
                                                                in_=NS[x_][:, qt, :], identity=ident[:, :]),
                      reads=[("NS", x_), "ident"], writes=["psX"])
            P.add("dve", lambda e, x_=x_, rows=rows: e.tensor_copy(out=QT[s][x_][rows, :], in_=psXb[rows, :]),
                  reads=["psX"], writes=[("QTaug", s, x_)])

    def phase3(l, do_ffn=True, tail=None):
        def rank_of(e):
            if "rank" not in ctx:
                ctx["rank"] = e.partition_id() % 4
            return ctx["rank"]
        for q in range(4):
            for kc in range(8):
                src, p = kc // 2, kc % 2
                P.add(OTQ, lambda e, kc=kc, src=src, p=p, q=q: e.dma_start(
                    out=OT[:, kc, q * 512:(q + 1) * 512],
                    in_=OS[l][p][q * 512 + src * 128:q * 512 + src * 128 + 128, bass.ds(rank_of(e) * 512, 512)]),
                    reads=[("OS", l, p, q)], writes=[("OT", kc, q)], dma="otld")
        wsrc = wout_in[l].rearrange("(kc p) n -> p kc n", p=128)
        for i in range(4):
            stg = (STG if i % 2 == 0 else STG2)[:, :].rearrange("p (k n) -> p k n", k=2)
            dma("sp", f"stg{i % 2}", stg, wsrc[:, 2 * i:2 * i + 2, :], [], [("STG", i % 2)])
            if l == 0:
                P.add("act", lambda e, stg=stg, i=i: e.activation(out=WO[:, 2 * i:2 * i + 2, :], in_=stg, func=AF.Copy,
                                                                  scale=small[:, 33:34]),
                      reads=[("STG", i % 2), "sub08"], writes=[("WO", i)])
            else:
                P.add("act", lambda e, stg=stg, i=i: e.copy(out=WO[:, 2 * i:2 * i + 2, :], in_=stg),
                      reads=[("STG", i % 2)], writes=[("WO", i)])
        yb = 0
        for t in range(16):
            for n2 in range(2):
                b = yb % 4
                yb += 1
                for kc in range(8):
                    wk = kc // 2 + 4 * (kc % 2)
                    P.add("pe", lambda e, kc=kc, wk=wk, b=b, t=t, n2=n2: e.matmul(
                        psY[b][:, :], lhsT=OT[:, kc, t * 128:(t + 1) * 128], rhs=WO[:, wk, n2 * 512:(n2 + 1) * 512],
                        start=(kc == 0), stop=(kc == 7)), reads=[("OT", k_, t // 4) for k_ in range(8)] + [("WO", wk // 2)], writes=[("psY", b)])
                P.add("dve", lambda e, b=b, t=t, n2=n2: e.tensor_tensor(
                    out=H[:, t, n2 * 512:(n2 + 1) * 512], in0=H[:, t, n2 * 512:(n2 + 1) * 512], in1=psY[b][:, :], op=ALU.add),
                    reads=[("psY", b), ("H", t, n2)], writes=[("H", t, n2)])
        if not do_ffn:
            return
        P.barrier()
        norm_all()
        w1src = wff1_in[l].rearrange("(kc p) n -> p kc n", p=128)
        w2src = wff2_in[l].rearrange("(fc p) n -> p fc n", p=128)
        cnt = {"u": 0, "y": yb}

        def load_weights(ei):
            ws = ei % 2
            for hf in range(2):
                stg = (STG if hf == 0 else STG2)[:, :].rearrange("p (k n) -> p k n", k=4)
                dma("sp", f"stg{hf}", stg, w1src[:, hf * 4:hf * 4 + 4, ei * 512:(ei + 1) * 512], [], [("STG", hf)])
                for k4 in range(4):
                    kc = hf * 4 + k4
                    P.add("act", lambda e, stg=stg, k4=k4, kc=kc, ws=ws: e.activation(
                        out=W1e[ws][:, kc, :], in_=stg[:, k4, :], func=AF.Copy, scale=gains[:, 16 + l * 8 + kc:17 + l * 8 + kc]),
                        reads=[("STG", hf), "gains"], writes=[("W1e", ws)])
            for hf in range(2):
                stg = (STG if hf == 0 else STG2)[:, :].rearrange("p (k n) -> p k n", k=2)
                dma("sp", f"stg{hf}", stg, w2src[:, ei * 4 + hf * 2:ei * 4 + hf * 2 + 2, :], [], [("STG", hf)])
                P.add("act", lambda e, stg=stg, hf=hf, ws=ws: e.copy(out=W2e[ws][:, 2 * hf:2 * hf + 2, :], in_=stg),
                      reads=[("STG", hf)], writes=[("W2e", ws)])

        def emit_u(ei, c):
            ws = ei % 2
            us = (ei * 4 + c) % 2
            for fc in range(4):
                b = cnt["u"] % 2
                cnt["u"] += 1
                for kc in range(8):
                    P.add("pe", lambda e, kc=kc, b=b, fc=fc: e.matmul(
                        psU[b][:, :], lhsT=W1e[ws][:, kc, fc * 128:(fc + 1) * 128], rhs=HNT[:, kc, c * 512:(c + 1) * 512],
                        start=(kc == 0), stop=(kc == 7)),
                        reads=[("W1e", ws)] + [("HNT", tt) for tt in range(4 * c, 4 * c + 4)], writes=[("psU", b)])
                P.add("act", lambda e, b=b: e.activation(out=RT[b][:, :], in_=psU[b][:, :], func=AF.Relu),
                      reads=[("psU", b)], writes=[("RT", b)])
                P.add("dve", lambda e, b=b, fc=fc: e.tensor_tensor(out=UT[us][:, fc, :], in0=RT[b][:, :], in1=RT[b][:, :],
                                                                  op=ALU.mult),
                      reads=[("RT", b)], writes=[("UT", us, fc)])

        def emit_y(ei, c):
            ws = ei % 2
            us = (ei * 4 + c) % 2
            for tt in range(4):
                t = 4 * c + tt
                for n2 in range(2):
                    b = cnt["y"] % 4
                    cnt["y"] += 1
                    for fc in range(4):
                        P.add("pe", lambda e, fc=fc, b=b, tt=tt, n2=n2: e.matmul(
                            psY[b][:, :], lhsT=UT[us][:, fc, tt * 128:(tt + 1) * 128], rhs=W2e[ws][:, fc, n2 * 512:(n2 + 1) * 512],
                            start=(fc == 0), stop=(fc == 3)),
                            reads=[("UT", us, fc), ("W2e", ws)], writes=[("psY", b)])
                    if n2 == 0:
                        P.add("dve", lambda e, b=b, t=t, n2=n2: e.tensor_tensor(
                            out=H[:, t, n2 * 512:(n2 + 1) * 512], in0=H[:, t, n2 * 512:(n2 + 1) * 512], in1=psY[b][:, :], op=ALU.add),
                            reads=[("psY", b), ("H", t, n2)], writes=[("H", t, n2)])
                    else:
                        ys = tt % 2
                        P.add("act", lambda e, b=b, ys=ys: e.copy(out=YS[ys][:, :], in_=psY[b][:, :]),
                              reads=[("psY", b)], writes=[("YS", ys)])
                        P.add("pool", lambda e, t=t, n2=n2, ys=ys: e.tensor_tensor(
                            out=H[:, t, n2 * 512:(n2 + 1) * 512], in0=H[:, t, n2 * 512:(n2 + 1) * 512], in1=YS[ys][:, :], op=ALU.add),
                            reads=[("YS", ys), ("H", t, n2)], writes=[("H", t, n2)])
            if ei == 7 and tail is not None:
                tail(c)

        steps = [(ei, c) for ei in range(8) for c in range(4)]
        for k in range(len(steps) + 1):
            if k < len(steps):
                ei, c = steps[k]
                if c == 0:
                    load_weights(ei)
                emit_u(ei, c)
            if k >= 1:
                emit_y(*steps[k - 1])

    def final_out(with_norm=True, tiles=range(16), load_fnb=True):
        if with_norm and load_fnb:
            dma("sp", "fnb", FNB[:, :], fnorm_in, [], ["fnb"])
        for t in tiles:
            slot = t % 2
            yt = YO[slot][:, :]
            if with_norm:
                P.add("dve", lambda e, t=t: e.memset(small[:, t:t + 1], 0.0), reads=[("ssq", t)], writes=[("ssq", t)])
                rms_tile(t, slot)
                P.add("dve", lambda e, t=t, yt=yt: e.scalar_tensor_tensor(
                    out=yt, in0=H[:, t, :], scalar=small[:, 16 + t:17 + t], in1=FNB[:, :], op0=ALU.mult, op1=ALU.mult),
                    reads=[("H", t, 0), ("H", t, 1), ("rstd", t), "fnb"], writes=[("yt", slot)])
            else:
                P.add("dve", lambda e, t=t, yt=yt: e.tensor_copy(out=yt, in_=H[:, t, :]),
                      reads=[("H", t, 0), ("H", t, 1)], writes=[("yt", slot)])
            dma("sp", f"yst{slot}", y_out[t * 128:(t + 1) * 128, :], yt, [("yt", slot)], [("yout", t)])

    def p1_tail(l):
        def tail(c):
            norm_a(4 * c)
            for t in range(4 * c, 4 * c + 4):
                if t + 1 < 4 * c + 4:
                    norm_a(t + 1)
                norm_b(t)
            dma("sp", f"htst{c}", HTin[l][c].ap().rearrange("(kc p) t -> p kc t", p=128),
                HNT[:, :, c * 512:(c + 1) * 512], [("HNT", tt) for tt in range(4 * c, 4 * c + 4)],
                [("HTin", l, c)])
            allgather(f"agh{l}_{c}", HTin[l][c].ap(), HT[l][c].ap(), [("HTin", l, c)], [("HT", l, c)])
        return tail

    def out_tail():
        def tail(c):
            final_out(True, tiles=range(4 * c, 4 * c + 4), load_fnb=(c == 0))
        return tail

    phase1(0)
    P.barrier(soft_cc=True)
    if stage != "F0":
        phase2(0)
        P.barrier(soft_cc=True)
    if stage == "F0":
        phase3(0)
        P.barrier()
        final_out(with_norm=False)
    elif stage == "A0":
        phase3(0, do_ffn=False)
        P.barrier()
        final_out(with_norm=False)
    elif stage == "L0":
        phase3(0)
        P.barrier()
        final_out(with_norm=False)
    else:
        phase3(0, tail=p1_tail(1))
        P.barrier(soft_cc=True)
        phase2(1)
        P.barrier(soft_cc=True)
        if stage == "A1":
            phase3(1, do_ffn=False)
            P.barrier()
            final_out(with_norm=False)
        else:
            phase3(1, tail=out_tail())
    P.barrier()
    P.finalize()

    with ExitStack() as st:
        sems = {}
        for e in Prog.ENGS:
            sems[("eng", e)] = st.enter_context(nc.semaphore(f"s_{e}"))
        for k in P.dma_keys:
            sems[("dma", k)] = st.enter_context(nc.semaphore(f"d_{k}"))
        for k in P.cc_keys:
            sems[("cc", k)] = st.enter_context(nc.semaphore(f"c_{k}"))
        block = st.enter_context(nc.Block())

        @block.sync
        def _(e):
            P.emit("sp", e, sems)

        @block.scalar
        def _(e):
            P.emit("act", e, sems)

        @block.vector
        def _(e):
            P.emit("dve", e, sems)

        @block.gpsimd
        def _(e):
            P.emit("pool", e, sems)

        @block.tensor
        def _(e):
            P.emit("pe", e, sems)
    return nc


def _slopes(n):
    return np.array([2.0 ** (-8.0 * (h + 1) / n) for h in range(n)], dtype=np.float64)


def _consts(r):
    bf = ml_dtypes.bfloat16
    s0 = _slopes(8)
    s1 = _slopes(16)
    ident = np.eye(128, dtype=np.float32).astype(bf)
    ki = np.arange(128)[:, None]
    qi = np.arange(128)[None, :]
    tri = np.where(qi < ki, NEG, 0.0).astype(np.float32).astype(bf)
    kaug = np.zeros((2, 32, T), np.float32)
    kaug[0, 0, :] = 1.0
    kaug[1, np.arange(T) // 256, np.arange(T)] = 1.0
    kaug = kaug.astype(bf)
    qaug0 = np.zeros((2, 32, 512), np.float32)
    btab = np.zeros((128, 2, 2, 2, NDC), np.float64)
    augq1 = np.zeros((128, 2, 2, 4), np.float64)
    pp = np.arange(128, dtype=np.float64)
    dc = np.arange(NDC, dtype=np.float64) - 3.0
    for p in range(2):
        s = s0[r + 4 * p]
        qaug0[p, 0, :] = -s * np.arange(512) / SCALE
        for x in range(2):
            btab[:, 0, p, x, :] = s * (pp[:, None] - 128.0 * dc[None, :])
            s_ = s1[2 * (r + 4 * p) + x]
            btab[:, 1, p, x, :] = s_ * (pp[:, None] - 128.0 * dc[None, :])
            for qt in range(4):
                augq1[:, p, x, qt] = -s_ * (qt * 128 + pp) / SCALE + NEG
    bsel = np.zeros((128, 2, 128), np.float32)
    bsel[64, 0, 0:64] = 1.0
    bsel[0, 1, 64:128] = 1.0
    return dict(ident=ident, trimask=tri, kaug=kaug, qaug0=qaug0.astype(bf),
                biastab=btab.reshape(128, -1).astype(np.float32), augq1=augq1.reshape(128, -1).astype(np.float32),
                bsel=bsel.reshape(128, 256))


_NC_CACHE = {}


def kernel(x, attn_norm, w_in, w_out, diff_lambda, diff_subln, mlp_norm, w_ff1, w_ff2, final_norm, _stage="full"):
    x = np.asarray(x, np.float32)
    attn_norm = np.asarray(attn_norm, np.float32)
    w_in = np.asarray(w_in, np.float32)
    w_out = np.ascontiguousarray(np.asarray(w_out, np.float32))
    diff_lambda = np.asarray(diff_lambda, np.float32)
    diff_subln = np.asarray(diff_subln, np.float32)
    mlp_norm = np.asarray(mlp_norm, np.float32)
    w_ff1 = np.ascontiguousarray(np.asarray(w_ff1, np.float32))
    w_ff2 = np.ascontiguousarray(np.asarray(w_ff2, np.float32))
    final_norm = np.asarray(final_norm, np.float32)

    if _stage not in _NC_CACHE:
        _NC_CACHE[_stage] = build_program(_stage)
    nc = _NC_CACHE[_stage]

    gains = np.zeros((128, 64), np.float32)
    for l in range(2):
        gains[:, l * 8:(l + 1) * 8] = attn_norm[l].reshape(8, 128).T
        gains[:, 16 + l * 8:16 + (l + 1) * 8] = mlp_norm[l].reshape(8, 128).T
    gains[:, 32] = diff_subln[0]
    fnb = np.ascontiguousarray(np.broadcast_to(final_norm[None, :], (128, D)))
    lamb = np.ascontiguousarray(np.broadcast_to(diff_lambda[0].reshape(1, 256), (128, 256)))

    in_maps = []
    for c in range(8):
        b, r = c // 4, c % 4
        win = np.zeros((4, D, 384), np.float32)
        for l in range(2):
            for p in range(2):
                c0 = 128 * (r + 4 * p)
                for j in range(3):
                    win[l * 2 + p, :, j * 128:(j + 1) * 128] = w_in[l, :, j * D + c0:j * D + c0 + 128]
        xr = x[b].reshape(4, 4, 512, D)[:, r].reshape(NT, D)
        m = dict(x=np.ascontiguousarray(xr), win=win, wout=w_out, wff1=w_ff1, wff2=w_ff2,
                 gains=gains, fnormb=fnb, lamb=lamb)
        m.update(_consts(r))
        in_maps.append(m)
    res = run_bass_kernel_spmd(nc, in_maps, core_ids=list(range(8)))
    out = np.zeros((2, T, D), np.float32)
    for c in range(8):
        b, r = c // 4, c % 4
        out[b].reshape(4, 4, 512, D)[:, r] = np.asarray(res.results[c]["y"], dtype=np.float32).reshape(4, 512, D)
    return out
```
